# Optimizing a Trainium2 kernel written in Bass

```python
import jax
import jax.numpy as jnp
from jax import lax
import numpy as np

D_MODEL = 1024
BATCH = 8
SEQ = 4096
DEPTH = 4

GRID_W = 64
CTX_LEN = 256
EPS = 1e-6
N_BRANCH = 4
MIX_W = D_MODEL // 4

ML_HEADS = 4
ML_W = MIX_W
ML_DH = ML_W // ML_HEADS
ML_CHUNK = 64
ML_CONV = 3
ML_STATE_COLS = 3 * ML_W + 4 * ML_HEADS
ML_COLS = ML_STATE_COLS + ML_W
GM_GROUPS = 4
GM_CHUNK = 128
GM_W = MIX_W
GM_DG = GM_W // GM_GROUPS
CV_W = MIX_W
CV_K = 31
FT_GROUPS = 4
FT_W = MIX_W
FT_DG = FT_W // FT_GROUPS
GM_OFF = ML_COLS
CV_OFF = GM_OFF + 2 * GM_W
FT_OFF = CV_OFF + 2 * CV_W
GATE_OFF = FT_OFF + FT_W
IN_COLS = GATE_OFF + N_BRANCH * D_MODEL
PEER_HEADS = 8
PEER_TOPK = 16
N_KEYS = 128
N_EXPERTS = N_KEYS * N_KEYS
PEER_DQ = 256
PEER_DK_HALF = PEER_DQ // 2
PEER_BLOCK = 128

kernel_name = 'hybrid_flow_mlstm_gmlp_conformer_fnet_peer'


def _rmsnorm(x, g):
    xf = x.astype(jnp.float32)
    y = xf * lax.rsqrt(jnp.mean(xf * xf, axis=-1, keepdims=True) + EPS)
    return (y * g.astype(jnp.float32)).astype(x.dtype)


def _layernorm(x, g, b):
    xf = x.astype(jnp.float32)
    mu = jnp.mean(xf, axis=-1, keepdims=True)
    xc = xf - mu
    y = xc * lax.rsqrt(jnp.mean(xc * xc, axis=-1, keepdims=True) + EPS)
    return (y * g.astype(jnp.float32) + b.astype(jnp.float32)).astype(x.dtype)


def _modulate(h, shift, scale):
    return h * (1 + scale) + shift


def _dwconv(x, w, b):
    k, ch = w.shape
    y = lax.conv_general_dilated(x, w[:, None, :].astype(x.dtype), window_strides=(1,),
                                 padding=((k // 2, k // 2),),
                                 dimension_numbers=('NWC', 'WIO', 'NWC'),
                                 feature_group_count=ch)
    return y + b


def _grid_sincos(n_tok, d):
    rows = n_tok // GRID_W
    n_freq = d // 4
    freq = 1.0 / (10000.0 ** (jnp.arange(n_freq, dtype=jnp.float32) / n_freq))
    r = jnp.repeat(jnp.arange(rows, dtype=jnp.float32), GRID_W)
    cc = jnp.tile(jnp.arange(GRID_W, dtype=jnp.float32), rows)
    ar = r[:, None] * freq[None, :]
    ac = cc[:, None] * freq[None, :]
    return jnp.concatenate([jnp.sin(ar), jnp.cos(ar), jnp.sin(ac), jnp.cos(ac)], axis=-1)


def _zero_state(b_sz):
    g = 2 * ML_HEADS
    return (jnp.zeros((b_sz, g, ML_DH, ML_DH), jnp.float32),
            jnp.zeros((b_sz, g, ML_DH), jnp.float32),
            jnp.zeros((b_sz, g), jnp.float32))


def _mlstm_scan(q, k, v, li, lf, state, need_out):
    b_sz, g, t_len, dh = q.shape
    nc = t_len // ML_CHUNK

    def chunks(a):
        a = a.reshape((b_sz, g, nc, ML_CHUNK) + a.shape[3:])
        return jnp.moveaxis(a, 2, 0)

    tril = jnp.tril(jnp.ones((ML_CHUNK, ML_CHUNK), dtype=bool))

    def step(carry, inp):
        c_st, n_st, m_st = carry
        qc, kc, vc, lic, lfc = inp
        bcum = jnp.cumsum(lfc, axis=-1)
        b_last = bcum[..., -1]
        w = b_last[..., None] - bcum + lic
        m_new = jnp.maximum(b_last + m_st, jnp.max(w, axis=-1))
        ws = jnp.exp(w - m_new[..., None])
        decay = jnp.exp(b_last + m_st - m_new)
        c_new = decay[..., None, None] * c_st + jnp.einsum('bgs,bgsd,bgse->bgde', ws, kc, vc)
        n_new = decay[..., None] * n_st + jnp.einsum('bgs,bgsd->bgd', ws, kc)
        if not need_out:
            return (c_new, n_new, m_new), None
        dlog = jnp.where(tril, bcum[..., :, None] - bcum[..., None, :] + lic[..., None, :], -jnp.inf)
        inter = bcum + m_st[..., None]
        m_t = jnp.maximum(inter, jnp.max(dlog, axis=-1))
        s = jnp.einsum('bgtd,bgsd->bgts', qc, kc) * jnp.exp(dlog - m_t[..., None])
        gi = jnp.exp(inter - m_t)
        num = gi[..., None] * jnp.einsum('bgtd,bgde->bgte', qc, c_st) + jnp.einsum('bgts,bgse->bgte', s, vc)
        den = gi * jnp.einsum('bgtd,bgd->bgt', qc, n_st) + jnp.sum(s, axis=-1)
        h = num / jnp.maximum(jnp.abs(den), jnp.exp(-m_t))[..., None]
        return (c_new, n_new, m_new), h

    state, h = lax.scan(step, state, (chunks(q), chunks(k), chunks(v), chunks(li), chunks(lf)))
    if need_out:
        h = jnp.moveaxis(h, 0, 2).reshape(b_sz, g, t_len, dh)
    return h, state


def _mlstm_branch(p, conv_w, conv_b, gate_b, norm_g, state, need_out):
    b_sz, t_len, _ = p.shape
    qk = jax.nn.silu(_dwconv(p[..., :2 * ML_W], conv_w, conv_b))

    def heads(a):
        return a.astype(jnp.float32).reshape(b_sz, t_len, ML_HEADS, ML_DH).transpose(0, 2, 1, 3)

    q = heads(qk[..., :ML_W])
    k = heads(qk[..., ML_W:]) * (ML_DH ** -0.5)
    v = heads(p[..., 2 * ML_W:3 * ML_W])
    gates = (p[..., 3 * ML_W:ML_STATE_COLS] + gate_b).astype(jnp.float32).transpose(0, 2, 1)
    li = gates[:, :2 * ML_HEADS]
    lf = jax.nn.log_sigmoid(gates[:, 2 * ML_HEADS:])

    def both(a):
        return jnp.concatenate([a, jnp.flip(a, axis=2)], axis=1)

    def dirs(a):
        return jnp.concatenate([a[:, :ML_HEADS], jnp.flip(a[:, ML_HEADS:], axis=2)], axis=1)

    h2, state = _mlstm_scan(both(q), both(k), both(v), dirs(li), dirs(lf), state, need_out)
    if not need_out:
        return None, state
    h = h2[:, :ML_HEADS] + jnp.flip(h2[:, ML_HEADS:], axis=2)
    h = h * lax.rsqrt(jnp.mean(h * h, axis=-1, keepdims=True) + EPS) * norm_g.astype(jnp.float32)[None, :, None, :]
    h = h.transpose(0, 2, 1, 3).reshape(b_sz, t_len, ML_W)
    o = jax.nn.sigmoid(p[..., ML_STATE_COLS:ML_COLS].astype(jnp.float32))
    return (h * o).astype(p.dtype), state


def _gmlp_branch(p, ln_g, ln_b, w_s, b_s):
    b_sz, t_len, _ = p.shape
    z = jax.nn.gelu(p)
    u, v = z[..., :GM_W], z[..., GM_W:]
    v = _layernorm(v, ln_g, ln_b).reshape(b_sz, t_len // GM_CHUNK, GM_CHUNK, GM_GROUPS, GM_DG)
    s = jnp.einsum('gts,bnsgc->bntgc', w_s, v) + b_s.T[:, :, None]
    return u * s.reshape(b_sz, t_len, GM_W)


def _conv_branch(p, dw_w, dw_b, ln_g, ln_b):
    z = p[..., :CV_W] * jax.nn.sigmoid(p[..., CV_W:])
    z = _dwconv(z, dw_w, dw_b)
    return jax.nn.silu(_layernorm(z, ln_g, ln_b))


def _fourier_branch(p):
    b_sz, t_len, _ = p.shape
    z = p.astype(jnp.float32).reshape(b_sz, t_len, FT_GROUPS, FT_DG)
    z = jnp.fft.fft2(z, axes=(1, 3), norm='ortho').real
    return z.reshape(b_sz, t_len, FT_W).astype(p.dtype)


def _mixer_merge(p, ml_out, gm_ln_g, gm_ln_b, gm_w_s, gm_b_s, cv_dw_w, cv_dw_b, cv_ln_g, cv_ln_b, w_branch, w_out):
    branches = (ml_out,
                _gmlp_branch(p[..., GM_OFF:CV_OFF], gm_ln_g, gm_ln_b, gm_w_s, gm_b_s),
                _conv_branch(p[..., CV_OFF:FT_OFF], cv_dw_w, cv_dw_b, cv_ln_g, cv_ln_b),
                _fourier_branch(p[..., FT_OFF:GATE_OFF]))
    y = None
    for i, z in enumerate(branches):
        gate = jax.nn.sigmoid(p[..., GATE_OFF + i * D_MODEL:GATE_OFF + (i + 1) * D_MODEL])
        term = gate * (z @ w_branch[i])
        y = term if y is None else y + term
    return y @ w_out


def _peer(h, w_q, keys, u_tab, v_tab):
    b_sz, t_len, d = h.shape
    hb_all = h.reshape(-1, PEER_BLOCK, d)

    def block(hb):
        n = hb.shape[0]
        q = (hb @ w_q).reshape(n, PEER_HEADS, 2, PEER_DK_HALF)
        s = jnp.einsum('nhpk,pek->nhpe', q, keys)
        v_top, i_top = lax.top_k(s, PEER_TOPK)
        cand = (v_top[:, :, 0, :, None] + v_top[:, :, 1, None, :]).reshape(n, PEER_HEADS, PEER_TOPK * PEER_TOPK)
        sc, pos = lax.top_k(cand, PEER_TOPK)
        i1 = jnp.take_along_axis(i_top[:, :, 0], pos // PEER_TOPK, axis=-1)
        i2 = jnp.take_along_axis(i_top[:, :, 1], pos % PEER_TOPK, axis=-1)
        e = i1 * N_KEYS + i2
        g = jax.nn.softmax(sc.astype(jnp.float32), axis=-1).astype(hb.dtype)
        a = jax.nn.gelu(jnp.einsum('nhkd,nd->nhk', u_tab[e], hb)) * g
        return jnp.einsum('nhk,nhkd->nd', a, v_tab[e])

    return lax.map(block, hb_all).reshape(b_sz, t_len, d)


def setup_inputs(seed: int = 0) -> dict:
    key = jax.random.key(seed)
    ks = jax.random.split(key, 32)
    f32 = jnp.float32
    L, D = DEPTH, D_MODEL

    def nrm(k, shape, s):
        return jax.random.normal(k, shape, f32) * s

    def gain(k, shape):
        return 1.0 + nrm(k, shape, 0.02)

    ml_gate_b = jnp.concatenate([nrm(ks[11], (L, 2 * ML_HEADS), 0.1),
                                 jax.random.uniform(ks[12], (L, 2 * ML_HEADS), f32, 3.0, 6.0)], axis=-1)
    return {
        'x': nrm(ks[0], (BATCH, SEQ, D), 1.0),
        'c': nrm(ks[1], (BATCH, D), 1.0),
        'ctx': nrm(ks[2], (BATCH, CTX_LEN, D), 1.0),
        'c_ctx': nrm(ks[3], (D,), 1.0),
        'w_ada': nrm(ks[4], (L, D, 6 * D), 0.5 * D ** -0.5),
        'b_ada': nrm(ks[5], (L, 6 * D), 0.02),
        'norm1_g': gain(ks[6], (L, D)),
        'norm2_g': gain(ks[7], (L, D)),
        'w_in': nrm(ks[8], (L, D, IN_COLS), D ** -0.5),
        'ml_conv_w': nrm(ks[9], (L, ML_CONV, 2 * ML_W), ML_CONV ** -0.5),
        'ml_conv_b': nrm(ks[10], (L, 2 * ML_W), 0.02),
        'ml_gate_b': ml_gate_b,
        'ml_norm_g': gain(ks[13], (L, ML_HEADS, ML_DH)),
        'gm_ln_g': gain(ks[14], (L, GM_W)),
        'gm_ln_b': nrm(ks[15], (L, GM_W), 0.02),
        'gm_w_s': nrm(ks[16], (L, GM_GROUPS, GM_CHUNK, GM_CHUNK), GM_CHUNK ** -0.5),
        'gm_b_s': gain(ks[17], (L, GM_GROUPS, GM_CHUNK)),
        'cv_dw_w': nrm(ks[18], (L, CV_K, CV_W), CV_K ** -0.5),
        'cv_dw_b': nrm(ks[19], (L, CV_W), 0.02),
        'cv_ln_g': gain(ks[20], (L, CV_W)),
        'cv_ln_b': nrm(ks[21], (L, CV_W), 0.02),
        'w_branch': nrm(ks[22], (L, N_BRANCH, MIX_W, D), MIX_W ** -0.5),
        'w_out': nrm(ks[23], (L, D, D), D ** -0.5),
        'peer_w_q': nrm(ks[24], (L, D, PEER_HEADS * PEER_DQ), D ** -0.5),
        'peer_keys': nrm(ks[25], (L, 2, N_KEYS, PEER_DK_HALF), PEER_DK_HALF ** -0.5),
        'peer_u': nrm(ks[26], (L, N_EXPERTS, D), D ** -0.5),
        'peer_v': nrm(ks[27], (L, N_EXPERTS, D), PEER_HEADS ** -0.5),
        'final_norm_g': gain(ks[28], (D,)),
    }


def reference(x, c, ctx, c_ctx, w_ada, b_ada, norm1_g, norm2_g, w_in, ml_conv_w, ml_conv_b, ml_gate_b,
              ml_norm_g, gm_ln_g, gm_ln_b, gm_w_s, gm_b_s, cv_dw_w, cv_dw_b, cv_ln_g, cv_ln_b, w_branch,
              w_out, peer_w_q, peer_keys, peer_u, peer_v, final_norm_g):
    b_sz, n_tok, d = x.shape
    x = x + _grid_sincos(n_tok, d).astype(x.dtype)[None]
    xc = ctx
    s_lat = jax.nn.silu(c)[:, None, :]
    s_ctx = jax.nn.silu(c_ctx)
    for l in range(DEPTH):
        last = l == DEPTH - 1
        mod = jnp.split(s_lat @ w_ada[l] + b_ada[l], 6, axis=-1)
        modc = jnp.split(s_ctx @ w_ada[l] + b_ada[l], 6, axis=-1)
        h = _modulate(_rmsnorm(x, norm1_g[l]), mod[0], mod[1])
        hc = _modulate(_rmsnorm(xc, norm1_g[l]), modc[0], modc[1])
        p = h @ w_in[l]
        pc = hc @ (w_in[l][:, :ML_STATE_COLS] if last else w_in[l])
        ml_args = (ml_conv_w[l], ml_conv_b[l], ml_gate_b[l], ml_norm_g[l])
        ml_c, state = _mlstm_branch(pc, *ml_args, _zero_state(b_sz), not last)
        ml_x, _ = _mlstm_branch(p, *ml_args, state, True)
        mix_args = (gm_ln_g[l], gm_ln_b[l], gm_w_s[l], gm_b_s[l], cv_dw_w[l], cv_dw_b[l],
                    cv_ln_g[l], cv_ln_b[l], w_branch[l], w_out[l])
        x = x + mod[2] * _mixer_merge(p, ml_x, *mix_args)
        peer_args = (peer_w_q[l], peer_keys[l], peer_u[l], peer_v[l])
        h = _modulate(_rmsnorm(x, norm2_g[l]), mod[3], mod[4])
        x = x + mod[5] * _peer(h, *peer_args)
        if not last:
            xc = xc + modc[2] * _mixer_merge(pc, ml_c, *mix_args)
            hc = _modulate(_rmsnorm(xc, norm2_g[l]), modc[3], modc[4])
            xc = xc + modc[5] * _peer(hc, *peer_args)
    return _rmsnorm(x, final_norm_g)
```

```python
import contextlib
import numpy as np
import ml_dtypes
import concourse.bass as bass
import concourse.mybir as mybir
from concourse.bass_utils import run_bass_kernel_spmd

F32 = mybir.dt.float32
BF16 = mybir.dt.bfloat16
I32 = mybir.dt.int32
U32 = mybir.dt.uint32
AF = mybir.ActivationFunctionType
ALU = mybir.AluOpType
AX = mybir.AxisListType

COMPUTE = ('pe', 'act', 'dve', 'pool')
NRING = 12
WRITE_KW = ('out', 'accum_out', 'ap')
EPS = 1e-6
NEG = -1.0e30


class V:
    __slots__ = ('buf', 'ap')

    def __init__(self, buf, ap):
        self.buf = buf
        self.ap = ap

    def __getitem__(self, k):
        return V(self.buf, self.ap[k])

    def re(self, pat, **kw):
        return V(self.buf, self.ap.rearrange(pat, **kw))

    def bc(self, shape):
        return V(self.buf, self.ap.to_broadcast(list(shape)))

    def unsq(self, ax):
        return V(self.buf, self.ap.unsqueeze(ax))

    def pbc(self, n):
        return V(self.buf, self.ap.partition_broadcast(n))


class Buf:
    __slots__ = ('t', 'lw', 'rd', 'name')

    def __init__(self, t, name=''):
        self.t = t
        self.lw = None
        self.rd = {}
        self.name = name

    def __getitem__(self, k):
        return V(self, self.t[k])

    @property
    def a(self):
        return V(self, self.t[:])

    def sub(self):
        return Buf(self.t, self.name + '_s')


class Prog:
    def __init__(self, nc):
        self.nc = nc
        self.ops = {e: [] for e in ('pe', 'act', 'dve', 'pool', 'sp')}
        self.count = {e: 0 for e in COMPUTE}
        self.known = {e: {} for e in self.ops}
        self.ndma = {'sp': 0, 'pool': 0, 'act': 0}
        self.es = contextlib.ExitStack()
        self.sems = {}
        for e in COMPUTE:
            self.sems[('c', e)] = self.es.enter_context(nc.semaphore('s_' + e))
        for q in ('sp', 'pool', 'act'):
            for i in range(NRING):
                self.sems[('d', q, i)] = self.es.enter_context(nc.semaphore('d_%s_%d' % (q, i)))
        self.nbuf = 0
        self.ninst = 0
        self.deferred = None

    def sb(self, shape, dt, name=None, es=None):
        self.nbuf += 1
        name = '%s_%d' % (name or 'sb', self.nbuf)
        t = (es or self.es).enter_context(self.nc.sbuf_tensor(name, list(shape), dt))
        return Buf(t, name)

    def ps(self, shape, dt, name=None, es=None):
        self.nbuf += 1
        name = '%s_%d' % (name or 'ps', self.nbuf)
        t = (es or self.es).enter_context(self.nc.psum_tensor(name, list(shape), dt))
        return Buf(t, name)

    def dram(self, name, shape, dt, kind='Internal'):
        t = self.nc.dram_tensor(name, list(shape), dt, kind=kind)
        return Buf(t, name)

    def _need(self, eng, tok, waits):
        if tok is None:
            return
        k, v = tok
        if self.known[eng].get(k, 0) >= v:
            return
        if waits.get(k, 0) < v:
            waits[k] = v

    def emit(self, eng, fn, reads=(), writes=(), dma=False):
        waits = {}
        own = ('c', eng) if (eng in COMPUTE and not dma) else None
        for b in reads:
            if b.lw is not None:
                if own is not None and b.lw[0] == own and eng == 'pe':
                    continue
                self._need(eng, b.lw, waits)
        for b in writes:
            if b.lw is not None:
                if not (own is not None and b.lw[0] == own and eng == 'pe'):
                    self._need(eng, b.lw, waits)
            for k, v in b.rd.items():
                if own is not None and k == own and eng == 'pe':
                    continue
                self._need(eng, (k, v), waits)
        if dma:
            i = self.ndma[eng]
            self.ndma[eng] = i + 1
            slot, gen = i % NRING, i // NRING
            key = ('d', eng, slot)
            if gen > 0:
                self._need(eng, (key, 16 * gen), waits)
            tok = (key, 16 * (gen + 1))
            inc = (key, 16)
        else:
            self.count[eng] += 1
            tok = (own, self.count[eng])
            inc = (own, 1)
        for k, v in waits.items():
            self.known[eng][k] = v
        self.ops[eng].append((fn, list(waits.items()), inc))
        self.ninst += 1
        for b in writes:
            b.lw = tok
            b.rd = {}
        for b in reads:
            if b in writes:
                continue
            if b.rd.get(tok[0], 0) < tok[1]:
                b.rd[tok[0]] = tok[1]
        return tok

    def run_deferred(self, lst, k=None):
        k = len(lst) if k is None else min(k, len(lst))
        for _ in range(k):
            eng, name, xr, xw, kw = lst.pop(0)
            self.op(eng, name, xr=xr, xw=xw, **kw)

    def op(self, eng, name, *, xr=(), xw=(), **kw):
        if self.deferred is not None:
            self.deferred.append((eng, name, xr, xw, kw))
            return None
        reads, writes, real = list(xr), list(xw), {}
        for k, v in kw.items():
            if isinstance(v, V):
                (writes if k in WRITE_KW else reads).append(v.buf)
                real[k] = v.ap
            else:
                real[k] = v
        isdma = name == 'dma_start'

        def fn(e, name=name, real=real):
            return getattr(e, name)(**real)
        return self.emit(eng, fn, reads, writes, dma=isdma)

    def dma(self, out, in_, q='sp', **kw):
        return self.op(q, 'dma_start', out=out, in_=in_, **kw)

    def mm(self, out, lhsT, rhs, start=True, stop=True):
        return self.op('pe', 'matmul', out=out, lhsT=lhsT, rhs=rhs, start=start, stop=stop)

    def barrier(self):
        toks = []
        for e in COMPUTE:
            if self.count[e] > 0:
                toks.append((('c', e), self.count[e]))
        for q, n in self.ndma.items():
            for i in range(max(0, n - NRING), n):
                toks.append((('d', q, i % NRING), 16 * (i // NRING + 1)))
        for e in self.ops:
            waits = {}
            for tok in toks:
                if tok[0] == ('c', e):
                    continue
                self._need(e, tok, waits)
            for k, v in waits.items():
                self.known[e][k] = v
            if waits:
                self.ops[e].append((None, list(waits.items()), None))

    def finish(self):
        self.barrier()
        nc = self.nc
        sems = self.sems
        ops = self.ops

        def replay(name, e):
            for fn, waits, inc in ops[name]:
                for k, v in waits:
                    e.wait_ge(sems[k], v)
                if fn is None:
                    continue
                ins = fn(e)
                if inc is not None:
                    ins.then_inc(sems[inc[0]], inc[1])

        with nc.Block() as block:
            @block.sync
            def _(e):
                replay('sp', e)

            @block.tensor
            def _(e):
                replay('pe', e)

            @block.scalar
            def _(e):
                replay('act', e)

            @block.vector
            def _(e):
                replay('dve', e)

            @block.gpsimd
            def _(e):
                replay('pool', e)
        self.es.close()


class Rot:
    def __init__(self, bufs):
        self.bufs = bufs
        self.i = 0

    def get(self):
        b = self.bufs[self.i % len(self.bufs)]
        self.i += 1
        return b


D = 1024
KD = 8
IN_COLS = 6416
GM_OFF = 1040
CV_OFF = 1552
FT_OFF = 2064
GATE_OFF = 2320
NEXP = 16384


class Cfg:
    def __init__(self, depth=4, t_lat=4096, t_ctx=256):
        self.depth = depth
        self.t_lat = t_lat
        self.t_ctx = t_ctx
        self.nt = t_lat + t_ctx
        self.groups = [(0, t_ctx, 1)] + [(t_ctx + i * 512, 512, 0) for i in range(t_lat // 512)]
        self.groups256 = [(i * 256, 256, 1 if i * 256 < t_ctx else 0) for i in range(self.nt // 256)]
        self.segs = [(0, t_ctx, 1), (t_ctx, t_lat, 0)]
        self.nch = self.nt // 64


def host_consts(cfg):
    T = cfg.t_lat
    k = np.arange(T, dtype=np.float64)
    ang = 2.0 * np.pi * ((k[:, None] * k[None, :]) % T) / T
    dftc = (np.cos(ang) / np.sqrt(T)).astype(ml_dtypes.bfloat16)
    dfts = (-np.sin(ang) / np.sqrt(T)).astype(ml_dtypes.bfloat16)
    c = np.arange(64, dtype=np.float64)
    a64 = 2.0 * np.pi * ((c[:, None] * c[None, :]) % 64) / 64
    cd = np.zeros((256, 512), np.float64)
    for g in range(4):
        cd[g * 64:(g + 1) * 64, g * 64:(g + 1) * 64] = np.cos(a64) / 8.0
        cd[g * 64:(g + 1) * 64, 256 + g * 64:256 + (g + 1) * 64] = np.sin(a64) / 8.0
    cd = cd.astype(ml_dtypes.bfloat16)
    cst = np.zeros((128, 1024), np.float32)
    cst[:, 0:128] = np.eye(128, dtype=np.float32)
    cst[:, 128:256] = np.arange(128, dtype=np.float32)[None, :]
    s = np.arange(64)
    cst[0:64, 256:320] = (s[:, None] <= s[None, :]).astype(np.float32)
    cst[0:64, 320:384] = (s[:, None] >= s[None, :]).astype(np.float32)
    for g in range(8):
        row = g if g < 4 else 32 + (g - 4)
        cst[row, 384 + g * 64:384 + (g + 1) * 64] = 1.0
    cst[:, 896:912] = np.arange(16, dtype=np.float32)[None, :]
    cst[:, 912] = EPS
    cst[:, 913] = 1.0
    return dftc, dfts, cd, cst


def grid_sincos(n_tok, d):
    rows = n_tok // 64
    n_freq = d // 4
    freq = (1.0 / (10000.0 ** (np.arange(n_freq, dtype=np.float32) / np.float32(n_freq)))).astype(np.float32)
    r = np.repeat(np.arange(rows, dtype=np.float32), 64)
    cc = np.tile(np.arange(64, dtype=np.float32), rows)
    ar = r[:, None] * freq[None, :]
    ac = cc[:, None] * freq[None, :]
    return np.concatenate([np.sin(ar), np.cos(ar), np.sin(ac), np.cos(ac)], axis=-1).astype(np.float32)


class MK:
    def __init__(self, cfg, debug=False):
        self.cfg = cfg
        self.nc = bass.Bass("TRN2", target_bir_lowering=False)
        self.P = Prog(self.nc)
        self.debug = debug
        P = self.P
        L = cfg.depth
        NT = cfg.nt
        din = lambda name, shape, dt=F32: P.dram(name, shape, dt, kind="ExternalInput")
        self.xin = din("xin", [NT, D])
        self.pos = din("pos", [cfg.t_lat, D])
        self.cvec = din("cvec", [2, D])
        self.w_ada = din("w_ada", [L, D, 6 * D])
        self.b_ada = din("b_ada", [L, 6 * D])
        self.norm1_g = din("norm1_g", [L, D])
        self.norm2_g = din("norm2_g", [L, D])
        self.w_in = din("w_in", [L, D, IN_COLS])
        self.ml_conv_w = din("ml_conv_w", [L, 3, 512])
        self.ml_conv_b = din("ml_conv_b", [L, 512])
        self.ml_gate_b = din("ml_gate_b", [L, 16])
        self.ml_norm_g = din("ml_norm_g", [L, 256])
        self.gm_ln_g = din("gm_ln_g", [L, 256])
        self.gm_ln_b = din("gm_ln_b", [L, 256])
        self.gm_w_s = din("gm_w_s", [L, 4, 128, 128])
        self.gm_b_s = din("gm_b_s", [L, 512])
        self.cv_dw_w = din("cv_dw_w", [L, 31, 256])
        self.cv_dw_b = din("cv_dw_b", [L, 256])
        self.cv_ln_g = din("cv_ln_g", [L, 256])
        self.cv_ln_b = din("cv_ln_b", [L, 256])
        self.w_branch = din("w_branch", [L, 4, 256, D])
        self.w_out = din("w_out", [L, D, D])
        self.peer_w_q = din("peer_w_q", [L, D, 2048])
        self.peer_keys = din("peer_keys", [L, 2, 128, 128])
        self.peer_u = din("peer_u", [L, NEXP, D])
        self.peer_v = din("peer_v", [L, NEXP, D])
        self.final_norm_g = din("final_norm_g", [D])
        self.dftc = din("dftc", [cfg.t_lat, cfg.t_lat], BF16)
        self.dfts = din("dfts", [cfg.t_lat, cfg.t_lat], BF16)
        self.cd = din("cd", [256, 512], BF16)
        self.cst_d = din("cst", [128, 1024])
        self.out = P.dram("out", [cfg.t_lat, D], F32, kind="ExternalOutput")
        sk = "ExternalOutput" if debug else "Internal"
        self.xT = P.dram("xT", [KD, 128, NT], F32, kind=sk)
        self.hT = P.dram("hT", [KD, 128, NT], BF16, kind=sk)
        self.Z = P.dram("Z", [4, 256, NT], BF16, kind=sk)
        self.Hf = P.dram("Hf", [cfg.nch, 64, 256], F32, kind=sk)
        self.Hb = P.dram("Hb", [cfg.nch, 64, 256], F32, kind=sk)
        self.UT = P.dram("UT", [128, 128, KD * 128], BF16)
        self.VB = P.dram("VB", [128, 128, D], BF16)
        self.cst = P.sb([128, 1024], F32, "cst")
        P.dma(self.cst.a, self.cst_d.a)
        c = self.cst
        self.identf = c[:, 0:128]
        self.iota128 = c[:, 128:256]
        self.iota16 = c[:, 896:912]
        self.eps = c[:, 912:913]
        self.identb_t = P.sb([128, 128], BF16, "identb")
        P.op('dve', 'tensor_copy', out=self.identb_t.a, in_=self.identf)
        self.identb = self.identb_t.a
        self.onesb_t = P.sb([128, 128], BF16, "onesb")
        P.op('pool', 'memset', ap=self.onesb_t.a, constant=1.0)
        self.onesb = self.onesb_t.a
        self.iota128b_t = P.sb([128, 128], BF16, "iota128b")
        P.op('dve', 'tensor_copy', out=self.iota128b_t.a, in_=self.iota128)
        self.onesf_t = P.sb([128, 128], F32, "onesf")
        P.op('pool', 'memset', ap=self.onesf_t.a, constant=1.0)
        self.onesf = self.onesf_t.a
        self.maskb_t = P.sb([64, 2, 4, 64], BF16, "maskb")
        for d_ in range(2):
            for h in range(4):
                P.op('dve', 'tensor_copy', out=self.maskb_t[:, d_, h, :], in_=c[0:64, 256 + 64 * d_:320 + 64 * d_])
        self.modT = P.sb([128, 48, 2], F32, "modT")
        cT = P.sb([2, D], F32, "cT")
        P.dma(cT.a, self.cvec.a)
        self.sT = P.sb([128, 2, KD], BF16, "sT")
        es = contextlib.ExitStack()
        ps = P.ps([128, 512], F32, es=es)
        for k in range(KD):
            P.op('pe', 'transpose', out=ps[:, 2 * k:2 * k + 2], in_=cT[:, k * 128:(k + 1) * 128], identity=self.identf[0:2, 0:2])
        P.op('act', 'activation', out=self.sT.a, in_=ps[:, 0:16].re("p (k s) -> p s k", s=2), func=AF.Silu)
        es.close()
        P.barrier()

    def load_rows_T(self, rows, es):
        P = self.P
        R = sum(v.ap.shape[0] for v in rows)
        w = rows[0].ap.shape[1]
        assert R <= 128
        rt = P.sb([128, 128], F32, "rows", es=es)
        r0 = 0
        for v in rows:
            r = v.ap.shape[0]
            P.dma(rt[r0:r0 + r, 0:w], v)
            r0 += r
        es_ = contextlib.ExitStack()
        ps = P.ps([128, 512], F32, es=es_)
        P.op('pe', 'transpose', out=ps[0:w, 0:R], in_=rt[0:R, 0:w], identity=self.identf[0:R, 0:R])
        ct = P.sb([128, R], F32, "colsT", es=es)
        P.op('dve', 'tensor_copy', out=ct[0:w, :], in_=ps[0:w, 0:R])
        P.barrier()
        es_.close()
        return ct

    def load_w(self, dst, src, q='pool'):
        self.P.dma(dst, src, q=q)

    def phase_init(self):
        P, cfg = self.P, self.cfg
        es = contextlib.ExitStack()
        xt = Rot([P.sb([128, D], F32, "xt", es=es) for _ in range(2)])
        pt = Rot([P.sb([128, D], F32, "pt", es=es) for _ in range(2)])
        pss = Rot([P.ps([128, 512], F32, es=es) for _ in range(4)])
        st = Rot([P.sb([128, KD, 128], F32, "xst", es=es) for _ in range(2)])
        for ti in range(cfg.nt // 128):
            x = xt.get()
            P.dma(x.a, self.xin[ti * 128:(ti + 1) * 128, :])
            if ti * 128 >= cfg.t_ctx:
                p = pt.get()
                r0 = ti * 128 - cfg.t_ctx
                P.dma(p.a, self.pos[r0:r0 + 128, :])
                P.op('dve', 'tensor_tensor', out=x.a, in0=x.a, in1=p.a, op=ALU.add)
            s = st.get()
            for half in range(2):
                ps = pss.get()
                for j in range(4):
                    k = half * 4 + j
                    P.op('pe', 'transpose', out=ps[:, j * 128:(j + 1) * 128], in_=x[:, k * 128:(k + 1) * 128], identity=self.identf)
                P.op('act', 'activation', out=s[:, half * 4:half * 4 + 4, :], in_=ps.a.re("p (j t) -> p j t", j=4), func=AF.Copy)
            P.dma(self.xT[:, :, ti * 128:(ti + 1) * 128].re("k p t -> p k t"), s.a)
        es.close()
        P.barrier()

    def phase_mod(self, l):
        P = self.P
        es = contextlib.ExitStack()
        wt = Rot([P.sb([128, KD, 1536], BF16, "wada", es=es) for _ in range(2)])
        ps = P.ps([128, 48, 2], F32, es=es)
        bT = self.load_rows_T([self.b_ada[l].re("(j p) -> j p", p=128)], es)
        for cg in range(4):
            w = wt.get()
            for k in range(KD):
                self.load_w(w[:, k, :], self.w_ada[l, k * 128:(k + 1) * 128, cg * 1536:(cg + 1) * 1536])
            for jj in range(12):
                j = cg * 12 + jj
                for k in range(KD):
                    P.mm(ps[:, j, :], w[:, k, jj * 128:(jj + 1) * 128], self.sT[:, :, k], start=(k == 0), stop=(k == KD - 1))
        P.op('dve', 'tensor_tensor', out=self.modT.a, in0=ps.a, in1=bT.a.unsq(2).bc([128, 48, 2]), op=ALU.add)
        es.close()
        P.barrier()

    def mod_scale_shift(self, l, which, es):
        P = self.P
        g = self.norm1_g if which == 0 else self.norm2_g
        gT = self.load_rows_T([g[l].re("(j p) -> j p", p=128)], es)
        base = 0 if which == 0 else 24
        gs = P.sb([128, KD, 2], F32, "gs", es=es)
        P.op('dve', 'tensor_scalar', out=gs.a, in0=self.modT[:, base + 8:base + 16, :], scalar1=1.0, scalar2=None, op0=ALU.add)
        P.op('dve', 'tensor_tensor', out=gs.a, in0=gs.a, in1=gT.a.unsq(2).bc([128, KD, 2]), op=ALU.mult)
        return gs, self.modT[:, base:base + 8, :], self.modT[:, base + 16:base + 24, :]

    def norm_group(self, xg, n, gs, sh, seg, hout, pss, tmps, sq, rsb):
        P = self.P
        P.op('act', 'activation', out=sq[:, :, 0:n], in_=xg, func=AF.Square)
        ps = pss.get()
        for k in range(KD):
            P.mm(ps[:, 0:n], self.onesb, sq[:, k, 0:n], start=(k == 0), stop=(k == KD - 1))
        rs = rsb
        P.op('act', 'activation', out=rs[:, 0:n], in_=ps[:, 0:n], func=AF.Sqrt, scale=1.0 / D, bias=self.eps)
        P.op('dve', 'reciprocal', out=rs[:, 0:n], in_=rs[:, 0:n])
        for k in range(KD):
            t = tmps.get()
            P.op('dve', 'scalar_tensor_tensor', out=t[:, 0:n], in0=xg[:, k, :], scalar=gs[:, k, seg:seg + 1], in1=rs[:, 0:n], op0=ALU.mult, op1=ALU.mult)
            if sh is None:
                P.op('act', 'activation', out=hout[:, k, :], in_=t[:, 0:n], func=AF.Copy)
            else:
                P.op('act', 'activation', out=hout[:, k, :], in_=t[:, 0:n], func=AF.Identity, bias=sh[:, k, seg:seg + 1])

    def phase_norm1(self, l):
        P, cfg = self.P, self.cfg
        es = contextlib.ExitStack()
        gs, sh, _ = self.mod_scale_shift(l, 0, es)
        xg = Rot([P.sb([128, KD, 512], F32, "xg", es=es) for _ in range(2)])
        hg = Rot([P.sb([128, KD, 512], BF16, "hg", es=es) for _ in range(2)])
        sq = P.sb([128, KD, 512], BF16, "sq", es=es)
        pss = Rot([P.ps([128, 512], F32, es=es) for _ in range(2)])
        tmps = Rot([P.sb([128, 512], F32, "nt", es=es) for _ in range(4)])
        rsb = P.sb([128, 512], F32, "rsb", es=es)
        for (n0, n, seg) in cfg.groups:
            x = xg.get()
            h = hg.get()
            P.dma(x[:, :, 0:n], self.xT[:, :, n0:n0 + n].re("k p t -> p k t"))
            self.norm_group(x[:, :, 0:n], n, gs, sh, seg, h[:, :, 0:n], pss, tmps, sq, rsb)
            P.dma(self.hT[:, :, n0:n0 + n].re("k p t -> p k t"), h[:, :, 0:n], q='pool')
        es.close()
        P.barrier()


def _gm(self, l):
    P, cfg = self.P, self.cfg
    es = contextlib.ExitStack()
    W = P.sb([128, KD, 512], BF16, "wgm", es=es)
    for k in range(KD):
        self.load_w(W[:, k, :], self.w_in[l, k * 128:(k + 1) * 128, GM_OFF:GM_OFF + 512])
    lng = P.sb([128, 256], F32, "lng", es=es)
    lnb = P.sb([128, 256], F32, "lnb", es=es)
    P.dma(lng.a, self.gm_ln_g[l:l + 1, :].pbc(128))
    P.dma(lnb.a, self.gm_ln_b[l:l + 1, :].pbc(128))
    bsr = P.sb([1, 512], F32, "bsr", es=es)
    P.dma(bsr.a, self.gm_b_s[l:l + 1, :])
    wsT = P.sb([128, 4, 128], BF16, "wsT", es=es)
    wsl = P.sb([128, 4, 128], BF16, "wsl", es=es)
    self.load_w(wsl.a, self.gm_w_s[l].re("g t s -> t g s"))
    pst = P.ps([128, 4, 128], BF16, es=es)
    for g in range(4):
        P.op('pe', 'transpose', out=pst[:, g, :], in_=wsl[:, g, :], identity=self.identb)
    P.op('dve', 'tensor_copy', out=wsT.a, in_=pst.a)
    hg = Rot([P.sb([128, KD, 512], BF16, "hg", es=es) for _ in range(2)])
    u64 = Rot([P.sb([64, 4, 512], BF16, "u64", es=es) for _ in range(2)])
    zst = Rot([P.sb([64, 4, 512], BF16, "zst", es=es) for _ in range(2)])
    psu = Rot([P.ps([128, 512], F32, es=es) for _ in range(2)])
    psv = Rot([P.ps([128, 512], F32, es=es) for _ in range(2)])
    pss = Rot([P.ps([64, 4, 128], F32, es=es) for _ in range(2)])
    vt = Rot([P.sb([128, 256], F32, "vt", es=es) for _ in range(2)])
    vn = Rot([P.sb([128, 256], BF16, "vn", es=es) for _ in range(2)])
    st6 = Rot([P.sb([128, 8], F32, "st6", es=es) for _ in range(2)])
    for (n0, n, seg) in cfg.groups:
        if self.skip_ctx and seg == 1:
            continue
        h = hg.get()
        P.dma(h[:, :, 0:n], self.hT[:, :, n0:n0 + n].re("k p t -> p k t"))
        u = u64.get()
        for g in range(4):
            ps = psu.get()
            for k in range(KD):
                P.mm(ps[0:64, 0:n], W[:, k, g * 64:(g + 1) * 64], h[:, k, 0:n], start=(k == 0), stop=(k == KD - 1))
            P.op('act', 'activation', out=u[:, g, 0:n], in_=ps[0:64, 0:n], func=AF.Gelu_apprx_tanh)
        z = zst.get()
        for sub in range(n // 128):
            ps = psv.get()
            for k in range(KD):
                P.mm(ps[:, 0:256], h[:, k, sub * 128:(sub + 1) * 128], W[:, k, 256:512], start=(k == 0), stop=(k == KD - 1))
            v = vt.get()
            P.op('act', 'activation', out=v.a, in_=ps[:, 0:256], func=AF.Gelu_apprx_tanh)
            s6 = st6.get()
            P.op('dve', 'bn_stats', out=s6[:, 0:6], in_=v.a)
            P.op('dve', 'bn_aggr', out=s6[:, 6:8], in_=s6[:, 0:6])
            P.op('act', 'activation', out=s6[:, 7:8], in_=s6[:, 7:8], func=AF.Sqrt, bias=self.eps)
            P.op('dve', 'reciprocal', out=s6[:, 7:8], in_=s6[:, 7:8])
            P.op('dve', 'tensor_scalar', out=v.a, in0=v.a, scalar1=s6[:, 6:7], scalar2=s6[:, 7:8], op0=ALU.subtract, op1=ALU.mult)
            P.op('dve', 'tensor_tensor', out=v.a, in0=v.a, in1=lng.a, op=ALU.mult)
            vb = vn.get()
            P.op('dve', 'tensor_tensor', out=vb.a, in0=v.a, in1=lnb.a, op=ALU.add)
            pg = pss.get()
            for g in range(4):
                P.mm(pg[:, g, :], vb[:, g * 64:(g + 1) * 64], wsT[:, g, :], start=True, stop=False)
                P.mm(pg[:, g, :], self.onesf[0:1, 0:64], bsr[0:1, g * 128:(g + 1) * 128], start=False, stop=True)
            P.op('dve', 'tensor_tensor', out=z[:, :, sub * 128:(sub + 1) * 128], in0=pg.a, in1=u[:, :, sub * 128:(sub + 1) * 128], op=ALU.mult)
        P.dma(self.Z[1, :, n0:n0 + n].re("(g p) t -> p g t", p=64), z[:, :, 0:n], q='pool')
    es.close()
    P.barrier()


MK.phase_gmlp = _gm


def _cv(self, l):
    P, cfg = self.P, self.cfg
    es = contextlib.ExitStack()
    PAD = 15
    W = P.sb([128, KD, 512], BF16, "wcv", es=es)
    for k in range(KD):
        self.load_w(W[:, k, :], self.w_in[l, k * 128:(k + 1) * 128, CV_OFF:CV_OFF + 512])
    cols = self.load_rows_T([self.cv_dw_w[l].re("j (c p) -> (j c) p", p=128), self.cv_dw_b[l].re("(c p) -> c p", p=128),
                             self.cv_ln_g[l].re("(c p) -> c p", p=128), self.cv_ln_b[l].re("(c p) -> c p", p=128)], es)
    diag = P.sb([128, 62, 128], BF16, "diag", es=es)
    for jc in range(62):
        P.op('dve', 'tensor_scalar', out=diag[:, jc, :], in0=self.identf, scalar1=cols[:, jc:jc + 1], scalar2=None, op0=ALU.mult)
    hg = Rot([P.sb([128, KD, 512], BF16, "hg", es=es) for _ in range(2)])
    psa = Rot([P.ps([128, 512], F32, es=es) for _ in range(2)])
    psb = Rot([P.ps([128, 512], F32, es=es) for _ in range(2)])
    psc = Rot([P.ps([128, 512], F32, es=es) for _ in range(2)])
    sg = Rot([P.sb([128, 512], F32, "sg", es=es) for _ in range(2)])
    for (s0, sn, seg) in cfg.segs:
        if self.skip_ctx and seg == 1:
            continue
        es2 = contextlib.ExitStack()
        zp = P.sb([128, 2, sn + 2 * PAD], BF16, "zp", es=es2)
        P.op('pool', 'memset', ap=zp.a, constant=0.0)
        y = P.sb([128, 2, 512], F32, "ycv", es=es2)
        y2 = P.sb([128, 2, 512], F32, "ycv2", es=es2)
        mean = P.sb([128, 512], F32, "mean", es=es2)
        rstd = P.sb([128, 512], F32, "rstd", es=es2)
        zo = Rot([P.sb([128, 2, 512], BF16, "zo", es=es2) for _ in range(2)])
        grp = [(a, n) for (a, n, sg_) in cfg.groups if sg_ == seg]
        for (n0, n) in grp:
            h = hg.get()
            P.dma(h[:, :, 0:n], self.hT[:, :, n0:n0 + n].re("k p t -> p k t"))
            for c in range(2):
                pa = psa.get()
                pb = psb.get()
                for k in range(KD):
                    P.mm(pa[:, 0:n], W[:, k, c * 128:(c + 1) * 128], h[:, k, 0:n], start=(k == 0), stop=(k == KD - 1))
                for k in range(KD):
                    P.mm(pb[:, 0:n], W[:, k, 256 + c * 128:256 + (c + 1) * 128], h[:, k, 0:n], start=(k == 0), stop=(k == KD - 1))
                s = sg.get()
                P.op('act', 'activation', out=s[:, 0:n], in_=pb[:, 0:n], func=AF.Sigmoid)
                o0 = PAD + n0 - s0
                P.op('dve', 'tensor_tensor', out=zp[:, c, o0:o0 + n], in0=pa[:, 0:n], in1=s[:, 0:n], op=ALU.mult)
        for (n0, n) in grp:
            o0 = n0 - s0
            for c in range(2):
                pc = psc.get()
                for j in range(31):
                    P.mm(pc[:, 0:n], diag[:, j * 2 + c, :], zp[:, c, o0 + j:o0 + j + n], start=(j == 0), stop=(j == 30))
                P.op('act', 'activation', out=y[:, c, 0:n], in_=pc[:, 0:n], func=AF.Identity, bias=cols[:, 62 + c:63 + c])
                P.op('act', 'activation', out=y2[:, c, 0:n], in_=y[:, c, 0:n], func=AF.Square)
            p1 = psa.get()
            p2 = psb.get()
            for c in range(2):
                P.mm(p1[:, 0:n], self.onesf, y[:, c, 0:n], start=(c == 0), stop=(c == 1))
            for c in range(2):
                P.mm(p2[:, 0:n], self.onesf, y2[:, c, 0:n], start=(c == 0), stop=(c == 1))
            P.op('act', 'activation', out=mean[:, 0:n], in_=p1[:, 0:n], func=AF.Identity, scale=1.0 / 256)
            P.op('dve', 'tensor_tensor', out=rstd[:, 0:n], in0=mean[:, 0:n], in1=mean[:, 0:n], op=ALU.mult)
            P.op('dve', 'scalar_tensor_tensor', out=rstd[:, 0:n], in0=p2[:, 0:n], scalar=1.0 / 256, in1=rstd[:, 0:n], op0=ALU.mult, op1=ALU.subtract)
            P.op('act', 'activation', out=rstd[:, 0:n], in_=rstd[:, 0:n], func=AF.Sqrt, bias=self.eps)
            P.op('dve', 'reciprocal', out=rstd[:, 0:n], in_=rstd[:, 0:n])
            z = zo.get()
            for c in range(2):
                P.op('dve', 'tensor_tensor', out=y[:, c, 0:n], in0=y[:, c, 0:n], in1=mean[:, 0:n], op=ALU.subtract)
                P.op('dve', 'tensor_tensor', out=y[:, c, 0:n], in0=y[:, c, 0:n], in1=rstd[:, 0:n], op=ALU.mult)
                P.op('act', 'activation', out=z[:, c, 0:n], in_=y[:, c, 0:n], func=AF.Silu, scale=cols[:, 64 + c:65 + c], bias=cols[:, 66 + c:67 + c])
            P.dma(self.Z[2, :, n0:n0 + n].re("(c p) t -> p c t", p=128), z[:, :, 0:n], q='pool')
        es2.close()
        P.barrier()
    es.close()
    P.barrier()


MK.phase_conv = _cv


def _ft(self, l):
    P, cfg = self.P, self.cfg
    es = contextlib.ExitStack()
    W = P.sb([128, KD, 256], BF16, "wft", es=es)
    for k in range(KD):
        self.load_w(W[:, k, :], self.w_in[l, k * 128:(k + 1) * 128, FT_OFF:FT_OFF + 256])
    CD = P.sb([128, 2, 512], BF16, "cdt", es=es)
    P.dma(CD.a, self.cd.a.re("(c p) n -> p c n", p=128))
    hg = Rot([P.sb([128, KD, 512], BF16, "hg", es=es) for _ in range(2)])
    zf = Rot([P.sb([128, 2, 512], BF16, "zf", es=es) for _ in range(2)])
    psa = Rot([P.ps([128, 512], F32, es=es) for _ in range(2)])
    psd = Rot([P.ps([128, 512], F32, es=es) for _ in range(4)])
    for (s0, sn, seg) in cfg.segs:
        if self.skip_ctx and seg == 1:
            continue
        es2 = contextlib.ExitStack()
        ntile = sn // 128
        zcs = P.sb([128, ntile, 512], BF16, "zcs", es=es2)
        grp = [(a, n) for (a, n, sg_) in cfg.groups if sg_ == seg]
        for (n0, n) in grp:
            h = hg.get()
            P.dma(h[:, :, 0:n], self.hT[:, :, n0:n0 + n].re("k p t -> p k t"))
            z = zf.get()
            for c in range(2):
                pa = psa.get()
                for k in range(KD):
                    P.mm(pa[:, 0:n], W[:, k, c * 128:(c + 1) * 128], h[:, k, 0:n], start=(k == 0), stop=(k == KD - 1))
                P.op('act', 'activation', out=z[:, c, 0:n], in_=pa[:, 0:n], func=AF.Copy)
            for sub in range(n // 128):
                pd = psd.get()
                for c in range(2):
                    P.mm(pd.a, z[:, c, sub * 128:(sub + 1) * 128], CD[:, c, :], start=(c == 0), stop=(c == 1))
                ti = (n0 - s0) // 128 + sub
                P.op('dve', 'tensor_copy', out=zcs[:, ti, :], in_=pd.a)
        kb_n = min(512, sn)
        nkb = sn // kb_n
        tc_ = P.sb([128, ntile, kb_n], BF16, "tc", es=es2)
        ts_ = P.sb([128, ntile, kb_n], BF16, "ts", es=es2)
        zo = Rot([P.sb([128, 2, 512], BF16, "zo", es=es2) for _ in range(2)])
        rstride = cfg.t_lat // sn
        for kb in range(nkb):
            if rstride == 1:
                P.dma(tc_.a, self.dftc[:, kb * kb_n:(kb + 1) * kb_n].re("(t p) n -> p t n", p=128))
                P.dma(ts_.a, self.dfts[:, kb * kb_n:(kb + 1) * kb_n].re("(t p) n -> p t n", p=128))
            else:
                P.dma(tc_.a, self.dftc.a.re("(r s) n -> r s n", s=rstride)[:, 0, kb * kb_n:(kb + 1) * kb_n].re("(t p) n -> p t n", p=128))
                P.dma(ts_.a, self.dfts.a.re("(r s) n -> r s n", s=rstride)[:, 0, kb * kb_n:(kb + 1) * kb_n].re("(t p) n -> p t n", p=128))
            z = zo.get()
            for c in range(2):
                pd = psd.get()
                for ti in range(ntile):
                    P.mm(pd[:, 0:kb_n], zcs[:, ti, c * 128:(c + 1) * 128], tc_[:, ti, :], start=(ti == 0), stop=False)
                    P.mm(pd[:, 0:kb_n], zcs[:, ti, 256 + c * 128:256 + (c + 1) * 128], ts_[:, ti, :], start=False, stop=(ti == ntile - 1))
                P.op('act', 'activation', out=z[:, c, 0:kb_n], in_=pd[:, 0:kb_n], func=AF.Identity, scale=float(np.sqrt(rstride)))
            P.dma(self.Z[3, :, s0 + kb * kb_n:s0 + (kb + 1) * kb_n].re("(c p) t -> p c t", p=128), z[:, :, 0:kb_n], q='pool')
        es2.close()
        P.barrier()
    es.close()
    P.barrier()


MK.phase_fnet = _ft


def _merge(self, l):
    P, cfg = self.P, self.cfg
    es = contextlib.ExitStack()
    Wg = P.sb([128, KD, 4096], BF16, "wgate", es=es)
    for k in range(KD):
        for i in range(4):
            self.load_w(Wg[:, k, i * 1024:(i + 1) * 1024], self.w_in[l, k * 128:(k + 1) * 128, GATE_OFF + i * 1024:GATE_OFF + (i + 1) * 1024])
    Wb = P.sb([128, 4, 2, 1024], BF16, "wbr", es=es)
    for i in range(4):
        for c in range(2):
            self.load_w(Wb[:, i, c, :], self.w_branch[l, i, c * 128:(c + 1) * 128, :])
    Wo = P.sb([128, KD, 1024], BF16, "wout", es=es)
    for k in range(KD):
        self.load_w(Wo[:, k, :], self.w_out[l, k * 128:(k + 1) * 128, :])
    gate1 = self.modT[:, 16:24, :]
    hg = Rot([P.sb([128, KD, 512], BF16, "hg", es=es) for _ in range(2)])
    zg = Rot([P.sb([128, 4, 2, 512], BF16, "zg", es=es) for _ in range(2)])
    xg = Rot([P.sb([128, KD, 512], F32, "xg", es=es) for _ in range(2)])
    yT = P.sb([128, KD, 512], BF16, "yT", es=es)
    psg = Rot([P.ps([128, 512], F32, es=es) for _ in range(3)])
    psl = Rot([P.ps([128, 512], F32, es=es) for _ in range(3)])
    pso = Rot([P.ps([128, 512], F32, es=es) for _ in range(2)])
    sg = Rot([P.sb([128, 512], F32, "sg", es=es) for _ in range(3)])
    acc = Rot([P.sb([128, 512], F32, "acc", es=es) for _ in range(2)])
    for (n0, n, seg) in cfg.groups:
        if self.skip_ctx and seg == 1:
            continue
        h = hg.get()
        P.dma(h[:, :, 0:n], self.hT[:, :, n0:n0 + n].re("k p t -> p k t"))
        z = zg.get()
        for i in range(4):
            P.dma(z[:, i, :, 0:n], self.Z[i, :, n0:n0 + n].re("(c p) t -> p c t", p=128))
        x = xg.get()
        P.dma(x[:, :, 0:n], self.xT[:, :, n0:n0 + n].re("k p t -> p k t"))
        for dc in range(KD):
            a = acc.get()
            for i in range(4):
                pg = psg.get()
                for k in range(KD):
                    P.mm(pg[:, 0:n], Wg[:, k, i * 1024 + dc * 128:i * 1024 + (dc + 1) * 128], h[:, k, 0:n], start=(k == 0), stop=(k == KD - 1))
                s = sg.get()
                P.op('act', 'activation', out=s[:, 0:n], in_=pg[:, 0:n], func=AF.Sigmoid)
                pl = psl.get()
                for c in range(2):
                    P.mm(pl[:, 0:n], Wb[:, i, c, dc * 128:(dc + 1) * 128], z[:, i, c, 0:n], start=(c == 0), stop=(c == 1))
                if i == 0:
                    P.op('dve', 'tensor_tensor', out=a[:, 0:n], in0=pl[:, 0:n], in1=s[:, 0:n], op=ALU.mult)
                else:
                    P.op('dve', 'tensor_tensor', out=s[:, 0:n], in0=pl[:, 0:n], in1=s[:, 0:n], op=ALU.mult)
                    if i < 3:
                        P.op('pool', 'tensor_tensor', out=a[:, 0:n], in0=a[:, 0:n], in1=s[:, 0:n], op=ALU.add)
                    else:
                        P.op('pool', 'tensor_tensor', out=yT[:, dc, 0:n], in0=a[:, 0:n], in1=s[:, 0:n], op=ALU.add)
        for dc in range(KD):
            po = pso.get()
            for k in range(KD):
                P.mm(po[:, 0:n], Wo[:, k, dc * 128:(dc + 1) * 128], yT[:, k, 0:n], start=(k == 0), stop=(k == KD - 1))
            P.op('dve', 'scalar_tensor_tensor', out=x[:, dc, 0:n], in0=po[:, 0:n], scalar=gate1[:, dc, seg:seg + 1], in1=x[:, dc, 0:n], op0=ALU.mult, op1=ALU.add)
        P.dma(self.xT[:, :, n0:n0 + n].re("k p t -> p k t"), x[:, :, 0:n], q='pool')
    es.close()
    P.barrier()


MK.phase_merge = _merge


def _final(self):
    P, cfg = self.P, self.cfg
    es = contextlib.ExitStack()
    gT = self.load_rows_T([self.final_norm_g.a.re("(j p) -> j p", p=128)], es)
    gs = P.sb([128, KD, 2], F32, "gsf", es=es)
    P.op('dve', 'tensor_copy', out=gs.a, in_=gT.a.unsq(2).bc([128, KD, 2]))
    xg = Rot([P.sb([128, KD, 512], F32, "xg", es=es) for _ in range(2)])
    yg = Rot([P.sb([128, KD, 512], F32, "yg", es=es) for _ in range(2)])
    sq = P.sb([128, KD, 512], BF16, "sq", es=es)
    pss = Rot([P.ps([128, 512], F32, es=es) for _ in range(2)])
    pst = Rot([P.ps([128, 512], F32, es=es) for _ in range(4)])
    tmps = Rot([P.sb([128, 512], F32, "nt", es=es) for _ in range(4)])
    rsb = P.sb([128, 512], F32, "rsb", es=es)
    ot = Rot([P.sb([128, D], F32, "ot", es=es) for _ in range(3)])
    for (n0, n, seg) in cfg.groups:
        if seg == 1:
            continue
        x = xg.get()
        y = yg.get()
        P.dma(x[:, :, 0:n], self.xT[:, :, n0:n0 + n].re("k p t -> p k t"))
        self.norm_group(x[:, :, 0:n], n, gs, None, 0, y[:, :, 0:n], pss, tmps, sq, rsb)
        for sub in range(n // 128):
            o = ot.get()
            for half in range(2):
                ps = pst.get()
                for j in range(4):
                    k = half * 4 + j
                    P.op('pe', 'transpose', out=ps[:, j * 128:(j + 1) * 128], in_=y[:, k, sub * 128:(sub + 1) * 128], identity=self.identf)
                P.op('act', 'activation', out=o[:, half * 512:(half + 1) * 512], in_=ps.a, func=AF.Copy)
            r0 = n0 - cfg.t_ctx + sub * 128
            P.dma(self.out[r0:r0 + 128, :], o.a, q='pool')
    es.close()
    P.barrier()


MK.phase_final = _final


def _ml(self, l):
    P, cfg = self.P, self.cfg
    NT, NCH = cfg.nt, cfg.nch
    nctx = cfg.t_ctx // 64
    order_b = list(range(nctx - 1, -1, -1)) + list(range(NCH - 1, nctx - 1, -1))
    order_f = list(range(NCH))
    es = contextlib.ExitStack()
    biasA = P.sb([64, 1], F32, "biasA", es=es)
    biasB = P.sb([64, 1], F32, "biasB", es=es)
    P.op('pool', 'memset', ap=biasA.a, constant=0.0)
    P.op('pool', 'memset', ap=biasB.a, constant=0.0)
    gb = self.ml_gate_b
    P.dma(biasA[0:4, :], gb[l, 0:4].re("(a b) -> a b", b=1))
    P.dma(biasA[32:36, :], gb[l, 4:8].re("(a b) -> a b", b=1))
    P.dma(biasB[0:4, :], gb[l, 8:12].re("(a b) -> a b", b=1))
    P.dma(biasB[32:36, :], gb[l, 12:16].re("(a b) -> a b", b=1))
    cols = self.load_rows_T([self.ml_conv_w[l].re("j (c p) -> (j c) p", p=128), self.ml_conv_b[l].re("(c p) -> c p", p=128)], es)
    colsB = self.load_rows_T([self.ml_conv_b[l].re("(c p) -> c p", p=64)], es)
    diag = P.sb([128, 12, 128], BF16, "diag3", es=es)
    for jc in range(12):
        P.op('dve', 'tensor_scalar', out=diag[:, jc, :], in0=self.identf, scalar1=cols[:, jc:jc + 1], scalar2=None, op0=ALU.mult)
    qk8 = [P.sb([64, NT], BF16, "qk8_%d" % i, es=es) for i in range(8)]
    Atok = P.sb([64, NCH, 8], F32, "Atok", es=es)
    Btok = P.sb([64, NCH, 8], F32, "Btok", es=es)
    Ctok = P.sb([64, NCH, 8], F32, "Ctok", es=es)
    decb = P.sb([64, 8, NCH], F32, "decb", es=es)
    emb = P.sb([64, 8, NCH], F32, "emb", es=es)
    es1 = contextlib.ExitStack()
    Wqk = P.sb([128, KD, 512], BF16, "wqk", es=es1)
    for k in range(KD):
        self.load_w(Wqk[:, k, :], self.w_in[l, k * 128:(k + 1) * 128, 0:512])
    pre = [P.sb([128, NT + 4], BF16, "pre%d" % i, es=es1) for i in range(4)]
    for i in range(4):
        P.op('pool', 'memset', ap=pre[i].a, constant=0.0)
    hg = Rot([P.sb([128, KD, 512], BF16, "hg", es=es1) for _ in range(2)])
    psQ = Rot([P.ps([128, 512], F32, es=es1) for _ in range(2)])

    def poff(seg):
        return 1 if seg == 1 else cfg.t_ctx + 3

    for (n0, n, seg) in cfg.groups:
        s0 = 0 if seg == 1 else cfg.t_ctx
        h = hg.get()
        P.dma(h[:, :, 0:n], self.hT[:, :, n0:n0 + n].re("k p t -> p k t"))
        for ci in range(4):
            pq = psQ.get()
            for k in range(KD):
                P.mm(pq[:, 0:n], Wqk[:, k, ci * 128:(ci + 1) * 128], h[:, k, 0:n], start=(k == 0), stop=(k == KD - 1))
            o0 = poff(seg) + n0 - s0
            P.op('dve', 'tensor_copy', out=pre[ci][:, o0:o0 + n], in_=pq[:, 0:n])
    for (n0, n, seg) in cfg.groups:
        s0 = 0 if seg == 1 else cfg.t_ctx
        for ci in range(4):
            for hp in range(2):
                pq = psQ.get()
                o0 = poff(seg) + n0 - s0 - 1
                for j in range(3):
                    P.mm(pq[0:64, 0:n], diag[:, j * 4 + ci, hp * 64:(hp + 1) * 64], pre[ci][:, o0 + j:o0 + j + n], start=(j == 0), stop=(j == 2))
                P.op('act', 'activation', out=qk8[ci * 2 + hp][:, n0:n0 + n], in_=pq[0:64, 0:n], func=AF.Silu, bias=colsB[0:64, ci * 2 + hp:ci * 2 + hp + 1])
    for h8 in range(4, 8):
        P.op('dve', 'tensor_scalar', out=qk8[h8].a, in0=qk8[h8].a, scalar1=0.125, scalar2=None, op0=ALU.mult)
    es1.close()
    P.barrier()
    esA = contextlib.ExitStack()
    LI = P.sb([64, NT], F32, "LI", es=esA)
    LF = P.sb([64, NT], F32, "LF", es=esA)
    es1 = contextlib.ExitStack()
    WgA = P.sb([128, KD, 64], BF16, "wga", es=es1)
    WgB = P.sb([128, KD, 64], BF16, "wgb", es=es1)
    P.op('pool', 'memset', ap=WgA.a, constant=0.0)
    P.op('pool', 'memset', ap=WgB.a, constant=0.0)
    for k in range(KD):
        rows = slice(k * 128, (k + 1) * 128)
        self.load_w(WgA[:, k, 0:4], self.w_in[l, rows, 768:772])
        self.load_w(WgA[:, k, 32:36], self.w_in[l, rows, 772:776])
        self.load_w(WgB[:, k, 0:4], self.w_in[l, rows, 776:780])
        self.load_w(WgB[:, k, 32:36], self.w_in[l, rows, 780:784])
    hg = Rot([P.sb([128, KD, 512], BF16, "hg", es=es1) for _ in range(2)])
    psA = Rot([P.ps([128, 512], F32, es=es1) for _ in range(2)])
    sgt = Rot([P.sb([64, 512], F32, "sgt", es=es1) for _ in range(2)])
    for (n0, n, seg) in cfg.groups:
        h = hg.get()
        P.dma(h[:, :, 0:n], self.hT[:, :, n0:n0 + n].re("k p t -> p k t"))
        pa = psA.get()
        for k in range(KD):
            P.mm(pa[0:64, 0:n], WgA[:, k, :], h[:, k, 0:n], start=(k == 0), stop=(k == KD - 1))
        P.op('act', 'activation', out=LI[:, n0:n0 + n], in_=pa[0:64, 0:n], func=AF.Identity, bias=biasA.a)
        pb = psA.get()
        for k in range(KD):
            P.mm(pb[0:64, 0:n], WgB[:, k, :], h[:, k, 0:n], start=(k == 0), stop=(k == KD - 1))
        s = sgt.get()
        P.op('act', 'activation', out=s[:, 0:n], in_=pb[0:64, 0:n], func=AF.Sigmoid, bias=biasB.a)
        P.op('act', 'activation', out=LF[:, n0:n0 + n], in_=s[:, 0:n], func=AF.Ln)
    es1.close()
    P.barrier()
    es1 = contextlib.ExitStack()
    ones = P.sb([64, NT], BF16, "ones", es=es1)
    P.op('pool', 'memset', ap=ones.a, constant=1.0)
    cs = P.sb([64, NT], F32, "cs", es=es1)
    P.op('dve', 'tensor_tensor_scan', out=cs.a, data0=ones.a, data1=LF.a, initial=0.0, op0=ALU.mult, op1=ALU.add)
    j3 = lambda b: b.a.re("p (c j) -> p c j", j=64)
    sm = lambda nm: P.sb([64, NCH], F32, nm, es=es1)
    base, tot, wmax, cmax, m_in, m_new, Mx, dec, em = [sm(x) for x in ("base", "tot", "wmax", "cmax", "m_in", "m_new", "Mx", "dec", "em")]
    bc3 = lambda b: b.a.unsq(2).bc([64, NCH, 64])
    P.op('dve', 'tensor_tensor', out=base.a, in0=j3(cs)[:, :, 0], in1=j3(LF)[:, :, 0], op=ALU.subtract)
    BC = cs
    P.op('dve', 'tensor_tensor', out=j3(BC), in0=j3(cs), in1=bc3(base), op=ALU.subtract)
    P.op('dve', 'tensor_copy', out=tot.a, in_=j3(BC)[:, :, 63])
    P.op('dve', 'tensor_tensor', out=j3(BC)[32:64], in0=bc3(tot)[32:64], in1=j3(BC)[32:64], op=ALU.subtract)
    P.op('dve', 'tensor_tensor', out=BC[32:64, :], in0=BC[32:64, :], in1=LF[32:64, :], op=ALU.add)
    Wt = LF
    Cq = LI
    P.op('dve', 'tensor_tensor', out=Cq.a, in0=LI.a, in1=BC.a, op=ALU.subtract)
    P.op('dve', 'tensor_tensor', out=j3(Wt), in0=j3(Cq), in1=bc3(tot), op=ALU.add)
    P.op('dve', 'tensor_reduce', out=wmax.a, in_=j3(Wt), axis=AX.X, op=ALU.max)
    P.op('dve', 'tensor_reduce', out=cmax.a, in_=j3(Cq), axis=AX.X, op=ALU.max)
    zero = P.sb([64, 1], F32, "zero", es=es1)
    P.op('pool', 'memset', ap=zero.a, constant=0.0)
    P.op('pool', 'memset', ap=m_in.a, constant=0.0)
    for (rows, order) in ((slice(0, 32), order_f), (slice(32, 64), order_b)):
        prev = None
        for i, c in enumerate(order):
            pv_ = zero[rows, 0:1] if prev is None else m_new[rows, prev:prev + 1]
            if prev is not None:
                P.op('dve', 'tensor_copy', out=m_in[rows, c:c + 1], in_=pv_)
            P.op('dve', 'tensor_scalar', out=m_new[rows, c:c + 1], in0=pv_, scalar1=tot[rows, c:c + 1], scalar2=wmax[rows, c:c + 1], op0=ALU.add, op1=ALU.max)
            prev = c
    P.op('dve', 'tensor_tensor', out=Mx.a, in0=m_in.a, in1=cmax.a, op=ALU.max)
    P.op('dve', 'tensor_tensor', out=j3(Cq), in0=j3(Cq), in1=bc3(Mx), op=ALU.subtract)
    P.op('act', 'activation', out=Cq.a, in_=Cq.a, func=AF.Exp)
    P.op('dve', 'tensor_tensor', out=j3(Wt), in0=j3(Wt), in1=bc3(m_new), op=ALU.subtract)
    P.op('act', 'activation', out=Wt.a, in_=Wt.a, func=AF.Exp)
    P.op('dve', 'tensor_tensor', out=j3(BC), in0=j3(BC), in1=bc3(Mx), op=ALU.add)
    P.op('act', 'activation', out=BC.a, in_=BC.a, func=AF.Exp, scale=-1.0)
    P.op('dve', 'tensor_tensor', out=dec.a, in0=tot.a, in1=m_in.a, op=ALU.add)
    P.op('dve', 'tensor_tensor', out=dec.a, in0=dec.a, in1=m_new.a, op=ALU.subtract)
    P.op('act', 'activation', out=dec.a, in_=dec.a, func=AF.Exp)
    P.op('dve', 'tensor_tensor', out=em.a, in0=m_in.a, in1=Mx.a, op=ALU.subtract)
    P.op('act', 'activation', out=em.a, in_=em.a, func=AF.Exp)
    pt = Rot([P.ps([64, 8, 64], F32, es=es1) for _ in range(2)])
    for (src, dst) in ((Cq, Atok), (Wt, Btok), (BC, Ctok)):
        for c0 in range(0, NCH, 8):
            nb = min(8, NCH - c0)
            p_ = pt.get()
            for cc in range(nb):
                P.op('pe', 'transpose', out=p_[:, cc, :], in_=src[:, (c0 + cc) * 64:(c0 + cc + 1) * 64], identity=self.identf[0:64, 0:64])
            P.op('dve', 'tensor_copy', out=dst[:, c0:c0 + nb, 0:4], in_=p_[:, 0:nb, 0:4])
            P.op('dve', 'tensor_copy', out=dst[:, c0:c0 + nb, 4:8], in_=p_[:, 0:nb, 32:36])
    pbq = Rot([P.ps([64, 4, NCH], F32, es=es1) for _ in range(2)])
    for (src, dst) in ((dec, decb), (em, emb)):
        for half in range(2):
            p_ = pbq.get()
            for gg in range(4):
                g = half * 4 + gg
                P.mm(p_[:, gg, :], self.cst[0:64, 384 + g * 64:384 + (g + 1) * 64], src.a, start=True, stop=True)
            P.op('dve', 'tensor_copy', out=dst[:, half * 4:half * 4 + 4, :], in_=p_.a)
    es1.close()
    esA.close()
    P.barrier()
    es3 = contextlib.ExitStack()
    v1 = P.sb([64, NCH, 4, 65], BF16, "v1", es=es3)
    P.op('pool', 'memset', ap=v1.a, constant=1.0)
    ktok = P.sb([64, NCH, 256], BF16, "ktok", es=es3)
    es3a = contextlib.ExitStack()
    Wv = P.sb([128, KD, 256], BF16, "wv", es=es3a)
    for k in range(KD):
        self.load_w(Wv[:, k, :], self.w_in[l, k * 128:(k + 1) * 128, 512:768])
    hg = Rot([P.sb([128, KD, 512], BF16, "hg", es=es3a) for _ in range(2)])
    psV = Rot([P.ps([128, 512], F32, es=es3a) for _ in range(2)])
    pk = Rot([P.ps([64, 256], BF16, es=es3a) for _ in range(2)])
    for (n0, n, seg) in cfg.groups:
        h = hg.get()
        P.dma(h[:, :, 0:n], self.hT[:, :, n0:n0 + n].re("k p t -> p k t"))
        for cc in range(n // 64):
            c = n0 // 64 + cc
            pv = psV.get()
            for k in range(KD):
                P.mm(pv[0:64, 0:256], h[:, k, cc * 64:(cc + 1) * 64], Wv[:, k, :], start=(k == 0), stop=(k == KD - 1))
            P.op('act', 'activation', out=v1[:, c, :, 0:64], in_=pv[0:64, 0:256].re("p (h d) -> p h d", h=4), func=AF.Copy)
    for c in range(NCH):
        p_ = pk.get()
        for hh in range(4):
            P.op('pe', 'transpose', out=p_[:, hh * 64:(hh + 1) * 64], in_=qk8[4 + hh][:, c * 64:(c + 1) * 64], identity=self.identb[0:64, 0:64])
        P.op('act', 'activation', out=ktok[:, c, :], in_=p_.a, func=AF.Copy)
    es3a.close()
    P.barrier()
    PS1 = [P.ps([64, 4, 64], F32, es=es3) for _ in range(2)]
    PS2 = [P.ps([64, 4, 65], F32, es=es3) for _ in range(2)]
    PS3 = [P.ps([64, 4, 65], F32, es=es3) for _ in range(2)]
    Cst = [P.sb([64, 4, 65], F32, "Cst", es=es3) for _ in range(2)]
    CnS = [P.sb([64, 4, 65], BF16, "CnS", es=es3) for _ in range(2)]
    for d_ in range(2):
        P.op('pool', 'memset', ap=Cst[d_].a, constant=0.0)
        P.op('pool', 'memset', ap=CnS[d_].a, constant=0.0)
    tmp = [Rot([P.sb([64, 4, 64], F32, "stmp", es=es3) for _ in range(2)]) for _ in range(2)]
    SD = [Rot([P.sb([64, 4, 64], BF16, "SD", es=es3) for _ in range(2)]) for _ in range(2)]
    kw = [Rot([P.sb([64, 4, 64], BF16, "kw", es=es3) for _ in range(2)]) for _ in range(2)]
    dn = [Rot([P.sb([64, 8], F32, "dn", es=es3) for _ in range(2)]) for _ in range(2)]
    hb = [Rot([P.sb([64, 4, 64], F32, "hbuf", es=es3) for _ in range(3)]) for _ in range(2)]
    Hd = [self.Hf, self.Hb]
    orders = [order_f, order_b]
    prep_lst = []
    prep_es = contextlib.ExitStack()
    if self.prep_in_mlstm:
        prep_lst = self.phase_peer_prep(l, es=prep_es, defer=True) or []
    per_step = (len(prep_lst) + NCH - 1) // NCH + 1
    for i in range(NCH):
        cs_ = [orders[d_][i] for d_ in range(2)]
        tks = [slice(c * 64, (c + 1) * 64) for c in cs_]
        qh = [[qk8[hh][:, tks[d_]] for hh in range(4)] for d_ in range(2)]
        kh = [[qk8[4 + hh][:, tks[d_]] for hh in range(4)] for d_ in range(2)]
        for d_ in range(2):
            for hh in range(4):
                P.mm(PS1[d_][:, hh, :], kh[d_][hh], qh[d_][hh])
        t_ = [tmp[d_].get() for d_ in range(2)]
        for d_ in range(2):
            P.op('dve', 'tensor_tensor', out=t_[d_].a, in0=PS1[d_].a, in1=Atok[:, cs_[d_], 4 * d_:4 * d_ + 4].unsq(2).bc([64, 4, 64]), op=ALU.mult)
        sd = [SD[d_].get() for d_ in range(2)]
        for d_ in range(2):
            P.op('pool', 'tensor_tensor', out=sd[d_].a, in0=t_[d_].a, in1=self.maskb_t[:, d_, :, :], op=ALU.mult)
        kw_ = [kw[d_].get() for d_ in range(2)]
        for d_ in range(2):
            P.op('pool', 'tensor_tensor', out=kw_[d_].a, in0=ktok[:, cs_[d_], :].re("p (h d) -> p h d", h=4), in1=Btok[:, cs_[d_], 4 * d_:4 * d_ + 4].unsq(2).bc([64, 4, 64]), op=ALU.mult)
        for d_ in range(2):
            for hh in range(4):
                P.mm(PS2[d_][:, hh, :], qh[d_][hh], CnS[d_][:, hh, :], start=True, stop=False)
                P.mm(PS2[d_][:, hh, :], sd[d_][:, hh, :], v1[:, cs_[d_], hh, :], start=False, stop=True)
        for d_ in range(2):
            for hh in range(4):
                P.mm(PS3[d_][:, hh, :], kw_[d_][:, hh, :], v1[:, cs_[d_], hh, :])
        for d_ in range(2):
            P.op('pool', 'tensor_tensor', out=Cst[d_].a, in0=Cst[d_].a, in1=decb[:, 4 * d_:4 * d_ + 4, cs_[d_]].unsq(2).bc([64, 4, 65]), op=ALU.mult)
        for d_ in range(2):
            P.op('dve', 'tensor_tensor', out=Cst[d_].a, in0=Cst[d_].a, in1=PS3[d_].a, op=ALU.add)
        if i + 1 < NCH:
            for d_ in range(2):
                cn = orders[d_][i + 1]
                P.op('pool', 'tensor_tensor', out=CnS[d_].a, in0=Cst[d_].a, in1=emb[:, 4 * d_:4 * d_ + 4, cn].unsq(2).bc([64, 4, 65]), op=ALU.mult)
        dd = [dn[d_].get() for d_ in range(2)]
        for d_ in range(2):
            P.op('act', 'activation', out=dd[d_][:, 0:4], in_=PS2[d_][:, :, 64], func=AF.Abs)
        for d_ in range(2):
            P.op('dve', 'tensor_tensor', out=dd[d_][:, 0:4], in0=dd[d_][:, 0:4], in1=Ctok[:, cs_[d_], 4 * d_:4 * d_ + 4], op=ALU.max)
        for d_ in range(2):
            P.op('dve', 'reciprocal', out=dd[d_][:, 4:8], in_=dd[d_][:, 0:4])
        for d_ in range(2):
            hbuf = hb[d_].get()
            P.op('dve', 'tensor_tensor', out=hbuf.a, in0=PS2[d_][:, :, 0:64], in1=dd[d_][:, 4:8].unsq(2).bc([64, 4, 64]), op=ALU.mult)
            P.dma(Hd[d_][cs_[d_]].re("p (h d) -> p h d", h=4), hbuf.a, q='sp')
        P.run_deferred(prep_lst, per_step)
    P.run_deferred(prep_lst)
    P.barrier()
    prep_es.close()
    es3.close()
    P.barrier()
    es4 = contextlib.ExitStack()
    Wo = P.sb([128, KD, 256], BF16, "wo", es=es4)
    for k in range(KD):
        self.load_w(Wo[:, k, :], self.w_in[l, k * 128:(k + 1) * 128, 784:1040])
    ng = P.sb([64, 256], F32, "mlng", es=es4)
    P.dma(ng.a, self.ml_norm_g[l:l + 1, :].pbc(64))
    hg = Rot([P.sb([128, KD, 512], BF16, "hg", es=es4) for _ in range(2)])
    hf_ = Rot([P.sb([64, 8, 256], F32, "hf", es=es4) for _ in range(2)])
    hb_ = Rot([P.sb([64, 8, 256], F32, "hb", es=es4) for _ in range(2)])
    sq = P.sb([64, 8, 256], F32, "sq4", es=es4)
    ss = P.sb([64, 32], F32, "ss4", es=es4)
    ob = Rot([P.sb([64, 8, 256], F32, "ob", es=es4) for _ in range(2)])
    hz = Rot([P.sb([64, 8, 256], BF16, "hz", es=es4) for _ in range(2)])
    zst = Rot([P.sb([128, 2, 512], BF16, "zst", es=es4) for _ in range(2)])
    pso = Rot([P.ps([128, 512], F32, es=es4) for _ in range(2)])
    pst = Rot([P.ps([128, 8, 64], BF16, es=es4) for _ in range(2)])
    for (n0, n, seg) in cfg.groups:
        if self.skip_ctx and seg == 1:
            continue
        nc_ = n // 64
        c0 = n0 // 64
        h = hg.get()
        P.dma(h[:, :, 0:n], self.hT[:, :, n0:n0 + n].re("k p t -> p k t"))
        a = hf_.get()
        b = hb_.get()
        P.dma(a[:, 0:nc_, :], self.Hf[c0:c0 + nc_].re("c p f -> p c f"))
        P.dma(b[:, 0:nc_, :], self.Hb[c0:c0 + nc_].re("c p f -> p c f"))
        P.op('dve', 'tensor_tensor', out=a[:, 0:nc_, :], in0=a[:, 0:nc_, :], in1=b[:, 0:nc_, :], op=ALU.add)
        P.op('act', 'activation', out=sq[:, 0:nc_, :], in_=a[:, 0:nc_, :], func=AF.Square)
        P.op('dve', 'tensor_reduce', out=ss[:, 0:nc_ * 4], in_=sq[:, 0:nc_, :].re("p c (h d) -> p (c h) d", h=4), axis=AX.X, op=ALU.add)
        P.op('act', 'activation', out=ss[:, 0:nc_ * 4], in_=ss[:, 0:nc_ * 4], func=AF.Sqrt, scale=1.0 / 64, bias=self.eps[0:64, :])
        P.op('dve', 'reciprocal', out=ss[:, 0:nc_ * 4], in_=ss[:, 0:nc_ * 4])
        P.op('dve', 'tensor_tensor', out=a[:, 0:nc_, :].re("p c (h d) -> p (c h) d", h=4), in0=a[:, 0:nc_, :].re("p c (h d) -> p (c h) d", h=4),
             in1=ss[:, 0:nc_ * 4].unsq(2).bc([64, nc_ * 4, 64]), op=ALU.mult)
        P.op('dve', 'tensor_tensor', out=a[:, 0:nc_, :], in0=a[:, 0:nc_, :], in1=ng.a.unsq(1).bc([64, nc_, 256]), op=ALU.mult)
        o = ob.get()
        for cc in range(nc_):
            po = pso.get()
            for k in range(KD):
                P.mm(po[0:64, 0:256], h[:, k, cc * 64:(cc + 1) * 64], Wo[:, k, :], start=(k == 0), stop=(k == KD - 1))
            P.op('act', 'activation', out=o[:, cc, :], in_=po[0:64, 0:256], func=AF.Sigmoid)
        z = hz.get()
        P.op('dve', 'tensor_tensor', out=z[:, 0:nc_, :], in0=a[:, 0:nc_, :], in1=o[:, 0:nc_, :], op=ALU.mult)
        zs = zst.get()
        for half in range(2):
            p_ = pst.get()
            for cc in range(nc_):
                P.op('pe', 'transpose', out=p_[:, cc, :], in_=z[:, cc, half * 128:(half + 1) * 128], identity=self.identb[0:64, 0:64])
            P.op('act', 'activation', out=zs[:, half, 0:n], in_=p_[:, 0:nc_, :].re("p c t -> p (c t)"), func=AF.Copy)
        P.dma(self.Z[0, :, n0:n0 + n].re("(c p) t -> p c t", p=128), zs[:, :, 0:n], q='pool')
    es4.close()
    es.close()
    P.barrier()


MK.phase_mlstm = _ml


def _peer_prep(self, l, es=None, defer=False):
    P = self.P
    if self.prep_done.get(l):
        return None
    self.prep_done[l] = True
    own_es = es is None
    es = es or contextlib.ExitStack()
    lst = []
    if defer:
        P.deferred = lst
    ub = Rot([P.sb([128, D], BF16, "ub", es=es) for _ in range(3)])
    vb = Rot([P.sb([128, D], BF16, "vb", es=es) for _ in range(3)])
    uo = Rot([P.sb([128, D], BF16, "uo", es=es) for _ in range(3)])
    pst = Rot([P.ps([128, KD, 128], BF16, es=es) for _ in range(2 if defer else 3)])
    Uv = self.peer_u[l].re("(a b) d -> b a d", b=128)
    Vv = self.peer_v[l].re("(a b) d -> b a d", b=128)
    for e2 in range(128):
        u = ub.get()
        P.dma(u.a, Uv[e2], q='pool')
        p_ = pst.get()
        for k in range(KD):
            P.op('pe', 'transpose', out=p_[:, k, :], in_=u[:, k * 128:(k + 1) * 128], identity=self.identb)
        o = uo.get()
        if e2 % 2:
            P.op('act', 'activation', out=o.a, in_=p_.a.re("p k e -> p (k e)"), func=AF.Copy)
        else:
            P.op('dve', 'tensor_copy', out=o.a, in_=p_.a.re("p k e -> p (k e)"))
        P.dma(self.UT[e2], o.a, q='sp')
        v = vb.get()
        P.dma(v.a, Vv[e2], q='pool')
        P.dma(self.VB[e2], v.a, q='sp')
    P.deferred = None
    if own_es:
        es.close()
        P.barrier()
    return lst


MK.phase_peer_prep = _peer_prep


def _peer(self, l):
    P, cfg = self.P, self.cfg
    es = contextlib.ExitStack()
    gs, sh, gate2 = self.mod_scale_shift(l, 1, es)
    Wq = P.sb([128, KD, 2048], BF16, "wq", es=es)
    for k in range(KD):
        self.load_w(Wq[:, k, :], self.peer_w_q[l, k * 128:(k + 1) * 128, :])
    psA = Rot([P.ps([128, 512], F32, es=es) for _ in range(2)])
    Wps = Rot([P.ps([128, 512], F32, es=es) for _ in range(2)])
    acc = [P.ps([128, 2, 256], F32, es=es) for _ in range(4)]
    kl = P.sb([128, 2, 128], BF16, "kl", es=es)
    self.load_w(kl.a, self.peer_keys[l].re("p e k -> e p k"))
    keysT = P.sb([128, 2, 128], BF16, "keysT", es=es)
    for p in range(2):
        pk_ = Wps.get()
        pkb = pk_.a.re("q (a b) -> q a b", b=128)
        klf = P.sb([128, 128], F32, "klf", es=es)
        P.op('dve', 'tensor_copy', out=klf.a, in_=kl[:, p, :])
        P.op('pe', 'transpose', out=pk_[:, 0:128], in_=klf.a, identity=self.identf)
        P.op('dve', 'tensor_copy', out=keysT[:, p, :], in_=pk_[:, 0:128])
    xgs = [P.sb([128, KD, 256], F32, "xg", es=es) for _ in range(2)]
    h2s = [P.sb([128, KD, 256], BF16, "h2", es=es) for _ in range(2)]
    qT = P.sb([128, 16, 256], BF16, "qT", es=es)
    S = P.sb([128, 16, 128], F32, "S", es=es)
    S2 = P.sb([128, 16, 128], F32, "S2", es=es)
    V1 = P.sb([128, 16, 16], F32, "V1", es=es)
    I1u = P.sb([128, 16, 16], U32, "I1u", es=es)
    I1f = P.sb([128, 16, 16], F32, "I1f", es=es)
    cand = P.sb([128, 8, 256], F32, "cand", es=es)
    cand2 = S2
    SC = P.sb([128, 8, 16], F32, "SC", es=es)
    POSu = P.sb([128, 8, 16], U32, "POSu", es=es)
    PIu = P.sb([128, 8, 16], U32, "PIu", es=es)
    PJu = P.sb([128, 8, 16], U32, "PJu", es=es)
    PIf = P.sb([128, 128], F32, "PIf", es=es)
    PJf = P.sb([128, 128], F32, "PJf", es=es)
    OH = S
    E1 = P.sb([128, 128], F32, "E1", es=es)
    E2 = P.sb([128, 128], F32, "E2", es=es)
    G = P.sb([128, 128], F32, "G", es=es)
    sm = P.sb([128, 16], F32, "sm", es=es)
    E1T = P.sb([128, 256], BF16, "E1T", es=es)
    E2T = P.sb([128, 256], BF16, "E2T", es=es)
    GT = P.sb([128, 256], BF16, "GT", es=es)
    An = Rot([P.sb([128, 128], BF16, "An", es=es) for _ in range(6)])
    Bn = Rot([P.sb([128, 128], BF16, "Bn", es=es) for _ in range(6)])
    Wbuf = P.sb([128, 256, 128], BF16, "Wbuf", es=es)
    ut = Rot([P.sb([128, KD, 128], BF16, "ut", es=es) for _ in range(4)])
    vt = Rot([P.sb([128, D], BF16, "vtb", es=es) for _ in range(4)])
    Ab = Rot([P.sb([128, 256], BF16, "Ab", es=es) for _ in range(3)])
    AW = Rot([P.sb([128, 256], BF16, "AW", es=es) for _ in range(3)])
    i16 = self.iota16
    V1s = [V1] + [V1.sub() for _ in range(15)]
    I1s = [I1u] + [I1u.sub() for _ in range(15)]
    S2s = [S2] + [S2.sub() for _ in range(15)]
    cands = [cand] + [cand.sub() for _ in range(7)]
    SCs = [SC] + [SC.sub() for _ in range(7)]
    POSs = [POSu] + [POSu.sub() for _ in range(7)]
    zlhs = P.sb([128, 128], BF16, "zlhs", es=es)
    zrhs = P.sb([128, 512], BF16, "zrhs", es=es)
    P.op('pool', 'memset', ap=zlhs.a, constant=0.0)
    P.op('pool', 'memset', ap=zrhs.a, constant=0.0)
    glist = [g for g in cfg.groups256 if not (self.skip_ctx and g[2] == 1)]
    sq = qT[:, 0:8, :]
    tmps = Rot([cands[i][:, i, :] for i in range(3)])
    rsb = cands[3][:, 3, :]

    def front(gi):
        (n0, n, seg) = glist[gi]
        x = xgs[gi % 2]
        h2 = h2s[gi % 2]
        P.dma(x.a, self.xT[:, :, n0:n0 + n].re("k p t -> p k t"))
        self.norm_group(x.a, n, gs, sh, seg, h2.a, Wps, tmps, sq, rsb)
        for j in range(16):
            pq = Wps.get()
            for k in range(KD):
                P.mm(pq[:, 0:n], Wq[:, k, j * 128:(j + 1) * 128], h2[:, k, :], start=(k == 0), stop=(k == KD - 1))
            P.op('act', 'activation', out=qT[:, j, :], in_=pq[:, 0:n], func=AF.Copy)
        for sub in range(2 if 'topk' in self.peer_parts else 0):
            tsl = slice(sub * 128, (sub + 1) * 128)
            for j4 in range(4):
                pscr = Wps.get()
                for jj in range(4):
                    j = j4 * 4 + jj
                    P.mm(pscr[:, jj * 128:(jj + 1) * 128], qT[:, j, tsl], keysT[:, j % 2, :])
                P.op('act', 'activation', out=S[:, j4 * 4:j4 * 4 + 4, :], in_=pscr.a.re("p (a b) -> p a b", b=128), func=AF.Copy)
            for j in range(16):
                P.op('dve', 'max', out=V1s[j][:, j, 0:8], in_=S[:, j, :])
            for j in range(16):
                P.op('dve', 'max_index', out=I1s[j][:, j, 0:8], in_max=V1s[j][:, j, 0:8], in_values=S[:, j, :])
            for j in range(16):
                P.op('dve', 'match_replace', out=S2s[j][:, j, :], in_to_replace=V1s[j][:, j, 0:8], in_values=S[:, j, :], imm_value=NEG)
            for j in range(16):
                P.op('dve', 'max', out=V1s[j][:, j, 8:16], in_=S2s[j][:, j, :])
            for j in range(16):
                P.op('dve', 'max_index', out=I1s[j][:, j, 8:16], in_max=V1s[j][:, j, 8:16], in_values=S2s[j][:, j, :])
            P.op('dve', 'tensor_copy', out=I1f.a, in_=I1s[0].a, xr=I1s[1:])
            V1v = V1s[0].a.re("q (h p) i -> q h p i", p=2)
            P.op('dve', 'tensor_tensor', out=cands[0].a.re("q h (i j) -> q h i j", j=16), in0=V1v[:, :, 0, :].unsq(3).bc([128, 8, 16, 16]),
                 in1=V1v[:, :, 1, :].unsq(2).bc([128, 8, 16, 16]), op=ALU.add, xr=V1s[1:], xw=cands[1:])
            c2v = S2.a.re("q a b -> q (a b)").re("q (h c) -> q h c", h=8)
            ohv = S.a.re("q a b -> q (a b)").re("q (j i) -> q j i", i=16)
            def c2(h):
                return V(S2s[2 * h], c2v.ap[:, h, :])
            for h in range(8):
                P.op('dve', 'max', out=SCs[h][:, h, 0:8], in_=cands[h][:, h, :])
            for h in range(8):
                P.op('dve', 'max_index', out=POSs[h][:, h, 0:8], in_max=SCs[h][:, h, 0:8], in_values=cands[h][:, h, :])
            for h in range(8):
                P.op('dve', 'match_replace', out=c2(h), in_to_replace=SCs[h][:, h, 0:8], in_values=cands[h][:, h, :], imm_value=NEG, xw=[S2s[2 * h + 1]])
            for h in range(8):
                P.op('dve', 'max', out=SCs[h][:, h, 8:16], in_=c2(h), xr=[S2s[2 * h + 1]])
            for h in range(8):
                P.op('dve', 'max_index', out=POSs[h][:, h, 8:16], in_max=SCs[h][:, h, 8:16], in_values=c2(h), xr=[S2s[2 * h + 1]])
            P.op('dve', 'tensor_single_scalar', out=PIu.a, in_=POSs[0].a, scalar=4, op=ALU.logical_shift_right, xr=POSs[1:])
            P.op('dve', 'tensor_single_scalar', out=PJu.a, in_=POSs[0].a, scalar=15, op=ALU.bitwise_and, xr=POSs[1:])
            P.op('dve', 'tensor_copy', out=PIf.a, in_=PIu.a.re("q h k -> q (h k)"))
            P.op('dve', 'tensor_copy', out=PJf.a, in_=PJu.a.re("q h k -> q (h k)"))
            I1v = I1f.a.re("q (h p) i -> q h p i", p=2)
            for (Pf, pp, Eo) in ((PIf, 0, E1), (PJf, 1, E2)):
                P.op('dve', 'tensor_tensor', out=ohv, in0=i16.unsq(1).bc([128, 128, 16]), in1=Pf.a.unsq(2).bc([128, 128, 16]), op=ALU.is_equal)
                P.op('dve', 'tensor_tensor', out=ohv.re("q (h k) i -> q h k i", h=8), in0=ohv.re("q (h k) i -> q h k i", h=8),
                     in1=I1v[:, :, pp, :].unsq(2).bc([128, 8, 16, 16]), op=ALU.mult)
                P.op('dve', 'tensor_reduce', out=Eo.a, in_=ohv, axis=AX.X, op=ALU.add)
            Gv = G.a.re("q (h k) -> q h k", h=8)
            P.op('dve', 'tensor_tensor', out=Gv, in0=SC.a, in1=SC[:, :, 0:1].bc([128, 8, 16]), op=ALU.subtract, xr=SCs[1:])
            P.op('act', 'activation', out=G.a, in_=G.a, func=AF.Exp)
            P.op('dve', 'tensor_reduce', out=sm[:, 0:8], in_=Gv, axis=AX.X, op=ALU.add)
            P.op('dve', 'reciprocal', out=sm[:, 8:16], in_=sm[:, 0:8])
            P.op('dve', 'tensor_tensor', out=Gv, in0=Gv, in1=sm[:, 8:16].unsq(2).bc([128, 8, 16]), op=ALU.mult)
            for (src, dst) in ((E1, E1T), (E2, E2T), (G, GT)):
                ptr = Wps.get()
                P.op('pe', 'transpose', out=ptr[:, 0:128], in_=src.a, identity=self.identf)
                P.op('act', 'activation', out=dst[:, tsl], in_=ptr[:, 0:128], func=AF.Copy)
    def run_front(gi):
        lst = []
        P.deferred = lst
        front(gi)
        P.deferred = None
        return lst

    P.run_deferred(run_front(0))
    for gi, (n0, n, seg) in enumerate(glist):
        x = xgs[gi % 2]
        h2 = h2s[gi % 2]
        for t4 in range(n // 4 if 'wb' in self.peer_parts else 0):
            wp = Wps.get()
            for tt in range(4):
                t = t4 * 4 + tt
                a_ = An.get()
                b_ = Bn.get()
                P.op('dve', 'tensor_scalar', out=a_.a, in0=self.iota128b_t.a, scalar1=E1T[:, t:t + 1], scalar2=GT[:, t:t + 1], op0=ALU.is_equal, op1=ALU.mult)
                P.op('dve', 'tensor_scalar', out=b_.a, in0=self.iota128b_t.a, scalar1=E2T[:, t:t + 1], scalar2=None, op0=ALU.is_equal)
                P.mm(wp[:, tt * 128:(tt + 1) * 128], a_.a, b_.a)
            P.op('act', 'activation', out=Wbuf[:, t4 * 4:t4 * 4 + 4, :], in_=wp.a.re("p (a b) -> p a b", b=128), func=AF.Copy)
        for bnk in range(4):
            P.mm(acc[bnk].a.re("p a b -> p (a b)"), zlhs.a, zrhs.a, start=True, stop=False)
        NE = 128 if 'ex' in self.peer_parts else 0

        def stage_a(e2):
            u = ut.get()
            v = vt.get()
            P.dma(u.a, self.UT[e2].re("p (k e) -> p k e", e=128), q='sp')
            P.dma(v.a, self.VB[e2], q='sp')
            pa = psA.get()
            for k in range(KD):
                P.mm(pa[:, 0:n], u[:, k, :], h2[:, k, :], start=(k == 0), stop=(k == KD - 1))
            ab = Ab.get()
            P.op('act', 'activation', out=ab.a, in_=pa[:, 0:n], func=AF.Gelu_apprx_tanh)
            aw = AW.get()
            P.op('dve', 'tensor_tensor', out=aw.a, in0=ab.a, in1=Wbuf[:, :, e2], op=ALU.mult)
            return v, aw

        nxt = run_front(gi + 1) if gi + 1 < len(glist) else []
        if not getattr(self, 'peer_pipe', True):
            pre_nxt, nxt = nxt, []
        per_chunk = (len(nxt) + 119) // 120 if NE else len(nxt)
        pend = stage_a(0) if NE else None
        for e2 in range(NE):
            v, aw = pend
            if e2 + 1 < NE:
                pend = stage_a(e2 + 1)
            P.run_deferred(nxt, per_chunk)
            for dc in range(KD):
                P.mm(acc[dc // 2][:, dc % 2, :], v[:, dc * 128:(dc + 1) * 128], aw.a, start=False, stop=(e2 == 127 and dc % 2 == 1))
        P.run_deferred(nxt)
        if not getattr(self, 'peer_pipe', True):
            P.run_deferred(pre_nxt)
        for dc in range(KD):
            P.op('dve', 'scalar_tensor_tensor', out=x[:, dc, :], in0=acc[dc // 2][:, dc % 2, :], scalar=gate2[:, dc, seg:seg + 1], in1=x[:, dc, :], op0=ALU.mult, op1=ALU.add)
        P.dma(self.xT[:, :, n0:n0 + n].re("k p t -> p k t"), x.a, q='pool')
    es.close()
    P.barrier()


MK.phase_peer = _peer


def build_program(cfg, debug=False, phases=None, **opts):
    mk = MK(cfg, debug=debug)
    mk.skip_ctx = False
    mk.prep_done = {}
    mk.prep_in_mlstm = (phases is None) and opts.get('prep_in_mlstm', False)
    mk.peer_pipe = opts.get('peer_pipe', True)
    mk.peer_parts = set(['topk', 'wb', 'ex']) if (phases is None or not any(p.startswith('pp_') for p in phases)) else set(p[3:] for p in phases if p.startswith('pp_'))
    on = lambda p: phases is None or p in phases
    mk.phase_init()
    for l in range(cfg.depth):
        mk.skip_ctx = False
        if on('mod'):
            mk.phase_mod(l)
        if on('norm1'):
            mk.phase_norm1(l)
        if on('mlstm'):
            mk.phase_mlstm(l)
        mk.skip_ctx = (l == cfg.depth - 1) and phases is None
        if on('gmlp'):
            mk.phase_gmlp(l)
        if on('conv'):
            mk.phase_conv(l)
        if on('fnet'):
            mk.phase_fnet(l)
        if on('merge'):
            mk.phase_merge(l)
        if on('prep'):
            mk.phase_peer_prep(l)
        if on('peer'):
            mk.phase_peer(l)
    mk.phase_final()
    mk.P.finish()
    return mk


_CACHE = {}


def make_in_maps(cfg, inp):
    dftc, dfts, cd, cst = host_consts(cfg)
    pos = grid_sincos(cfg.t_lat, D)
    L = cfg.depth
    shared = {"pos": pos, "dftc": dftc, "dfts": dfts, "cd": cd, "cst": cst}
    for k in ["w_ada", "b_ada", "norm1_g", "norm2_g", "w_in", "ml_conv_w", "ml_conv_b", "ml_gate_b", "gm_ln_g", "gm_ln_b",
              "gm_w_s", "cv_dw_w", "cv_dw_b", "cv_ln_g", "cv_ln_b", "w_branch", "w_out", "peer_w_q", "peer_keys",
              "peer_u", "peer_v", "final_norm_g"]:
        shared[k] = np.ascontiguousarray(np.asarray(inp[k], dtype=np.float32))
    shared["ml_norm_g"] = np.ascontiguousarray(np.asarray(inp["ml_norm_g"], np.float32).reshape(L, 256))
    shared["gm_b_s"] = np.ascontiguousarray(np.asarray(inp["gm_b_s"], np.float32).reshape(L, 512))
    x = np.asarray(inp["x"], np.float32)
    ctx = np.asarray(inp["ctx"], np.float32)
    c = np.asarray(inp["c"], np.float32)
    c_ctx = np.asarray(inp["c_ctx"], np.float32)
    maps = []
    for b in range(x.shape[0]):
        m = dict(shared)
        m["xin"] = np.ascontiguousarray(np.concatenate([ctx[b], x[b]], 0))
        m["cvec"] = np.ascontiguousarray(np.stack([c[b], c_ctx], 0))
        maps.append(m)
    return maps


def kernel(**inputs):
    x = np.asarray(inputs["x"])
    B, T, _ = x.shape
    depth = np.asarray(inputs["w_ada"]).shape[0]
    cfg = Cfg(depth=depth, t_lat=T, t_ctx=np.asarray(inputs["ctx"]).shape[1])
    key = (depth, T, cfg.t_ctx)
    if key not in _CACHE:
        _CACHE[key] = build_program(cfg)
    mk = _CACHE[key]
    maps = make_in_maps(cfg, inputs)
    res = run_bass_kernel_spmd(mk.nc, maps, core_ids=list(range(B)))
    return np.stack([np.asarray(r["out"], dtype=np.float32) for r in res.results], 0)
```

```python
import contextlib
import numpy as np
import ml_dtypes
import concourse.bass as bass
import concourse.mybir as mybir
from concourse.bass_utils import run_bass_kernel_spmd

F32 = mybir.dt.float32
BF16 = mybir.dt.bfloat16
I32 = mybir.dt.int32
U32 = mybir.dt.uint32
AF = mybir.ActivationFunctionType
ALU = mybir.AluOpType
AX = mybir.AxisListType

COMPUTE = ('pe', 'act', 'dve', 'pool')
NRING = 12
WRITE_KW = ('out', 'accum_out', 'ap')
EPS = 1e-6
NEG = -1.0e30


class V:
    __slots__ = ('buf', 'ap')

    def __init__(self, buf, ap):
        self.buf = buf
        self.ap = ap

    def __getitem__(self, k):
        return V(self.buf, self.ap[k])

    def re(self, pat, **kw):
        return V(self.buf, self.ap.rearrange(pat, **kw))

    def bc(self, shape):
        return V(self.buf, self.ap.to_broadcast(list(shape)))

    def unsq(self, ax):
        return V(self.buf, self.ap.unsqueeze(ax))

    def pbc(self, n):
        return V(self.buf, self.ap.partition_broadcast(n))


class Buf:
    __slots__ = ('t', 'lw', 'rd', 'name')

    def __init__(self, t, name=''):
        self.t = t
        self.lw = None
        self.rd = {}
        self.name = name

    def __getitem__(self, k):
        return V(self, self.t[k])

    @property
    def a(self):
        return V(self, self.t[:])

    def sub(self):
        return Buf(self.t, self.name + '_s')


class Prog:
    def __init__(self, nc):
        self.nc = nc
        self.ops = {e: [] for e in ('pe', 'act', 'dve', 'pool', 'sp')}
        self.count = {e: 0 for e in COMPUTE}
        self.known = {e: {} for e in self.ops}
        self.ndma = {'sp': 0, 'pool': 0, 'act': 0}
        self.es = contextlib.ExitStack()
        self.sems = {}
        for e in COMPUTE:
            self.sems[('c', e)] = self.es.enter_context(nc.semaphore('s_' + e))
        for q in ('sp', 'pool', 'act'):
            for i in range(NRING):
                self.sems[('d', q, i)] = self.es.enter_context(nc.semaphore('d_%s_%d' % (q, i)))
        self.nbuf = 0
        self.ninst = 0
        self.deferred = None

    def sb(self, shape, dt, name=None, es=None):
        self.nbuf += 1
        name = '%s_%d' % (name or 'sb', self.nbuf)
        t = (es or self.es).enter_context(self.nc.sbuf_tensor(name, list(shape), dt))
        return Buf(t, name)

    def ps(self, shape, dt, name=None, es=None):
        self.nbuf += 1
        name = '%s_%d' % (name or 'ps', self.nbuf)
        t = (es or self.es).enter_context(self.nc.psum_tensor(name, list(shape), dt))
        return Buf(t, name)

    def dram(self, name, shape, dt, kind='Internal'):
        t = self.nc.dram_tensor(name, list(shape), dt, kind=kind)
        return Buf(t, name)

    def _need(self, eng, tok, waits):
        if tok is None:
            return
        k, v = tok
        if self.known[eng].get(k, 0) >= v:
            return
        if waits.get(k, 0) < v:
            waits[k] = v

    def emit(self, eng, fn, reads=(), writes=(), dma=False):
        waits = {}
        own = ('c', eng) if (eng in COMPUTE and not dma) else None
        for b in reads:
            if b.lw is not None:
                if own is not None and b.lw[0] == own and eng == 'pe':
                    continue
                self._need(eng, b.lw, waits)
        for b in writes:
            if b.lw is not None:
                if not (own is not None and b.lw[0] == own and eng == 'pe'):
                    self._need(eng, b.lw, waits)
            for k, v in b.rd.items():
                if own is not None and k == own and eng == 'pe':
                    continue
                self._need(eng, (k, v), waits)
        if dma:
            i = self.ndma[eng]
            self.ndma[eng] = i + 1
            slot, gen = i % NRING, i // NRING
            key = ('d', eng, slot)
            if gen > 0:
                self._need(eng, (key, 16 * gen), waits)
            tok = (key, 16 * (gen + 1))
            inc = (key, 16)
        else:
            self.count[eng] += 1
            tok = (own, self.count[eng])
            inc = (own, 1)
        for k, v in waits.items():
            self.known[eng][k] = v
        self.ops[eng].append((fn, list(waits.items()), inc))
        self.ninst += 1
        for b in writes:
            b.lw = tok
            b.rd = {}
        for b in reads:
            if b in writes:
                continue
            if b.rd.get(tok[0], 0) < tok[1]:
                b.rd[tok[0]] = tok[1]
        return tok

    def run_deferred(self, lst, k=None):
        k = len(lst) if k is None else min(k, len(lst))
        for _ in range(k):
            eng, name, xr, xw, kw = lst.pop(0)
            self.op(eng, name, xr=xr, xw=xw, **kw)

    def op(self, eng, name, *, xr=(), xw=(), **kw):
        if self.deferred is not None:
            self.deferred.append((eng, name, xr, xw, kw))
            return None
        reads, writes, real = list(xr), list(xw), {}
        for k, v in kw.items():
            if isinstance(v, V):
                (writes if k in WRITE_KW else reads).append(v.buf)
                real[k] = v.ap
            else:
                real[k] = v
        isdma = name == 'dma_start'

        def fn(e, name=name, real=real):
            return getattr(e, name)(**real)
        return self.emit(eng, fn, reads, writes, dma=isdma)

    def dma(self, out, in_, q='sp', **kw):
        return self.op(q, 'dma_start', out=out, in_=in_, **kw)

    def mm(self, out, lhsT, rhs, start=True, stop=True):
        return self.op('pe', 'matmul', out=out, lhsT=lhsT, rhs=rhs, start=start, stop=stop)

    def barrier(self):
        toks = []
        for e in COMPUTE:
            if self.count[e] > 0:
                toks.append((('c', e), self.count[e]))
        for q, n in self.ndma.items():
            for i in range(max(0, n - NRING), n):
                toks.append((('d', q, i % NRING), 16 * (i // NRING + 1)))
        for e in self.ops:
            waits = {}
            for tok in toks:
                if tok[0] == ('c', e):
                    continue
                self._need(e, tok, waits)
            for k, v in waits.items():
                self.known[e][k] = v
            if waits:
                self.ops[e].append((None, list(waits.items()), None))

    def finish(self):
        self.barrier()
        nc = self.nc
        sems = self.sems
        ops = self.ops

        def replay(name, e):
            for fn, waits, inc in ops[name]:
                for k, v in waits:
                    e.wait_ge(sems[k], v)
                if fn is None:
                    continue
                ins = fn(e)
                if inc is not None:
                    ins.then_inc(sems[inc[0]], inc[1])

        with nc.Block() as block:
            @block.sync
            def _(e):
                replay('sp', e)

            @block.tensor
            def _(e):
                replay('pe', e)

            @block.scalar
            def _(e):
                replay('act', e)

            @block.vector
            def _(e):
                replay('dve', e)

            @block.gpsimd
            def _(e):
                replay('pool', e)
        self.es.close()


class Rot:
    def __init__(self, bufs):
        self.bufs = bufs
        self.i = 0

    def get(self):
        b = self.bufs[self.i % len(self.bufs)]
        self.i += 1
        return b


D = 1024
KD = 8
IN_COLS = 6416
GM_OFF = 1040
CV_OFF = 1552
FT_OFF = 2064
GATE_OFF = 2320
NEXP = 16384


class Cfg:
    def __init__(self, depth=4, t_lat=4096, t_ctx=256):
        self.depth = depth
        self.t_lat = t_lat
        self.t_ctx = t_ctx
        self.nt = t_lat + t_ctx
        self.groups = [(0, t_ctx, 1)] + [(t_ctx + i * 512, 512, 0) for i in range(t_lat // 512)]
        self.groups256 = [(i * 256, 256, 1 if i * 256 < t_ctx else 0) for i in range(self.nt // 256)]
        self.segs = [(0, t_ctx, 1), (t_ctx, t_lat, 0)]
        self.nch = self.nt // 64


def host_consts(cfg):
    T = cfg.t_lat
    k = np.arange(T, dtype=np.float64)
    ang = 2.0 * np.pi * ((k[:, None] * k[None, :]) % T) / T
    dftc = (np.cos(ang) / np.sqrt(T)).astype(ml_dtypes.bfloat16)
    dfts = (-np.sin(ang) / np.sqrt(T)).astype(ml_dtypes.bfloat16)
    c = np.arange(64, dtype=np.float64)
    a64 = 2.0 * np.pi * ((c[:, None] * c[None, :]) % 64) / 64
    cd = np.zeros((256, 512), np.float64)
    for g in range(4):
        cd[g * 64:(g + 1) * 64, g * 64:(g + 1) * 64] = np.cos(a64) / 8.0
        cd[g * 64:(g + 1) * 64, 256 + g * 64:256 + (g + 1) * 64] = np.sin(a64) / 8.0
    cd = cd.astype(ml_dtypes.bfloat16)
    cst = np.zeros((128, 1024), np.float32)
    cst[:, 0:128] = np.eye(128, dtype=np.float32)
    cst[:, 128:256] = np.arange(128, dtype=np.float32)[None, :]
    s = np.arange(64)
    cst[0:64, 256:320] = (s[:, None] <= s[None, :]).astype(np.float32)
    cst[0:64, 320:384] = (s[:, None] >= s[None, :]).astype(np.float32)
    for g in range(8):
        row = g if g < 4 else 32 + (g - 4)
        cst[row, 384 + g * 64:384 + (g + 1) * 64] = 1.0
    cst[:, 896:912] = np.arange(16, dtype=np.float32)[None, :]
    cst[:, 912] = EPS
    cst[:, 913] = 1.0
    return dftc, dfts, cd, cst


def grid_sincos(n_tok, d):
    rows = n_tok // 64
    n_freq = d // 4
    freq = (1.0 / (10000.0 ** (np.arange(n_freq, dtype=np.float32) / np.float32(n_freq)))).astype(np.float32)
    r = np.repeat(np.arange(rows, dtype=np.float32), 64)
    cc = np.tile(np.arange(64, dtype=np.float32), rows)
    ar = r[:, None] * freq[None, :]
    ac = cc[:, None] * freq[None, :]
    return np.concatenate([np.sin(ar), np.cos(ar), np.sin(ac), np.cos(ac)], axis=-1).astype(np.float32)


class MK:
    def __init__(self, cfg, debug=False):
        self.cfg = cfg
        self.nc = bass.Bass("TRN2", target_bir_lowering=False)
        self.P = Prog(self.nc)
        self.debug = debug
        P = self.P
        L = cfg.depth
        NT = cfg.nt
        din = lambda name, shape, dt=F32: P.dram(name, shape, dt, kind="ExternalInput")
        self.xin = din("xin", [NT, D])
        self.pos = din("pos", [cfg.t_lat, D])
        self.cvec = din("cvec", [2, D])
        self.w_ada = din("w_ada", [L, D, 6 * D])
        self.b_ada = din("b_ada", [L, 6 * D])
        self.norm1_g = din("norm1_g", [L, D])
        self.norm2_g = din("norm2_g", [L, D])
        self.w_in = din("w_in", [L, D, IN_COLS])
        self.ml_conv_w = din("ml_conv_w", [L, 3, 512])
        self.ml_conv_b = din("ml_conv_b", [L, 512])
        self.ml_gate_b = din("ml_gate_b", [L, 16])
        self.ml_norm_g = din("ml_norm_g", [L, 256])
        self.gm_ln_g = din("gm_ln_g", [L, 256])
        self.gm_ln_b = din("gm_ln_b", [L, 256])
        self.gm_w_s = din("gm_w_s", [L, 4, 128, 128])
        self.gm_b_s = din("gm_b_s", [L, 512])
        self.cv_dw_w = din("cv_dw_w", [L, 31, 256])
        self.cv_dw_b = din("cv_dw_b", [L, 256])
        self.cv_ln_g = din("cv_ln_g", [L, 256])
        self.cv_ln_b = din("cv_ln_b", [L, 256])
        self.w_branch = din("w_branch", [L, 4, 256, D])
        self.w_out = din("w_out", [L, D, D])
        self.peer_w_q = din("peer_w_q", [L, D, 2048])
        self.peer_keys = din("peer_keys", [L, 2, 128, 128])
        self.peer_u = din("peer_u", [L, NEXP, D])
        self.peer_v = din("peer_v", [L, NEXP, D])
        self.final_norm_g = din("final_norm_g", [D])
        self.dftc = din("dftc", [cfg.t_lat, cfg.t_lat], BF16)
        self.dfts = din("dfts", [cfg.t_lat, cfg.t_lat], BF16)
        self.cd = din("cd", [256, 512], BF16)
        self.cst_d = din("cst", [128, 1024])
        self.out = P.dram("out", [cfg.t_lat, D], F32, kind="ExternalOutput")
        sk = "ExternalOutput" if debug else "Internal"
        self.xT = P.dram("xT", [KD, 128, NT], F32, kind=sk)
        self.hT = P.dram("hT", [KD, 128, NT], BF16, kind=sk)
        self.Z = P.dram("Z", [4, 256, NT], BF16, kind=sk)
        self.Hf = P.dram("Hf", [cfg.nch, 64, 256], F32, kind=sk)
        self.Hb = P.dram("Hb", [cfg.nch, 64, 256], F32, kind=sk)
        self.UTs = [P.dram("UT%d" % i, [128, 128, KD * 128], BF16) for i in range(2)]
        self.VBs = [P.dram("VB%d" % i, [128, 128, D], BF16) for i in range(2)]
        self.cst = P.sb([128, 1024], F32, "cst")
        P.dma(self.cst.a, self.cst_d.a)
        c = self.cst
        self.identf = c[:, 0:128]
        self.iota128 = c[:, 128:256]
        self.iota16 = c[:, 896:912]
        self.eps = c[:, 912:913]
        self.identb_t = P.sb([128, 128], BF16, "identb")
        P.op('dve', 'tensor_copy', out=self.identb_t.a, in_=self.identf)
        self.identb = self.identb_t.a
        self.onesb_t = P.sb([128, 128], BF16, "onesb")
        P.op('pool', 'memset', ap=self.onesb_t.a, constant=1.0)
        self.onesb = self.onesb_t.a
        self.iota128b_t = P.sb([128, 128], BF16, "iota128b")
        P.op('dve', 'tensor_copy', out=self.iota128b_t.a, in_=self.iota128)
        self.onesf_t = P.sb([128, 128], F32, "onesf")
        P.op('pool', 'memset', ap=self.onesf_t.a, constant=1.0)
        self.onesf = self.onesf_t.a
        self.maskb_t = P.sb([64, 2, 4, 64], BF16, "maskb")
        for d_ in range(2):
            for h in range(4):
                P.op('dve', 'tensor_copy', out=self.maskb_t[:, d_, h, :], in_=c[0:64, 256 + 64 * d_:320 + 64 * d_])
        self.modT = P.sb([128, 48, 2], F32, "modT")
        cT = P.sb([2, D], F32, "cT")
        P.dma(cT.a, self.cvec.a)
        self.sT = P.sb([128, 2, KD], BF16, "sT")
        es = contextlib.ExitStack()
        ps = P.ps([128, 512], F32, es=es)
        for k in range(KD):
            P.op('pe', 'transpose', out=ps[:, 2 * k:2 * k + 2], in_=cT[:, k * 128:(k + 1) * 128], identity=self.identf[0:2, 0:2])
        P.op('act', 'activation', out=self.sT.a, in_=ps[:, 0:16].re("p (k s) -> p s k", s=2), func=AF.Silu)
        es.close()
        P.barrier()

    def load_rows_T(self, rows, es):
        P = self.P
        R = sum(v.ap.shape[0] for v in rows)
        w = rows[0].ap.shape[1]
        assert R <= 128
        rt = P.sb([128, 128], F32, "rows", es=es)
        r0 = 0
        for v in rows:
            r = v.ap.shape[0]
            P.dma(rt[r0:r0 + r, 0:w], v)
            r0 += r
        es_ = contextlib.ExitStack()
        ps = P.ps([128, 512], F32, es=es_)
        P.op('pe', 'transpose', out=ps[0:w, 0:R], in_=rt[0:R, 0:w], identity=self.identf[0:R, 0:R])
        ct = P.sb([128, R], F32, "colsT", es=es)
        P.op('dve', 'tensor_copy', out=ct[0:w, :], in_=ps[0:w, 0:R])
        P.barrier()
        es_.close()
        return ct

    def load_w(self, dst, src, q='pool'):
        self.P.dma(dst, src, q=q)

    def phase_init(self):
        P, cfg = self.P, self.cfg
        es = contextlib.ExitStack()
        xt = Rot([P.sb([128, D], F32, "xt", es=es) for _ in range(2)])
        pt = Rot([P.sb([128, D], F32, "pt", es=es) for _ in range(2)])
        pss = Rot([P.ps([128, 512], F32, es=es) for _ in range(4)])
        st = Rot([P.sb([128, KD, 128], F32, "xst", es=es) for _ in range(2)])
        for ti in range(cfg.nt // 128):
            x = xt.get()
            P.dma(x.a, self.xin[ti * 128:(ti + 1) * 128, :])
            if ti * 128 >= cfg.t_ctx:
                p = pt.get()
                r0 = ti * 128 - cfg.t_ctx
                P.dma(p.a, self.pos[r0:r0 + 128, :])
                P.op('dve', 'tensor_tensor', out=x.a, in0=x.a, in1=p.a, op=ALU.add)
            s = st.get()
            for half in range(2):
                ps = pss.get()
                for j in range(4):
                    k = half * 4 + j
                    P.op('pe', 'transpose', out=ps[:, j * 128:(j + 1) * 128], in_=x[:, k * 128:(k + 1) * 128], identity=self.identf)
                P.op('act', 'activation', out=s[:, half * 4:half * 4 + 4, :], in_=ps.a.re("p (j t) -> p j t", j=4), func=AF.Copy)
            P.dma(self.xT[:, :, ti * 128:(ti + 1) * 128].re("k p t -> p k t"), s.a)
        es.close()
        P.barrier()

    def phase_mod(self, l):
        P = self.P
        es = contextlib.ExitStack()
        wt = Rot([P.sb([128, KD, 1536], BF16, "wada", es=es) for _ in range(2)])
        ps = P.ps([128, 48, 2], F32, es=es)
        bT = self.load_rows_T([self.b_ada[l].re("(j p) -> j p", p=128)], es)
        for cg in range(4):
            w = wt.get()
            for k in range(KD):
                self.load_w(w[:, k, :], self.w_ada[l, k * 128:(k + 1) * 128, cg * 1536:(cg + 1) * 1536])
            for jj in range(12):
                j = cg * 12 + jj
                for k in range(KD):
                    P.mm(ps[:, j, :], w[:, k, jj * 128:(jj + 1) * 128], self.sT[:, :, k], start=(k == 0), stop=(k == KD - 1))
        P.op('dve', 'tensor_tensor', out=self.modT.a, in0=ps.a, in1=bT.a.unsq(2).bc([128, 48, 2]), op=ALU.add)
        es.close()
        P.barrier()

    def mod_scale_shift(self, l, which, es):
        P = self.P
        g = self.norm1_g if which == 0 else self.norm2_g
        gT = self.load_rows_T([g[l].re("(j p) -> j p", p=128)], es)
        base = 0 if which == 0 else 24
        gs = P.sb([128, KD, 2], F32, "gs", es=es)
        P.op('dve', 'tensor_scalar', out=gs.a, in0=self.modT[:, base + 8:base + 16, :], scalar1=1.0, scalar2=None, op0=ALU.add)
        P.op('dve', 'tensor_tensor', out=gs.a, in0=gs.a, in1=gT.a.unsq(2).bc([128, KD, 2]), op=ALU.mult)
        return gs, self.modT[:, base:base + 8, :], self.modT[:, base + 16:base + 24, :]

    def norm_group(self, xg, n, gs, sh, seg, hout, pss, tmps, sq, rsb):
        P = self.P
        P.op('act', 'activation', out=sq[:, :, 0:n], in_=xg, func=AF.Square)
        ps = pss.get()
        for k in range(KD):
            P.mm(ps[:, 0:n], self.onesb, sq[:, k, 0:n], start=(k == 0), stop=(k == KD - 1))
        rs = rsb
        P.op('act', 'activation', out=rs[:, 0:n], in_=ps[:, 0:n], func=AF.Sqrt, scale=1.0 / D, bias=self.eps)
        P.op('dve', 'reciprocal', out=rs[:, 0:n], in_=rs[:, 0:n])
        for k in range(KD):
            t = tmps.get()
            P.op('dve', 'scalar_tensor_tensor', out=t[:, 0:n], in0=xg[:, k, :], scalar=gs[:, k, seg:seg + 1], in1=rs[:, 0:n], op0=ALU.mult, op1=ALU.mult)
            if sh is None:
                P.op('act', 'activation', out=hout[:, k, :], in_=t[:, 0:n], func=AF.Copy)
            else:
                P.op('act', 'activation', out=hout[:, k, :], in_=t[:, 0:n], func=AF.Identity, bias=sh[:, k, seg:seg + 1])

    def phase_norm1(self, l):
        P, cfg = self.P, self.cfg
        es = contextlib.ExitStack()
        gs, sh, _ = self.mod_scale_shift(l, 0, es)
        xg = Rot([P.sb([128, KD, 512], F32, "xg", es=es) for _ in range(2)])
        hg = Rot([P.sb([128, KD, 512], BF16, "hg", es=es) for _ in range(2)])
        sq = P.sb([128, KD, 512], BF16, "sq", es=es)
        pss = Rot([P.ps([128, 512], F32, es=es) for _ in range(2)])
        tmps = Rot([P.sb([128, 512], F32, "nt", es=es) for _ in range(4)])
        rsb = P.sb([128, 512], F32, "rsb", es=es)
        for (n0, n, seg) in cfg.groups:
            x = xg.get()
            h = hg.get()
            P.dma(x[:, :, 0:n], self.xT[:, :, n0:n0 + n].re("k p t -> p k t"))
            self.norm_group(x[:, :, 0:n], n, gs, sh, seg, h[:, :, 0:n], pss, tmps, sq, rsb)
            P.dma(self.hT[:, :, n0:n0 + n].re("k p t -> p k t"), h[:, :, 0:n], q='pool')
        es.close()
        P.barrier()


def _gm(self, l):
    P, cfg = self.P, self.cfg
    es = contextlib.ExitStack()
    W = P.sb([128, KD, 512], BF16, "wgm", es=es)
    for k in range(KD):
        self.load_w(W[:, k, :], self.w_in[l, k * 128:(k + 1) * 128, GM_OFF:GM_OFF + 512])
    lng = P.sb([128, 256], F32, "lng", es=es)
    lnb = P.sb([128, 256], F32, "lnb", es=es)
    P.dma(lng.a, self.gm_ln_g[l:l + 1, :].pbc(128))
    P.dma(lnb.a, self.gm_ln_b[l:l + 1, :].pbc(128))
    bsr = P.sb([1, 512], F32, "bsr", es=es)
    P.dma(bsr.a, self.gm_b_s[l:l + 1, :])
    wsT = P.sb([128, 4, 128], BF16, "wsT", es=es)
    wsl = P.sb([128, 4, 128], BF16, "wsl", es=es)
    self.load_w(wsl.a, self.gm_w_s[l].re("g t s -> t g s"))
    pst = P.ps([128, 4, 128], BF16, es=es)
    for g in range(4):
        P.op('pe', 'transpose', out=pst[:, g, :], in_=wsl[:, g, :], identity=self.identb)
    P.op('dve', 'tensor_copy', out=wsT.a, in_=pst.a)
    hg = Rot([P.sb([128, KD, 512], BF16, "hg", es=es) for _ in range(2)])
    u64 = Rot([P.sb([64, 4, 512], BF16, "u64", es=es) for _ in range(2)])
    zst = Rot([P.sb([64, 4, 512], BF16, "zst", es=es) for _ in range(2)])
    psu = Rot([P.ps([128, 512], F32, es=es) for _ in range(2)])
    psv = Rot([P.ps([128, 512], F32, es=es) for _ in range(2)])
    pss = Rot([P.ps([64, 4, 128], F32, es=es) for _ in range(2)])
    vt = Rot([P.sb([128, 256], F32, "vt", es=es) for _ in range(2)])
    vn = Rot([P.sb([128, 256], BF16, "vn", es=es) for _ in range(2)])
    st6 = Rot([P.sb([128, 8], F32, "st6", es=es) for _ in range(2)])
    for (n0, n, seg) in cfg.groups:
        if self.skip_ctx and seg == 1:
            continue
        h = hg.get()
        P.dma(h[:, :, 0:n], self.hT[:, :, n0:n0 + n].re("k p t -> p k t"))
        u = u64.get()
        for g in range(4):
            ps = psu.get()
            for k in range(KD):
                P.mm(ps[0:64, 0:n], W[:, k, g * 64:(g + 1) * 64], h[:, k, 0:n], start=(k == 0), stop=(k == KD - 1))
            P.op('act', 'activation', out=u[:, g, 0:n], in_=ps[0:64, 0:n], func=AF.Gelu_apprx_tanh)
        z = zst.get()
        for sub in range(n // 128):
            ps = psv.get()
            for k in range(KD):
                P.mm(ps[:, 0:256], h[:, k, sub * 128:(sub + 1) * 128], W[:, k, 256:512], start=(k == 0), stop=(k == KD - 1))
            v = vt.get()
            P.op('act', 'activation', out=v.a, in_=ps[:, 0:256], func=AF.Gelu_apprx_tanh)
            s6 = st6.get()
            P.op('dve', 'bn_stats', out=s6[:, 0:6], in_=v.a)
            P.op('dve', 'bn_aggr', out=s6[:, 6:8], in_=s6[:, 0:6])
            P.op('act', 'activation', out=s6[:, 7:8], in_=s6[:, 7:8], func=AF.Sqrt, bias=self.eps)
            P.op('dve', 'reciprocal', out=s6[:, 7:8], in_=s6[:, 7:8])
            P.op('dve', 'tensor_scalar', out=v.a, in0=v.a, scalar1=s6[:, 6:7], scalar2=s6[:, 7:8], op0=ALU.subtract, op1=ALU.mult)
            P.op('dve', 'tensor_tensor', out=v.a, in0=v.a, in1=lng.a, op=ALU.mult)
            vb = vn.get()
            P.op('dve', 'tensor_tensor', out=vb.a, in0=v.a, in1=lnb.a, op=ALU.add)
            pg = pss.get()
            for g in range(4):
                P.mm(pg[:, g, :], vb[:, g * 64:(g + 1) * 64], wsT[:, g, :], start=True, stop=False)
                P.mm(pg[:, g, :], self.onesf[0:1, 0:64], bsr[0:1, g * 128:(g + 1) * 128], start=False, stop=True)
            P.op('dve', 'tensor_tensor', out=z[:, :, sub * 128:(sub + 1) * 128], in0=pg.a, in1=u[:, :, sub * 128:(sub + 1) * 128], op=ALU.mult)
        P.dma(self.Z[1, :, n0:n0 + n].re("(g p) t -> p g t", p=64), z[:, :, 0:n], q='pool')
    es.close()
    P.barrier()


MK.phase_gmlp = _gm


def _cv(self, l):
    P, cfg = self.P, self.cfg
    es = contextlib.ExitStack()
    PAD = 15
    W = P.sb([128, KD, 512], BF16, "wcv", es=es)
    for k in range(KD):
        self.load_w(W[:, k, :], self.w_in[l, k * 128:(k + 1) * 128, CV_OFF:CV_OFF + 512])
    cols = self.load_rows_T([self.cv_dw_w[l].re("j (c p) -> (j c) p", p=128), self.cv_dw_b[l].re("(c p) -> c p", p=128),
                             self.cv_ln_g[l].re("(c p) -> c p", p=128), self.cv_ln_b[l].re("(c p) -> c p", p=128)], es)
    diag = P.sb([128, 62, 128], BF16, "diag", es=es)
    for jc in range(62):
        P.op('dve', 'tensor_scalar', out=diag[:, jc, :], in0=self.identf, scalar1=cols[:, jc:jc + 1], scalar2=None, op0=ALU.mult)
    hg = Rot([P.sb([128, KD, 512], BF16, "hg", es=es) for _ in range(2)])
    psa = Rot([P.ps([128, 512], F32, es=es) for _ in range(2)])
    psb = Rot([P.ps([128, 512], F32, es=es) for _ in range(2)])
    psc = Rot([P.ps([128, 512], F32, es=es) for _ in range(2)])
    sg = Rot([P.sb([128, 512], F32, "sg", es=es) for _ in range(2)])
    for (s0, sn, seg) in cfg.segs:
        if self.skip_ctx and seg == 1:
            continue
        es2 = contextlib.ExitStack()
        zp = P.sb([128, 2, sn + 2 * PAD], BF16, "zp", es=es2)
        P.op('pool', 'memset', ap=zp.a, constant=0.0)
        y = P.sb([128, 2, 512], F32, "ycv", es=es2)
        y2 = P.sb([128, 2, 512], F32, "ycv2", es=es2)
        mean = P.sb([128, 512], F32, "mean", es=es2)
        rstd = P.sb([128, 512], F32, "rstd", es=es2)
        zo = Rot([P.sb([128, 2, 512], BF16, "zo", es=es2) for _ in range(2)])
        grp = [(a, n) for (a, n, sg_) in cfg.groups if sg_ == seg]
        for (n0, n) in grp:
            h = hg.get()
            P.dma(h[:, :, 0:n], self.hT[:, :, n0:n0 + n].re("k p t -> p k t"))
            for c in range(2):
                pa = psa.get()
                pb = psb.get()
                for k in range(KD):
                    P.mm(pa[:, 0:n], W[:, k, c * 128:(c + 1) * 128], h[:, k, 0:n], start=(k == 0), stop=(k == KD - 1))
                for k in range(KD):
                    P.mm(pb[:, 0:n], W[:, k, 256 + c * 128:256 + (c + 1) * 128], h[:, k, 0:n], start=(k == 0), stop=(k == KD - 1))
                s = sg.get()
                P.op('act', 'activation', out=s[:, 0:n], in_=pb[:, 0:n], func=AF.Sigmoid)
                o0 = PAD + n0 - s0
                P.op('dve', 'tensor_tensor', out=zp[:, c, o0:o0 + n], in0=pa[:, 0:n], in1=s[:, 0:n], op=ALU.mult)
        for (n0, n) in grp:
            o0 = n0 - s0
            for c in range(2):
                pc = psc.get()
                for j in range(31):
                    P.mm(pc[:, 0:n], diag[:, j * 2 + c, :], zp[:, c, o0 + j:o0 + j + n], start=(j == 0), stop=(j == 30))
                P.op('act', 'activation', out=y[:, c, 0:n], in_=pc[:, 0:n], func=AF.Identity, bias=cols[:, 62 + c:63 + c])
                P.op('act', 'activation', out=y2[:, c, 0:n], in_=y[:, c, 0:n], func=AF.Square)
            p1 = psa.get()
            p2 = psb.get()
            for c in range(2):
                P.mm(p1[:, 0:n], self.onesf, y[:, c, 0:n], start=(c == 0), stop=(c == 1))
            for c in range(2):
                P.mm(p2[:, 0:n], self.onesf, y2[:, c, 0:n], start=(c == 0), stop=(c == 1))
            P.op('act', 'activation', out=mean[:, 0:n], in_=p1[:, 0:n], func=AF.Identity, scale=1.0 / 256)
            P.op('dve', 'tensor_tensor', out=rstd[:, 0:n], in0=mean[:, 0:n], in1=mean[:, 0:n], op=ALU.mult)
            P.op('dve', 'scalar_tensor_tensor', out=rstd[:, 0:n], in0=p2[:, 0:n], scalar=1.0 / 256, in1=rstd[:, 0:n], op0=ALU.mult, op1=ALU.subtract)
            P.op('act', 'activation', out=rstd[:, 0:n], in_=rstd[:, 0:n], func=AF.Sqrt, bias=self.eps)
            P.op('dve', 'reciprocal', out=rstd[:, 0:n], in_=rstd[:, 0:n])
            z = zo.get()
            for c in range(2):
                P.op('dve', 'tensor_tensor', out=y[:, c, 0:n], in0=y[:, c, 0:n], in1=mean[:, 0:n], op=ALU.subtract)
                P.op('dve', 'tensor_tensor', out=y[:, c, 0:n], in0=y[:, c, 0:n], in1=rstd[:, 0:n], op=ALU.mult)
                P.op('act', 'activation', out=z[:, c, 0:n], in_=y[:, c, 0:n], func=AF.Silu, scale=cols[:, 64 + c:65 + c], bias=cols[:, 66 + c:67 + c])
            P.dma(self.Z[2, :, n0:n0 + n].re("(c p) t -> p c t", p=128), z[:, :, 0:n], q='pool')
        es2.close()
        P.barrier()
    es.close()
    P.barrier()


MK.phase_conv = _cv


def _ft(self, l):
    P, cfg = self.P, self.cfg
    es = contextlib.ExitStack()
    W = P.sb([128, KD, 256], BF16, "wft", es=es)
    for k in range(KD):
        self.load_w(W[:, k, :], self.w_in[l, k * 128:(k + 1) * 128, FT_OFF:FT_OFF + 256])
    CD = P.sb([128, 2, 512], BF16, "cdt", es=es)
    P.dma(CD.a, self.cd.a.re("(c p) n -> p c n", p=128))
    hg = Rot([P.sb([128, KD, 512], BF16, "hg", es=es) for _ in range(2)])
    zf = Rot([P.sb([128, 2, 512], BF16, "zf", es=es) for _ in range(2)])
    psa = Rot([P.ps([128, 512], F32, es=es) for _ in range(2)])
    psd = Rot([P.ps([128, 512], F32, es=es) for _ in range(4)])
    for (s0, sn, seg) in cfg.segs:
        if self.skip_ctx and seg == 1:
            continue
        es2 = contextlib.ExitStack()
        ntile = sn // 128
        zcs = P.sb([128, ntile, 512], BF16, "zcs", es=es2)
        grp = [(a, n) for (a, n, sg_) in cfg.groups if sg_ == seg]
        for (n0, n) in grp:
            h = hg.get()
            P.dma(h[:, :, 0:n], self.hT[:, :, n0:n0 + n].re("k p t -> p k t"))
            z = zf.get()
            for c in range(2):
                pa = psa.get()
                for k in range(KD):
                    P.mm(pa[:, 0:n], W[:, k, c * 128:(c + 1) * 128], h[:, k, 0:n], start=(k == 0), stop=(k == KD - 1))
                P.op('act', 'activation', out=z[:, c, 0:n], in_=pa[:, 0:n], func=AF.Copy)
            for sub in range(n // 128):
                pd = psd.get()
                for c in range(2):
                    P.mm(pd.a, z[:, c, sub * 128:(sub + 1) * 128], CD[:, c, :], start=(c == 0), stop=(c == 1))
                ti = (n0 - s0) // 128 + sub
                P.op('dve', 'tensor_copy', out=zcs[:, ti, :], in_=pd.a)
        kb_n = min(512, sn)
        nkb = sn // kb_n
        tcs = Rot([P.sb([128, ntile, kb_n], BF16, "tc", es=es2) for _ in range(2)])
        tss = Rot([P.sb([128, ntile, kb_n], BF16, "ts", es=es2) for _ in range(2)])
        zo = Rot([P.sb([128, 2, 512], BF16, "zo", es=es2) for _ in range(2)])
        rstride = cfg.t_lat // sn
        for kb in range(nkb):
            tc_ = tcs.get()
            ts_ = tss.get()
            if rstride == 1:
                P.dma(tc_.a, self.dftc[:, kb * kb_n:(kb + 1) * kb_n].re("(t p) n -> p t n", p=128))
                P.dma(ts_.a, self.dfts[:, kb * kb_n:(kb + 1) * kb_n].re("(t p) n -> p t n", p=128))
            else:
                P.dma(tc_.a, self.dftc.a.re("(r s) n -> r s n", s=rstride)[:, 0, kb * kb_n:(kb + 1) * kb_n].re("(t p) n -> p t n", p=128))
                P.dma(ts_.a, self.dfts.a.re("(r s) n -> r s n", s=rstride)[:, 0, kb * kb_n:(kb + 1) * kb_n].re("(t p) n -> p t n", p=128))
            z = zo.get()
            for c in range(2):
                pd = psd.get()
                for ti in range(ntile):
                    P.mm(pd[:, 0:kb_n], zcs[:, ti, c * 128:(c + 1) * 128], tc_[:, ti, :], start=(ti == 0), stop=False)
                    P.mm(pd[:, 0:kb_n], zcs[:, ti, 256 + c * 128:256 + (c + 1) * 128], ts_[:, ti, :], start=False, stop=(ti == ntile - 1))
                P.op('act', 'activation', out=z[:, c, 0:kb_n], in_=pd[:, 0:kb_n], func=AF.Identity, scale=float(np.sqrt(rstride)))
            P.dma(self.Z[3, :, s0 + kb * kb_n:s0 + (kb + 1) * kb_n].re("(c p) t -> p c t", p=128), z[:, :, 0:kb_n], q='pool')
        es2.close()
        P.barrier()
    es.close()
    P.barrier()


MK.phase_fnet = _ft


def _merge(self, l):
    P, cfg = self.P, self.cfg
    es = contextlib.ExitStack()
    Wg = P.sb([128, KD, 4096], BF16, "wgate", es=es)
    for k in range(KD):
        for i in range(4):
            self.load_w(Wg[:, k, i * 1024:(i + 1) * 1024], self.w_in[l, k * 128:(k + 1) * 128, GATE_OFF + i * 1024:GATE_OFF + (i + 1) * 1024])
    Wb = P.sb([128, 4, 2, 1024], BF16, "wbr", es=es)
    for i in range(4):
        for c in range(2):
            self.load_w(Wb[:, i, c, :], self.w_branch[l, i, c * 128:(c + 1) * 128, :])
    Wo = P.sb([128, KD, 1024], BF16, "wout", es=es)
    for k in range(KD):
        self.load_w(Wo[:, k, :], self.w_out[l, k * 128:(k + 1) * 128, :])
    gate1 = self.modT[:, 16:24, :]
    hg = Rot([P.sb([128, KD, 512], BF16, "hg", es=es) for _ in range(2)])
    zg = Rot([P.sb([128, 4, 2, 512], BF16, "zg", es=es) for _ in range(2)])
    xg = Rot([P.sb([128, KD, 512], F32, "xg", es=es) for _ in range(2)])
    yT = P.sb([128, KD, 512], BF16, "yT", es=es)
    psg = Rot([P.ps([128, 512], F32, es=es) for _ in range(3)])
    psl = Rot([P.ps([128, 512], F32, es=es) for _ in range(3)])
    pso = Rot([P.ps([128, 512], F32, es=es) for _ in range(2)])
    sg = Rot([P.sb([128, 512], F32, "sg", es=es) for _ in range(3)])
    acc = Rot([P.sb([128, 512], F32, "acc", es=es) for _ in range(2)])
    for (n0, n, seg) in cfg.groups:
        if self.skip_ctx and seg == 1:
            continue
        h = hg.get()
        P.dma(h[:, :, 0:n], self.hT[:, :, n0:n0 + n].re("k p t -> p k t"))
        z = zg.get()
        for i in range(4):
            P.dma(z[:, i, :, 0:n], self.Z[i, :, n0:n0 + n].re("(c p) t -> p c t", p=128))
        x = xg.get()
        P.dma(x[:, :, 0:n], self.xT[:, :, n0:n0 + n].re("k p t -> p k t"))
        for dc in range(KD):
            a = acc.get()
            for i in range(4):
                pg = psg.get()
                for k in range(KD):
                    P.mm(pg[:, 0:n], Wg[:, k, i * 1024 + dc * 128:i * 1024 + (dc + 1) * 128], h[:, k, 0:n], start=(k == 0), stop=(k == KD - 1))
                s = sg.get()
                P.op('act', 'activation', out=s[:, 0:n], in_=pg[:, 0:n], func=AF.Sigmoid)
                pl = psl.get()
                for c in range(2):
                    P.mm(pl[:, 0:n], Wb[:, i, c, dc * 128:(dc + 1) * 128], z[:, i, c, 0:n], start=(c == 0), stop=(c == 1))
                if i == 0:
                    P.op('dve', 'tensor_tensor', out=a[:, 0:n], in0=pl[:, 0:n], in1=s[:, 0:n], op=ALU.mult)
                else:
                    P.op('dve', 'tensor_tensor', out=s[:, 0:n], in0=pl[:, 0:n], in1=s[:, 0:n], op=ALU.mult)
                    if i < 3:
                        P.op(self.merge_eng, 'tensor_tensor', out=a[:, 0:n], in0=a[:, 0:n], in1=s[:, 0:n], op=ALU.add)
                    else:
                        P.op(self.merge_eng, 'tensor_tensor', out=yT[:, dc, 0:n], in0=a[:, 0:n], in1=s[:, 0:n], op=ALU.add)
        for dc in range(KD):
            po = pso.get()
            for k in range(KD):
                P.mm(po[:, 0:n], Wo[:, k, dc * 128:(dc + 1) * 128], yT[:, k, 0:n], start=(k == 0), stop=(k == KD - 1))
            P.op('dve', 'scalar_tensor_tensor', out=x[:, dc, 0:n], in0=po[:, 0:n], scalar=gate1[:, dc, seg:seg + 1], in1=x[:, dc, 0:n], op0=ALU.mult, op1=ALU.add)
        P.dma(self.xT[:, :, n0:n0 + n].re("k p t -> p k t"), x[:, :, 0:n], q='pool')
    es.close()
    P.barrier()


MK.phase_merge = _merge


def _final(self):
    P, cfg = self.P, self.cfg
    es = contextlib.ExitStack()
    gT = self.load_rows_T([self.final_norm_g.a.re("(j p) -> j p", p=128)], es)
    gs = P.sb([128, KD, 2], F32, "gsf", es=es)
    P.op('dve', 'tensor_copy', out=gs.a, in_=gT.a.unsq(2).bc([128, KD, 2]))
    xg = Rot([P.sb([128, KD, 512], F32, "xg", es=es) for _ in range(2)])
    yg = Rot([P.sb([128, KD, 512], F32, "yg", es=es) for _ in range(2)])
    sq = P.sb([128, KD, 512], BF16, "sq", es=es)
    pss = Rot([P.ps([128, 512], F32, es=es) for _ in range(2)])
    pst = Rot([P.ps([128, 512], F32, es=es) for _ in range(4)])
    tmps = Rot([P.sb([128, 512], F32, "nt", es=es) for _ in range(4)])
    rsb = P.sb([128, 512], F32, "rsb", es=es)
    ot = Rot([P.sb([128, D], F32, "ot", es=es) for _ in range(3)])
    for (n0, n, seg) in cfg.groups:
        if seg == 1:
            continue
        x = xg.get()
        y = yg.get()
        P.dma(x[:, :, 0:n], self.xT[:, :, n0:n0 + n].re("k p t -> p k t"))
        self.norm_group(x[:, :, 0:n], n, gs, None, 0, y[:, :, 0:n], pss, tmps, sq, rsb)
        for sub in range(n // 128):
            o = ot.get()
            for half in range(2):
                ps = pst.get()
                for j in range(4):
                    k = half * 4 + j
                    P.op('pe', 'transpose', out=ps[:, j * 128:(j + 1) * 128], in_=y[:, k, sub * 128:(sub + 1) * 128], identity=self.identf)
                P.op('act', 'activation', out=o[:, half * 512:(half + 1) * 512], in_=ps.a, func=AF.Copy)
            r0 = n0 - cfg.t_ctx + sub * 128
            P.dma(self.out[r0:r0 + 128, :], o.a, q='pool')
    es.close()
    P.barrier()


MK.phase_final = _final


def _ml(self, l):
    P, cfg = self.P, self.cfg
    NT, NCH = cfg.nt, cfg.nch
    nctx = cfg.t_ctx // 64
    order_b = list(range(nctx - 1, -1, -1)) + list(range(NCH - 1, nctx - 1, -1))
    order_f = list(range(NCH))
    es = contextlib.ExitStack()
    biasA = P.sb([64, 1], F32, "biasA", es=es)
    biasB = P.sb([64, 1], F32, "biasB", es=es)
    P.op('pool', 'memset', ap=biasA.a, constant=0.0)
    P.op('pool', 'memset', ap=biasB.a, constant=0.0)
    gb = self.ml_gate_b
    P.dma(biasA[0:4, :], gb[l, 0:4].re("(a b) -> a b", b=1))
    P.dma(biasA[32:36, :], gb[l, 4:8].re("(a b) -> a b", b=1))
    P.dma(biasB[0:4, :], gb[l, 8:12].re("(a b) -> a b", b=1))
    P.dma(biasB[32:36, :], gb[l, 12:16].re("(a b) -> a b", b=1))
    cols = self.load_rows_T([self.ml_conv_w[l].re("j (c p) -> (j c) p", p=128), self.ml_conv_b[l].re("(c p) -> c p", p=128)], es)
    colsB = self.load_rows_T([self.ml_conv_b[l].re("(c p) -> c p", p=64)], es)
    diag = P.sb([128, 12, 128], BF16, "diag3", es=es)
    for jc in range(12):
        P.op('dve', 'tensor_scalar', out=diag[:, jc, :], in0=self.identf, scalar1=cols[:, jc:jc + 1], scalar2=None, op0=ALU.mult)
    qk8 = [P.sb([64, NT], BF16, "qk8_%d" % i, es=es) for i in range(8)]
    Atok = P.sb([64, NCH, 8], F32, "Atok", es=es)
    Btok = P.sb([64, NCH, 8], F32, "Btok", es=es)
    Ctok = P.sb([64, NCH, 8], F32, "Ctok", es=es)
    decb = P.sb([64, 8, NCH], F32, "decb", es=es)
    emb = P.sb([64, 8, NCH], F32, "emb", es=es)
    es1 = contextlib.ExitStack()
    Wqk = P.sb([128, KD, 512], BF16, "wqk", es=es1)
    for k in range(KD):
        self.load_w(Wqk[:, k, :], self.w_in[l, k * 128:(k + 1) * 128, 0:512])
    pre = [P.sb([128, NT + 4], BF16, "pre%d" % i, es=es1) for i in range(4)]
    for i in range(4):
        P.op('pool', 'memset', ap=pre[i].a, constant=0.0)
    hg = Rot([P.sb([128, KD, 512], BF16, "hg", es=es1) for _ in range(2)])
    psQ = Rot([P.ps([128, 512], F32, es=es1) for _ in range(2)])

    def poff(seg):
        return 1 if seg == 1 else cfg.t_ctx + 3

    for (n0, n, seg) in cfg.groups:
        s0 = 0 if seg == 1 else cfg.t_ctx
        h = hg.get()
        P.dma(h[:, :, 0:n], self.hT[:, :, n0:n0 + n].re("k p t -> p k t"))
        for ci in range(4):
            pq = psQ.get()
            for k in range(KD):
                P.mm(pq[:, 0:n], Wqk[:, k, ci * 128:(ci + 1) * 128], h[:, k, 0:n], start=(k == 0), stop=(k == KD - 1))
            o0 = poff(seg) + n0 - s0
            P.op('dve', 'tensor_copy', out=pre[ci][:, o0:o0 + n], in_=pq[:, 0:n])
    for (n0, n, seg) in cfg.groups:
        s0 = 0 if seg == 1 else cfg.t_ctx
        for ci in range(4):
            for hp in range(2):
                pq = psQ.get()
                o0 = poff(seg) + n0 - s0 - 1
                for j in range(3):
                    P.mm(pq[0:64, 0:n], diag[:, j * 4 + ci, hp * 64:(hp + 1) * 64], pre[ci][:, o0 + j:o0 + j + n], start=(j == 0), stop=(j == 2))
                P.op('act', 'activation', out=qk8[ci * 2 + hp][:, n0:n0 + n], in_=pq[0:64, 0:n], func=AF.Silu, bias=colsB[0:64, ci * 2 + hp:ci * 2 + hp + 1])
    for h8 in range(4, 8):
        P.op('dve', 'tensor_scalar', out=qk8[h8].a, in0=qk8[h8].a, scalar1=0.125, scalar2=None, op0=ALU.mult)
    es1.close()
    P.barrier()
    esA = contextlib.ExitStack()
    LI = P.sb([64, NT], F32, "LI", es=esA)
    LF = P.sb([64, NT], F32, "LF", es=esA)
    es1 = contextlib.ExitStack()
    WgA = P.sb([128, KD, 64], BF16, "wga", es=es1)
    WgB = P.sb([128, KD, 64], BF16, "wgb", es=es1)
    P.op('pool', 'memset', ap=WgA.a, constant=0.0)
    P.op('pool', 'memset', ap=WgB.a, constant=0.0)
    for k in range(KD):
        rows = slice(k * 128, (k + 1) * 128)
        self.load_w(WgA[:, k, 0:4], self.w_in[l, rows, 768:772])
        self.load_w(WgA[:, k, 32:36], self.w_in[l, rows, 772:776])
        self.load_w(WgB[:, k, 0:4], self.w_in[l, rows, 776:780])
        self.load_w(WgB[:, k, 32:36], self.w_in[l, rows, 780:784])
    hg = Rot([P.sb([128, KD, 512], BF16, "hg", es=es1) for _ in range(2)])
    psA = Rot([P.ps([128, 512], F32, es=es1) for _ in range(2)])
    sgt = Rot([P.sb([64, 512], F32, "sgt", es=es1) for _ in range(2)])
    for (n0, n, seg) in cfg.groups:
        h = hg.get()
        P.dma(h[:, :, 0:n], self.hT[:, :, n0:n0 + n].re("k p t -> p k t"))
        pa = psA.get()
        for k in range(KD):
            P.mm(pa[0:64, 0:n], WgA[:, k, :], h[:, k, 0:n], start=(k == 0), stop=(k == KD - 1))
        P.op('act', 'activation', out=LI[:, n0:n0 + n], in_=pa[0:64, 0:n], func=AF.Identity, bias=biasA.a)
        pb = psA.get()
        for k in range(KD):
            P.mm(pb[0:64, 0:n], WgB[:, k, :], h[:, k, 0:n], start=(k == 0), stop=(k == KD - 1))
        s = sgt.get()
        P.op('act', 'activation', out=s[:, 0:n], in_=pb[0:64, 0:n], func=AF.Sigmoid, bias=biasB.a)
        P.op('act', 'activation', out=LF[:, n0:n0 + n], in_=s[:, 0:n], func=AF.Ln)
    es1.close()
    P.barrier()
    es1 = contextlib.ExitStack()
    ones = P.sb([64, NT], BF16, "ones", es=es1)
    P.op('pool', 'memset', ap=ones.a, constant=1.0)
    cs = P.sb([64, NT], F32, "cs", es=es1)
    P.op('dve', 'tensor_tensor_scan', out=cs.a, data0=ones.a, data1=LF.a, initial=0.0, op0=ALU.mult, op1=ALU.add)
    j3 = lambda b: b.a.re("p (c j) -> p c j", j=64)
    sm = lambda nm: P.sb([64, NCH], F32, nm, es=es1)
    base, tot, wmax, cmax, m_in, m_new, Mx, dec, em = [sm(x) for x in ("base", "tot", "wmax", "cmax", "m_in", "m_new", "Mx", "dec", "em")]
    bc3 = lambda b: b.a.unsq(2).bc([64, NCH, 64])
    P.op('dve', 'tensor_tensor', out=base.a, in0=j3(cs)[:, :, 0], in1=j3(LF)[:, :, 0], op=ALU.subtract)
    BC = cs
    P.op('dve', 'tensor_tensor', out=j3(BC), in0=j3(cs), in1=bc3(base), op=ALU.subtract)
    P.op('dve', 'tensor_copy', out=tot.a, in_=j3(BC)[:, :, 63])
    P.op('dve', 'tensor_tensor', out=j3(BC)[32:64], in0=bc3(tot)[32:64], in1=j3(BC)[32:64], op=ALU.subtract)
    P.op('dve', 'tensor_tensor', out=BC[32:64, :], in0=BC[32:64, :], in1=LF[32:64, :], op=ALU.add)
    Wt = LF
    Cq = LI
    P.op('dve', 'tensor_tensor', out=Cq.a, in0=LI.a, in1=BC.a, op=ALU.subtract)
    P.op('dve', 'tensor_tensor', out=j3(Wt), in0=j3(Cq), in1=bc3(tot), op=ALU.add)
    P.op('dve', 'tensor_reduce', out=wmax.a, in_=j3(Wt), axis=AX.X, op=ALU.max)
    P.op('dve', 'tensor_reduce', out=cmax.a, in_=j3(Cq), axis=AX.X, op=ALU.max)
    zero = P.sb([64, 1], F32, "zero", es=es1)
    P.op('pool', 'memset', ap=zero.a, constant=0.0)
    P.op('pool', 'memset', ap=m_in.a, constant=0.0)
    for (rows, order) in ((slice(0, 32), order_f), (slice(32, 64), order_b)):
        prev = None
        for i, c in enumerate(order):
            pv_ = zero[rows, 0:1] if prev is None else m_new[rows, prev:prev + 1]
            if prev is not None:
                P.op('dve', 'tensor_copy', out=m_in[rows, c:c + 1], in_=pv_)
            P.op('dve', 'tensor_scalar', out=m_new[rows, c:c + 1], in0=pv_, scalar1=tot[rows, c:c + 1], scalar2=wmax[rows, c:c + 1], op0=ALU.add, op1=ALU.max)
            prev = c
    P.op('dve', 'tensor_tensor', out=Mx.a, in0=m_in.a, in1=cmax.a, op=ALU.max)
    P.op('dve', 'tensor_tensor', out=j3(Cq), in0=j3(Cq), in1=bc3(Mx), op=ALU.subtract)
    P.op('act', 'activation', out=Cq.a, in_=Cq.a, func=AF.Exp)
    P.op('dve', 'tensor_tensor', out=j3(Wt), in0=j3(Wt), in1=bc3(m_new), op=ALU.subtract)
    P.op('act', 'activation', out=Wt.a, in_=Wt.a, func=AF.Exp)
    P.op('dve', 'tensor_tensor', out=j3(BC), in0=j3(BC), in1=bc3(Mx), op=ALU.add)
    P.op('act', 'activation', out=BC.a, in_=BC.a, func=AF.Exp, scale=-1.0)
    P.op('dve', 'tensor_tensor', out=dec.a, in0=tot.a, in1=m_in.a, op=ALU.add)
    P.op('dve', 'tensor_tensor', out=dec.a, in0=dec.a, in1=m_new.a, op=ALU.subtract)
    P.op('act', 'activation', out=dec.a, in_=dec.a, func=AF.Exp)
    P.op('dve', 'tensor_tensor', out=em.a, in0=m_in.a, in1=Mx.a, op=ALU.subtract)
    P.op('act', 'activation', out=em.a, in_=em.a, func=AF.Exp)
    pt = Rot([P.ps([64, 8, 64], F32, es=es1) for _ in range(2)])
    for (src, dst) in ((Cq, Atok), (Wt, Btok), (BC, Ctok)):
        for c0 in range(0, NCH, 8):
            nb = min(8, NCH - c0)
            p_ = pt.get()
            for cc in range(nb):
                P.op('pe', 'transpose', out=p_[:, cc, :], in_=src[:, (c0 + cc) * 64:(c0 + cc + 1) * 64], identity=self.identf[0:64, 0:64])
            P.op('dve', 'tensor_copy', out=dst[:, c0:c0 + nb, 0:4], in_=p_[:, 0:nb, 0:4])
            P.op('dve', 'tensor_copy', out=dst[:, c0:c0 + nb, 4:8], in_=p_[:, 0:nb, 32:36])
    pbq = Rot([P.ps([64, 4, NCH], F32, es=es1) for _ in range(2)])
    for (src, dst) in ((dec, decb), (em, emb)):
        for half in range(2):
            p_ = pbq.get()
            for gg in range(4):
                g = half * 4 + gg
                P.mm(p_[:, gg, :], self.cst[0:64, 384 + g * 64:384 + (g + 1) * 64], src.a, start=True, stop=True)
            P.op('dve', 'tensor_copy', out=dst[:, half * 4:half * 4 + 4, :], in_=p_.a)
    es1.close()
    esA.close()
    P.barrier()
    es3 = contextlib.ExitStack()
    v1 = P.sb([64, NCH, 4, 65], BF16, "v1", es=es3)
    P.op('pool', 'memset', ap=v1.a, constant=1.0)
    ktok = P.sb([64, NCH, 256], BF16, "ktok", es=es3)
    es3a = contextlib.ExitStack()
    Wv = P.sb([128, KD, 256], BF16, "wv", es=es3a)
    for k in range(KD):
        self.load_w(Wv[:, k, :], self.w_in[l, k * 128:(k + 1) * 128, 512:768])
    hg = Rot([P.sb([128, KD, 512], BF16, "hg", es=es3a) for _ in range(2)])
    psV = Rot([P.ps([128, 512], F32, es=es3a) for _ in range(2)])
    pk = Rot([P.ps([64, 256], BF16, es=es3a) for _ in range(2)])
    for (n0, n, seg) in cfg.groups:
        h = hg.get()
        P.dma(h[:, :, 0:n], self.hT[:, :, n0:n0 + n].re("k p t -> p k t"))
        for cc in range(n // 64):
            c = n0 // 64 + cc
            pv = psV.get()
            for k in range(KD):
                P.mm(pv[0:64, 0:256], h[:, k, cc * 64:(cc + 1) * 64], Wv[:, k, :], start=(k == 0), stop=(k == KD - 1))
            P.op('act', 'activation', out=v1[:, c, :, 0:64], in_=pv[0:64, 0:256].re("p (h d) -> p h d", h=4), func=AF.Copy)
    for c in range(NCH):
        p_ = pk.get()
        for hh in range(4):
            P.op('pe', 'transpose', out=p_[:, hh * 64:(hh + 1) * 64], in_=qk8[4 + hh][:, c * 64:(c + 1) * 64], identity=self.identb[0:64, 0:64])
        P.op('act', 'activation', out=ktok[:, c, :], in_=p_.a, func=AF.Copy)
    es3a.close()
    P.barrier()
    PS1 = [P.ps([64, 4, 64], F32, es=es3) for _ in range(2)]
    PS2 = [P.ps([64, 4, 65], F32, es=es3) for _ in range(2)]
    PS3 = [P.ps([64, 4, 65], F32, es=es3) for _ in range(2)]
    Cst = [P.sb([64, 4, 65], F32, "Cst", es=es3) for _ in range(2)]
    CnS = [P.sb([64, 4, 65], BF16, "CnS", es=es3) for _ in range(2)]
    for d_ in range(2):
        P.op('pool', 'memset', ap=Cst[d_].a, constant=0.0)
        P.op('pool', 'memset', ap=CnS[d_].a, constant=0.0)
    tmp = [Rot([P.sb([64, 4, 64], F32, "stmp", es=es3) for _ in range(2)]) for _ in range(2)]
    SD = [Rot([P.sb([64, 4, 64], BF16, "SD", es=es3) for _ in range(2)]) for _ in range(2)]
    kw = [Rot([P.sb([64, 4, 64], BF16, "kw", es=es3) for _ in range(2)]) for _ in range(2)]
    dn = [Rot([P.sb([64, 8], F32, "dn", es=es3) for _ in range(2)]) for _ in range(2)]
    hb = [Rot([P.sb([64, 4, 64], F32, "hbuf", es=es3) for _ in range(3)]) for _ in range(2)]
    Hd = [self.Hf, self.Hb]
    orders = [order_f, order_b]
    prep_lst = []
    prep_es = contextlib.ExitStack()
    if self.prep_in_mlstm:
        prep_lst = self.phase_peer_prep(l, es=prep_es, defer=True) or []
    per_step = (len(prep_lst) + NCH - 1) // NCH + 1
    for i in range(NCH):
        cs_ = [orders[d_][i] for d_ in range(2)]
        tks = [slice(c * 64, (c + 1) * 64) for c in cs_]
        qh = [[qk8[hh][:, tks[d_]] for hh in range(4)] for d_ in range(2)]
        kh = [[qk8[4 + hh][:, tks[d_]] for hh in range(4)] for d_ in range(2)]
        for d_ in range(2):
            for hh in range(4):
                P.mm(PS1[d_][:, hh, :], kh[d_][hh], qh[d_][hh])
        t_ = [tmp[d_].get() for d_ in range(2)]
        for d_ in range(2):
            P.op('dve', 'tensor_tensor', out=t_[d_].a, in0=PS1[d_].a, in1=Atok[:, cs_[d_], 4 * d_:4 * d_ + 4].unsq(2).bc([64, 4, 64]), op=ALU.mult)
        sd = [SD[d_].get() for d_ in range(2)]
        for d_ in range(2):
            P.op(self.scan_eng, 'tensor_tensor', out=sd[d_].a, in0=t_[d_].a, in1=self.maskb_t[:, d_, :, :], op=ALU.mult)
        kw_ = [kw[d_].get() for d_ in range(2)]
        for d_ in range(2):
            P.op(self.scan_eng, 'tensor_tensor', out=kw_[d_].a, in0=ktok[:, cs_[d_], :].re("p (h d) -> p h d", h=4), in1=Btok[:, cs_[d_], 4 * d_:4 * d_ + 4].unsq(2).bc([64, 4, 64]), op=ALU.mult)
        for d_ in range(2):
            for hh in range(4):
                P.mm(PS2[d_][:, hh, :], qh[d_][hh], CnS[d_][:, hh, :], start=True, stop=False)
                P.mm(PS2[d_][:, hh, :], sd[d_][:, hh, :], v1[:, cs_[d_], hh, :], start=False, stop=True)
        for d_ in range(2):
            for hh in range(4):
                P.mm(PS3[d_][:, hh, :], kw_[d_][:, hh, :], v1[:, cs_[d_], hh, :])
        for d_ in range(2):
            P.op(self.scan_eng, 'tensor_tensor', out=Cst[d_].a, in0=Cst[d_].a, in1=decb[:, 4 * d_:4 * d_ + 4, cs_[d_]].unsq(2).bc([64, 4, 65]), op=ALU.mult)
        for d_ in range(2):
            P.op('dve', 'tensor_tensor', out=Cst[d_].a, in0=Cst[d_].a, in1=PS3[d_].a, op=ALU.add)
        if i + 1 < NCH:
            for d_ in range(2):
                cn = orders[d_][i + 1]
                P.op(self.scan_eng, 'tensor_tensor', out=CnS[d_].a, in0=Cst[d_].a, in1=emb[:, 4 * d_:4 * d_ + 4, cn].unsq(2).bc([64, 4, 65]), op=ALU.mult)
        dd = [dn[d_].get() for d_ in range(2)]
        for d_ in range(2):
            P.op('act', 'activation', out=dd[d_][:, 0:4], in_=PS2[d_][:, :, 64], func=AF.Abs)
        for d_ in range(2):
            P.op('dve', 'tensor_tensor', out=dd[d_][:, 0:4], in0=dd[d_][:, 0:4], in1=Ctok[:, cs_[d_], 4 * d_:4 * d_ + 4], op=ALU.max)
        for d_ in range(2):
            P.op('dve', 'reciprocal', out=dd[d_][:, 4:8], in_=dd[d_][:, 0:4])
        for d_ in range(2):
            hbuf = hb[d_].get()
            P.op('dve', 'tensor_tensor', out=hbuf.a, in0=PS2[d_][:, :, 0:64], in1=dd[d_][:, 4:8].unsq(2).bc([64, 4, 64]), op=ALU.mult)
            P.dma(Hd[d_][cs_[d_]].re("p (h d) -> p h d", h=4), hbuf.a, q='sp')
        P.run_deferred(prep_lst, per_step)
    P.run_deferred(prep_lst)
    P.barrier()
    prep_es.close()
    es3.close()
    P.barrier()
    es4 = contextlib.ExitStack()
    Wo = P.sb([128, KD, 256], BF16, "wo", es=es4)
    for k in range(KD):
        self.load_w(Wo[:, k, :], self.w_in[l, k * 128:(k + 1) * 128, 784:1040])
    ng = P.sb([64, 256], F32, "mlng", es=es4)
    P.dma(ng.a, self.ml_norm_g[l:l + 1, :].pbc(64))
    hg = Rot([P.sb([128, KD, 512], BF16, "hg", es=es4) for _ in range(2)])
    hf_ = Rot([P.sb([64, 8, 256], F32, "hf", es=es4) for _ in range(2)])
    hb_ = Rot([P.sb([64, 8, 256], F32, "hb", es=es4) for _ in range(2)])
    sq = P.sb([64, 8, 256], F32, "sq4", es=es4)
    ss = P.sb([64, 32], F32, "ss4", es=es4)
    ob = Rot([P.sb([64, 8, 256], F32, "ob", es=es4) for _ in range(2)])
    hz = Rot([P.sb([64, 8, 256], BF16, "hz", es=es4) for _ in range(2)])
    zst = Rot([P.sb([128, 2, 512], BF16, "zst", es=es4) for _ in range(2)])
    pso = Rot([P.ps([128, 512], F32, es=es4) for _ in range(2)])
    pst = Rot([P.ps([128, 8, 64], BF16, es=es4) for _ in range(2)])
    for (n0, n, seg) in cfg.groups:
        if self.skip_ctx and seg == 1:
            continue
        nc_ = n // 64
        c0 = n0 // 64
        h = hg.get()
        P.dma(h[:, :, 0:n], self.hT[:, :, n0:n0 + n].re("k p t -> p k t"))
        a = hf_.get()
        b = hb_.get()
        P.dma(a[:, 0:nc_, :], self.Hf[c0:c0 + nc_].re("c p f -> p c f"))
        P.dma(b[:, 0:nc_, :], self.Hb[c0:c0 + nc_].re("c p f -> p c f"))
        P.op('dve', 'tensor_tensor', out=a[:, 0:nc_, :], in0=a[:, 0:nc_, :], in1=b[:, 0:nc_, :], op=ALU.add)
        P.op('act', 'activation', out=sq[:, 0:nc_, :], in_=a[:, 0:nc_, :], func=AF.Square)
        P.op('dve', 'tensor_reduce', out=ss[:, 0:nc_ * 4], in_=sq[:, 0:nc_, :].re("p c (h d) -> p (c h) d", h=4), axis=AX.X, op=ALU.add)
        P.op('act', 'activation', out=ss[:, 0:nc_ * 4], in_=ss[:, 0:nc_ * 4], func=AF.Sqrt, scale=1.0 / 64, bias=self.eps[0:64, :])
        P.op('dve', 'reciprocal', out=ss[:, 0:nc_ * 4], in_=ss[:, 0:nc_ * 4])
        P.op('dve', 'tensor_tensor', out=a[:, 0:nc_, :].re("p c (h d) -> p (c h) d", h=4), in0=a[:, 0:nc_, :].re("p c (h d) -> p (c h) d", h=4),
             in1=ss[:, 0:nc_ * 4].unsq(2).bc([64, nc_ * 4, 64]), op=ALU.mult)
        P.op('dve', 'tensor_tensor', out=a[:, 0:nc_, :], in0=a[:, 0:nc_, :], in1=ng.a.unsq(1).bc([64, nc_, 256]), op=ALU.mult)
        o = ob.get()
        for cc in range(nc_):
            po = pso.get()
            for k in range(KD):
                P.mm(po[0:64, 0:256], h[:, k, cc * 64:(cc + 1) * 64], Wo[:, k, :], start=(k == 0), stop=(k == KD - 1))
            P.op('act', 'activation', out=o[:, cc, :], in_=po[0:64, 0:256], func=AF.Sigmoid)
        z = hz.get()
        P.op('dve', 'tensor_tensor', out=z[:, 0:nc_, :], in0=a[:, 0:nc_, :], in1=o[:, 0:nc_, :], op=ALU.mult)
        zs = zst.get()
        for half in range(2):
            p_ = pst.get()
            for cc in range(nc_):
                P.op('pe', 'transpose', out=p_[:, cc, :], in_=z[:, cc, half * 128:(half + 1) * 128], identity=self.identb[0:64, 0:64])
            P.op('act', 'activation', out=zs[:, half, 0:n], in_=p_[:, 0:nc_, :].re("p c t -> p (c t)"), func=AF.Copy)
        P.dma(self.Z[0, :, n0:n0 + n].re("(c p) t -> p c t", p=128), zs[:, :, 0:n], q='pool')
    es4.close()
    es.close()
    P.barrier()


MK.phase_mlstm = _ml


def _peer_prep(self, l, es=None, defer=False):
    P = self.P
    if self.prep_done.get(l):
        return None
    self.prep_done[l] = True
    own_es = es is None
    es = es or contextlib.ExitStack()
    lst = []
    if defer:
        P.deferred = lst
    ub = Rot([P.sb([128, D], BF16, "ub", es=es) for _ in range(3)])
    vb = Rot([P.sb([128, D], BF16, "vb", es=es) for _ in range(3)])
    uo = Rot([P.sb([128, D], BF16, "uo", es=es) for _ in range(3)])
    pst = Rot([P.ps([128, KD, 128], BF16, es=es) for _ in range(2 if defer else 3)])
    Uv = self.peer_u[l].re("(a b) d -> b a d", b=128)
    Vv = self.peer_v[l].re("(a b) d -> b a d", b=128)
    for e2 in range(128):
        u = ub.get()
        P.dma(u.a, Uv[e2], q='pool')
        p_ = pst.get()
        for k in range(KD):
            P.op('pe', 'transpose', out=p_[:, k, :], in_=u[:, k * 128:(k + 1) * 128], identity=self.identb)
        o = uo.get()
        if e2 % 2:
            P.op('act', 'activation', out=o.a, in_=p_.a.re("p k e -> p (k e)"), func=AF.Copy)
        else:
            P.op('dve', 'tensor_copy', out=o.a, in_=p_.a.re("p k e -> p (k e)"))
        P.dma(self.UTs[l % 2][e2], o.a, q='sp')
        v = vb.get()
        P.dma(v.a, Vv[e2], q='pool')
        P.dma(self.VBs[l % 2][e2], v.a, q='sp')
    P.deferred = None
    if own_es:
        es.close()
        P.barrier()
    return lst


MK.phase_peer_prep = _peer_prep


def _peer(self, l):
    P, cfg = self.P, self.cfg
    es = contextlib.ExitStack()
    gs, sh, gate2 = self.mod_scale_shift(l, 1, es)
    Wq = P.sb([128, KD, 2048], BF16, "wq", es=es)
    for k in range(KD):
        self.load_w(Wq[:, k, :], self.peer_w_q[l, k * 128:(k + 1) * 128, :])
    psA = Rot([P.ps([128, 512], F32, es=es) for _ in range(2)])
    Wps = Rot([P.ps([128, 512], F32, es=es) for _ in range(2)])
    acc = [P.ps([128, 2, 256], F32, es=es) for _ in range(4)]
    kl = P.sb([128, 2, 128], BF16, "kl", es=es)
    self.load_w(kl.a, self.peer_keys[l].re("p e k -> e p k"))
    keysT = P.sb([128, 2, 128], BF16, "keysT", es=es)
    for p in range(2):
        pk_ = Wps.get()
        pkb = pk_.a.re("q (a b) -> q a b", b=128)
        klf = P.sb([128, 128], F32, "klf", es=es)
        P.op('dve', 'tensor_copy', out=klf.a, in_=kl[:, p, :])
        P.op('pe', 'transpose', out=pk_[:, 0:128], in_=klf.a, identity=self.identf)
        P.op('dve', 'tensor_copy', out=keysT[:, p, :], in_=pk_[:, 0:128])
    xgs = [P.sb([128, KD, 256], F32, "xg", es=es) for _ in range(2)]
    h2s = [P.sb([128, KD, 256], BF16, "h2", es=es) for _ in range(2)]
    qT = P.sb([128, 16, 256], BF16, "qT", es=es)
    S = P.sb([128, 16, 128], F32, "S", es=es)
    S2 = P.sb([128, 16, 128], F32, "S2", es=es)
    V1 = P.sb([128, 16, 16], F32, "V1", es=es)
    I1u = P.sb([128, 16, 16], U32, "I1u", es=es)
    I1f = P.sb([128, 16, 16], F32, "I1f", es=es)
    cand = P.sb([128, 8, 256], F32, "cand", es=es)
    cand2 = S2
    SC = P.sb([128, 8, 16], F32, "SC", es=es)
    POSu = P.sb([128, 8, 16], U32, "POSu", es=es)
    PIu = P.sb([128, 8, 16], U32, "PIu", es=es)
    PJu = P.sb([128, 8, 16], U32, "PJu", es=es)
    PIf = P.sb([128, 128], F32, "PIf", es=es)
    PJf = P.sb([128, 128], F32, "PJf", es=es)
    OH = S
    E1 = P.sb([128, 128], F32, "E1", es=es)
    E2 = P.sb([128, 128], F32, "E2", es=es)
    G = P.sb([128, 128], F32, "G", es=es)
    sm = P.sb([128, 16], F32, "sm", es=es)
    E1T = P.sb([128, 256], BF16, "E1T", es=es)
    E2T = P.sb([128, 256], BF16, "E2T", es=es)
    GT = P.sb([128, 256], BF16, "GT", es=es)
    An = Rot([P.sb([128, 128], BF16, "An", es=es) for _ in range(6)])
    Bn = Rot([P.sb([128, 128], BF16, "Bn", es=es) for _ in range(6)])
    Wbuf = P.sb([128, 256, 128], BF16, "Wbuf", es=es)
    ut = Rot([P.sb([128, KD, 128], BF16, "ut", es=es) for _ in range(4)])
    vt = Rot([P.sb([128, D], BF16, "vtb", es=es) for _ in range(4)])
    Ab = Rot([P.sb([128, 256], BF16, "Ab", es=es) for _ in range(3)])
    AW = Rot([P.sb([128, 256], BF16, "AW", es=es) for _ in range(3)])
    i16 = self.iota16
    V1s = [V1] + [V1.sub() for _ in range(15)]
    I1s = [I1u] + [I1u.sub() for _ in range(15)]
    S2s = [S2] + [S2.sub() for _ in range(15)]
    cands = [cand] + [cand.sub() for _ in range(7)]
    SCs = [SC] + [SC.sub() for _ in range(7)]
    POSs = [POSu] + [POSu.sub() for _ in range(7)]
    zlhs = P.sb([128, 128], BF16, "zlhs", es=es)
    zrhs = P.sb([128, 512], BF16, "zrhs", es=es)
    P.op('pool', 'memset', ap=zlhs.a, constant=0.0)
    P.op('pool', 'memset', ap=zrhs.a, constant=0.0)
    glist = [g for g in cfg.groups256 if not (self.skip_ctx and g[2] == 1)]
    sq = qT[:, 0:8, :]
    tmps = Rot([cands[i][:, i, :] for i in range(3)])
    rsb = cands[3][:, 3, :]

    def front(gi):
        (n0, n, seg) = glist[gi]
        x = xgs[gi % 2]
        h2 = h2s[gi % 2]
        P.dma(x.a, self.xT[:, :, n0:n0 + n].re("k p t -> p k t"))
        self.norm_group(x.a, n, gs, sh, seg, h2.a, Wps, tmps, sq, rsb)
        for j in range(16):
            pq = Wps.get()
            for k in range(KD):
                P.mm(pq[:, 0:n], Wq[:, k, j * 128:(j + 1) * 128], h2[:, k, :], start=(k == 0), stop=(k == KD - 1))
            P.op('act', 'activation', out=qT[:, j, :], in_=pq[:, 0:n], func=AF.Copy)
        for sub in range(2 if 'topk' in self.peer_parts else 0):
            tsl = slice(sub * 128, (sub + 1) * 128)
            for j4 in range(4):
                pscr = Wps.get()
                for jj in range(4):
                    j = j4 * 4 + jj
                    P.mm(pscr[:, jj * 128:(jj + 1) * 128], qT[:, j, tsl], keysT[:, j % 2, :])
                P.op('act', 'activation', out=S[:, j4 * 4:j4 * 4 + 4, :], in_=pscr.a.re("p (a b) -> p a b", b=128), func=AF.Copy)
            for j in range(16):
                P.op('dve', 'max', out=V1s[j][:, j, 0:8], in_=S[:, j, :])
            for j in range(16):
                P.op('dve', 'max_index', out=I1s[j][:, j, 0:8], in_max=V1s[j][:, j, 0:8], in_values=S[:, j, :])
            for j in range(16):
                P.op('dve', 'match_replace', out=S2s[j][:, j, :], in_to_replace=V1s[j][:, j, 0:8], in_values=S[:, j, :], imm_value=NEG)
            for j in range(16):
                P.op('dve', 'max', out=V1s[j][:, j, 8:16], in_=S2s[j][:, j, :])
            for j in range(16):
                P.op('dve', 'max_index', out=I1s[j][:, j, 8:16], in_max=V1s[j][:, j, 8:16], in_values=S2s[j][:, j, :])
            P.op('dve', 'tensor_copy', out=I1f.a, in_=I1s[0].a, xr=I1s[1:])
            V1v = V1s[0].a.re("q (h p) i -> q h p i", p=2)
            P.op('dve', 'tensor_tensor', out=cands[0].a.re("q h (i j) -> q h i j", j=16), in0=V1v[:, :, 0, :].unsq(3).bc([128, 8, 16, 16]),
                 in1=V1v[:, :, 1, :].unsq(2).bc([128, 8, 16, 16]), op=ALU.add, xr=V1s[1:], xw=cands[1:])
            c2v = S2.a.re("q a b -> q (a b)").re("q (h c) -> q h c", h=8)
            ohv = S.a.re("q a b -> q (a b)").re("q (j i) -> q j i", i=16)
            def c2(h):
                return V(S2s[2 * h], c2v.ap[:, h, :])
            for h in range(8):
                P.op('dve', 'max', out=SCs[h][:, h, 0:8], in_=cands[h][:, h, :])
            for h in range(8):
                P.op('dve', 'max_index', out=POSs[h][:, h, 0:8], in_max=SCs[h][:, h, 0:8], in_values=cands[h][:, h, :])
            for h in range(8):
                P.op('dve', 'match_replace', out=c2(h), in_to_replace=SCs[h][:, h, 0:8], in_values=cands[h][:, h, :], imm_value=NEG, xw=[S2s[2 * h + 1]])
            for h in range(8):
                P.op('dve', 'max', out=SCs[h][:, h, 8:16], in_=c2(h), xr=[S2s[2 * h + 1]])
            for h in range(8):
                P.op('dve', 'max_index', out=POSs[h][:, h, 8:16], in_max=SCs[h][:, h, 8:16], in_values=c2(h), xr=[S2s[2 * h + 1]])
            P.op('dve', 'tensor_single_scalar', out=PIu.a, in_=POSs[0].a, scalar=4, op=ALU.logical_shift_right, xr=POSs[1:])
            P.op('dve', 'tensor_single_scalar', out=PJu.a, in_=POSs[0].a, scalar=15, op=ALU.bitwise_and, xr=POSs[1:])
            P.op('dve', 'tensor_copy', out=PIf.a, in_=PIu.a.re("q h k -> q (h k)"))
            P.op('dve', 'tensor_copy', out=PJf.a, in_=PJu.a.re("q h k -> q (h k)"))
            I1v = I1f.a.re("q (h p) i -> q h p i", p=2)
            for (Pf, pp, Eo) in ((PIf, 0, E1), (PJf, 1, E2)):
                P.op('dve', 'tensor_tensor', out=ohv, in0=i16.unsq(1).bc([128, 128, 16]), in1=Pf.a.unsq(2).bc([128, 128, 16]), op=ALU.is_equal)
                P.op('dve', 'tensor_tensor', out=ohv.re("q (h k) i -> q h k i", h=8), in0=ohv.re("q (h k) i -> q h k i", h=8),
                     in1=I1v[:, :, pp, :].unsq(2).bc([128, 8, 16, 16]), op=ALU.mult)
                P.op('dve', 'tensor_reduce', out=Eo.a, in_=ohv, axis=AX.X, op=ALU.add)
            Gv = G.a.re("q (h k) -> q h k", h=8)
            P.op('dve', 'tensor_tensor', out=Gv, in0=SC.a, in1=SC[:, :, 0:1].bc([128, 8, 16]), op=ALU.subtract, xr=SCs[1:])
            P.op('act', 'activation', out=G.a, in_=G.a, func=AF.Exp)
            P.op('dve', 'tensor_reduce', out=sm[:, 0:8], in_=Gv, axis=AX.X, op=ALU.add)
            P.op('dve', 'reciprocal', out=sm[:, 8:16], in_=sm[:, 0:8])
            P.op('dve', 'tensor_tensor', out=Gv, in0=Gv, in1=sm[:, 8:16].unsq(2).bc([128, 8, 16]), op=ALU.mult)
            for (src, dst) in ((E1, E1T), (E2, E2T), (G, GT)):
                ptr = Wps.get()
                P.op('pe', 'transpose', out=ptr[:, 0:128], in_=src.a, identity=self.identf)
                P.op('act', 'activation', out=dst[:, tsl], in_=ptr[:, 0:128], func=AF.Copy)
    def run_front(gi):
        lst = []
        P.deferred = lst
        front(gi)
        P.deferred = None
        return lst

    P.run_deferred(run_front(0))
    for gi, (n0, n, seg) in enumerate(glist):
        x = xgs[gi % 2]
        h2 = h2s[gi % 2]
        for t4 in range(n // 4 if 'wb' in self.peer_parts else 0):
            wp = Wps.get()
            for tt in range(4):
                t = t4 * 4 + tt
                a_ = An.get()
                b_ = Bn.get()
                P.op('dve', 'tensor_scalar', out=a_.a, in0=self.iota128b_t.a, scalar1=E1T[:, t:t + 1], scalar2=GT[:, t:t + 1], op0=ALU.is_equal, op1=ALU.mult)
                P.op('pool' if getattr(self, 'wb_pool', False) else 'dve', 'tensor_scalar', out=b_.a, in0=self.iota128b_t.a, scalar1=E2T[:, t:t + 1], scalar2=None, op0=ALU.is_equal)
                P.mm(wp[:, tt * 128:(tt + 1) * 128], a_.a, b_.a)
            P.op('act', 'activation', out=Wbuf[:, t4 * 4:t4 * 4 + 4, :], in_=wp.a.re("p (a b) -> p a b", b=128), func=AF.Copy)
        for bnk in range(4):
            P.mm(acc[bnk].a.re("p a b -> p (a b)"), zlhs.a, zrhs.a, start=True, stop=False)
        NE = 128 if 'ex' in self.peer_parts else 0

        def stage_a(e2):
            u = ut.get()
            v = vt.get()
            P.dma(u.a, self.UTs[l % 2][e2].re("p (k e) -> p k e", e=128), q='sp')
            P.dma(v.a, self.VBs[l % 2][e2], q='sp')
            pa = psA.get()
            for k in range(KD):
                P.mm(pa[:, 0:n], u[:, k, :], h2[:, k, :], start=(k == 0), stop=(k == KD - 1))
            ab = Ab.get()
            P.op('act', 'activation', out=ab.a, in_=pa[:, 0:n], func=AF.Gelu_apprx_tanh)
            aw = AW.get()
            P.op('dve', 'tensor_tensor', out=aw.a, in0=ab.a, in1=Wbuf[:, :, e2], op=ALU.mult)
            return v, aw

        nxt = run_front(gi + 1) if gi + 1 < len(glist) else []
        if not getattr(self, 'peer_pipe', True):
            pre_nxt, nxt = nxt, []
        per_chunk = (len(nxt) + 119) // 120 if NE else len(nxt)
        pend = stage_a(0) if NE else None
        for e2 in range(NE):
            v, aw = pend
            if e2 + 1 < NE:
                pend = stage_a(e2 + 1)
            P.run_deferred(nxt, per_chunk)
            for dc in range(KD):
                P.mm(acc[dc // 2][:, dc % 2, :], v[:, dc * 128:(dc + 1) * 128], aw.a, start=False, stop=(e2 == 127 and dc % 2 == 1))
        P.run_deferred(nxt)
        if not getattr(self, 'peer_pipe', True):
            P.run_deferred(pre_nxt)
        for dc in range(KD):
            P.op('dve', 'scalar_tensor_tensor', out=x[:, dc, :], in0=acc[dc // 2][:, dc % 2, :], scalar=gate2[:, dc, seg:seg + 1], in1=x[:, dc, :], op0=ALU.mult, op1=ALU.add)
        P.dma(self.xT[:, :, n0:n0 + n].re("k p t -> p k t"), x.a, q='pool')
    es.close()
    P.barrier()


MK.phase_peer = _peer


def build_program(cfg, debug=False, phases=None, **opts):
    mk = MK(cfg, debug=debug)
    mk.skip_ctx = False
    mk.prep_done = {}
    mk.prep_in_mlstm = (phases is None) and opts.get('prep_in_mlstm', False)
    mk.peer_pipe = opts.get('peer_pipe', True)
    mk.scan_eng = 'dve' if (opts.get('scan_dve', True) and not (phases is not None and 'scan_pool' in phases)) else 'pool'
    mk.merge_eng = 'dve' if (opts.get('merge_dve', True) and not (phases is not None and 'merge_pool' in phases)) else 'pool'
    mk.wb_pool = opts.get('wb_pool', False) or (phases is not None and 'wb_pool' in phases)
    mk.peer_parts = set(['topk', 'wb', 'ex']) if (phases is None or not any(p.startswith('pp_') for p in phases)) else set(p[3:] for p in phases if p.startswith('pp_'))
    on = lambda p: phases is None or p in phases
    mk.phase_init()
    for l in range(cfg.depth):
        mk.skip_ctx = False
        if on('mod'):
            mk.phase_mod(l)
        if on('norm1'):
            mk.phase_norm1(l)
        if on('mlstm'):
            mk.phase_mlstm(l)
        mk.skip_ctx = (l == cfg.depth - 1) and phases is None
        if on('gmlp'):
            mk.phase_gmlp(l)
        if on('conv'):
            mk.phase_conv(l)
        if on('fnet'):
            mk.phase_fnet(l)
        if on('merge'):
            mk.phase_merge(l)
        if on('prep'):
            mk.phase_peer_prep(l)
        if on('peer'):
            mk.phase_peer(l)
    mk.phase_final()
    mk.P.finish()
    return mk


_CACHE = {}


def make_in_maps(cfg, inp):
    dftc, dfts, cd, cst = host_consts(cfg)
    pos = grid_sincos(cfg.t_lat, D)
    L = cfg.depth
    shared = {"pos": pos, "dftc": dftc, "dfts": dfts, "cd": cd, "cst": cst}
    for k in ["w_ada", "b_ada", "norm1_g", "norm2_g", "w_in", "ml_conv_w", "ml_conv_b", "ml_gate_b", "gm_ln_g", "gm_ln_b",
              "gm_w_s", "cv_dw_w", "cv_dw_b", "cv_ln_g", "cv_ln_b", "w_branch", "w_out", "peer_w_q", "peer_keys",
              "peer_u", "peer_v", "final_norm_g"]:
        shared[k] = np.ascontiguousarray(np.asarray(inp[k], dtype=np.float32))
    shared["ml_norm_g"] = np.ascontiguousarray(np.asarray(inp["ml_norm_g"], np.float32).reshape(L, 256))
    shared["gm_b_s"] = np.ascontiguousarray(np.asarray(inp["gm_b_s"], np.float32).reshape(L, 512))
    x = np.asarray(inp["x"], np.float32)
    ctx = np.asarray(inp["ctx"], np.float32)
    c = np.asarray(inp["c"], np.float32)
    c_ctx = np.asarray(inp["c_ctx"], np.float32)
    maps = []
    for b in range(x.shape[0]):
        m = dict(shared)
        m["xin"] = np.ascontiguousarray(np.concatenate([ctx[b], x[b]], 0))
        m["cvec"] = np.ascontiguousarray(np.stack([c[b], c_ctx], 0))
        maps.append(m)
    return maps


def kernel(**inputs):
    x = np.asarray(inputs["x"])
    B, T, _ = x.shape
    depth = np.asarray(inputs["w_ada"]).shape[0]
    cfg = Cfg(depth=depth, t_lat=T, t_ctx=np.asarray(inputs["ctx"]).shape[1])
    key = (depth, T, cfg.t_ctx)
    if key not in _CACHE:
        _CACHE[key] = build_program(cfg)
    mk = _CACHE[key]
    maps = make_in_maps(cfg, inputs)
    res = run_bass_kernel_spmd(mk.nc, maps, core_ids=list(range(B)))
    return np.stack([np.asarray(r["out"], dtype=np.float32) for r in res.results], 0)
```

```python
import contextlib
import numpy as np
import ml_dtypes
import concourse.bass as bass
import concourse.mybir as mybir
from concourse.bass_utils import run_bass_kernel_spmd

F32 = mybir.dt.float32
BF16 = mybir.dt.bfloat16
I32 = mybir.dt.int32
U32 = mybir.dt.uint32
AF = mybir.ActivationFunctionType
ALU = mybir.AluOpType
AX = mybir.AxisListType

COMPUTE = ('pe', 'act', 'dve', 'pool')
NRING = 12
WRITE_KW = ('out', 'accum_out', 'ap')
EPS = 1e-6
NEG = -1.0e30


class V:
    __slots__ = ('buf', 'ap')

    def __init__(self, buf, ap):
        self.buf = buf
        self.ap = ap

    def __getitem__(self, k):
        return V(self.buf, self.ap[k])

    def re(self, pat, **kw):
        return V(self.buf, self.ap.rearrange(pat, **kw))

    def bc(self, shape):
        return V(self.buf, self.ap.to_broadcast(list(shape)))

    def unsq(self, ax):
        return V(self.buf, self.ap.unsqueeze(ax))

    def pbc(self, n):
        return V(self.buf, self.ap.partition_broadcast(n))


class Buf:
    __slots__ = ('t', 'lw', 'rd', 'name')

    def __init__(self, t, name=''):
        self.t = t
        self.lw = None
        self.rd = {}
        self.name = name

    def __getitem__(self, k):
        return V(self, self.t[k])

    @property
    def a(self):
        return V(self, self.t[:])

    def sub(self):
        return Buf(self.t, self.name + '_s')


class Prog:
    def __init__(self, nc):
        self.nc = nc
        self.ops = {e: [] for e in ('pe', 'act', 'dve', 'pool', 'sp')}
        self.count = {e: 0 for e in COMPUTE}
        self.known = {e: {} for e in self.ops}
        self.ndma = {'sp': 0, 'pool': 0, 'act': 0}
        self.es = contextlib.ExitStack()
        self.sems = {}
        for e in COMPUTE:
            self.sems[('c', e)] = self.es.enter_context(nc.semaphore('s_' + e))
        for q in ('sp', 'pool', 'act'):
            for i in range(NRING):
                self.sems[('d', q, i)] = self.es.enter_context(nc.semaphore('d_%s_%d' % (q, i)))
        self.nbuf = 0
        self.ninst = 0
        self.deferred = None

    def sb(self, shape, dt, name=None, es=None):
        self.nbuf += 1
        name = '%s_%d' % (name or 'sb', self.nbuf)
        t = (es or self.es).enter_context(self.nc.sbuf_tensor(name, list(shape), dt))
        return Buf(t, name)

    def ps(self, shape, dt, name=None, es=None):
        self.nbuf += 1
        name = '%s_%d' % (name or 'ps', self.nbuf)
        t = (es or self.es).enter_context(self.nc.psum_tensor(name, list(shape), dt))
        return Buf(t, name)

    def dram(self, name, shape, dt, kind='Internal'):
        t = self.nc.dram_tensor(name, list(shape), dt, kind=kind)
        return Buf(t, name)

    def _need(self, eng, tok, waits):
        if tok is None:
            return
        k, v = tok
        if self.known[eng].get(k, 0) >= v:
            return
        if waits.get(k, 0) < v:
            waits[k] = v

    def emit(self, eng, fn, reads=(), writes=(), dma=False):
        waits = {}
        own = ('c', eng) if (eng in COMPUTE and not dma) else None
        for b in reads:
            if b.lw is not None:
                if own is not None and b.lw[0] == own and eng == 'pe':
                    continue
                self._need(eng, b.lw, waits)
        for b in writes:
            if b.lw is not None:
                if not (own is not None and b.lw[0] == own and eng == 'pe'):
                    self._need(eng, b.lw, waits)
            for k, v in b.rd.items():
                if own is not None and k == own and eng == 'pe':
                    continue
                self._need(eng, (k, v), waits)
        if dma:
            i = self.ndma[eng]
            self.ndma[eng] = i + 1
            slot, gen = i % NRING, i // NRING
            key = ('d', eng, slot)
            if gen > 0:
                self._need(eng, (key, 16 * gen), waits)
            tok = (key, 16 * (gen + 1))
            inc = (key, 16)
        else:
            self.count[eng] += 1
            tok = (own, self.count[eng])
            inc = (own, 1)
        for k, v in waits.items():
            self.known[eng][k] = v
        self.ops[eng].append((fn, list(waits.items()), inc))
        self.ninst += 1
        for b in writes:
            b.lw = tok
            b.rd = {}
        for b in reads:
            if b in writes:
                continue
            if b.rd.get(tok[0], 0) < tok[1]:
                b.rd[tok[0]] = tok[1]
        return tok

    def run_deferred(self, lst, k=None):
        k = len(lst) if k is None else min(k, len(lst))
        for _ in range(k):
            eng, name, xr, xw, kw = lst.pop(0)
            self.op(eng, name, xr=xr, xw=xw, **kw)

    def op(self, eng, name, *, xr=(), xw=(), **kw):
        if self.deferred is not None:
            self.deferred.append((eng, name, xr, xw, kw))
            return None
        reads, writes, real = list(xr), list(xw), {}
        for k, v in kw.items():
            if isinstance(v, V):
                (writes if k in WRITE_KW else reads).append(v.buf)
                real[k] = v.ap
            else:
                real[k] = v
        isdma = name == 'dma_start'

        def fn(e, name=name, real=real):
            return getattr(e, name)(**real)
        return self.emit(eng, fn, reads, writes, dma=isdma)

    def dma(self, out, in_, q='sp', **kw):
        return self.op(q, 'dma_start', out=out, in_=in_, **kw)

    def mm(self, out, lhsT, rhs, start=True, stop=True):
        return self.op('pe', 'matmul', out=out, lhsT=lhsT, rhs=rhs, start=start, stop=stop)

    def barrier(self):
        toks = []
        for e in COMPUTE:
            if self.count[e] > 0:
                toks.append((('c', e), self.count[e]))
        for q, n in self.ndma.items():
            for i in range(max(0, n - NRING), n):
                toks.append((('d', q, i % NRING), 16 * (i // NRING + 1)))
        for e in self.ops:
            waits = {}
            for tok in toks:
                if tok[0] == ('c', e):
                    continue
                self._need(e, tok, waits)
            for k, v in waits.items():
                self.known[e][k] = v
            if waits:
                self.ops[e].append((None, list(waits.items()), None))

    def finish(self):
        self.barrier()
        nc = self.nc
        sems = self.sems
        ops = self.ops

        def replay(name, e):
            for fn, waits, inc in ops[name]:
                for k, v in waits:
                    e.wait_ge(sems[k], v)
                if fn is None:
                    continue
                ins = fn(e)
                if inc is not None:
                    ins.then_inc(sems[inc[0]], inc[1])

        with nc.Block() as block:
            @block.sync
            def _(e):
                replay('sp', e)

            @block.tensor
            def _(e):
                replay('pe', e)

            @block.scalar
            def _(e):
                replay('act', e)

            @block.vector
            def _(e):
                replay('dve', e)

            @block.gpsimd
            def _(e):
                replay('pool', e)
        self.es.close()


class Rot:
    def __init__(self, bufs):
        self.bufs = bufs
        self.i = 0

    def get(self):
        b = self.bufs[self.i % len(self.bufs)]
        self.i += 1
        return b


D = 1024
KD = 8
IN_COLS = 6416
GM_OFF = 1040
CV_OFF = 1552
FT_OFF = 2064
GATE_OFF = 2320
NEXP = 16384


class Cfg:
    def __init__(self, depth=4, t_lat=4096, t_ctx=256):
        self.depth = depth
        self.t_lat = t_lat
        self.t_ctx = t_ctx
        self.nt = t_lat + t_ctx
        self.groups = [(0, t_ctx, 1)] + [(t_ctx + i * 512, 512, 0) for i in range(t_lat // 512)]
        self.groups256 = [(i * 256, 256, 1 if i * 256 < t_ctx else 0) for i in range(self.nt // 256)]
        self.segs = [(0, t_ctx, 1), (t_ctx, t_lat, 0)]
        self.nch = self.nt // 64


def host_consts(cfg):
    T = cfg.t_lat
    k = np.arange(T, dtype=np.float64)
    ang = 2.0 * np.pi * ((k[:, None] * k[None, :]) % T) / T
    dftc = (np.cos(ang) / np.sqrt(T)).astype(ml_dtypes.bfloat16)
    dfts = (-np.sin(ang) / np.sqrt(T)).astype(ml_dtypes.bfloat16)
    c = np.arange(64, dtype=np.float64)
    a64 = 2.0 * np.pi * ((c[:, None] * c[None, :]) % 64) / 64
    cd = np.zeros((256, 512), np.float64)
    for g in range(4):
        cd[g * 64:(g + 1) * 64, g * 64:(g + 1) * 64] = np.cos(a64) / 8.0
        cd[g * 64:(g + 1) * 64, 256 + g * 64:256 + (g + 1) * 64] = np.sin(a64) / 8.0
    cd = cd.astype(ml_dtypes.bfloat16)
    cst = np.zeros((128, 1024), np.float32)
    cst[:, 0:128] = np.eye(128, dtype=np.float32)
    cst[:, 128:256] = np.arange(128, dtype=np.float32)[None, :]
    s = np.arange(64)
    cst[0:64, 256:320] = (s[:, None] <= s[None, :]).astype(np.float32)
    cst[0:64, 320:384] = (s[:, None] >= s[None, :]).astype(np.float32)
    for g in range(8):
        row = g if g < 4 else 32 + (g - 4)
        cst[row, 384 + g * 64:384 + (g + 1) * 64] = 1.0
    cst[:, 896:912] = np.arange(16, dtype=np.float32)[None, :]
    cst[:, 912] = EPS
    cst[:, 913] = 1.0
    return dftc, dfts, cd, cst


def grid_sincos(n_tok, d):
    rows = n_tok // 64
    n_freq = d // 4
    freq = (1.0 / (10000.0 ** (np.arange(n_freq, dtype=np.float32) / np.float32(n_freq)))).astype(np.float32)
    r = np.repeat(np.arange(rows, dtype=np.float32), 64)
    cc = np.tile(np.arange(64, dtype=np.float32), rows)
    ar = r[:, None] * freq[None, :]
    ac = cc[:, None] * freq[None, :]
    return np.concatenate([np.sin(ar), np.cos(ar), np.sin(ac), np.cos(ac)], axis=-1).astype(np.float32)


class MK:
    def __init__(self, cfg, debug=False):
        self.cfg = cfg
        self.nc = bass.Bass("TRN2", target_bir_lowering=False)
        self.P = Prog(self.nc)
        self.debug = debug
        P = self.P
        L = cfg.depth
        NT = cfg.nt
        din = lambda name, shape, dt=F32: P.dram(name, shape, dt, kind="ExternalInput")
        self.xin = din("xin", [NT, D])
        self.pos = din("pos", [cfg.t_lat, D])
        self.cvec = din("cvec", [2, D])
        self.w_ada = din("w_ada", [L, D, 6 * D])
        self.b_ada = din("b_ada", [L, 6 * D])
        self.norm1_g = din("norm1_g", [L, D])
        self.norm2_g = din("norm2_g", [L, D])
        self.w_in = din("w_in", [L, D, IN_COLS])
        self.ml_conv_w = din("ml_conv_w", [L, 3, 512])
        self.ml_conv_b = din("ml_conv_b", [L, 512])
        self.ml_gate_b = din("ml_gate_b", [L, 16])
        self.ml_norm_g = din("ml_norm_g", [L, 256])
        self.gm_ln_g = din("gm_ln_g", [L, 256])
        self.gm_ln_b = din("gm_ln_b", [L, 256])
        self.gm_w_s = din("gm_w_s", [L, 4, 128, 128])
        self.gm_b_s = din("gm_b_s", [L, 512])
        self.cv_dw_w = din("cv_dw_w", [L, 31, 256])
        self.cv_dw_b = din("cv_dw_b", [L, 256])
        self.cv_ln_g = din("cv_ln_g", [L, 256])
        self.cv_ln_b = din("cv_ln_b", [L, 256])
        self.w_branch = din("w_branch", [L, 4, 256, D])
        self.w_out = din("w_out", [L, D, D])
        self.peer_w_q = din("peer_w_q", [L, D, 2048])
        self.peer_keys = din("peer_keys", [L, 2, 128, 128])
        self.peer_u = din("peer_u", [L, NEXP, D])
        self.peer_v = din("peer_v", [L, NEXP, D])
        self.final_norm_g = din("final_norm_g", [D])
        self.dftc = din("dftc", [cfg.t_lat, cfg.t_lat], BF16)
        self.dfts = din("dfts", [cfg.t_lat, cfg.t_lat], BF16)
        self.cd = din("cd", [256, 512], BF16)
        self.cst_d = din("cst", [128, 1024])
        self.out = P.dram("out", [cfg.t_lat, D], F32, kind="ExternalOutput")
        sk = "ExternalOutput" if debug else "Internal"
        self.xT = P.dram("xT", [KD, 128, NT], F32, kind=sk)
        self.hT = P.dram("hT", [KD, 128, NT], BF16, kind=sk)
        self.Z = P.dram("Z", [4, 256, NT], BF16, kind=sk)
        self.Hf = P.dram("Hf", [cfg.nch, 64, 256], F32, kind=sk)
        self.Hb = P.dram("Hb", [cfg.nch, 64, 256], F32, kind=sk)
        self.UTs = [P.dram("UT%d" % i, [128, 128, KD * 128], BF16) for i in range(2)]
        self.VBs = [P.dram("VB%d" % i, [128, 128, D], BF16) for i in range(2)]
        self.cst = P.sb([128, 1024], F32, "cst")
        P.dma(self.cst.a, self.cst_d.a)
        c = self.cst
        self.identf = c[:, 0:128]
        self.iota128 = c[:, 128:256]
        self.iota16 = c[:, 896:912]
        self.eps = c[:, 912:913]
        self.identb_t = P.sb([128, 128], BF16, "identb")
        P.op('dve', 'tensor_copy', out=self.identb_t.a, in_=self.identf)
        self.identb = self.identb_t.a
        self.onesb_t = P.sb([128, 128], BF16, "onesb")
        P.op('pool', 'memset', ap=self.onesb_t.a, constant=1.0)
        self.onesb = self.onesb_t.a
        self.iota128b_t = P.sb([128, 128], BF16, "iota128b")
        P.op('dve', 'tensor_copy', out=self.iota128b_t.a, in_=self.iota128)
        self.onesf_t = P.sb([128, 128], F32, "onesf")
        P.op('pool', 'memset', ap=self.onesf_t.a, constant=1.0)
        self.onesf = self.onesf_t.a
        self.maskb_t = P.sb([64, 2, 4, 64], BF16, "maskb")
        for d_ in range(2):
            for h in range(4):
                P.op('dve', 'tensor_copy', out=self.maskb_t[:, d_, h, :], in_=c[0:64, 256 + 64 * d_:320 + 64 * d_])
        self.modT = P.sb([128, 48, 2], F32, "modT")
        cT = P.sb([2, D], F32, "cT")
        P.dma(cT.a, self.cvec.a)
        self.sT = P.sb([128, 2, KD], BF16, "sT")
        es = contextlib.ExitStack()
        ps = P.ps([128, 512], F32, es=es)
        for k in range(KD):
            P.op('pe', 'transpose', out=ps[:, 2 * k:2 * k + 2], in_=cT[:, k * 128:(k + 1) * 128], identity=self.identf[0:2, 0:2])
        P.op('act', 'activation', out=self.sT.a, in_=ps[:, 0:16].re("p (k s) -> p s k", s=2), func=AF.Silu)
        es.close()
        P.barrier()

    def load_rows_T(self, rows, es):
        P = self.P
        R = sum(v.ap.shape[0] for v in rows)
        w = rows[0].ap.shape[1]
        assert R <= 128
        rt = P.sb([128, 128], F32, "rows", es=es)
        r0 = 0
        for v in rows:
            r = v.ap.shape[0]
            P.dma(rt[r0:r0 + r, 0:w], v)
            r0 += r
        es_ = contextlib.ExitStack()
        ps = P.ps([128, 512], F32, es=es_)
        P.op('pe', 'transpose', out=ps[0:w, 0:R], in_=rt[0:R, 0:w], identity=self.identf[0:R, 0:R])
        ct = P.sb([128, R], F32, "colsT", es=es)
        P.op('dve', 'tensor_copy', out=ct[0:w, :], in_=ps[0:w, 0:R])
        P.barrier()
        es_.close()
        return ct

    def load_w(self, dst, src, q='pool'):
        self.P.dma(dst, src, q=q)

    def phase_init(self):
        P, cfg = self.P, self.cfg
        es = contextlib.ExitStack()
        xt = Rot([P.sb([128, D], F32, "xt", es=es) for _ in range(2)])
        pt = Rot([P.sb([128, D], F32, "pt", es=es) for _ in range(2)])
        pss = Rot([P.ps([128, 512], F32, es=es) for _ in range(4)])
        st = Rot([P.sb([128, KD, 128], F32, "xst", es=es) for _ in range(2)])
        for ti in range(cfg.nt // 128):
            x = xt.get()
            P.dma(x.a, self.xin[ti * 128:(ti + 1) * 128, :])
            if ti * 128 >= cfg.t_ctx:
                p = pt.get()
                r0 = ti * 128 - cfg.t_ctx
                P.dma(p.a, self.pos[r0:r0 + 128, :])
                P.op('dve', 'tensor_tensor', out=x.a, in0=x.a, in1=p.a, op=ALU.add)
            s = st.get()
            for half in range(2):
                ps = pss.get()
                for j in range(4):
                    k = half * 4 + j
                    P.op('pe', 'transpose', out=ps[:, j * 128:(j + 1) * 128], in_=x[:, k * 128:(k + 1) * 128], identity=self.identf)
                P.op('act', 'activation', out=s[:, half * 4:half * 4 + 4, :], in_=ps.a.re("p (j t) -> p j t", j=4), func=AF.Copy)
            P.dma(self.xT[:, :, ti * 128:(ti + 1) * 128].re("k p t -> p k t"), s.a)
        es.close()
        P.barrier()

    def phase_mod(self, l):
        P = self.P
        es = contextlib.ExitStack()
        wt = Rot([P.sb([128, KD, 1536], BF16, "wada", es=es) for _ in range(2)])
        ps = P.ps([128, 48, 2], F32, es=es)
        bT = self.load_rows_T([self.b_ada[l].re("(j p) -> j p", p=128)], es)
        for cg in range(4):
            w = wt.get()
            for k in range(KD):
                self.load_w(w[:, k, :], self.w_ada[l, k * 128:(k + 1) * 128, cg * 1536:(cg + 1) * 1536])
            for jj in range(12):
                j = cg * 12 + jj
                for k in range(KD):
                    P.mm(ps[:, j, :], w[:, k, jj * 128:(jj + 1) * 128], self.sT[:, :, k], start=(k == 0), stop=(k == KD - 1))
        P.op('dve', 'tensor_tensor', out=self.modT.a, in0=ps.a, in1=bT.a.unsq(2).bc([128, 48, 2]), op=ALU.add)
        es.close()
        P.barrier()

    def mod_scale_shift(self, l, which, es):
        P = self.P
        g = self.norm1_g if which == 0 else self.norm2_g
        gT = self.load_rows_T([g[l].re("(j p) -> j p", p=128)], es)
        base = 0 if which == 0 else 24
        gs = P.sb([128, KD, 2], F32, "gs", es=es)
        P.op('dve', 'tensor_scalar', out=gs.a, in0=self.modT[:, base + 8:base + 16, :], scalar1=1.0, scalar2=None, op0=ALU.add)
        P.op('dve', 'tensor_tensor', out=gs.a, in0=gs.a, in1=gT.a.unsq(2).bc([128, KD, 2]), op=ALU.mult)
        return gs, self.modT[:, base:base + 8, :], self.modT[:, base + 16:base + 24, :]

    def norm_group(self, xg, n, gs, sh, seg, hout, pss, tmps, sq, rsb):
        P = self.P
        P.op('act', 'activation', out=sq[:, :, 0:n], in_=xg, func=AF.Square)
        ps = pss.get()
        for k in range(KD):
            P.mm(ps[:, 0:n], self.onesb, sq[:, k, 0:n], start=(k == 0), stop=(k == KD - 1))
        rs = rsb
        P.op('act', 'activation', out=rs[:, 0:n], in_=ps[:, 0:n], func=AF.Sqrt, scale=1.0 / D, bias=self.eps)
        P.op('dve', 'reciprocal', out=rs[:, 0:n], in_=rs[:, 0:n])
        for k in range(KD):
            t = tmps.get()
            P.op('dve', 'scalar_tensor_tensor', out=t[:, 0:n], in0=xg[:, k, :], scalar=gs[:, k, seg:seg + 1], in1=rs[:, 0:n], op0=ALU.mult, op1=ALU.mult)
            if sh is None:
                P.op('act', 'activation', out=hout[:, k, :], in_=t[:, 0:n], func=AF.Copy)
            else:
                P.op('act', 'activation', out=hout[:, k, :], in_=t[:, 0:n], func=AF.Identity, bias=sh[:, k, seg:seg + 1])

    def phase_norm1(self, l):
        P, cfg = self.P, self.cfg
        es = contextlib.ExitStack()
        gs, sh, _ = self.mod_scale_shift(l, 0, es)
        xg = Rot([P.sb([128, KD, 512], F32, "xg", es=es) for _ in range(2)])
        hg = Rot([P.sb([128, KD, 512], BF16, "hg", es=es) for _ in range(2)])
        sq = P.sb([128, KD, 512], BF16, "sq", es=es)
        pss = Rot([P.ps([128, 512], F32, es=es) for _ in range(2)])
        tmps = Rot([P.sb([128, 512], F32, "nt", es=es) for _ in range(4)])
        rsb = P.sb([128, 512], F32, "rsb", es=es)
        for (n0, n, seg) in cfg.groups:
            x = xg.get()
            h = hg.get()
            P.dma(x[:, :, 0:n], self.xT[:, :, n0:n0 + n].re("k p t -> p k t"))
            self.norm_group(x[:, :, 0:n], n, gs, sh, seg, h[:, :, 0:n], pss, tmps, sq, rsb)
            P.dma(self.hT[:, :, n0:n0 + n].re("k p t -> p k t"), h[:, :, 0:n], q='pool')
        es.close()
        P.barrier()


def _gm(self, l):
    P, cfg = self.P, self.cfg
    es = contextlib.ExitStack()
    W = P.sb([128, KD, 512], BF16, "wgm", es=es)
    for k in range(KD):
        self.load_w(W[:, k, :], self.w_in[l, k * 128:(k + 1) * 128, GM_OFF:GM_OFF + 512])
    lng = P.sb([128, 256], F32, "lng", es=es)
    lnb = P.sb([128, 256], F32, "lnb", es=es)
    P.dma(lng.a, self.gm_ln_g[l:l + 1, :].pbc(128))
    P.dma(lnb.a, self.gm_ln_b[l:l + 1, :].pbc(128))
    bsr = P.sb([1, 512], F32, "bsr", es=es)
    P.dma(bsr.a, self.gm_b_s[l:l + 1, :])
    wsT = P.sb([128, 4, 128], BF16, "wsT", es=es)
    wsl = P.sb([128, 4, 128], BF16, "wsl", es=es)
    self.load_w(wsl.a, self.gm_w_s[l].re("g t s -> t g s"))
    pst = P.ps([128, 4, 128], BF16, es=es)
    for g in range(4):
        P.op('pe', 'transpose', out=pst[:, g, :], in_=wsl[:, g, :], identity=self.identb)
    P.op('dve', 'tensor_copy', out=wsT.a, in_=pst.a)
    hg = Rot([P.sb([128, KD, 512], BF16, "hg", es=es) for _ in range(2)])
    u64 = Rot([P.sb([64, 4, 512], BF16, "u64", es=es) for _ in range(2)])
    zst = Rot([P.sb([64, 4, 512], BF16, "zst", es=es) for _ in range(2)])
    psu = Rot([P.ps([128, 512], F32, es=es) for _ in range(2)])
    psv = Rot([P.ps([128, 512], F32, es=es) for _ in range(2)])
    pss = Rot([P.ps([64, 4, 128], F32, es=es) for _ in range(2)])
    vt = Rot([P.sb([128, 256], F32, "vt", es=es) for _ in range(2)])
    vn = Rot([P.sb([128, 256], BF16, "vn", es=es) for _ in range(2)])
    st6 = Rot([P.sb([128, 8], F32, "st6", es=es) for _ in range(2)])
    for (n0, n, seg) in cfg.groups:
        if self.skip_ctx and seg == 1:
            continue
        h = hg.get()
        P.dma(h[:, :, 0:n], self.hT[:, :, n0:n0 + n].re("k p t -> p k t"))
        u = u64.get()
        for g in range(4):
            ps = psu.get()
            for k in range(KD):
                P.mm(ps[0:64, 0:n], W[:, k, g * 64:(g + 1) * 64], h[:, k, 0:n], start=(k == 0), stop=(k == KD - 1))
            P.op('act', 'activation', out=u[:, g, 0:n], in_=ps[0:64, 0:n], func=AF.Gelu_apprx_tanh)
        z = zst.get()
        for sub in range(n // 128):
            ps = psv.get()
            for k in range(KD):
                P.mm(ps[:, 0:256], h[:, k, sub * 128:(sub + 1) * 128], W[:, k, 256:512], start=(k == 0), stop=(k == KD - 1))
            v = vt.get()
            P.op('act', 'activation', out=v.a, in_=ps[:, 0:256], func=AF.Gelu_apprx_tanh)
            s6 = st6.get()
            P.op('dve', 'bn_stats', out=s6[:, 0:6], in_=v.a)
            P.op('dve', 'bn_aggr', out=s6[:, 6:8], in_=s6[:, 0:6])
            P.op('act', 'activation', out=s6[:, 7:8], in_=s6[:, 7:8], func=AF.Sqrt, bias=self.eps)
            P.op('dve', 'reciprocal', out=s6[:, 7:8], in_=s6[:, 7:8])
            P.op('dve', 'tensor_scalar', out=v.a, in0=v.a, scalar1=s6[:, 6:7], scalar2=s6[:, 7:8], op0=ALU.subtract, op1=ALU.mult)
            P.op('dve', 'tensor_tensor', out=v.a, in0=v.a, in1=lng.a, op=ALU.mult)
            vb = vn.get()
            P.op('dve', 'tensor_tensor', out=vb.a, in0=v.a, in1=lnb.a, op=ALU.add)
            pg = pss.get()
            for g in range(4):
                P.mm(pg[:, g, :], vb[:, g * 64:(g + 1) * 64], wsT[:, g, :], start=True, stop=False)
                P.mm(pg[:, g, :], self.onesf[0:1, 0:64], bsr[0:1, g * 128:(g + 1) * 128], start=False, stop=True)
            P.op('dve', 'tensor_tensor', out=z[:, :, sub * 128:(sub + 1) * 128], in0=pg.a, in1=u[:, :, sub * 128:(sub + 1) * 128], op=ALU.mult)
        P.dma(self.Z[1, :, n0:n0 + n].re("(g p) t -> p g t", p=64), z[:, :, 0:n], q='pool')
    es.close()
    P.barrier()


MK.phase_gmlp = _gm


def _cv(self, l):
    P, cfg = self.P, self.cfg
    es = contextlib.ExitStack()
    PAD = 15
    W = P.sb([128, KD, 512], BF16, "wcv", es=es)
    for k in range(KD):
        self.load_w(W[:, k, :], self.w_in[l, k * 128:(k + 1) * 128, CV_OFF:CV_OFF + 512])
    cols = self.load_rows_T([self.cv_dw_w[l].re("j (c p) -> (j c) p", p=128), self.cv_dw_b[l].re("(c p) -> c p", p=128),
                             self.cv_ln_g[l].re("(c p) -> c p", p=128), self.cv_ln_b[l].re("(c p) -> c p", p=128)], es)
    diag = P.sb([128, 62, 128], BF16, "diag", es=es)
    for jc in range(62):
        P.op('dve', 'tensor_scalar', out=diag[:, jc, :], in0=self.identf, scalar1=cols[:, jc:jc + 1], scalar2=None, op0=ALU.mult)
    hg = Rot([P.sb([128, KD, 512], BF16, "hg", es=es) for _ in range(2)])
    psa = Rot([P.ps([128, 512], F32, es=es) for _ in range(2)])
    psb = Rot([P.ps([128, 512], F32, es=es) for _ in range(2)])
    psc = Rot([P.ps([128, 512], F32, es=es) for _ in range(2)])
    sg = Rot([P.sb([128, 512], F32, "sg", es=es) for _ in range(2)])
    for (s0, sn, seg) in cfg.segs:
        if self.skip_ctx and seg == 1:
            continue
        es2 = contextlib.ExitStack()
        zp = P.sb([128, 2, sn + 2 * PAD], BF16, "zp", es=es2)
        P.op('pool', 'memset', ap=zp.a, constant=0.0)
        y = P.sb([128, 2, 512], F32, "ycv", es=es2)
        y2 = P.sb([128, 2, 512], F32, "ycv2", es=es2)
        mean = P.sb([128, 512], F32, "mean", es=es2)
        rstd = P.sb([128, 512], F32, "rstd", es=es2)
        zo = Rot([P.sb([128, 2, 512], BF16, "zo", es=es2) for _ in range(2)])
        grp = [(a, n) for (a, n, sg_) in cfg.groups if sg_ == seg]
        for (n0, n) in grp:
            h = hg.get()
            P.dma(h[:, :, 0:n], self.hT[:, :, n0:n0 + n].re("k p t -> p k t"))
            for c in range(2):
                pa = psa.get()
                pb = psb.get()
                for k in range(KD):
                    P.mm(pa[:, 0:n], W[:, k, c * 128:(c + 1) * 128], h[:, k, 0:n], start=(k == 0), stop=(k == KD - 1))
                for k in range(KD):
                    P.mm(pb[:, 0:n], W[:, k, 256 + c * 128:256 + (c + 1) * 128], h[:, k, 0:n], start=(k == 0), stop=(k == KD - 1))
                s = sg.get()
                P.op('act', 'activation', out=s[:, 0:n], in_=pb[:, 0:n], func=AF.Sigmoid)
                o0 = PAD + n0 - s0
                P.op('dve', 'tensor_tensor', out=zp[:, c, o0:o0 + n], in0=pa[:, 0:n], in1=s[:, 0:n], op=ALU.mult)
        for (n0, n) in grp:
            o0 = n0 - s0
            for c in range(2):
                pc = psc.get()
                for j in range(31):
                    P.mm(pc[:, 0:n], diag[:, j * 2 + c, :], zp[:, c, o0 + j:o0 + j + n], start=(j == 0), stop=(j == 30))
                P.op('act', 'activation', out=y[:, c, 0:n], in_=pc[:, 0:n], func=AF.Identity, bias=cols[:, 62 + c:63 + c])
                P.op('act', 'activation', out=y2[:, c, 0:n], in_=y[:, c, 0:n], func=AF.Square)
            p1 = psa.get()
            p2 = psb.get()
            for c in range(2):
                P.mm(p1[:, 0:n], self.onesf, y[:, c, 0:n], start=(c == 0), stop=(c == 1))
            for c in range(2):
                P.mm(p2[:, 0:n], self.onesf, y2[:, c, 0:n], start=(c == 0), stop=(c == 1))
            P.op('act', 'activation', out=mean[:, 0:n], in_=p1[:, 0:n], func=AF.Identity, scale=1.0 / 256)
            P.op('dve', 'tensor_tensor', out=rstd[:, 0:n], in0=mean[:, 0:n], in1=mean[:, 0:n], op=ALU.mult)
            P.op('dve', 'scalar_tensor_tensor', out=rstd[:, 0:n], in0=p2[:, 0:n], scalar=1.0 / 256, in1=rstd[:, 0:n], op0=ALU.mult, op1=ALU.subtract)
            P.op('act', 'activation', out=rstd[:, 0:n], in_=rstd[:, 0:n], func=AF.Sqrt, bias=self.eps)
            P.op('dve', 'reciprocal', out=rstd[:, 0:n], in_=rstd[:, 0:n])
            z = zo.get()
            for c in range(2):
                P.op('dve', 'tensor_tensor', out=y[:, c, 0:n], in0=y[:, c, 0:n], in1=mean[:, 0:n], op=ALU.subtract)
                P.op('dve', 'tensor_tensor', out=y[:, c, 0:n], in0=y[:, c, 0:n], in1=rstd[:, 0:n], op=ALU.mult)
                P.op('act', 'activation', out=z[:, c, 0:n], in_=y[:, c, 0:n], func=AF.Silu, scale=cols[:, 64 + c:65 + c], bias=cols[:, 66 + c:67 + c])
            P.dma(self.Z[2, :, n0:n0 + n].re("(c p) t -> p c t", p=128), z[:, :, 0:n], q='pool')
        es2.close()
        P.barrier()
    es.close()
    P.barrier()


MK.phase_conv = _cv


def _ft(self, l):
    P, cfg = self.P, self.cfg
    es = contextlib.ExitStack()
    W = P.sb([128, KD, 256], BF16, "wft", es=es)
    for k in range(KD):
        self.load_w(W[:, k, :], self.w_in[l, k * 128:(k + 1) * 128, FT_OFF:FT_OFF + 256])
    CD = P.sb([128, 2, 512], BF16, "cdt", es=es)
    P.dma(CD.a, self.cd.a.re("(c p) n -> p c n", p=128))
    hg = Rot([P.sb([128, KD, 512], BF16, "hg", es=es) for _ in range(2)])
    zf = Rot([P.sb([128, 2, 512], BF16, "zf", es=es) for _ in range(2)])
    psa = Rot([P.ps([128, 512], F32, es=es) for _ in range(2)])
    psd = Rot([P.ps([128, 512], F32, es=es) for _ in range(4)])
    for (s0, sn, seg) in cfg.segs:
        if self.skip_ctx and seg == 1:
            continue
        es2 = contextlib.ExitStack()
        ntile = sn // 128
        zcs = P.sb([128, ntile, 512], BF16, "zcs", es=es2)
        grp = [(a, n) for (a, n, sg_) in cfg.groups if sg_ == seg]
        for (n0, n) in grp:
            h = hg.get()
            P.dma(h[:, :, 0:n], self.hT[:, :, n0:n0 + n].re("k p t -> p k t"))
            z = zf.get()
            for c in range(2):
                pa = psa.get()
                for k in range(KD):
                    P.mm(pa[:, 0:n], W[:, k, c * 128:(c + 1) * 128], h[:, k, 0:n], start=(k == 0), stop=(k == KD - 1))
                P.op('act', 'activation', out=z[:, c, 0:n], in_=pa[:, 0:n], func=AF.Copy)
            for sub in range(n // 128):
                pd = psd.get()
                for c in range(2):
                    P.mm(pd.a, z[:, c, sub * 128:(sub + 1) * 128], CD[:, c, :], start=(c == 0), stop=(c == 1))
                ti = (n0 - s0) // 128 + sub
                P.op('dve', 'tensor_copy', out=zcs[:, ti, :], in_=pd.a)
        kb_n = min(512, sn)
        nkb = sn // kb_n
        tcs = Rot([P.sb([128, ntile, kb_n], BF16, "tc", es=es2) for _ in range(2)])
        tss = Rot([P.sb([128, ntile, kb_n], BF16, "ts", es=es2) for _ in range(2)])
        zo = Rot([P.sb([128, 2, 512], BF16, "zo", es=es2) for _ in range(2)])
        rstride = cfg.t_lat // sn
        for kb in range(nkb):
            tc_ = tcs.get()
            ts_ = tss.get()
            if rstride == 1:
                P.dma(tc_.a, self.dftc[:, kb * kb_n:(kb + 1) * kb_n].re("(t p) n -> p t n", p=128))
                P.dma(ts_.a, self.dfts[:, kb * kb_n:(kb + 1) * kb_n].re("(t p) n -> p t n", p=128))
            else:
                P.dma(tc_.a, self.dftc.a.re("(r s) n -> r s n", s=rstride)[:, 0, kb * kb_n:(kb + 1) * kb_n].re("(t p) n -> p t n", p=128))
                P.dma(ts_.a, self.dfts.a.re("(r s) n -> r s n", s=rstride)[:, 0, kb * kb_n:(kb + 1) * kb_n].re("(t p) n -> p t n", p=128))
            z = zo.get()
            for c in range(2):
                pd = psd.get()
                for ti in range(ntile):
                    P.mm(pd[:, 0:kb_n], zcs[:, ti, c * 128:(c + 1) * 128], tc_[:, ti, :], start=(ti == 0), stop=False)
                    P.mm(pd[:, 0:kb_n], zcs[:, ti, 256 + c * 128:256 + (c + 1) * 128], ts_[:, ti, :], start=False, stop=(ti == ntile - 1))
                P.op('act', 'activation', out=z[:, c, 0:kb_n], in_=pd[:, 0:kb_n], func=AF.Identity, scale=float(np.sqrt(rstride)))
            P.dma(self.Z[3, :, s0 + kb * kb_n:s0 + (kb + 1) * kb_n].re("(c p) t -> p c t", p=128), z[:, :, 0:kb_n], q='pool')
        es2.close()
        P.barrier()
    es.close()
    P.barrier()


MK.phase_fnet = _ft


def _merge(self, l):
    P, cfg = self.P, self.cfg
    es = contextlib.ExitStack()
    Wg = P.sb([128, KD, 4096], BF16, "wgate", es=es)
    for k in range(KD):
        for i in range(4):
            self.load_w(Wg[:, k, i * 1024:(i + 1) * 1024], self.w_in[l, k * 128:(k + 1) * 128, GATE_OFF + i * 1024:GATE_OFF + (i + 1) * 1024])
    Wb = P.sb([128, 4, 2, 1024], BF16, "wbr", es=es)
    for i in range(4):
        for c in range(2):
            self.load_w(Wb[:, i, c, :], self.w_branch[l, i, c * 128:(c + 1) * 128, :])
    Wo = P.sb([128, KD, 1024], BF16, "wout", es=es)
    for k in range(KD):
        self.load_w(Wo[:, k, :], self.w_out[l, k * 128:(k + 1) * 128, :])
    gate1 = self.modT[:, 16:24, :]
    hg = Rot([P.sb([128, KD, 512], BF16, "hg", es=es) for _ in range(2)])
    zg = Rot([P.sb([128, 4, 2, 512], BF16, "zg", es=es) for _ in range(2)])
    xg = Rot([P.sb([128, KD, 512], F32, "xg", es=es) for _ in range(2)])
    yT = P.sb([128, KD, 512], BF16, "yT", es=es)
    psg = Rot([P.ps([128, 512], F32, es=es) for _ in range(3)])
    psl = Rot([P.ps([128, 512], F32, es=es) for _ in range(3)])
    pso = Rot([P.ps([128, 512], F32, es=es) for _ in range(2)])
    sg = Rot([P.sb([128, 512], F32, "sg", es=es) for _ in range(3)])
    acc = Rot([P.sb([128, 512], F32, "acc", es=es) for _ in range(2)])
    for (n0, n, seg) in cfg.groups:
        if self.skip_ctx and seg == 1:
            continue
        h = hg.get()
        P.dma(h[:, :, 0:n], self.hT[:, :, n0:n0 + n].re("k p t -> p k t"))
        z = zg.get()
        for i in range(4):
            P.dma(z[:, i, :, 0:n], self.Z[i, :, n0:n0 + n].re("(c p) t -> p c t", p=128))
        x = xg.get()
        P.dma(x[:, :, 0:n], self.xT[:, :, n0:n0 + n].re("k p t -> p k t"))
        for dc in range(KD):
            a = acc.get()
            for i in range(4):
                pg = psg.get()
                for k in range(KD):
                    P.mm(pg[:, 0:n], Wg[:, k, i * 1024 + dc * 128:i * 1024 + (dc + 1) * 128], h[:, k, 0:n], start=(k == 0), stop=(k == KD - 1))
                s = sg.get()
                P.op('act', 'activation', out=s[:, 0:n], in_=pg[:, 0:n], func=AF.Sigmoid)
                pl = psl.get()
                for c in range(2):
                    P.mm(pl[:, 0:n], Wb[:, i, c, dc * 128:(dc + 1) * 128], z[:, i, c, 0:n], start=(c == 0), stop=(c == 1))
                if i == 0:
                    P.op('dve', 'tensor_tensor', out=a[:, 0:n], in0=pl[:, 0:n], in1=s[:, 0:n], op=ALU.mult)
                else:
                    P.op('dve', 'tensor_tensor', out=s[:, 0:n], in0=pl[:, 0:n], in1=s[:, 0:n], op=ALU.mult)
                    if i < 3:
                        P.op(self.merge_eng, 'tensor_tensor', out=a[:, 0:n], in0=a[:, 0:n], in1=s[:, 0:n], op=ALU.add)
                    else:
                        P.op(self.merge_eng, 'tensor_tensor', out=yT[:, dc, 0:n], in0=a[:, 0:n], in1=s[:, 0:n], op=ALU.add)
        for dc in range(KD):
            po = pso.get()
            for k in range(KD):
                P.mm(po[:, 0:n], Wo[:, k, dc * 128:(dc + 1) * 128], yT[:, k, 0:n], start=(k == 0), stop=(k == KD - 1))
            P.op('dve', 'scalar_tensor_tensor', out=x[:, dc, 0:n], in0=po[:, 0:n], scalar=gate1[:, dc, seg:seg + 1], in1=x[:, dc, 0:n], op0=ALU.mult, op1=ALU.add)
        P.dma(self.xT[:, :, n0:n0 + n].re("k p t -> p k t"), x[:, :, 0:n], q='pool')
    es.close()
    P.barrier()


MK.phase_merge = _merge


def _final(self):
    P, cfg = self.P, self.cfg
    es = contextlib.ExitStack()
    gT = self.load_rows_T([self.final_norm_g.a.re("(j p) -> j p", p=128)], es)
    gs = P.sb([128, KD, 2], F32, "gsf", es=es)
    P.op('dve', 'tensor_copy', out=gs.a, in_=gT.a.unsq(2).bc([128, KD, 2]))
    xg = Rot([P.sb([128, KD, 512], F32, "xg", es=es) for _ in range(2)])
    yg = Rot([P.sb([128, KD, 512], F32, "yg", es=es) for _ in range(2)])
    sq = P.sb([128, KD, 512], BF16, "sq", es=es)
    pss = Rot([P.ps([128, 512], F32, es=es) for _ in range(2)])
    pst = Rot([P.ps([128, 512], F32, es=es) for _ in range(4)])
    tmps = Rot([P.sb([128, 512], F32, "nt", es=es) for _ in range(4)])
    rsb = P.sb([128, 512], F32, "rsb", es=es)
    ot = Rot([P.sb([128, D], F32, "ot", es=es) for _ in range(3)])
    for (n0, n, seg) in cfg.groups:
        if seg == 1:
            continue
        x = xg.get()
        y = yg.get()
        P.dma(x[:, :, 0:n], self.xT[:, :, n0:n0 + n].re("k p t -> p k t"))
        self.norm_group(x[:, :, 0:n], n, gs, None, 0, y[:, :, 0:n], pss, tmps, sq, rsb)
        for sub in range(n // 128):
            o = ot.get()
            for half in range(2):
                ps = pst.get()
                for j in range(4):
                    k = half * 4 + j
                    P.op('pe', 'transpose', out=ps[:, j * 128:(j + 1) * 128], in_=y[:, k, sub * 128:(sub + 1) * 128], identity=self.identf)
                P.op('act', 'activation', out=o[:, half * 512:(half + 1) * 512], in_=ps.a, func=AF.Copy)
            r0 = n0 - cfg.t_ctx + sub * 128
            P.dma(self.out[r0:r0 + 128, :], o.a, q='pool')
    es.close()
    P.barrier()


MK.phase_final = _final


def _ml(self, l):
    P, cfg = self.P, self.cfg
    NT, NCH = cfg.nt, cfg.nch
    nctx = cfg.t_ctx // 64
    order_b = list(range(nctx - 1, -1, -1)) + list(range(NCH - 1, nctx - 1, -1))
    order_f = list(range(NCH))
    es = contextlib.ExitStack()
    biasA = P.sb([64, 1], F32, "biasA", es=es)
    biasB = P.sb([64, 1], F32, "biasB", es=es)
    P.op('pool', 'memset', ap=biasA.a, constant=0.0)
    P.op('pool', 'memset', ap=biasB.a, constant=0.0)
    gb = self.ml_gate_b
    P.dma(biasA[0:4, :], gb[l, 0:4].re("(a b) -> a b", b=1))
    P.dma(biasA[32:36, :], gb[l, 4:8].re("(a b) -> a b", b=1))
    P.dma(biasB[0:4, :], gb[l, 8:12].re("(a b) -> a b", b=1))
    P.dma(biasB[32:36, :], gb[l, 12:16].re("(a b) -> a b", b=1))
    cols = self.load_rows_T([self.ml_conv_w[l].re("j (c p) -> (j c) p", p=128), self.ml_conv_b[l].re("(c p) -> c p", p=128)], es)
    colsB = self.load_rows_T([self.ml_conv_b[l].re("(c p) -> c p", p=64)], es)
    diag = P.sb([128, 12, 128], BF16, "diag3", es=es)
    for jc in range(12):
        P.op('dve', 'tensor_scalar', out=diag[:, jc, :], in0=self.identf, scalar1=cols[:, jc:jc + 1], scalar2=None, op0=ALU.mult)
    qk8 = [P.sb([64, NT], BF16, "qk8_%d" % i, es=es) for i in range(8)]
    Atok = P.sb([64, NCH, 8], F32, "Atok", es=es)
    Btok = P.sb([64, NCH, 8], F32, "Btok", es=es)
    Ctok = P.sb([64, NCH, 8], F32, "Ctok", es=es)
    decb = P.sb([64, 8, NCH], F32, "decb", es=es)
    emb = P.sb([64, 8, NCH], F32, "emb", es=es)
    es1 = contextlib.ExitStack()
    Wqk = P.sb([128, KD, 512], BF16, "wqk", es=es1)
    for k in range(KD):
        self.load_w(Wqk[:, k, :], self.w_in[l, k * 128:(k + 1) * 128, 0:512])
    pre = [P.sb([128, NT + 4], BF16, "pre%d" % i, es=es1) for i in range(4)]
    for i in range(4):
        P.op('pool', 'memset', ap=pre[i].a, constant=0.0)
    hg = Rot([P.sb([128, KD, 512], BF16, "hg", es=es1) for _ in range(2)])
    psQ = Rot([P.ps([128, 512], F32, es=es1) for _ in range(2)])

    def poff(seg):
        return 1 if seg == 1 else cfg.t_ctx + 3

    for (n0, n, seg) in cfg.groups:
        s0 = 0 if seg == 1 else cfg.t_ctx
        h = hg.get()
        P.dma(h[:, :, 0:n], self.hT[:, :, n0:n0 + n].re("k p t -> p k t"))
        for ci in range(4):
            pq = psQ.get()
            for k in range(KD):
                P.mm(pq[:, 0:n], Wqk[:, k, ci * 128:(ci + 1) * 128], h[:, k, 0:n], start=(k == 0), stop=(k == KD - 1))
            o0 = poff(seg) + n0 - s0
            P.op('dve', 'tensor_copy', out=pre[ci][:, o0:o0 + n], in_=pq[:, 0:n])
    for (n0, n, seg) in cfg.groups:
        s0 = 0 if seg == 1 else cfg.t_ctx
        for ci in range(4):
            for hp in range(2):
                pq = psQ.get()
                o0 = poff(seg) + n0 - s0 - 1
                for j in range(3):
                    P.mm(pq[0:64, 0:n], diag[:, j * 4 + ci, hp * 64:(hp + 1) * 64], pre[ci][:, o0 + j:o0 + j + n], start=(j == 0), stop=(j == 2))
                P.op('act', 'activation', out=qk8[ci * 2 + hp][:, n0:n0 + n], in_=pq[0:64, 0:n], func=AF.Silu, bias=colsB[0:64, ci * 2 + hp:ci * 2 + hp + 1])
    for h8 in range(4, 8):
        P.op('dve', 'tensor_scalar', out=qk8[h8].a, in0=qk8[h8].a, scalar1=0.125, scalar2=None, op0=ALU.mult)
    es1.close()
    P.barrier()
    esA = contextlib.ExitStack()
    LI = P.sb([64, NT], F32, "LI", es=esA)
    LF = P.sb([64, NT], F32, "LF", es=esA)
    es1 = contextlib.ExitStack()
    WgA = P.sb([128, KD, 64], BF16, "wga", es=es1)
    WgB = P.sb([128, KD, 64], BF16, "wgb", es=es1)
    P.op('pool', 'memset', ap=WgA.a, constant=0.0)
    P.op('pool', 'memset', ap=WgB.a, constant=0.0)
    for k in range(KD):
        rows = slice(k * 128, (k + 1) * 128)
        self.load_w(WgA[:, k, 0:4], self.w_in[l, rows, 768:772])
        self.load_w(WgA[:, k, 32:36], self.w_in[l, rows, 772:776])
        self.load_w(WgB[:, k, 0:4], self.w_in[l, rows, 776:780])
        self.load_w(WgB[:, k, 32:36], self.w_in[l, rows, 780:784])
    hg = Rot([P.sb([128, KD, 512], BF16, "hg", es=es1) for _ in range(2)])
    psA = Rot([P.ps([128, 512], F32, es=es1) for _ in range(2)])
    sgt = Rot([P.sb([64, 512], F32, "sgt", es=es1) for _ in range(2)])
    for (n0, n, seg) in cfg.groups:
        h = hg.get()
        P.dma(h[:, :, 0:n], self.hT[:, :, n0:n0 + n].re("k p t -> p k t"))
        pa = psA.get()
        for k in range(KD):
            P.mm(pa[0:64, 0:n], WgA[:, k, :], h[:, k, 0:n], start=(k == 0), stop=(k == KD - 1))
        P.op('act', 'activation', out=LI[:, n0:n0 + n], in_=pa[0:64, 0:n], func=AF.Identity, bias=biasA.a)
        pb = psA.get()
        for k in range(KD):
            P.mm(pb[0:64, 0:n], WgB[:, k, :], h[:, k, 0:n], start=(k == 0), stop=(k == KD - 1))
        s = sgt.get()
        P.op('act', 'activation', out=s[:, 0:n], in_=pb[0:64, 0:n], func=AF.Sigmoid, bias=biasB.a)
        P.op('act', 'activation', out=LF[:, n0:n0 + n], in_=s[:, 0:n], func=AF.Ln)
    es1.close()
    P.barrier()
    es1 = contextlib.ExitStack()
    ones = P.sb([64, NT], BF16, "ones", es=es1)
    P.op('pool', 'memset', ap=ones.a, constant=1.0)
    cs = P.sb([64, NT], F32, "cs", es=es1)
    P.op('dve', 'tensor_tensor_scan', out=cs.a, data0=ones.a, data1=LF.a, initial=0.0, op0=ALU.mult, op1=ALU.add)
    j3 = lambda b: b.a.re("p (c j) -> p c j", j=64)
    sm = lambda nm: P.sb([64, NCH], F32, nm, es=es1)
    base, tot, wmax, cmax, m_in, m_new, Mx, dec, em = [sm(x) for x in ("base", "tot", "wmax", "cmax", "m_in", "m_new", "Mx", "dec", "em")]
    bc3 = lambda b: b.a.unsq(2).bc([64, NCH, 64])
    P.op('dve', 'tensor_tensor', out=base.a, in0=j3(cs)[:, :, 0], in1=j3(LF)[:, :, 0], op=ALU.subtract)
    BC = cs
    P.op('dve', 'tensor_tensor', out=j3(BC), in0=j3(cs), in1=bc3(base), op=ALU.subtract)
    P.op('dve', 'tensor_copy', out=tot.a, in_=j3(BC)[:, :, 63])
    P.op('dve', 'tensor_tensor', out=j3(BC)[32:64], in0=bc3(tot)[32:64], in1=j3(BC)[32:64], op=ALU.subtract)
    P.op('dve', 'tensor_tensor', out=BC[32:64, :], in0=BC[32:64, :], in1=LF[32:64, :], op=ALU.add)
    Wt = LF
    Cq = LI
    P.op('dve', 'tensor_tensor', out=Cq.a, in0=LI.a, in1=BC.a, op=ALU.subtract)
    P.op('dve', 'tensor_tensor', out=j3(Wt), in0=j3(Cq), in1=bc3(tot), op=ALU.add)
    P.op('dve', 'tensor_reduce', out=wmax.a, in_=j3(Wt), axis=AX.X, op=ALU.max)
    P.op('dve', 'tensor_reduce', out=cmax.a, in_=j3(Cq), axis=AX.X, op=ALU.max)
    zero = P.sb([64, 1], F32, "zero", es=es1)
    P.op('pool', 'memset', ap=zero.a, constant=0.0)
    P.op('pool', 'memset', ap=m_in.a, constant=0.0)
    for (rows, order) in ((slice(0, 32), order_f), (slice(32, 64), order_b)):
        prev = None
        for i, c in enumerate(order):
            pv_ = zero[rows, 0:1] if prev is None else m_new[rows, prev:prev + 1]
            if prev is not None:
                P.op('dve', 'tensor_copy', out=m_in[rows, c:c + 1], in_=pv_)
            P.op('dve', 'tensor_scalar', out=m_new[rows, c:c + 1], in0=pv_, scalar1=tot[rows, c:c + 1], scalar2=wmax[rows, c:c + 1], op0=ALU.add, op1=ALU.max)
            prev = c
    P.op('dve', 'tensor_tensor', out=Mx.a, in0=m_in.a, in1=cmax.a, op=ALU.max)
    P.op('dve', 'tensor_tensor', out=j3(Cq), in0=j3(Cq), in1=bc3(Mx), op=ALU.subtract)
    P.op('act', 'activation', out=Cq.a, in_=Cq.a, func=AF.Exp)
    P.op('dve', 'tensor_tensor', out=j3(Wt), in0=j3(Wt), in1=bc3(m_new), op=ALU.subtract)
    P.op('act', 'activation', out=Wt.a, in_=Wt.a, func=AF.Exp)
    P.op('dve', 'tensor_tensor', out=j3(BC), in0=j3(BC), in1=bc3(Mx), op=ALU.add)
    P.op('act', 'activation', out=BC.a, in_=BC.a, func=AF.Exp, scale=-1.0)
    P.op('dve', 'tensor_tensor', out=dec.a, in0=tot.a, in1=m_in.a, op=ALU.add)
    P.op('dve', 'tensor_tensor', out=dec.a, in0=dec.a, in1=m_new.a, op=ALU.subtract)
    P.op('act', 'activation', out=dec.a, in_=dec.a, func=AF.Exp)
    P.op('dve', 'tensor_tensor', out=em.a, in0=m_in.a, in1=Mx.a, op=ALU.subtract)
    P.op('act', 'activation', out=em.a, in_=em.a, func=AF.Exp)
    pt = Rot([P.ps([64, 8, 64], F32, es=es1) for _ in range(2)])
    for (src, dst) in ((Cq, Atok), (Wt, Btok), (BC, Ctok)):
        for c0 in range(0, NCH, 8):
            nb = min(8, NCH - c0)
            p_ = pt.get()
            for cc in range(nb):
                P.op('pe', 'transpose', out=p_[:, cc, :], in_=src[:, (c0 + cc) * 64:(c0 + cc + 1) * 64], identity=self.identf[0:64, 0:64])
            P.op('dve', 'tensor_copy', out=dst[:, c0:c0 + nb, 0:4], in_=p_[:, 0:nb, 0:4])
            P.op('dve', 'tensor_copy', out=dst[:, c0:c0 + nb, 4:8], in_=p_[:, 0:nb, 32:36])
    pbq = Rot([P.ps([64, 4, NCH], F32, es=es1) for _ in range(2)])
    for (src, dst) in ((dec, decb), (em, emb)):
        for half in range(2):
            p_ = pbq.get()
            for gg in range(4):
                g = half * 4 + gg
                P.mm(p_[:, gg, :], self.cst[0:64, 384 + g * 64:384 + (g + 1) * 64], src.a, start=True, stop=True)
            P.op('dve', 'tensor_copy', out=dst[:, half * 4:half * 4 + 4, :], in_=p_.a)
    es1.close()
    esA.close()
    P.barrier()
    es3 = contextlib.ExitStack()
    v1 = P.sb([64, NCH, 4, 65], BF16, "v1", es=es3)
    P.op('pool', 'memset', ap=v1.a, constant=1.0)
    ktok = P.sb([64, NCH, 256], BF16, "ktok", es=es3)
    es3a = contextlib.ExitStack()
    Wv = P.sb([128, KD, 256], BF16, "wv", es=es3a)
    for k in range(KD):
        self.load_w(Wv[:, k, :], self.w_in[l, k * 128:(k + 1) * 128, 512:768])
    hg = Rot([P.sb([128, KD, 512], BF16, "hg", es=es3a) for _ in range(2)])
    psV = Rot([P.ps([128, 512], F32, es=es3a) for _ in range(2)])
    pk = Rot([P.ps([64, 256], BF16, es=es3a) for _ in range(2)])
    for (n0, n, seg) in cfg.groups:
        h = hg.get()
        P.dma(h[:, :, 0:n], self.hT[:, :, n0:n0 + n].re("k p t -> p k t"))
        for cc in range(n // 64):
            c = n0 // 64 + cc
            pv = psV.get()
            for k in range(KD):
                P.mm(pv[0:64, 0:256], h[:, k, cc * 64:(cc + 1) * 64], Wv[:, k, :], start=(k == 0), stop=(k == KD - 1))
            P.op('act', 'activation', out=v1[:, c, :, 0:64], in_=pv[0:64, 0:256].re("p (h d) -> p h d", h=4), func=AF.Copy)
    for c in range(NCH):
        p_ = pk.get()
        for hh in range(4):
            P.op('pe', 'transpose', out=p_[:, hh * 64:(hh + 1) * 64], in_=qk8[4 + hh][:, c * 64:(c + 1) * 64], identity=self.identb[0:64, 0:64])
        P.op('act', 'activation', out=ktok[:, c, :], in_=p_.a, func=AF.Copy)
    es3a.close()
    P.barrier()
    PS1 = [P.ps([64, 4, 64], F32, es=es3) for _ in range(2)]
    PS2 = [P.ps([64, 4, 65], F32, es=es3) for _ in range(2)]
    PS3 = [P.ps([64, 4, 65], F32, es=es3) for _ in range(2)]
    Cst = [P.sb([64, 4, 65], F32, "Cst", es=es3) for _ in range(2)]
    CnS = [P.sb([64, 4, 65], BF16, "CnS", es=es3) for _ in range(2)]
    for d_ in range(2):
        P.op('pool', 'memset', ap=Cst[d_].a, constant=0.0)
        P.op('pool', 'memset', ap=CnS[d_].a, constant=0.0)
    tmp = [Rot([P.sb([64, 4, 64], F32, "stmp", es=es3) for _ in range(2)]) for _ in range(2)]
    SD = [Rot([P.sb([64, 4, 64], BF16, "SD", es=es3) for _ in range(2)]) for _ in range(2)]
    kw = [Rot([P.sb([64, 4, 64], BF16, "kw", es=es3) for _ in range(2)]) for _ in range(2)]
    dn = [Rot([P.sb([64, 8], F32, "dn", es=es3) for _ in range(2)]) for _ in range(2)]
    hb = [Rot([P.sb([64, 4, 64], F32, "hbuf", es=es3) for _ in range(3)]) for _ in range(2)]
    Hd = [self.Hf, self.Hb]
    orders = [order_f, order_b]
    prep_lst = []
    prep_es = contextlib.ExitStack()
    if self.prep_in_mlstm:
        prep_lst = self.phase_peer_prep(l, es=prep_es, defer=True) or []
    per_step = (len(prep_lst) + NCH - 1) // NCH + 1
    for i in range(NCH):
        cs_ = [orders[d_][i] for d_ in range(2)]
        tks = [slice(c * 64, (c + 1) * 64) for c in cs_]
        qh = [[qk8[hh][:, tks[d_]] for hh in range(4)] for d_ in range(2)]
        kh = [[qk8[4 + hh][:, tks[d_]] for hh in range(4)] for d_ in range(2)]
        for d_ in range(2):
            for hh in range(4):
                P.mm(PS1[d_][:, hh, :], kh[d_][hh], qh[d_][hh])
        t_ = [tmp[d_].get() for d_ in range(2)]
        for d_ in range(2):
            P.op('dve', 'tensor_tensor', out=t_[d_].a, in0=PS1[d_].a, in1=Atok[:, cs_[d_], 4 * d_:4 * d_ + 4].unsq(2).bc([64, 4, 64]), op=ALU.mult)
        sd = [SD[d_].get() for d_ in range(2)]
        for d_ in range(2):
            P.op(self.scan_eng, 'tensor_tensor', out=sd[d_].a, in0=t_[d_].a, in1=self.maskb_t[:, d_, :, :], op=ALU.mult)
        kw_ = [kw[d_].get() for d_ in range(2)]
        for d_ in range(2):
            P.op(self.scan_eng, 'tensor_tensor', out=kw_[d_].a, in0=ktok[:, cs_[d_], :].re("p (h d) -> p h d", h=4), in1=Btok[:, cs_[d_], 4 * d_:4 * d_ + 4].unsq(2).bc([64, 4, 64]), op=ALU.mult)
        for d_ in range(2):
            for hh in range(4):
                P.mm(PS2[d_][:, hh, :], qh[d_][hh], CnS[d_][:, hh, :], start=True, stop=False)
                P.mm(PS2[d_][:, hh, :], sd[d_][:, hh, :], v1[:, cs_[d_], hh, :], start=False, stop=True)
        for d_ in range(2):
            for hh in range(4):
                P.mm(PS3[d_][:, hh, :], kw_[d_][:, hh, :], v1[:, cs_[d_], hh, :])
        for d_ in range(2):
            P.op(self.scan_eng, 'tensor_tensor', out=Cst[d_].a, in0=Cst[d_].a, in1=decb[:, 4 * d_:4 * d_ + 4, cs_[d_]].unsq(2).bc([64, 4, 65]), op=ALU.mult)
        for d_ in range(2):
            P.op('dve', 'tensor_tensor', out=Cst[d_].a, in0=Cst[d_].a, in1=PS3[d_].a, op=ALU.add)
        if i + 1 < NCH:
            for d_ in range(2):
                cn = orders[d_][i + 1]
                P.op(self.scan_eng, 'tensor_tensor', out=CnS[d_].a, in0=Cst[d_].a, in1=emb[:, 4 * d_:4 * d_ + 4, cn].unsq(2).bc([64, 4, 65]), op=ALU.mult)
        dd = [dn[d_].get() for d_ in range(2)]
        for d_ in range(2):
            P.op('act', 'activation', out=dd[d_][:, 0:4], in_=PS2[d_][:, :, 64], func=AF.Abs)
        for d_ in range(2):
            P.op('dve', 'tensor_tensor', out=dd[d_][:, 0:4], in0=dd[d_][:, 0:4], in1=Ctok[:, cs_[d_], 4 * d_:4 * d_ + 4], op=ALU.max)
        for d_ in range(2):
            P.op('dve', 'reciprocal', out=dd[d_][:, 4:8], in_=dd[d_][:, 0:4])
        for d_ in range(2):
            hbuf = hb[d_].get()
            P.op('dve', 'tensor_tensor', out=hbuf.a, in0=PS2[d_][:, :, 0:64], in1=dd[d_][:, 4:8].unsq(2).bc([64, 4, 64]), op=ALU.mult)
            P.dma(Hd[d_][cs_[d_]].re("p (h d) -> p h d", h=4), hbuf.a, q='sp')
        P.run_deferred(prep_lst, per_step)
    P.run_deferred(prep_lst)
    P.barrier()
    prep_es.close()
    es3.close()
    P.barrier()
    es4 = contextlib.ExitStack()
    Wo = P.sb([128, KD, 256], BF16, "wo", es=es4)
    for k in range(KD):
        self.load_w(Wo[:, k, :], self.w_in[l, k * 128:(k + 1) * 128, 784:1040])
    ng = P.sb([64, 256], F32, "mlng", es=es4)
    P.dma(ng.a, self.ml_norm_g[l:l + 1, :].pbc(64))
    hg = Rot([P.sb([128, KD, 512], BF16, "hg", es=es4) for _ in range(2)])
    hf_ = Rot([P.sb([64, 8, 256], F32, "hf", es=es4) for _ in range(2)])
    hb_ = Rot([P.sb([64, 8, 256], F32, "hb", es=es4) for _ in range(2)])
    sq = P.sb([64, 8, 256], F32, "sq4", es=es4)
    ss = P.sb([64, 32], F32, "ss4", es=es4)
    ob = Rot([P.sb([64, 8, 256], F32, "ob", es=es4) for _ in range(2)])
    hz = Rot([P.sb([64, 8, 256], BF16, "hz", es=es4) for _ in range(2)])
    zst = Rot([P.sb([128, 2, 512], BF16, "zst", es=es4) for _ in range(2)])
    pso = Rot([P.ps([128, 512], F32, es=es4) for _ in range(2)])
    pst = Rot([P.ps([128, 8, 64], BF16, es=es4) for _ in range(2)])
    for (n0, n, seg) in cfg.groups:
        if self.skip_ctx and seg == 1:
            continue
        nc_ = n // 64
        c0 = n0 // 64
        h = hg.get()
        P.dma(h[:, :, 0:n], self.hT[:, :, n0:n0 + n].re("k p t -> p k t"))
        a = hf_.get()
        b = hb_.get()
        P.dma(a[:, 0:nc_, :], self.Hf[c0:c0 + nc_].re("c p f -> p c f"))
        P.dma(b[:, 0:nc_, :], self.Hb[c0:c0 + nc_].re("c p f -> p c f"))
        P.op('dve', 'tensor_tensor', out=a[:, 0:nc_, :], in0=a[:, 0:nc_, :], in1=b[:, 0:nc_, :], op=ALU.add)
        P.op('act', 'activation', out=sq[:, 0:nc_, :], in_=a[:, 0:nc_, :], func=AF.Square)
        P.op('dve', 'tensor_reduce', out=ss[:, 0:nc_ * 4], in_=sq[:, 0:nc_, :].re("p c (h d) -> p (c h) d", h=4), axis=AX.X, op=ALU.add)
        P.op('act', 'activation', out=ss[:, 0:nc_ * 4], in_=ss[:, 0:nc_ * 4], func=AF.Sqrt, scale=1.0 / 64, bias=self.eps[0:64, :])
        P.op('dve', 'reciprocal', out=ss[:, 0:nc_ * 4], in_=ss[:, 0:nc_ * 4])
        P.op('dve', 'tensor_tensor', out=a[:, 0:nc_, :].re("p c (h d) -> p (c h) d", h=4), in0=a[:, 0:nc_, :].re("p c (h d) -> p (c h) d", h=4),
             in1=ss[:, 0:nc_ * 4].unsq(2).bc([64, nc_ * 4, 64]), op=ALU.mult)
        P.op('dve', 'tensor_tensor', out=a[:, 0:nc_, :], in0=a[:, 0:nc_, :], in1=ng.a.unsq(1).bc([64, nc_, 256]), op=ALU.mult)
        o = ob.get()
        for cc in range(nc_):
            po = pso.get()
            for k in range(KD):
                P.mm(po[0:64, 0:256], h[:, k, cc * 64:(cc + 1) * 64], Wo[:, k, :], start=(k == 0), stop=(k == KD - 1))
            P.op('act', 'activation', out=o[:, cc, :], in_=po[0:64, 0:256], func=AF.Sigmoid)
        z = hz.get()
        P.op('dve', 'tensor_tensor', out=z[:, 0:nc_, :], in0=a[:, 0:nc_, :], in1=o[:, 0:nc_, :], op=ALU.mult)
        zs = zst.get()
        for half in range(2):
            p_ = pst.get()
            for cc in range(nc_):
                P.op('pe', 'transpose', out=p_[:, cc, :], in_=z[:, cc, half * 128:(half + 1) * 128], identity=self.identb[0:64, 0:64])
            P.op('act', 'activation', out=zs[:, half, 0:n], in_=p_[:, 0:nc_, :].re("p c t -> p (c t)"), func=AF.Copy)
        P.dma(self.Z[0, :, n0:n0 + n].re("(c p) t -> p c t", p=128), zs[:, :, 0:n], q='pool')
    es4.close()
    es.close()
    P.barrier()


MK.phase_mlstm = _ml


def _peer_prep(self, l, es=None, defer=False):
    P = self.P
    if self.prep_done.get(l):
        return None
    self.prep_done[l] = True
    own_es = es is None
    es = es or contextlib.ExitStack()
    lst = []
    if defer:
        P.deferred = lst
    ub = Rot([P.sb([128, D], BF16, "ub", es=es) for _ in range(3)])
    vb = Rot([P.sb([128, D], BF16, "vb", es=es) for _ in range(3)])
    uo = Rot([P.sb([128, D], BF16, "uo", es=es) for _ in range(3)])
    pst = Rot([P.ps([128, KD, 128], BF16, es=es) for _ in range(2 if defer else 3)])
    Uv = self.peer_u[l].re("(a b) d -> b a d", b=128)
    Vv = self.peer_v[l].re("(a b) d -> b a d", b=128)
    for e2 in range(128):
        u = ub.get()
        P.dma(u.a, Uv[e2], q='pool')
        p_ = pst.get()
        for k in range(KD):
            P.op('pe', 'transpose', out=p_[:, k, :], in_=u[:, k * 128:(k + 1) * 128], identity=self.identb)
        o = uo.get()
        if e2 % 2:
            P.op('act', 'activation', out=o.a, in_=p_.a.re("p k e -> p (k e)"), func=AF.Copy)
        else:
            P.op('dve', 'tensor_copy', out=o.a, in_=p_.a.re("p k e -> p (k e)"))
        P.dma(self.UTs[l % 2][e2], o.a, q='sp')
        v = vb.get()
        P.dma(v.a, Vv[e2], q='pool')
        P.dma(self.VBs[l % 2][e2], v.a, q='sp')
    P.deferred = None
    if own_es:
        es.close()
        P.barrier()
    return lst


MK.phase_peer_prep = _peer_prep


def _peer(self, l):
    P, cfg = self.P, self.cfg
    es = contextlib.ExitStack()
    gs, sh, gate2 = self.mod_scale_shift(l, 1, es)
    Wq = P.sb([128, KD, 2048], BF16, "wq", es=es)
    for k in range(KD):
        self.load_w(Wq[:, k, :], self.peer_w_q[l, k * 128:(k + 1) * 128, :])
    psA = Rot([P.ps([128, 512], F32, es=es) for _ in range(2)])
    Wps = Rot([P.ps([128, 512], F32, es=es) for _ in range(2)])
    acc = [P.ps([128, 2, 256], F32, es=es) for _ in range(4)]
    kl = P.sb([128, 2, 128], BF16, "kl", es=es)
    self.load_w(kl.a, self.peer_keys[l].re("p e k -> e p k"))
    keysT = P.sb([128, 2, 128], BF16, "keysT", es=es)
    for p in range(2):
        pk_ = Wps.get()
        pkb = pk_.a.re("q (a b) -> q a b", b=128)
        klf = P.sb([128, 128], F32, "klf", es=es)
        P.op('dve', 'tensor_copy', out=klf.a, in_=kl[:, p, :])
        P.op('pe', 'transpose', out=pk_[:, 0:128], in_=klf.a, identity=self.identf)
        P.op('dve', 'tensor_copy', out=keysT[:, p, :], in_=pk_[:, 0:128])
    xgs = [P.sb([128, KD, 256], F32, "xg", es=es) for _ in range(2)]
    h2s = [P.sb([128, KD, 256], BF16, "h2", es=es) for _ in range(2)]
    qT = P.sb([128, 16, 256], BF16, "qT", es=es)
    S = P.sb([128, 16, 128], F32, "S", es=es)
    S2 = P.sb([128, 16, 128], F32, "S2", es=es)
    V1 = P.sb([128, 16, 16], F32, "V1", es=es)
    I1u = P.sb([128, 16, 16], U32, "I1u", es=es)
    I1f = P.sb([128, 16, 16], F32, "I1f", es=es)
    cand = P.sb([128, 8, 256], F32, "cand", es=es)
    cand2 = S2
    SC = P.sb([128, 8, 16], F32, "SC", es=es)
    POSu = P.sb([128, 8, 16], U32, "POSu", es=es)
    PIu = P.sb([128, 8, 16], U32, "PIu", es=es)
    PJu = P.sb([128, 8, 16], U32, "PJu", es=es)
    PIf = P.sb([128, 128], F32, "PIf", es=es)
    PJf = P.sb([128, 128], F32, "PJf", es=es)
    OH = S
    E1 = P.sb([128, 128], F32, "E1", es=es)
    E2 = P.sb([128, 128], F32, "E2", es=es)
    G = P.sb([128, 128], F32, "G", es=es)
    sm = P.sb([128, 16], F32, "sm", es=es)
    E1T = P.sb([128, 256], BF16, "E1T", es=es)
    E2T = P.sb([128, 256], BF16, "E2T", es=es)
    GT = P.sb([128, 256], BF16, "GT", es=es)
    if self.wb4:
        An4 = Rot([P.sb([128, 4, 128], BF16, "An", es=es) for _ in range(2)])
        Bn4 = Rot([P.sb([128, 4, 128], BF16, "Bn", es=es) for _ in range(2)])
        iota4 = P.sb([128, 4, 128], BF16, "iota4", es=es)
        for tt in range(4):
            P.op('dve', 'tensor_copy', out=iota4[:, tt, :], in_=self.iota128b_t.a)
    else:
        An = Rot([P.sb([128, 128], BF16, "An", es=es) for _ in range(6)])
        Bn = Rot([P.sb([128, 128], BF16, "Bn", es=es) for _ in range(6)])
    Wbuf = P.sb([128, 256, 128], BF16, "Wbuf", es=es)
    ut = Rot([P.sb([128, KD, 128], BF16, "ut", es=es) for _ in range(4)])
    vt = Rot([P.sb([128, D], BF16, "vtb", es=es) for _ in range(4)])
    Ab = Rot([P.sb([128, 256], BF16, "Ab", es=es) for _ in range(3)])
    AW = Rot([P.sb([128, 256], BF16, "AW", es=es) for _ in range(3)])
    i16 = self.iota16
    V1s = [V1] + [V1.sub() for _ in range(15)]
    I1s = [I1u] + [I1u.sub() for _ in range(15)]
    S2s = [S2] + [S2.sub() for _ in range(15)]
    cands = [cand] + [cand.sub() for _ in range(7)]
    SCs = [SC] + [SC.sub() for _ in range(7)]
    POSs = [POSu] + [POSu.sub() for _ in range(7)]
    zlhs = P.sb([128, 128], BF16, "zlhs", es=es)
    zrhs = P.sb([128, 512], BF16, "zrhs", es=es)
    P.op('pool', 'memset', ap=zlhs.a, constant=0.0)
    P.op('pool', 'memset', ap=zrhs.a, constant=0.0)
    glist = [g for g in cfg.groups256 if not (self.skip_ctx and g[2] == 1)]
    sq = qT[:, 0:8, :]
    tmps = Rot([cands[i][:, i, :] for i in range(3)])
    rsb = cands[3][:, 3, :]

    def front(gi):
        (n0, n, seg) = glist[gi]
        x = xgs[gi % 2]
        h2 = h2s[gi % 2]
        P.dma(x.a, self.xT[:, :, n0:n0 + n].re("k p t -> p k t"))
        self.norm_group(x.a, n, gs, sh, seg, h2.a, Wps, tmps, sq, rsb)
        for j in range(16):
            pq = Wps.get()
            for k in range(KD):
                P.mm(pq[:, 0:n], Wq[:, k, j * 128:(j + 1) * 128], h2[:, k, :], start=(k == 0), stop=(k == KD - 1))
            P.op('act', 'activation', out=qT[:, j, :], in_=pq[:, 0:n], func=AF.Copy)
        for sub in range(2 if 'topk' in self.peer_parts else 0):
            tsl = slice(sub * 128, (sub + 1) * 128)
            for j4 in range(4):
                pscr = Wps.get()
                for jj in range(4):
                    j = j4 * 4 + jj
                    P.mm(pscr[:, jj * 128:(jj + 1) * 128], qT[:, j, tsl], keysT[:, j % 2, :])
                P.op('act', 'activation', out=S[:, j4 * 4:j4 * 4 + 4, :], in_=pscr.a.re("p (a b) -> p a b", b=128), func=AF.Copy)
            for j in range(16):
                P.op('dve', 'max', out=V1s[j][:, j, 0:8], in_=S[:, j, :])
            for j in range(16):
                P.op('dve', 'max_index', out=I1s[j][:, j, 0:8], in_max=V1s[j][:, j, 0:8], in_values=S[:, j, :])
            for j in range(16):
                P.op('dve', 'match_replace', out=S2s[j][:, j, :], in_to_replace=V1s[j][:, j, 0:8], in_values=S[:, j, :], imm_value=NEG)
            for j in range(16):
                P.op('dve', 'max', out=V1s[j][:, j, 8:16], in_=S2s[j][:, j, :])
            for j in range(16):
                P.op('dve', 'max_index', out=I1s[j][:, j, 8:16], in_max=V1s[j][:, j, 8:16], in_values=S2s[j][:, j, :])
            P.op('dve', 'tensor_copy', out=I1f.a, in_=I1s[0].a, xr=I1s[1:])
            V1v = V1s[0].a.re("q (h p) i -> q h p i", p=2)
            P.op('dve', 'tensor_tensor', out=cands[0].a.re("q h (i j) -> q h i j", j=16), in0=V1v[:, :, 0, :].unsq(3).bc([128, 8, 16, 16]),
                 in1=V1v[:, :, 1, :].unsq(2).bc([128, 8, 16, 16]), op=ALU.add, xr=V1s[1:], xw=cands[1:])
            c2v = S2.a.re("q a b -> q (a b)").re("q (h c) -> q h c", h=8)
            ohv = S.a.re("q a b -> q (a b)").re("q (j i) -> q j i", i=16)
            def c2(h):
                return V(S2s[2 * h], c2v.ap[:, h, :])
            for h in range(8):
                P.op('dve', 'max', out=SCs[h][:, h, 0:8], in_=cands[h][:, h, :])
            for h in range(8):
                P.op('dve', 'max_index', out=POSs[h][:, h, 0:8], in_max=SCs[h][:, h, 0:8], in_values=cands[h][:, h, :])
            for h in range(8):
                P.op('dve', 'match_replace', out=c2(h), in_to_replace=SCs[h][:, h, 0:8], in_values=cands[h][:, h, :], imm_value=NEG, xw=[S2s[2 * h + 1]])
            for h in range(8):
                P.op('dve', 'max', out=SCs[h][:, h, 8:16], in_=c2(h), xr=[S2s[2 * h + 1]])
            for h in range(8):
                P.op('dve', 'max_index', out=POSs[h][:, h, 8:16], in_max=SCs[h][:, h, 8:16], in_values=c2(h), xr=[S2s[2 * h + 1]])
            P.op('dve', 'tensor_single_scalar', out=PIu.a, in_=POSs[0].a, scalar=4, op=ALU.logical_shift_right, xr=POSs[1:])
            P.op('dve', 'tensor_single_scalar', out=PJu.a, in_=POSs[0].a, scalar=15, op=ALU.bitwise_and, xr=POSs[1:])
            P.op('dve', 'tensor_copy', out=PIf.a, in_=PIu.a.re("q h k -> q (h k)"))
            P.op('dve', 'tensor_copy', out=PJf.a, in_=PJu.a.re("q h k -> q (h k)"))
            I1v = I1f.a.re("q (h p) i -> q h p i", p=2)
            for (Pf, pp, Eo) in ((PIf, 0, E1), (PJf, 1, E2)):
                P.op('dve', 'tensor_tensor', out=ohv, in0=i16.unsq(1).bc([128, 128, 16]), in1=Pf.a.unsq(2).bc([128, 128, 16]), op=ALU.is_equal)
                P.op('dve', 'tensor_tensor', out=ohv.re("q (h k) i -> q h k i", h=8), in0=ohv.re("q (h k) i -> q h k i", h=8),
                     in1=I1v[:, :, pp, :].unsq(2).bc([128, 8, 16, 16]), op=ALU.mult)
                P.op('dve', 'tensor_reduce', out=Eo.a, in_=ohv, axis=AX.X, op=ALU.add)
            Gv = G.a.re("q (h k) -> q h k", h=8)
            P.op('dve', 'tensor_tensor', out=Gv, in0=SC.a, in1=SC[:, :, 0:1].bc([128, 8, 16]), op=ALU.subtract, xr=SCs[1:])
            P.op('act', 'activation', out=G.a, in_=G.a, func=AF.Exp)
            P.op('dve', 'tensor_reduce', out=sm[:, 0:8], in_=Gv, axis=AX.X, op=ALU.add)
            P.op('dve', 'reciprocal', out=sm[:, 8:16], in_=sm[:, 0:8])
            P.op('dve', 'tensor_tensor', out=Gv, in0=Gv, in1=sm[:, 8:16].unsq(2).bc([128, 8, 16]), op=ALU.mult)
            for (src, dst) in ((E1, E1T), (E2, E2T), (G, GT)):
                ptr = Wps.get()
                P.op('pe', 'transpose', out=ptr[:, 0:128], in_=src.a, identity=self.identf)
                P.op('act', 'activation', out=dst[:, tsl], in_=ptr[:, 0:128], func=AF.Copy)
    def run_front(gi):
        lst = []
        P.deferred = lst
        front(gi)
        P.deferred = None
        return lst

    P.run_deferred(run_front(0))
    for gi, (n0, n, seg) in enumerate(glist):
        x = xgs[gi % 2]
        h2 = h2s[gi % 2]
        for t4 in range(n // 4 if 'wb' in self.peer_parts else 0):
            wp = Wps.get()
            if self.wb4:
                a_ = An4.get()
                b_ = Bn4.get()
                t0_ = t4 * 4
                P.op('dve', 'tensor_tensor', out=a_.a, in0=iota4.a, in1=E1T[:, t0_:t0_ + 4].unsq(2).bc([128, 4, 128]), op=ALU.is_equal)
                P.op('dve', 'tensor_tensor', out=a_.a, in0=a_.a, in1=GT[:, t0_:t0_ + 4].unsq(2).bc([128, 4, 128]), op=ALU.mult)
                P.op('dve', 'tensor_tensor', out=b_.a, in0=iota4.a, in1=E2T[:, t0_:t0_ + 4].unsq(2).bc([128, 4, 128]), op=ALU.is_equal)
                for tt in range(4):
                    P.mm(wp[:, tt * 128:(tt + 1) * 128], a_[:, tt, :], b_[:, tt, :])
            for tt in range(0 if self.wb4 else 4):
                t = t4 * 4 + tt
                a_ = An.get()
                b_ = Bn.get()
                P.op('dve', 'tensor_scalar', out=a_.a, in0=self.iota128b_t.a, scalar1=E1T[:, t:t + 1], scalar2=GT[:, t:t + 1], op0=ALU.is_equal, op1=ALU.mult)
                P.op('dve', 'tensor_scalar', out=b_.a, in0=self.iota128b_t.a, scalar1=E2T[:, t:t + 1], scalar2=None, op0=ALU.is_equal)
                P.mm(wp[:, tt * 128:(tt + 1) * 128], a_.a, b_.a)
            P.op('act', 'activation', out=Wbuf[:, t4 * 4:t4 * 4 + 4, :], in_=wp.a.re("p (a b) -> p a b", b=128), func=AF.Copy)
        for bnk in range(4):
            P.mm(acc[bnk].a.re("p a b -> p (a b)"), zlhs.a, zrhs.a, start=True, stop=False)
        NE = 128 if 'ex' in self.peer_parts else 0

        def stage_a(e2):
            u = ut.get()
            v = vt.get()
            P.dma(u.a, self.UTs[l % 2][e2].re("p (k e) -> p k e", e=128), q='sp')
            P.dma(v.a, self.VBs[l % 2][e2], q='sp')
            pa = psA.get()
            for k in range(KD):
                P.mm(pa[:, 0:n], u[:, k, :], h2[:, k, :], start=(k == 0), stop=(k == KD - 1))
            ab = Ab.get()
            P.op('act', 'activation', out=ab.a, in_=pa[:, 0:n], func=AF.Gelu_apprx_tanh)
            aw = AW.get()
            P.op(self.aw_eng, 'tensor_tensor', out=aw.a, in0=ab.a, in1=Wbuf[:, :, e2], op=ALU.mult)
            return v, aw

        nxt = run_front(gi + 1) if gi + 1 < len(glist) else []
        if not getattr(self, 'peer_pipe', True):
            pre_nxt, nxt = nxt, []
        per_chunk = (len(nxt) + 119) // 120 if NE else len(nxt)
        pend = stage_a(0) if NE else None
        for e2 in range(NE):
            v, aw = pend
            if e2 + 1 < NE:
                pend = stage_a(e2 + 1)
            P.run_deferred(nxt, per_chunk)
            for dc in range(KD):
                P.mm(acc[dc // 2][:, dc % 2, :], v[:, dc * 128:(dc + 1) * 128], aw.a, start=False, stop=(e2 == 127 and dc % 2 == 1))
        P.run_deferred(nxt)
        if not getattr(self, 'peer_pipe', True):
            P.run_deferred(pre_nxt)
        for dc in range(KD):
            P.op('dve', 'scalar_tensor_tensor', out=x[:, dc, :], in0=acc[dc // 2][:, dc % 2, :], scalar=gate2[:, dc, seg:seg + 1], in1=x[:, dc, :], op0=ALU.mult, op1=ALU.add)
        P.dma(self.xT[:, :, n0:n0 + n].re("k p t -> p k t"), x.a, q='pool')
    es.close()
    P.barrier()


MK.phase_peer = _peer


def build_program(cfg, debug=False, phases=None, **opts):
    mk = MK(cfg, debug=debug)
    mk.skip_ctx = False
    mk.prep_done = {}
    mk.prep_in_mlstm = (phases is None) and opts.get('prep_in_mlstm', False)
    mk.peer_pipe = opts.get('peer_pipe', True)
    mk.wb4 = opts.get('wb4', False) or (phases is not None and 'wb4' in phases)
    mk.aw_eng = 'pool' if (opts.get('aw_pool', True) or (phases is not None and 'aw_pool' in phases)) else 'dve'
    mk.scan_eng = 'dve' if (opts.get('scan_dve', True) and not (phases is not None and 'scan_pool' in phases)) else 'pool'
    mk.merge_eng = 'dve' if (opts.get('merge_dve', True) and not (phases is not None and 'merge_pool' in phases)) else 'pool'
    mk.wb_pool = opts.get('wb_pool', False) or (phases is not None and 'wb_pool' in phases)
    mk.peer_parts = set(['topk', 'wb', 'ex']) if (phases is None or not any(p.startswith('pp_') for p in phases)) else set(p[3:] for p in phases if p.startswith('pp_'))
    on = lambda p: phases is None or p in phases
    mk.phase_init()
    for l in range(cfg.depth):
        mk.skip_ctx = False
        if on('mod'):
            mk.phase_mod(l)
        if on('norm1'):
            mk.phase_norm1(l)
        if on('mlstm'):
            mk.phase_mlstm(l)
        mk.skip_ctx = (l == cfg.depth - 1) and phases is None
        if on('gmlp'):
            mk.phase_gmlp(l)
        if on('conv'):
            mk.phase_conv(l)
        if on('fnet'):
            mk.phase_fnet(l)
        if on('merge'):
            mk.phase_merge(l)
        if on('prep'):
            mk.phase_peer_prep(l)
        if on('peer'):
            mk.phase_peer(l)
    mk.phase_final()
    mk.P.finish()
    return mk


_CACHE = {}


def make_in_maps(cfg, inp):
    dftc, dfts, cd, cst = host_consts(cfg)
    pos = grid_sincos(cfg.t_lat, D)
    L = cfg.depth
    shared = {"pos": pos, "dftc": dftc, "dfts": dfts, "cd": cd, "cst": cst}
    for k in ["w_ada", "b_ada", "norm1_g", "norm2_g", "w_in", "ml_conv_w", "ml_conv_b", "ml_gate_b", "gm_ln_g", "gm_ln_b",
              "gm_w_s", "cv_dw_w", "cv_dw_b", "cv_ln_g", "cv_ln_b", "w_branch", "w_out", "peer_w_q", "peer_keys",
              "peer_u", "peer_v", "final_norm_g"]:
        shared[k] = np.ascontiguousarray(np.asarray(inp[k], dtype=np.float32))
    shared["ml_norm_g"] = np.ascontiguousarray(np.asarray(inp["ml_norm_g"], np.float32).reshape(L, 256))
    shared["gm_b_s"] = np.ascontiguousarray(np.asarray(inp["gm_b_s"], np.float32).reshape(L, 512))
    x = np.asarray(inp["x"], np.float32)
    ctx = np.asarray(inp["ctx"], np.float32)
    c = np.asarray(inp["c"], np.float32)
    c_ctx = np.asarray(inp["c_ctx"], np.float32)
    maps = []
    for b in range(x.shape[0]):
        m = dict(shared)
        m["xin"] = np.ascontiguousarray(np.concatenate([ctx[b], x[b]], 0))
        m["cvec"] = np.ascontiguousarray(np.stack([c[b], c_ctx], 0))
        maps.append(m)
    return maps


def kernel(**inputs):
    x = np.asarray(inputs["x"])
    B, T, _ = x.shape
    depth = np.asarray(inputs["w_ada"]).shape[0]
    cfg = Cfg(depth=depth, t_lat=T, t_ctx=np.asarray(inputs["ctx"]).shape[1])
    key = (depth, T, cfg.t_ctx)
    if key not in _CACHE:
        _CACHE[key] = build_program(cfg)
    mk = _CACHE[key]
    maps = make_in_maps(cfg, inputs)
    res = run_bass_kernel_spmd(mk.nc, maps, core_ids=list(range(B)))
    return np.stack([np.asarray(r["out"], dtype=np.float32) for r in res.results], 0)
```

```python
import contextlib
import numpy as np
import ml_dtypes
import concourse.bass as bass
import concourse.mybir as mybir
from concourse.bass_utils import run_bass_kernel_spmd

F32 = mybir.dt.float32
BF16 = mybir.dt.bfloat16
I32 = mybir.dt.int32
U32 = mybir.dt.uint32
AF = mybir.ActivationFunctionType
ALU = mybir.AluOpType
AX = mybir.AxisListType

COMPUTE = ('pe', 'act', 'dve', 'pool')
NRING = 12
WRITE_KW = ('out', 'accum_out', 'ap')
EPS = 1e-6
NEG = -1.0e30


class V:
    __slots__ = ('buf', 'ap')

    def __init__(self, buf, ap):
        self.buf = buf
        self.ap = ap

    def __getitem__(self, k):
        return V(self.buf, self.ap[k])

    def re(self, pat, **kw):
        return V(self.buf, self.ap.rearrange(pat, **kw))

    def bc(self, shape):
        return V(self.buf, self.ap.to_broadcast(list(shape)))

    def unsq(self, ax):
        return V(self.buf, self.ap.unsqueeze(ax))

    def pbc(self, n):
        return V(self.buf, self.ap.partition_broadcast(n))


class Buf:
    __slots__ = ('t', 'lw', 'rd', 'name')

    def __init__(self, t, name=''):
        self.t = t
        self.lw = None
        self.rd = {}
        self.name = name

    def __getitem__(self, k):
        return V(self, self.t[k])

    @property
    def a(self):
        return V(self, self.t[:])

    def sub(self):
        return Buf(self.t, self.name + '_s')


class Prog:
    def __init__(self, nc):
        self.nc = nc
        self.ops = {e: [] for e in ('pe', 'act', 'dve', 'pool', 'sp')}
        self.count = {e: 0 for e in COMPUTE}
        self.known = {e: {} for e in self.ops}
        self.ndma = {'sp': 0, 'pool': 0, 'act': 0}
        self.es = contextlib.ExitStack()
        self.sems = {}
        for e in COMPUTE:
            self.sems[('c', e)] = self.es.enter_context(nc.semaphore('s_' + e))
        for q in ('sp', 'pool', 'act'):
            for i in range(NRING):
                self.sems[('d', q, i)] = self.es.enter_context(nc.semaphore('d_%s_%d' % (q, i)))
        self.nbuf = 0
        self.ninst = 0
        self.deferred = None

    def sb(self, shape, dt, name=None, es=None):
        self.nbuf += 1
        name = '%s_%d' % (name or 'sb', self.nbuf)
        t = (es or self.es).enter_context(self.nc.sbuf_tensor(name, list(shape), dt))
        return Buf(t, name)

    def ps(self, shape, dt, name=None, es=None):
        self.nbuf += 1
        name = '%s_%d' % (name or 'ps', self.nbuf)
        t = (es or self.es).enter_context(self.nc.psum_tensor(name, list(shape), dt))
        return Buf(t, name)

    def dram(self, name, shape, dt, kind='Internal'):
        t = self.nc.dram_tensor(name, list(shape), dt, kind=kind)
        return Buf(t, name)

    def _need(self, eng, tok, waits):
        if tok is None:
            return
        k, v = tok
        if self.known[eng].get(k, 0) >= v:
            return
        if waits.get(k, 0) < v:
            waits[k] = v

    def emit(self, eng, fn, reads=(), writes=(), dma=False):
        waits = {}
        own = ('c', eng) if (eng in COMPUTE and not dma) else None
        for b in reads:
            if b.lw is not None:
                if own is not None and b.lw[0] == own and eng == 'pe':
                    continue
                self._need(eng, b.lw, waits)
        for b in writes:
            if b.lw is not None:
                if not (own is not None and b.lw[0] == own and eng == 'pe'):
                    self._need(eng, b.lw, waits)
            for k, v in b.rd.items():
                if own is not None and k == own and eng == 'pe':
                    continue
                self._need(eng, (k, v), waits)
        if dma:
            i = self.ndma[eng]
            self.ndma[eng] = i + 1
            slot, gen = i % NRING, i // NRING
            key = ('d', eng, slot)
            if gen > 0:
                self._need(eng, (key, 16 * gen), waits)
            tok = (key, 16 * (gen + 1))
            inc = (key, 16)
        else:
            self.count[eng] += 1
            tok = (own, self.count[eng])
            inc = (own, 1)
        for k, v in waits.items():
            self.known[eng][k] = v
        self.ops[eng].append((fn, list(waits.items()), inc))
        self.ninst += 1
        for b in writes:
            b.lw = tok
            b.rd = {}
        for b in reads:
            if b in writes:
                continue
            if b.rd.get(tok[0], 0) < tok[1]:
                b.rd[tok[0]] = tok[1]
        return tok

    def run_deferred(self, lst, k=None):
        k = len(lst) if k is None else min(k, len(lst))
        for _ in range(k):
            eng, name, xr, xw, kw = lst.pop(0)
            self.op(eng, name, xr=xr, xw=xw, **kw)

    def op(self, eng, name, *, xr=(), xw=(), **kw):
        if self.deferred is not None:
            self.deferred.append((eng, name, xr, xw, kw))
            return None
        reads, writes, real = list(xr), list(xw), {}
        for k, v in kw.items():
            if isinstance(v, V):
                (writes if k in WRITE_KW else reads).append(v.buf)
                real[k] = v.ap
            else:
                real[k] = v
        isdma = name == 'dma_start'

        def fn(e, name=name, real=real):
            return getattr(e, name)(**real)
        return self.emit(eng, fn, reads, writes, dma=isdma)

    def dma(self, out, in_, q='sp', **kw):
        return self.op(q, 'dma_start', out=out, in_=in_, **kw)

    def mm(self, out, lhsT, rhs, start=True, stop=True):
        return self.op('pe', 'matmul', out=out, lhsT=lhsT, rhs=rhs, start=start, stop=stop)

    def barrier(self):
        toks = []
        for e in COMPUTE:
            if self.count[e] > 0:
                toks.append((('c', e), self.count[e]))
        for q, n in self.ndma.items():
            for i in range(max(0, n - NRING), n):
                toks.append((('d', q, i % NRING), 16 * (i // NRING + 1)))
        for e in self.ops:
            waits = {}
            for tok in toks:
                if tok[0] == ('c', e):
                    continue
                self._need(e, tok, waits)
            for k, v in waits.items():
                self.known[e][k] = v
            if waits:
                self.ops[e].append((None, list(waits.items()), None))

    def finish(self):
        self.barrier()
        nc = self.nc
        sems = self.sems
        ops = self.ops

        def replay(name, e):
            for fn, waits, inc in ops[name]:
                for k, v in waits:
                    e.wait_ge(sems[k], v)
                if fn is None:
                    continue
                ins = fn(e)
                if inc is not None:
                    ins.then_inc(sems[inc[0]], inc[1])

        with nc.Block() as block:
            @block.sync
            def _(e):
                replay('sp', e)

            @block.tensor
            def _(e):
                replay('pe', e)

            @block.scalar
            def _(e):
                replay('act', e)

            @block.vector
            def _(e):
                replay('dve', e)

            @block.gpsimd
            def _(e):
                replay('pool', e)
        self.es.close()


class Rot:
    def __init__(self, bufs):
        self.bufs = bufs
        self.i = 0

    def get(self):
        b = self.bufs[self.i % len(self.bufs)]
        self.i += 1
        return b


D = 1024
KD = 8
IN_COLS = 6416
GM_OFF = 1040
CV_OFF = 1552
FT_OFF = 2064
GATE_OFF = 2320
NEXP = 16384


class Cfg:
    def __init__(self, depth=4, t_lat=4096, t_ctx=256):
        self.depth = depth
        self.t_lat = t_lat
        self.t_ctx = t_ctx
        self.nt = t_lat + t_ctx
        self.groups = [(0, t_ctx, 1)] + [(t_ctx + i * 512, 512, 0) for i in range(t_lat // 512)]
        self.groups256 = [(i * 256, 256, 1 if i * 256 < t_ctx else 0) for i in range(self.nt // 256)]
        self.segs = [(0, t_ctx, 1), (t_ctx, t_lat, 0)]
        self.nch = self.nt // 64


def host_consts(cfg):
    T = cfg.t_lat
    k = np.arange(T, dtype=np.float64)
    ang = 2.0 * np.pi * ((k[:, None] * k[None, :]) % T) / T
    dftc = (np.cos(ang) / np.sqrt(T)).astype(ml_dtypes.bfloat16)
    dfts = (-np.sin(ang) / np.sqrt(T)).astype(ml_dtypes.bfloat16)
    c = np.arange(64, dtype=np.float64)
    a64 = 2.0 * np.pi * ((c[:, None] * c[None, :]) % 64) / 64
    cd = np.zeros((256, 512), np.float64)
    for g in range(4):
        cd[g * 64:(g + 1) * 64, g * 64:(g + 1) * 64] = np.cos(a64) / 8.0
        cd[g * 64:(g + 1) * 64, 256 + g * 64:256 + (g + 1) * 64] = np.sin(a64) / 8.0
    cd = cd.astype(ml_dtypes.bfloat16)
    cst = np.zeros((128, 1024), np.float32)
    cst[:, 0:128] = np.eye(128, dtype=np.float32)
    cst[:, 128:256] = np.arange(128, dtype=np.float32)[None, :]
    s = np.arange(64)
    cst[0:64, 256:320] = (s[:, None] <= s[None, :]).astype(np.float32)
    cst[0:64, 320:384] = (s[:, None] >= s[None, :]).astype(np.float32)
    for g in range(8):
        row = g if g < 4 else 32 + (g - 4)
        cst[row, 384 + g * 64:384 + (g + 1) * 64] = 1.0
    cst[:, 896:912] = np.arange(16, dtype=np.float32)[None, :]
    cst[:, 912] = EPS
    cst[:, 913] = 1.0
    return dftc, dfts, cd, cst


def grid_sincos(n_tok, d):
    rows = n_tok // 64
    n_freq = d // 4
    freq = (1.0 / (10000.0 ** (np.arange(n_freq, dtype=np.float32) / np.float32(n_freq)))).astype(np.float32)
    r = np.repeat(np.arange(rows, dtype=np.float32), 64)
    cc = np.tile(np.arange(64, dtype=np.float32), rows)
    ar = r[:, None] * freq[None, :]
    ac = cc[:, None] * freq[None, :]
    return np.concatenate([np.sin(ar), np.cos(ar), np.sin(ac), np.cos(ac)], axis=-1).astype(np.float32)


class MK:
    def __init__(self, cfg, debug=False):
        self.cfg = cfg
        self.nc = bass.Bass("TRN2", target_bir_lowering=False)
        self.P = Prog(self.nc)
        self.debug = debug
        P = self.P
        L = cfg.depth
        NT = cfg.nt
        din = lambda name, shape, dt=F32: P.dram(name, shape, dt, kind="ExternalInput")
        self.xin = din("xin", [NT, D])
        self.pos = din("pos", [cfg.t_lat, D])
        self.cvec = din("cvec", [2, D])
        self.w_ada = din("w_ada", [L, D, 6 * D])
        self.b_ada = din("b_ada", [L, 6 * D])
        self.norm1_g = din("norm1_g", [L, D])
        self.norm2_g = din("norm2_g", [L, D])
        self.w_in = din("w_in", [L, D, IN_COLS])
        self.ml_conv_w = din("ml_conv_w", [L, 3, 512])
        self.ml_conv_b = din("ml_conv_b", [L, 512])
        self.ml_gate_b = din("ml_gate_b", [L, 16])
        self.ml_norm_g = din("ml_norm_g", [L, 256])
        self.gm_ln_g = din("gm_ln_g", [L, 256])
        self.gm_ln_b = din("gm_ln_b", [L, 256])
        self.gm_w_s = din("gm_w_s", [L, 4, 128, 128])
        self.gm_b_s = din("gm_b_s", [L, 512])
        self.cv_dw_w = din("cv_dw_w", [L, 31, 256])
        self.cv_dw_b = din("cv_dw_b", [L, 256])
        self.cv_ln_g = din("cv_ln_g", [L, 256])
        self.cv_ln_b = din("cv_ln_b", [L, 256])
        self.w_branch = din("w_branch", [L, 4, 256, D])
        self.w_out = din("w_out", [L, D, D])
        self.peer_w_q = din("peer_w_q", [L, D, 2048])
        self.peer_keys = din("peer_keys", [L, 2, 128, 128])
        self.peer_u = din("peer_u", [L, NEXP, D])
        self.peer_v = din("peer_v", [L, NEXP, D])
        self.final_norm_g = din("final_norm_g", [D])
        self.dftc = din("dftc", [cfg.t_lat, cfg.t_lat], BF16)
        self.dfts = din("dfts", [cfg.t_lat, cfg.t_lat], BF16)
        self.cd = din("cd", [256, 512], BF16)
        self.cst_d = din("cst", [128, 1024])
        self.out = P.dram("out", [cfg.t_lat, D], F32, kind="ExternalOutput")
        sk = "ExternalOutput" if debug else "Internal"
        self.xT = P.dram("xT", [KD, 128, NT], F32, kind=sk)
        self.hT = P.dram("hT", [KD, 128, NT], BF16, kind=sk)
        self.Z = P.dram("Z", [4, 256, NT], BF16, kind=sk)
        self.Hf = P.dram("Hf", [cfg.nch, 64, 256], F32, kind=sk)
        self.Hb = P.dram("Hb", [cfg.nch, 64, 256], F32, kind=sk)
        self.UTs = [P.dram("UT%d" % i, [128, 128, KD * 128], BF16) for i in range(2)]
        self.VBs = [P.dram("VB%d" % i, [128, 128, D], BF16) for i in range(2)]
        self.cst = P.sb([128, 1024], F32, "cst")
        P.dma(self.cst.a, self.cst_d.a)
        c = self.cst
        self.identf = c[:, 0:128]
        self.iota128 = c[:, 128:256]
        self.iota16 = c[:, 896:912]
        self.eps = c[:, 912:913]
        self.identb_t = P.sb([128, 128], BF16, "identb")
        P.op('dve', 'tensor_copy', out=self.identb_t.a, in_=self.identf)
        self.identb = self.identb_t.a
        self.onesb_t = P.sb([128, 128], BF16, "onesb")
        P.op('pool', 'memset', ap=self.onesb_t.a, constant=1.0)
        self.onesb = self.onesb_t.a
        self.iota128b_t = P.sb([128, 128], BF16, "iota128b")
        P.op('dve', 'tensor_copy', out=self.iota128b_t.a, in_=self.iota128)
        self.onesf_t = P.sb([128, 128], F32, "onesf")
        P.op('pool', 'memset', ap=self.onesf_t.a, constant=1.0)
        self.onesf = self.onesf_t.a
        self.maskb_t = P.sb([64, 2, 4, 64], BF16, "maskb")
        for d_ in range(2):
            for h in range(4):
                P.op('dve', 'tensor_copy', out=self.maskb_t[:, d_, h, :], in_=c[0:64, 256 + 64 * d_:320 + 64 * d_])
        self.modT = P.sb([128, 48, 2], F32, "modT")
        cT = P.sb([2, D], F32, "cT")
        P.dma(cT.a, self.cvec.a)
        self.sT = P.sb([128, 2, KD], BF16, "sT")
        es = contextlib.ExitStack()
        ps = P.ps([128, 512], F32, es=es)
        for k in range(KD):
            P.op('pe', 'transpose', out=ps[:, 2 * k:2 * k + 2], in_=cT[:, k * 128:(k + 1) * 128], identity=self.identf[0:2, 0:2])
        P.op('act', 'activation', out=self.sT.a, in_=ps[:, 0:16].re("p (k s) -> p s k", s=2), func=AF.Silu)
        es.close()
        P.barrier()

    def load_rows_T(self, rows, es):
        P = self.P
        R = sum(v.ap.shape[0] for v in rows)
        w = rows[0].ap.shape[1]
        assert R <= 128
        rt = P.sb([128, 128], F32, "rows", es=es)
        r0 = 0
        for v in rows:
            r = v.ap.shape[0]
            P.dma(rt[r0:r0 + r, 0:w], v)
            r0 += r
        es_ = contextlib.ExitStack()
        ps = P.ps([128, 512], F32, es=es_)
        P.op('pe', 'transpose', out=ps[0:w, 0:R], in_=rt[0:R, 0:w], identity=self.identf[0:R, 0:R])
        ct = P.sb([128, R], F32, "colsT", es=es)
        P.op('dve', 'tensor_copy', out=ct[0:w, :], in_=ps[0:w, 0:R])
        P.barrier()
        es_.close()
        return ct

    def load_w(self, dst, src, q='pool'):
        self.P.dma(dst, src, q=q)

    def phase_init(self):
        P, cfg = self.P, self.cfg
        es = contextlib.ExitStack()
        xt = Rot([P.sb([128, D], F32, "xt", es=es) for _ in range(2)])
        pt = Rot([P.sb([128, D], F32, "pt", es=es) for _ in range(2)])
        pss = Rot([P.ps([128, 512], F32, es=es) for _ in range(4)])
        st = Rot([P.sb([128, KD, 128], F32, "xst", es=es) for _ in range(2)])
        for ti in range(cfg.nt // 128):
            x = xt.get()
            P.dma(x.a, self.xin[ti * 128:(ti + 1) * 128, :])
            if ti * 128 >= cfg.t_ctx:
                p = pt.get()
                r0 = ti * 128 - cfg.t_ctx
                P.dma(p.a, self.pos[r0:r0 + 128, :])
                P.op('dve', 'tensor_tensor', out=x.a, in0=x.a, in1=p.a, op=ALU.add)
            s = st.get()
            for half in range(2):
                ps = pss.get()
                for j in range(4):
                    k = half * 4 + j
                    P.op('pe', 'transpose', out=ps[:, j * 128:(j + 1) * 128], in_=x[:, k * 128:(k + 1) * 128], identity=self.identf)
                P.op('act', 'activation', out=s[:, half * 4:half * 4 + 4, :], in_=ps.a.re("p (j t) -> p j t", j=4), func=AF.Copy)
            P.dma(self.xT[:, :, ti * 128:(ti + 1) * 128].re("k p t -> p k t"), s.a)
        es.close()
        P.barrier()

    def phase_mod(self, l):
        P = self.P
        es = contextlib.ExitStack()
        wt = Rot([P.sb([128, KD, 1536], BF16, "wada", es=es) for _ in range(2)])
        ps = P.ps([128, 48, 2], F32, es=es)
        bT = self.load_rows_T([self.b_ada[l].re("(j p) -> j p", p=128)], es)
        for cg in range(4):
            w = wt.get()
            for k in range(KD):
                self.load_w(w[:, k, :], self.w_ada[l, k * 128:(k + 1) * 128, cg * 1536:(cg + 1) * 1536])
            for jj in range(12):
                j = cg * 12 + jj
                for k in range(KD):
                    P.mm(ps[:, j, :], w[:, k, jj * 128:(jj + 1) * 128], self.sT[:, :, k], start=(k == 0), stop=(k == KD - 1))
        P.op('dve', 'tensor_tensor', out=self.modT.a, in0=ps.a, in1=bT.a.unsq(2).bc([128, 48, 2]), op=ALU.add)
        es.close()
        P.barrier()

    def mod_scale_shift(self, l, which, es):
        P = self.P
        g = self.norm1_g if which == 0 else self.norm2_g
        gT = self.load_rows_T([g[l].re("(j p) -> j p", p=128)], es)
        base = 0 if which == 0 else 24
        gs = P.sb([128, KD, 2], F32, "gs", es=es)
        P.op('dve', 'tensor_scalar', out=gs.a, in0=self.modT[:, base + 8:base + 16, :], scalar1=1.0, scalar2=None, op0=ALU.add)
        P.op('dve', 'tensor_tensor', out=gs.a, in0=gs.a, in1=gT.a.unsq(2).bc([128, KD, 2]), op=ALU.mult)
        return gs, self.modT[:, base:base + 8, :], self.modT[:, base + 16:base + 24, :]

    def norm_group(self, xg, n, gs, sh, seg, hout, pss, tmps, sq, rsb):
        P = self.P
        P.op('act', 'activation', out=sq[:, :, 0:n], in_=xg, func=AF.Square)
        ps = pss.get()
        for k in range(KD):
            P.mm(ps[:, 0:n], self.onesb, sq[:, k, 0:n], start=(k == 0), stop=(k == KD - 1))
        rs = rsb
        P.op('act', 'activation', out=rs[:, 0:n], in_=ps[:, 0:n], func=AF.Sqrt, scale=1.0 / D, bias=self.eps)
        P.op('dve', 'reciprocal', out=rs[:, 0:n], in_=rs[:, 0:n])
        for k in range(KD):
            t = tmps.get()
            P.op('dve', 'scalar_tensor_tensor', out=t[:, 0:n], in0=xg[:, k, :], scalar=gs[:, k, seg:seg + 1], in1=rs[:, 0:n], op0=ALU.mult, op1=ALU.mult)
            if sh is None:
                P.op('act', 'activation', out=hout[:, k, :], in_=t[:, 0:n], func=AF.Copy)
            else:
                P.op('act', 'activation', out=hout[:, k, :], in_=t[:, 0:n], func=AF.Identity, bias=sh[:, k, seg:seg + 1])

    def phase_norm1(self, l):
        P, cfg = self.P, self.cfg
        es = contextlib.ExitStack()
        gs, sh, _ = self.mod_scale_shift(l, 0, es)
        xg = Rot([P.sb([128, KD, 512], F32, "xg", es=es) for _ in range(2)])
        hg = Rot([P.sb([128, KD, 512], BF16, "hg", es=es) for _ in range(2)])
        sq = P.sb([128, KD, 512], BF16, "sq", es=es)
        pss = Rot([P.ps([128, 512], F32, es=es) for _ in range(2)])
        tmps = Rot([P.sb([128, 512], F32, "nt", es=es) for _ in range(4)])
        rsb = P.sb([128, 512], F32, "rsb", es=es)
        for (n0, n, seg) in cfg.groups:
            x = xg.get()
            h = hg.get()
            P.dma(x[:, :, 0:n], self.xT[:, :, n0:n0 + n].re("k p t -> p k t"))
            self.norm_group(x[:, :, 0:n], n, gs, sh, seg, h[:, :, 0:n], pss, tmps, sq, rsb)
            P.dma(self.hT[:, :, n0:n0 + n].re("k p t -> p k t"), h[:, :, 0:n], q='pool')
        es.close()
        P.barrier()


def _gm(self, l):
    P, cfg = self.P, self.cfg
    es = contextlib.ExitStack()
    W = P.sb([128, KD, 512], BF16, "wgm", es=es)
    for k in range(KD):
        self.load_w(W[:, k, :], self.w_in[l, k * 128:(k + 1) * 128, GM_OFF:GM_OFF + 512])
    lng = P.sb([128, 256], F32, "lng", es=es)
    lnb = P.sb([128, 256], F32, "lnb", es=es)
    P.dma(lng.a, self.gm_ln_g[l:l + 1, :].pbc(128))
    P.dma(lnb.a, self.gm_ln_b[l:l + 1, :].pbc(128))
    bsr = P.sb([1, 512], F32, "bsr", es=es)
    P.dma(bsr.a, self.gm_b_s[l:l + 1, :])
    wsT = P.sb([128, 4, 128], BF16, "wsT", es=es)
    wsl = P.sb([128, 4, 128], BF16, "wsl", es=es)
    self.load_w(wsl.a, self.gm_w_s[l].re("g t s -> t g s"))
    pst = P.ps([128, 4, 128], BF16, es=es)
    for g in range(4):
        P.op('pe', 'transpose', out=pst[:, g, :], in_=wsl[:, g, :], identity=self.identb)
    P.op('dve', 'tensor_copy', out=wsT.a, in_=pst.a)
    hg = Rot([P.sb([128, KD, 512], BF16, "hg", es=es) for _ in range(2)])
    u64 = Rot([P.sb([64, 4, 512], BF16, "u64", es=es) for _ in range(2)])
    zst = Rot([P.sb([64, 4, 512], BF16, "zst", es=es) for _ in range(2)])
    psu = Rot([P.ps([128, 512], F32, es=es) for _ in range(1)])
    psv = Rot([P.ps([128, 512], F32, es=es) for _ in range(4)])
    pss = Rot([P.ps([64, 4, 128], F32, es=es) for _ in range(2)])
    vt = Rot([P.sb([128, 256], F32, "vt", es=es) for _ in range(4)])
    vn = Rot([P.sb([128, 256], BF16, "vn", es=es) for _ in range(4)])
    st6 = Rot([P.sb([128, 8], F32, "st6", es=es) for _ in range(4)])
    for (n0, n, seg) in cfg.groups:
        if self.skip_ctx and seg == 1:
            continue
        h = hg.get()
        P.dma(h[:, :, 0:n], self.hT[:, :, n0:n0 + n].re("k p t -> p k t"))
        u = u64.get()
        for g in range(4):
            ps = psu.get()
            for k in range(KD):
                P.mm(ps[0:64, 0:n], W[:, k, g * 64:(g + 1) * 64], h[:, k, 0:n], start=(k == 0), stop=(k == KD - 1))
            P.op('act', 'activation', out=u[:, g, 0:n], in_=ps[0:64, 0:n], func=AF.Gelu_apprx_tanh)
        z = zst.get()
        subs = list(range(n // 128))
        pv_ = [psv.get() for _ in subs]
        v_ = [vt.get() for _ in subs]
        s6_ = [st6.get() for _ in subs]
        vb_ = [vn.get() for _ in subs]
        for sub in subs:
            for k in range(KD):
                P.mm(pv_[sub][:, 0:256], h[:, k, sub * 128:(sub + 1) * 128], W[:, k, 256:512], start=(k == 0), stop=(k == KD - 1))
        for sub in subs:
            P.op('act', 'activation', out=v_[sub].a, in_=pv_[sub][:, 0:256], func=AF.Gelu_apprx_tanh)
        for sub in subs:
            P.op('dve', 'bn_stats', out=s6_[sub][:, 0:6], in_=v_[sub].a)
        for sub in subs:
            P.op('dve', 'bn_aggr', out=s6_[sub][:, 6:8], in_=s6_[sub][:, 0:6])
        for sub in subs:
            P.op('act', 'activation', out=s6_[sub][:, 7:8], in_=s6_[sub][:, 7:8], func=AF.Sqrt, bias=self.eps)
        for sub in subs:
            P.op('dve', 'reciprocal', out=s6_[sub][:, 7:8], in_=s6_[sub][:, 7:8])
        for sub in subs:
            P.op('dve', 'tensor_scalar', out=v_[sub].a, in0=v_[sub].a, scalar1=s6_[sub][:, 6:7], scalar2=s6_[sub][:, 7:8], op0=ALU.subtract, op1=ALU.mult)
        for sub in subs:
            P.op('dve', 'tensor_tensor', out=v_[sub].a, in0=v_[sub].a, in1=lng.a, op=ALU.mult)
        for sub in subs:
            P.op('dve', 'tensor_tensor', out=vb_[sub].a, in0=v_[sub].a, in1=lnb.a, op=ALU.add)
        for sub in subs:
            pg = pss.get()
            for g in range(4):
                P.mm(pg[:, g, :], vb_[sub][:, g * 64:(g + 1) * 64], wsT[:, g, :], start=True, stop=False)
                P.mm(pg[:, g, :], self.onesf[0:1, 0:64], bsr[0:1, g * 128:(g + 1) * 128], start=False, stop=True)
            P.op('dve', 'tensor_tensor', out=z[:, :, sub * 128:(sub + 1) * 128], in0=pg.a, in1=u[:, :, sub * 128:(sub + 1) * 128], op=ALU.mult)
        P.dma(self.Z[1, :, n0:n0 + n].re("(g p) t -> p g t", p=64), z[:, :, 0:n], q='pool')
    es.close()
    P.barrier()


MK.phase_gmlp = _gm


def _cv(self, l):
    P, cfg = self.P, self.cfg
    es = contextlib.ExitStack()
    PAD = 15
    W = P.sb([128, KD, 512], BF16, "wcv", es=es)
    for k in range(KD):
        self.load_w(W[:, k, :], self.w_in[l, k * 128:(k + 1) * 128, CV_OFF:CV_OFF + 512])
    cols = self.load_rows_T([self.cv_dw_w[l].re("j (c p) -> (j c) p", p=128), self.cv_dw_b[l].re("(c p) -> c p", p=128),
                             self.cv_ln_g[l].re("(c p) -> c p", p=128), self.cv_ln_b[l].re("(c p) -> c p", p=128)], es)
    diag = P.sb([128, 62, 128], BF16, "diag", es=es)
    for jc in range(62):
        P.op('dve', 'tensor_scalar', out=diag[:, jc, :], in0=self.identf, scalar1=cols[:, jc:jc + 1], scalar2=None, op0=ALU.mult)
    hg = Rot([P.sb([128, KD, 512], BF16, "hg", es=es) for _ in range(2)])
    psa = Rot([P.ps([128, 512], F32, es=es) for _ in range(2)])
    psb = Rot([P.ps([128, 512], F32, es=es) for _ in range(2)])
    psc = Rot([P.ps([128, 512], F32, es=es) for _ in range(2)])
    sg = Rot([P.sb([128, 512], F32, "sg", es=es) for _ in range(2)])
    for (s0, sn, seg) in cfg.segs:
        if self.skip_ctx and seg == 1:
            continue
        es2 = contextlib.ExitStack()
        zp = P.sb([128, 2, sn + 2 * PAD], BF16, "zp", es=es2)
        P.op('pool', 'memset', ap=zp.a, constant=0.0)
        y = P.sb([128, 2, 512], F32, "ycv", es=es2)
        y2 = P.sb([128, 2, 512], F32, "ycv2", es=es2)
        mean = P.sb([128, 512], F32, "mean", es=es2)
        rstd = P.sb([128, 512], F32, "rstd", es=es2)
        zo = Rot([P.sb([128, 2, 512], BF16, "zo", es=es2) for _ in range(2)])
        grp = [(a, n) for (a, n, sg_) in cfg.groups if sg_ == seg]
        for (n0, n) in grp:
            h = hg.get()
            P.dma(h[:, :, 0:n], self.hT[:, :, n0:n0 + n].re("k p t -> p k t"))
            for c in range(2):
                pa = psa.get()
                pb = psb.get()
                for k in range(KD):
                    P.mm(pa[:, 0:n], W[:, k, c * 128:(c + 1) * 128], h[:, k, 0:n], start=(k == 0), stop=(k == KD - 1))
                for k in range(KD):
                    P.mm(pb[:, 0:n], W[:, k, 256 + c * 128:256 + (c + 1) * 128], h[:, k, 0:n], start=(k == 0), stop=(k == KD - 1))
                s = sg.get()
                P.op('act', 'activation', out=s[:, 0:n], in_=pb[:, 0:n], func=AF.Sigmoid)
                o0 = PAD + n0 - s0
                P.op('dve', 'tensor_tensor', out=zp[:, c, o0:o0 + n], in0=pa[:, 0:n], in1=s[:, 0:n], op=ALU.mult)
        for (n0, n) in grp:
            o0 = n0 - s0
            for c in range(2):
                pc = psc.get()
                for j in range(31):
                    P.mm(pc[:, 0:n], diag[:, j * 2 + c, :], zp[:, c, o0 + j:o0 + j + n], start=(j == 0), stop=(j == 30))
                P.op('act', 'activation', out=y[:, c, 0:n], in_=pc[:, 0:n], func=AF.Identity, bias=cols[:, 62 + c:63 + c])
                P.op('act', 'activation', out=y2[:, c, 0:n], in_=y[:, c, 0:n], func=AF.Square)
            p1 = psa.get()
            p2 = psb.get()
            for c in range(2):
                P.mm(p1[:, 0:n], self.onesf, y[:, c, 0:n], start=(c == 0), stop=(c == 1))
            for c in range(2):
                P.mm(p2[:, 0:n], self.onesf, y2[:, c, 0:n], start=(c == 0), stop=(c == 1))
            P.op('act', 'activation', out=mean[:, 0:n], in_=p1[:, 0:n], func=AF.Identity, scale=1.0 / 256)
            P.op('dve', 'tensor_tensor', out=rstd[:, 0:n], in0=mean[:, 0:n], in1=mean[:, 0:n], op=ALU.mult)
            P.op('dve', 'scalar_tensor_tensor', out=rstd[:, 0:n], in0=p2[:, 0:n], scalar=1.0 / 256, in1=rstd[:, 0:n], op0=ALU.mult, op1=ALU.subtract)
            P.op('act', 'activation', out=rstd[:, 0:n], in_=rstd[:, 0:n], func=AF.Sqrt, bias=self.eps)
            P.op('dve', 'reciprocal', out=rstd[:, 0:n], in_=rstd[:, 0:n])
            z = zo.get()
            for c in range(2):
                P.op('dve', 'tensor_tensor', out=y[:, c, 0:n], in0=y[:, c, 0:n], in1=mean[:, 0:n], op=ALU.subtract)
                P.op('dve', 'tensor_tensor', out=y[:, c, 0:n], in0=y[:, c, 0:n], in1=rstd[:, 0:n], op=ALU.mult)
                P.op('act', 'activation', out=z[:, c, 0:n], in_=y[:, c, 0:n], func=AF.Silu, scale=cols[:, 64 + c:65 + c], bias=cols[:, 66 + c:67 + c])
            P.dma(self.Z[2, :, n0:n0 + n].re("(c p) t -> p c t", p=128), z[:, :, 0:n], q='pool')
        es2.close()
        P.barrier()
    es.close()
    P.barrier()


MK.phase_conv = _cv


def _ft(self, l):
    P, cfg = self.P, self.cfg
    es = contextlib.ExitStack()
    W = P.sb([128, KD, 256], BF16, "wft", es=es)
    for k in range(KD):
        self.load_w(W[:, k, :], self.w_in[l, k * 128:(k + 1) * 128, FT_OFF:FT_OFF + 256])
    CD = P.sb([128, 2, 512], BF16, "cdt", es=es)
    P.dma(CD.a, self.cd.a.re("(c p) n -> p c n", p=128))
    hg = Rot([P.sb([128, KD, 512], BF16, "hg", es=es) for _ in range(2)])
    zf = Rot([P.sb([128, 2, 512], BF16, "zf", es=es) for _ in range(2)])
    psa = Rot([P.ps([128, 512], F32, es=es) for _ in range(2)])
    psd = Rot([P.ps([128, 512], F32, es=es) for _ in range(4)])
    for (s0, sn, seg) in cfg.segs:
        if self.skip_ctx and seg == 1:
            continue
        es2 = contextlib.ExitStack()
        ntile = sn // 128
        zcs = P.sb([128, ntile, 512], BF16, "zcs", es=es2)
        grp = [(a, n) for (a, n, sg_) in cfg.groups if sg_ == seg]
        for (n0, n) in grp:
            h = hg.get()
            P.dma(h[:, :, 0:n], self.hT[:, :, n0:n0 + n].re("k p t -> p k t"))
            z = zf.get()
            for c in range(2):
                pa = psa.get()
                for k in range(KD):
                    P.mm(pa[:, 0:n], W[:, k, c * 128:(c + 1) * 128], h[:, k, 0:n], start=(k == 0), stop=(k == KD - 1))
                P.op('act', 'activation', out=z[:, c, 0:n], in_=pa[:, 0:n], func=AF.Copy)
            for sub in range(n // 128):
                pd = psd.get()
                for c in range(2):
                    P.mm(pd.a, z[:, c, sub * 128:(sub + 1) * 128], CD[:, c, :], start=(c == 0), stop=(c == 1))
                ti = (n0 - s0) // 128 + sub
                P.op('dve', 'tensor_copy', out=zcs[:, ti, :], in_=pd.a)
        kb_n = min(512, sn)
        nkb = sn // kb_n
        tcs = Rot([P.sb([128, ntile, kb_n], BF16, "tc", es=es2) for _ in range(2)])
        tss = Rot([P.sb([128, ntile, kb_n], BF16, "ts", es=es2) for _ in range(2)])
        zo = Rot([P.sb([128, 2, 512], BF16, "zo", es=es2) for _ in range(2)])
        rstride = cfg.t_lat // sn
        for kb in range(nkb):
            tc_ = tcs.get()
            ts_ = tss.get()
            if rstride == 1:
                P.dma(tc_.a, self.dftc[:, kb * kb_n:(kb + 1) * kb_n].re("(t p) n -> p t n", p=128))
                P.dma(ts_.a, self.dfts[:, kb * kb_n:(kb + 1) * kb_n].re("(t p) n -> p t n", p=128))
            else:
                P.dma(tc_.a, self.dftc.a.re("(r s) n -> r s n", s=rstride)[:, 0, kb * kb_n:(kb + 1) * kb_n].re("(t p) n -> p t n", p=128))
                P.dma(ts_.a, self.dfts.a.re("(r s) n -> r s n", s=rstride)[:, 0, kb * kb_n:(kb + 1) * kb_n].re("(t p) n -> p t n", p=128))
            z = zo.get()
            for c in range(2):
                pd = psd.get()
                for ti in range(ntile):
                    P.mm(pd[:, 0:kb_n], zcs[:, ti, c * 128:(c + 1) * 128], tc_[:, ti, :], start=(ti == 0), stop=False)
                    P.mm(pd[:, 0:kb_n], zcs[:, ti, 256 + c * 128:256 + (c + 1) * 128], ts_[:, ti, :], start=False, stop=(ti == ntile - 1))
                P.op('act', 'activation', out=z[:, c, 0:kb_n], in_=pd[:, 0:kb_n], func=AF.Identity, scale=float(np.sqrt(rstride)))
            P.dma(self.Z[3, :, s0 + kb * kb_n:s0 + (kb + 1) * kb_n].re("(c p) t -> p c t", p=128), z[:, :, 0:kb_n], q='pool')
        es2.close()
        P.barrier()
    es.close()
    P.barrier()


MK.phase_fnet = _ft


def _merge(self, l):
    P, cfg = self.P, self.cfg
    es = contextlib.ExitStack()
    Wg = P.sb([128, KD, 4096], BF16, "wgate", es=es)
    for k in range(KD):
        for i in range(4):
            self.load_w(Wg[:, k, i * 1024:(i + 1) * 1024], self.w_in[l, k * 128:(k + 1) * 128, GATE_OFF + i * 1024:GATE_OFF + (i + 1) * 1024])
    Wb = P.sb([128, 4, 2, 1024], BF16, "wbr", es=es)
    for i in range(4):
        for c in range(2):
            self.load_w(Wb[:, i, c, :], self.w_branch[l, i, c * 128:(c + 1) * 128, :])
    Wo = P.sb([128, KD, 1024], BF16, "wout", es=es)
    for k in range(KD):
        self.load_w(Wo[:, k, :], self.w_out[l, k * 128:(k + 1) * 128, :])
    gate1 = self.modT[:, 16:24, :]
    hg = Rot([P.sb([128, KD, 512], BF16, "hg", es=es) for _ in range(2)])
    zg = Rot([P.sb([128, 4, 2, 512], BF16, "zg", es=es) for _ in range(2)])
    xg = Rot([P.sb([128, KD, 512], F32, "xg", es=es) for _ in range(2)])
    yT = P.sb([128, KD, 512], BF16, "yT", es=es)
    psg = Rot([P.ps([128, 512], F32, es=es) for _ in range(3)])
    psl = Rot([P.ps([128, 512], F32, es=es) for _ in range(3)])
    pso = Rot([P.ps([128, 512], F32, es=es) for _ in range(2)])
    sg = Rot([P.sb([128, 512], F32, "sg", es=es) for _ in range(3)])
    acc = Rot([P.sb([128, 512], F32, "acc", es=es) for _ in range(2)])
    for (n0, n, seg) in cfg.groups:
        if self.skip_ctx and seg == 1:
            continue
        h = hg.get()
        P.dma(h[:, :, 0:n], self.hT[:, :, n0:n0 + n].re("k p t -> p k t"))
        z = zg.get()
        for i in range(4):
            P.dma(z[:, i, :, 0:n], self.Z[i, :, n0:n0 + n].re("(c p) t -> p c t", p=128))
        x = xg.get()
        P.dma(x[:, :, 0:n], self.xT[:, :, n0:n0 + n].re("k p t -> p k t"))
        for dc in range(KD):
            a = acc.get()
            for i in range(4):
                pg = psg.get()
                for k in range(KD):
                    P.mm(pg[:, 0:n], Wg[:, k, i * 1024 + dc * 128:i * 1024 + (dc + 1) * 128], h[:, k, 0:n], start=(k == 0), stop=(k == KD - 1))
                s = sg.get()
                P.op('act', 'activation', out=s[:, 0:n], in_=pg[:, 0:n], func=AF.Sigmoid)
                pl = psl.get()
                for c in range(2):
                    P.mm(pl[:, 0:n], Wb[:, i, c, dc * 128:(dc + 1) * 128], z[:, i, c, 0:n], start=(c == 0), stop=(c == 1))
                if i == 0:
                    P.op('dve', 'tensor_tensor', out=a[:, 0:n], in0=pl[:, 0:n], in1=s[:, 0:n], op=ALU.mult)
                else:
                    P.op('dve', 'tensor_tensor', out=s[:, 0:n], in0=pl[:, 0:n], in1=s[:, 0:n], op=ALU.mult)
                    if i < 3:
                        P.op(self.merge_eng, 'tensor_tensor', out=a[:, 0:n], in0=a[:, 0:n], in1=s[:, 0:n], op=ALU.add)
                    else:
                        P.op(self.merge_eng, 'tensor_tensor', out=yT[:, dc, 0:n], in0=a[:, 0:n], in1=s[:, 0:n], op=ALU.add)
        for dc in range(KD):
            po = pso.get()
            for k in range(KD):
                P.mm(po[:, 0:n], Wo[:, k, dc * 128:(dc + 1) * 128], yT[:, k, 0:n], start=(k == 0), stop=(k == KD - 1))
            P.op('dve', 'scalar_tensor_tensor', out=x[:, dc, 0:n], in0=po[:, 0:n], scalar=gate1[:, dc, seg:seg + 1], in1=x[:, dc, 0:n], op0=ALU.mult, op1=ALU.add)
        P.dma(self.xT[:, :, n0:n0 + n].re("k p t -> p k t"), x[:, :, 0:n], q='pool')
    es.close()
    P.barrier()


MK.phase_merge = _merge


def _final(self):
    P, cfg = self.P, self.cfg
    es = contextlib.ExitStack()
    gT = self.load_rows_T([self.final_norm_g.a.re("(j p) -> j p", p=128)], es)
    gs = P.sb([128, KD, 2], F32, "gsf", es=es)
    P.op('dve', 'tensor_copy', out=gs.a, in_=gT.a.unsq(2).bc([128, KD, 2]))
    xg = Rot([P.sb([128, KD, 512], F32, "xg", es=es) for _ in range(2)])
    yg = Rot([P.sb([128, KD, 512], F32, "yg", es=es) for _ in range(2)])
    sq = P.sb([128, KD, 512], BF16, "sq", es=es)
    pss = Rot([P.ps([128, 512], F32, es=es) for _ in range(2)])
    pst = Rot([P.ps([128, 512], F32, es=es) for _ in range(4)])
    tmps = Rot([P.sb([128, 512], F32, "nt", es=es) for _ in range(4)])
    rsb = P.sb([128, 512], F32, "rsb", es=es)
    ot = Rot([P.sb([128, D], F32, "ot", es=es) for _ in range(3)])
    for (n0, n, seg) in cfg.groups:
        if seg == 1:
            continue
        x = xg.get()
        y = yg.get()
        P.dma(x[:, :, 0:n], self.xT[:, :, n0:n0 + n].re("k p t -> p k t"))
        self.norm_group(x[:, :, 0:n], n, gs, None, 0, y[:, :, 0:n], pss, tmps, sq, rsb)
        for sub in range(n // 128):
            o = ot.get()
            for half in range(2):
                ps = pst.get()
                for j in range(4):
                    k = half * 4 + j
                    P.op('pe', 'transpose', out=ps[:, j * 128:(j + 1) * 128], in_=y[:, k, sub * 128:(sub + 1) * 128], identity=self.identf)
                P.op('act', 'activation', out=o[:, half * 512:(half + 1) * 512], in_=ps.a, func=AF.Copy)
            r0 = n0 - cfg.t_ctx + sub * 128
            P.dma(self.out[r0:r0 + 128, :], o.a, q='pool')
    es.close()
    P.barrier()


MK.phase_final = _final


def _ml(self, l):
    P, cfg = self.P, self.cfg
    NT, NCH = cfg.nt, cfg.nch
    nctx = cfg.t_ctx // 64
    order_b = list(range(nctx - 1, -1, -1)) + list(range(NCH - 1, nctx - 1, -1))
    order_f = list(range(NCH))
    es = contextlib.ExitStack()
    biasA = P.sb([64, 1], F32, "biasA", es=es)
    biasB = P.sb([64, 1], F32, "biasB", es=es)
    P.op('pool', 'memset', ap=biasA.a, constant=0.0)
    P.op('pool', 'memset', ap=biasB.a, constant=0.0)
    gb = self.ml_gate_b
    P.dma(biasA[0:4, :], gb[l, 0:4].re("(a b) -> a b", b=1))
    P.dma(biasA[32:36, :], gb[l, 4:8].re("(a b) -> a b", b=1))
    P.dma(biasB[0:4, :], gb[l, 8:12].re("(a b) -> a b", b=1))
    P.dma(biasB[32:36, :], gb[l, 12:16].re("(a b) -> a b", b=1))
    cols = self.load_rows_T([self.ml_conv_w[l].re("j (c p) -> (j c) p", p=128), self.ml_conv_b[l].re("(c p) -> c p", p=128)], es)
    colsB = self.load_rows_T([self.ml_conv_b[l].re("(c p) -> c p", p=64)], es)
    diag = P.sb([128, 12, 128], BF16, "diag3", es=es)
    for jc in range(12):
        P.op('dve', 'tensor_scalar', out=diag[:, jc, :], in0=self.identf, scalar1=cols[:, jc:jc + 1], scalar2=None, op0=ALU.mult)
    qk8 = [P.sb([64, NT], BF16, "qk8_%d" % i, es=es) for i in range(8)]
    Atok = P.sb([64, NCH, 8], F32, "Atok", es=es)
    Btok = P.sb([64, NCH, 8], F32, "Btok", es=es)
    Ctok = P.sb([64, NCH, 8], F32, "Ctok", es=es)
    decb = P.sb([64, 8, NCH], F32, "decb", es=es)
    emb = P.sb([64, 8, NCH], F32, "emb", es=es)
    es1 = contextlib.ExitStack()
    Wqk = P.sb([128, KD, 512], BF16, "wqk", es=es1)
    for k in range(KD):
        self.load_w(Wqk[:, k, :], self.w_in[l, k * 128:(k + 1) * 128, 0:512])
    pre = [P.sb([128, NT + 4], BF16, "pre%d" % i, es=es1) for i in range(4)]
    for i in range(4):
        P.op('pool', 'memset', ap=pre[i].a, constant=0.0)
    hg = Rot([P.sb([128, KD, 512], BF16, "hg", es=es1) for _ in range(2)])
    psQ = Rot([P.ps([128, 512], F32, es=es1) for _ in range(2)])

    def poff(seg):
        return 1 if seg == 1 else cfg.t_ctx + 3

    for (n0, n, seg) in cfg.groups:
        s0 = 0 if seg == 1 else cfg.t_ctx
        h = hg.get()
        P.dma(h[:, :, 0:n], self.hT[:, :, n0:n0 + n].re("k p t -> p k t"))
        for ci in range(4):
            pq = psQ.get()
            for k in range(KD):
                P.mm(pq[:, 0:n], Wqk[:, k, ci * 128:(ci + 1) * 128], h[:, k, 0:n], start=(k == 0), stop=(k == KD - 1))
            o0 = poff(seg) + n0 - s0
            P.op('dve', 'tensor_copy', out=pre[ci][:, o0:o0 + n], in_=pq[:, 0:n])
    for (n0, n, seg) in cfg.groups:
        s0 = 0 if seg == 1 else cfg.t_ctx
        for ci in range(4):
            for hp in range(2):
                pq = psQ.get()
                o0 = poff(seg) + n0 - s0 - 1
                for j in range(3):
                    P.mm(pq[0:64, 0:n], diag[:, j * 4 + ci, hp * 64:(hp + 1) * 64], pre[ci][:, o0 + j:o0 + j + n], start=(j == 0), stop=(j == 2))
                P.op('act', 'activation', out=qk8[ci * 2 + hp][:, n0:n0 + n], in_=pq[0:64, 0:n], func=AF.Silu, bias=colsB[0:64, ci * 2 + hp:ci * 2 + hp + 1])
    for h8 in range(4, 8):
        P.op('dve', 'tensor_scalar', out=qk8[h8].a, in0=qk8[h8].a, scalar1=0.125, scalar2=None, op0=ALU.mult)
    es1.close()
    P.barrier()
    esA = contextlib.ExitStack()
    LI = P.sb([64, NT], F32, "LI", es=esA)
    LF = P.sb([64, NT], F32, "LF", es=esA)
    es1 = contextlib.ExitStack()
    WgA = P.sb([128, KD, 64], BF16, "wga", es=es1)
    WgB = P.sb([128, KD, 64], BF16, "wgb", es=es1)
    P.op('pool', 'memset', ap=WgA.a, constant=0.0)
    P.op('pool', 'memset', ap=WgB.a, constant=0.0)
    for k in range(KD):
        rows = slice(k * 128, (k + 1) * 128)
        self.load_w(WgA[:, k, 0:4], self.w_in[l, rows, 768:772])
        self.load_w(WgA[:, k, 32:36], self.w_in[l, rows, 772:776])
        self.load_w(WgB[:, k, 0:4], self.w_in[l, rows, 776:780])
        self.load_w(WgB[:, k, 32:36], self.w_in[l, rows, 780:784])
    hg = Rot([P.sb([128, KD, 512], BF16, "hg", es=es1) for _ in range(2)])
    psA = Rot([P.ps([128, 512], F32, es=es1) for _ in range(2)])
    sgt = Rot([P.sb([64, 512], F32, "sgt", es=es1) for _ in range(2)])
    for (n0, n, seg) in cfg.groups:
        h = hg.get()
        P.dma(h[:, :, 0:n], self.hT[:, :, n0:n0 + n].re("k p t -> p k t"))
        pa = psA.get()
        for k in range(KD):
            P.mm(pa[0:64, 0:n], WgA[:, k, :], h[:, k, 0:n], start=(k == 0), stop=(k == KD - 1))
        P.op('act', 'activation', out=LI[:, n0:n0 + n], in_=pa[0:64, 0:n], func=AF.Identity, bias=biasA.a)
        pb = psA.get()
        for k in range(KD):
            P.mm(pb[0:64, 0:n], WgB[:, k, :], h[:, k, 0:n], start=(k == 0), stop=(k == KD - 1))
        s = sgt.get()
        P.op('act', 'activation', out=s[:, 0:n], in_=pb[0:64, 0:n], func=AF.Sigmoid, bias=biasB.a)
        P.op('act', 'activation', out=LF[:, n0:n0 + n], in_=s[:, 0:n], func=AF.Ln)
    es1.close()
    P.barrier()
    es1 = contextlib.ExitStack()
    ones = P.sb([64, NT], BF16, "ones", es=es1)
    P.op('pool', 'memset', ap=ones.a, constant=1.0)
    cs = P.sb([64, NT], F32, "cs", es=es1)
    P.op('dve', 'tensor_tensor_scan', out=cs.a, data0=ones.a, data1=LF.a, initial=0.0, op0=ALU.mult, op1=ALU.add)
    j3 = lambda b: b.a.re("p (c j) -> p c j", j=64)
    sm = lambda nm: P.sb([64, NCH], F32, nm, es=es1)
    base, tot, wmax, cmax, m_in, m_new, Mx, dec, em = [sm(x) for x in ("base", "tot", "wmax", "cmax", "m_in", "m_new", "Mx", "dec", "em")]
    bc3 = lambda b: b.a.unsq(2).bc([64, NCH, 64])
    P.op('dve', 'tensor_tensor', out=base.a, in0=j3(cs)[:, :, 0], in1=j3(LF)[:, :, 0], op=ALU.subtract)
    BC = cs
    P.op('dve', 'tensor_tensor', out=j3(BC), in0=j3(cs), in1=bc3(base), op=ALU.subtract)
    P.op('dve', 'tensor_copy', out=tot.a, in_=j3(BC)[:, :, 63])
    P.op('dve', 'tensor_tensor', out=j3(BC)[32:64], in0=bc3(tot)[32:64], in1=j3(BC)[32:64], op=ALU.subtract)
    P.op('dve', 'tensor_tensor', out=BC[32:64, :], in0=BC[32:64, :], in1=LF[32:64, :], op=ALU.add)
    Wt = LF
    Cq = LI
    P.op('dve', 'tensor_tensor', out=Cq.a, in0=LI.a, in1=BC.a, op=ALU.subtract)
    P.op('dve', 'tensor_tensor', out=j3(Wt), in0=j3(Cq), in1=bc3(tot), op=ALU.add)
    P.op('dve', 'tensor_reduce', out=wmax.a, in_=j3(Wt), axis=AX.X, op=ALU.max)
    P.op('dve', 'tensor_reduce', out=cmax.a, in_=j3(Cq), axis=AX.X, op=ALU.max)
    zero = P.sb([64, 1], F32, "zero", es=es1)
    P.op('pool', 'memset', ap=zero.a, constant=0.0)
    P.op('pool', 'memset', ap=m_in.a, constant=0.0)
    for (rows, order) in ((slice(0, 32), order_f), (slice(32, 64), order_b)):
        prev = None
        for i, c in enumerate(order):
            pv_ = zero[rows, 0:1] if prev is None else m_new[rows, prev:prev + 1]
            if prev is not None:
                P.op('dve', 'tensor_copy', out=m_in[rows, c:c + 1], in_=pv_)
            P.op('dve', 'tensor_scalar', out=m_new[rows, c:c + 1], in0=pv_, scalar1=tot[rows, c:c + 1], scalar2=wmax[rows, c:c + 1], op0=ALU.add, op1=ALU.max)
            prev = c
    P.op('dve', 'tensor_tensor', out=Mx.a, in0=m_in.a, in1=cmax.a, op=ALU.max)
    P.op('dve', 'tensor_tensor', out=j3(Cq), in0=j3(Cq), in1=bc3(Mx), op=ALU.subtract)
    P.op('act', 'activation', out=Cq.a, in_=Cq.a, func=AF.Exp)
    P.op('dve', 'tensor_tensor', out=j3(Wt), in0=j3(Wt), in1=bc3(m_new), op=ALU.subtract)
    P.op('act', 'activation', out=Wt.a, in_=Wt.a, func=AF.Exp)
    P.op('dve', 'tensor_tensor', out=j3(BC), in0=j3(BC), in1=bc3(Mx), op=ALU.add)
    P.op('act', 'activation', out=BC.a, in_=BC.a, func=AF.Exp, scale=-1.0)
    P.op('dve', 'tensor_tensor', out=dec.a, in0=tot.a, in1=m_in.a, op=ALU.add)
    P.op('dve', 'tensor_tensor', out=dec.a, in0=dec.a, in1=m_new.a, op=ALU.subtract)
    P.op('act', 'activation', out=dec.a, in_=dec.a, func=AF.Exp)
    P.op('dve', 'tensor_tensor', out=em.a, in0=m_in.a, in1=Mx.a, op=ALU.subtract)
    P.op('act', 'activation', out=em.a, in_=em.a, func=AF.Exp)
    pt = Rot([P.ps([64, 8, 64], F32, es=es1) for _ in range(2)])
    for (src, dst) in ((Cq, Atok), (Wt, Btok), (BC, Ctok)):
        for c0 in range(0, NCH, 8):
            nb = min(8, NCH - c0)
            p_ = pt.get()
            for cc in range(nb):
                P.op('pe', 'transpose', out=p_[:, cc, :], in_=src[:, (c0 + cc) * 64:(c0 + cc + 1) * 64], identity=self.identf[0:64, 0:64])
            P.op('dve', 'tensor_copy', out=dst[:, c0:c0 + nb, 0:4], in_=p_[:, 0:nb, 0:4])
            P.op('dve', 'tensor_copy', out=dst[:, c0:c0 + nb, 4:8], in_=p_[:, 0:nb, 32:36])
    pbq = Rot([P.ps([64, 4, NCH], F32, es=es1) for _ in range(2)])
    for (src, dst) in ((dec, decb), (em, emb)):
        for half in range(2):
            p_ = pbq.get()
            for gg in range(4):
                g = half * 4 + gg
                P.mm(p_[:, gg, :], self.cst[0:64, 384 + g * 64:384 + (g + 1) * 64], src.a, start=True, stop=True)
            P.op('dve', 'tensor_copy', out=dst[:, half * 4:half * 4 + 4, :], in_=p_.a)
    es1.close()
    esA.close()
    P.barrier()
    es3 = contextlib.ExitStack()
    v1 = P.sb([64, NCH, 4, 65], BF16, "v1", es=es3)
    P.op('pool', 'memset', ap=v1.a, constant=1.0)
    ktok = P.sb([64, NCH, 256], BF16, "ktok", es=es3)
    es3a = contextlib.ExitStack()
    Wv = P.sb([128, KD, 256], BF16, "wv", es=es3a)
    for k in range(KD):
        self.load_w(Wv[:, k, :], self.w_in[l, k * 128:(k + 1) * 128, 512:768])
    hg = Rot([P.sb([128, KD, 512], BF16, "hg", es=es3a) for _ in range(2)])
    psV = Rot([P.ps([128, 512], F32, es=es3a) for _ in range(2)])
    pk = Rot([P.ps([64, 256], BF16, es=es3a) for _ in range(2)])
    for (n0, n, seg) in cfg.groups:
        h = hg.get()
        P.dma(h[:, :, 0:n], self.hT[:, :, n0:n0 + n].re("k p t -> p k t"))
        for cc in range(n // 64):
            c = n0 // 64 + cc
            pv = psV.get()
            for k in range(KD):
                P.mm(pv[0:64, 0:256], h[:, k, cc * 64:(cc + 1) * 64], Wv[:, k, :], start=(k == 0), stop=(k == KD - 1))
            P.op('act', 'activation', out=v1[:, c, :, 0:64], in_=pv[0:64, 0:256].re("p (h d) -> p h d", h=4), func=AF.Copy)
    for c in range(NCH):
        p_ = pk.get()
        for hh in range(4):
            P.op('pe', 'transpose', out=p_[:, hh * 64:(hh + 1) * 64], in_=qk8[4 + hh][:, c * 64:(c + 1) * 64], identity=self.identb[0:64, 0:64])
        P.op('act', 'activation', out=ktok[:, c, :], in_=p_.a, func=AF.Copy)
    es3a.close()
    P.barrier()
    PS1 = [P.ps([64, 4, 64], F32, es=es3) for _ in range(2)]
    PS2 = [P.ps([64, 4, 65], F32, es=es3) for _ in range(2)]
    PS3 = [P.ps([64, 4, 65], F32, es=es3) for _ in range(2)]
    Cst = [P.sb([64, 4, 65], F32, "Cst", es=es3) for _ in range(2)]
    CnS = [P.sb([64, 4, 65], BF16, "CnS", es=es3) for _ in range(2)]
    for d_ in range(2):
        P.op('pool', 'memset', ap=Cst[d_].a, constant=0.0)
        P.op('pool', 'memset', ap=CnS[d_].a, constant=0.0)
    tmp = [Rot([P.sb([64, 4, 64], F32, "stmp", es=es3) for _ in range(2)]) for _ in range(2)]
    SD = [Rot([P.sb([64, 4, 64], BF16, "SD", es=es3) for _ in range(2)]) for _ in range(2)]
    kw = [Rot([P.sb([64, 4, 64], BF16, "kw", es=es3) for _ in range(2)]) for _ in range(2)]
    dn = [Rot([P.sb([64, 8], F32, "dn", es=es3) for _ in range(2)]) for _ in range(2)]
    hb = [Rot([P.sb([64, 4, 64], F32, "hbuf", es=es3) for _ in range(3)]) for _ in range(2)]
    Hd = [self.Hf, self.Hb]
    orders = [order_f, order_b]
    prep_lst = []
    prep_es = contextlib.ExitStack()
    if self.prep_in_mlstm:
        prep_lst = self.phase_peer_prep(l, es=prep_es, defer=True) or []
    per_step = (len(prep_lst) + NCH - 1) // NCH + 1
    for i in range(NCH):
        cs_ = [orders[d_][i] for d_ in range(2)]
        tks = [slice(c * 64, (c + 1) * 64) for c in cs_]
        qh = [[qk8[hh][:, tks[d_]] for hh in range(4)] for d_ in range(2)]
        kh = [[qk8[4 + hh][:, tks[d_]] for hh in range(4)] for d_ in range(2)]
        for d_ in range(2):
            for hh in range(4):
                P.mm(PS1[d_][:, hh, :], kh[d_][hh], qh[d_][hh])
        t_ = [tmp[d_].get() for d_ in range(2)]
        for d_ in range(2):
            P.op('dve', 'tensor_tensor', out=t_[d_].a, in0=PS1[d_].a, in1=Atok[:, cs_[d_], 4 * d_:4 * d_ + 4].unsq(2).bc([64, 4, 64]), op=ALU.mult)
        sd = [SD[d_].get() for d_ in range(2)]
        for d_ in range(2):
            P.op(self.scan_eng, 'tensor_tensor', out=sd[d_].a, in0=t_[d_].a, in1=self.maskb_t[:, d_, :, :], op=ALU.mult)
        kw_ = [kw[d_].get() for d_ in range(2)]
        for d_ in range(2):
            P.op(self.scan_eng, 'tensor_tensor', out=kw_[d_].a, in0=ktok[:, cs_[d_], :].re("p (h d) -> p h d", h=4), in1=Btok[:, cs_[d_], 4 * d_:4 * d_ + 4].unsq(2).bc([64, 4, 64]), op=ALU.mult)
        for d_ in range(2):
            for hh in range(4):
                P.mm(PS2[d_][:, hh, :], qh[d_][hh], CnS[d_][:, hh, :], start=True, stop=False)
                P.mm(PS2[d_][:, hh, :], sd[d_][:, hh, :], v1[:, cs_[d_], hh, :], start=False, stop=True)
        for d_ in range(2):
            for hh in range(4):
                P.mm(PS3[d_][:, hh, :], kw_[d_][:, hh, :], v1[:, cs_[d_], hh, :])
        for d_ in range(2):
            P.op(self.scan_eng, 'tensor_tensor', out=Cst[d_].a, in0=Cst[d_].a, in1=decb[:, 4 * d_:4 * d_ + 4, cs_[d_]].unsq(2).bc([64, 4, 65]), op=ALU.mult)
        for d_ in range(2):
            P.op('dve', 'tensor_tensor', out=Cst[d_].a, in0=Cst[d_].a, in1=PS3[d_].a, op=ALU.add)
        if i + 1 < NCH:
            for d_ in range(2):
                cn = orders[d_][i + 1]
                P.op(self.scan_eng, 'tensor_tensor', out=CnS[d_].a, in0=Cst[d_].a, in1=emb[:, 4 * d_:4 * d_ + 4, cn].unsq(2).bc([64, 4, 65]), op=ALU.mult)
        dd = [dn[d_].get() for d_ in range(2)]
        for d_ in range(2):
            P.op('act', 'activation', out=dd[d_][:, 0:4], in_=PS2[d_][:, :, 64], func=AF.Abs)
        for d_ in range(2):
            P.op('dve', 'tensor_tensor', out=dd[d_][:, 0:4], in0=dd[d_][:, 0:4], in1=Ctok[:, cs_[d_], 4 * d_:4 * d_ + 4], op=ALU.max)
        for d_ in range(2):
            P.op('dve', 'reciprocal', out=dd[d_][:, 4:8], in_=dd[d_][:, 0:4])
        for d_ in range(2):
            hbuf = hb[d_].get()
            P.op('dve', 'tensor_tensor', out=hbuf.a, in0=PS2[d_][:, :, 0:64], in1=dd[d_][:, 4:8].unsq(2).bc([64, 4, 64]), op=ALU.mult)
            P.dma(Hd[d_][cs_[d_]].re("p (h d) -> p h d", h=4), hbuf.a, q='sp')
        P.run_deferred(prep_lst, per_step)
    P.run_deferred(prep_lst)
    P.barrier()
    prep_es.close()
    es3.close()
    P.barrier()
    es4 = contextlib.ExitStack()
    Wo = P.sb([128, KD, 256], BF16, "wo", es=es4)
    for k in range(KD):
        self.load_w(Wo[:, k, :], self.w_in[l, k * 128:(k + 1) * 128, 784:1040])
    ng = P.sb([64, 256], F32, "mlng", es=es4)
    P.dma(ng.a, self.ml_norm_g[l:l + 1, :].pbc(64))
    hg = Rot([P.sb([128, KD, 512], BF16, "hg", es=es4) for _ in range(2)])
    hf_ = Rot([P.sb([64, 8, 256], F32, "hf", es=es4) for _ in range(2)])
    hb_ = Rot([P.sb([64, 8, 256], F32, "hb", es=es4) for _ in range(2)])
    sq = P.sb([64, 8, 256], F32, "sq4", es=es4)
    ss = P.sb([64, 32], F32, "ss4", es=es4)
    ob = Rot([P.sb([64, 8, 256], F32, "ob", es=es4) for _ in range(2)])
    hz = Rot([P.sb([64, 8, 256], BF16, "hz", es=es4) for _ in range(2)])
    zst = Rot([P.sb([128, 2, 512], BF16, "zst", es=es4) for _ in range(2)])
    pso = Rot([P.ps([128, 512], F32, es=es4) for _ in range(2)])
    pst = Rot([P.ps([128, 8, 64], BF16, es=es4) for _ in range(2)])
    for (n0, n, seg) in cfg.groups:
        if self.skip_ctx and seg == 1:
            continue
        nc_ = n // 64
        c0 = n0 // 64
        h = hg.get()
        P.dma(h[:, :, 0:n], self.hT[:, :, n0:n0 + n].re("k p t -> p k t"))
        a = hf_.get()
        b = hb_.get()
        P.dma(a[:, 0:nc_, :], self.Hf[c0:c0 + nc_].re("c p f -> p c f"))
        P.dma(b[:, 0:nc_, :], self.Hb[c0:c0 + nc_].re("c p f -> p c f"))
        P.op('dve', 'tensor_tensor', out=a[:, 0:nc_, :], in0=a[:, 0:nc_, :], in1=b[:, 0:nc_, :], op=ALU.add)
        P.op('act', 'activation', out=sq[:, 0:nc_, :], in_=a[:, 0:nc_, :], func=AF.Square)
        P.op('dve', 'tensor_reduce', out=ss[:, 0:nc_ * 4], in_=sq[:, 0:nc_, :].re("p c (h d) -> p (c h) d", h=4), axis=AX.X, op=ALU.add)
        P.op('act', 'activation', out=ss[:, 0:nc_ * 4], in_=ss[:, 0:nc_ * 4], func=AF.Sqrt, scale=1.0 / 64, bias=self.eps[0:64, :])
        P.op('dve', 'reciprocal', out=ss[:, 0:nc_ * 4], in_=ss[:, 0:nc_ * 4])
        P.op('dve', 'tensor_tensor', out=a[:, 0:nc_, :].re("p c (h d) -> p (c h) d", h=4), in0=a[:, 0:nc_, :].re("p c (h d) -> p (c h) d", h=4),
             in1=ss[:, 0:nc_ * 4].unsq(2).bc([64, nc_ * 4, 64]), op=ALU.mult)
        P.op('dve', 'tensor_tensor', out=a[:, 0:nc_, :], in0=a[:, 0:nc_, :], in1=ng.a.unsq(1).bc([64, nc_, 256]), op=ALU.mult)
        o = ob.get()
        for cc in range(nc_):
            po = pso.get()
            for k in range(KD):
                P.mm(po[0:64, 0:256], h[:, k, cc * 64:(cc + 1) * 64], Wo[:, k, :], start=(k == 0), stop=(k == KD - 1))
            P.op('act', 'activation', out=o[:, cc, :], in_=po[0:64, 0:256], func=AF.Sigmoid)
        z = hz.get()
        P.op('dve', 'tensor_tensor', out=z[:, 0:nc_, :], in0=a[:, 0:nc_, :], in1=o[:, 0:nc_, :], op=ALU.mult)
        zs = zst.get()
        for half in range(2):
            p_ = pst.get()
            for cc in range(nc_):
                P.op('pe', 'transpose', out=p_[:, cc, :], in_=z[:, cc, half * 128:(half + 1) * 128], identity=self.identb[0:64, 0:64])
            P.op('act', 'activation', out=zs[:, half, 0:n], in_=p_[:, 0:nc_, :].re("p c t -> p (c t)"), func=AF.Copy)
        P.dma(self.Z[0, :, n0:n0 + n].re("(c p) t -> p c t", p=128), zs[:, :, 0:n], q='pool')
    es4.close()
    es.close()
    P.barrier()


MK.phase_mlstm = _ml


def _peer_prep(self, l, es=None, defer=False):
    P = self.P
    if self.prep_done.get(l):
        return None
    self.prep_done[l] = True
    own_es = es is None
    es = es or contextlib.ExitStack()
    lst = []
    if defer:
        P.deferred = lst
    ub = Rot([P.sb([128, D], BF16, "ub", es=es) for _ in range(3)])
    vb = Rot([P.sb([128, D], BF16, "vb", es=es) for _ in range(3)])
    uo = Rot([P.sb([128, D], BF16, "uo", es=es) for _ in range(3)])
    pst = Rot([P.ps([128, KD, 128], BF16, es=es) for _ in range(2 if defer else 3)])
    Uv = self.peer_u[l].re("(a b) d -> b a d", b=128)
    Vv = self.peer_v[l].re("(a b) d -> b a d", b=128)
    for e2 in range(128):
        u = ub.get()
        P.dma(u.a, Uv[e2], q='pool')
        p_ = pst.get()
        for k in range(KD):
            P.op('pe', 'transpose', out=p_[:, k, :], in_=u[:, k * 128:(k + 1) * 128], identity=self.identb)
        o = uo.get()
        if e2 % 2:
            P.op('act', 'activation', out=o.a, in_=p_.a.re("p k e -> p (k e)"), func=AF.Copy)
        else:
            P.op('dve', 'tensor_copy', out=o.a, in_=p_.a.re("p k e -> p (k e)"))
        P.dma(self.UTs[l % 2][e2], o.a, q='sp')
        v = vb.get()
        P.dma(v.a, Vv[e2], q='pool')
        P.dma(self.VBs[l % 2][e2], v.a, q='sp')
    P.deferred = None
    if own_es:
        es.close()
        P.barrier()
    return lst


MK.phase_peer_prep = _peer_prep


def _peer(self, l):
    P, cfg = self.P, self.cfg
    es = contextlib.ExitStack()
    gs, sh, gate2 = self.mod_scale_shift(l, 1, es)
    Wq = P.sb([128, KD, 2048], BF16, "wq", es=es)
    for k in range(KD):
        self.load_w(Wq[:, k, :], self.peer_w_q[l, k * 128:(k + 1) * 128, :])
    psA = Rot([P.ps([128, 512], F32, es=es) for _ in range(2)])
    Wps = Rot([P.ps([128, 512], F32, es=es) for _ in range(2)])
    acc = [P.ps([128, 2, 256], F32, es=es) for _ in range(4)]
    kl = P.sb([128, 2, 128], BF16, "kl", es=es)
    self.load_w(kl.a, self.peer_keys[l].re("p e k -> e p k"))
    keysT = P.sb([128, 2, 128], BF16, "keysT", es=es)
    for p in range(2):
        pk_ = Wps.get()
        pkb = pk_.a.re("q (a b) -> q a b", b=128)
        klf = P.sb([128, 128], F32, "klf", es=es)
        P.op('dve', 'tensor_copy', out=klf.a, in_=kl[:, p, :])
        P.op('pe', 'transpose', out=pk_[:, 0:128], in_=klf.a, identity=self.identf)
        P.op('dve', 'tensor_copy', out=keysT[:, p, :], in_=pk_[:, 0:128])
    xgs = [P.sb([128, KD, 256], F32, "xg", es=es) for _ in range(2)]
    h2s = [P.sb([128, KD, 256], BF16, "h2", es=es) for _ in range(2)]
    qT = P.sb([128, 16, 256], BF16, "qT", es=es)
    S = P.sb([128, 16, 128], F32, "S", es=es)
    S2 = P.sb([128, 16, 128], F32, "S2", es=es)
    V1 = P.sb([128, 16, 16], F32, "V1", es=es)
    I1u = P.sb([128, 16, 16], U32, "I1u", es=es)
    I1f = P.sb([128, 16, 16], F32, "I1f", es=es)
    cand = P.sb([128, 8, 256], F32, "cand", es=es)
    cand2 = S2
    SC = P.sb([128, 8, 16], F32, "SC", es=es)
    POSu = P.sb([128, 8, 16], U32, "POSu", es=es)
    PIu = P.sb([128, 8, 16], U32, "PIu", es=es)
    PJu = P.sb([128, 8, 16], U32, "PJu", es=es)
    PIf = P.sb([128, 128], F32, "PIf", es=es)
    PJf = P.sb([128, 128], F32, "PJf", es=es)
    OH = S
    E1 = P.sb([128, 128], F32, "E1", es=es)
    E2 = P.sb([128, 128], F32, "E2", es=es)
    G = P.sb([128, 128], F32, "G", es=es)
    sm = P.sb([128, 16], F32, "sm", es=es)
    E1T = P.sb([128, 256], BF16, "E1T", es=es)
    E2T = P.sb([128, 256], BF16, "E2T", es=es)
    GT = P.sb([128, 256], BF16, "GT", es=es)
    if self.wb4:
        An4 = Rot([P.sb([128, 4, 128], BF16, "An", es=es) for _ in range(2)])
        Bn4 = Rot([P.sb([128, 4, 128], BF16, "Bn", es=es) for _ in range(2)])
        iota4 = P.sb([128, 4, 128], BF16, "iota4", es=es)
        for tt in range(4):
            P.op('dve', 'tensor_copy', out=iota4[:, tt, :], in_=self.iota128b_t.a)
    else:
        An = Rot([P.sb([128, 128], BF16, "An", es=es) for _ in range(6)])
        Bn = Rot([P.sb([128, 128], BF16, "Bn", es=es) for _ in range(6)])
    Wbuf = P.sb([128, 256, 128], BF16, "Wbuf", es=es)
    ut = Rot([P.sb([128, KD, 128], BF16, "ut", es=es) for _ in range(4)])
    vt = Rot([P.sb([128, D], BF16, "vtb", es=es) for _ in range(4)])
    Ab = Rot([P.sb([128, 256], BF16, "Ab", es=es) for _ in range(3)])
    AW = Rot([P.sb([128, 256], BF16, "AW", es=es) for _ in range(3)])
    i16 = self.iota16
    V1s = [V1] + [V1.sub() for _ in range(15)]
    I1s = [I1u] + [I1u.sub() for _ in range(15)]
    S2s = [S2] + [S2.sub() for _ in range(15)]
    cands = [cand] + [cand.sub() for _ in range(7)]
    SCs = [SC] + [SC.sub() for _ in range(7)]
    POSs = [POSu] + [POSu.sub() for _ in range(7)]
    zlhs = P.sb([128, 128], BF16, "zlhs", es=es)
    zrhs = P.sb([128, 512], BF16, "zrhs", es=es)
    P.op('pool', 'memset', ap=zlhs.a, constant=0.0)
    P.op('pool', 'memset', ap=zrhs.a, constant=0.0)
    glist = [g for g in cfg.groups256 if not (self.skip_ctx and g[2] == 1)]
    sq = qT[:, 0:8, :]
    tmps = Rot([cands[i][:, i, :] for i in range(3)])
    rsb = cands[3][:, 3, :]

    def front(gi):
        (n0, n, seg) = glist[gi]
        x = xgs[gi % 2]
        h2 = h2s[gi % 2]
        P.dma(x.a, self.xT[:, :, n0:n0 + n].re("k p t -> p k t"))
        self.norm_group(x.a, n, gs, sh, seg, h2.a, Wps, tmps, sq, rsb)
        for j in range(16):
            pq = Wps.get()
            for k in range(KD):
                P.mm(pq[:, 0:n], Wq[:, k, j * 128:(j + 1) * 128], h2[:, k, :], start=(k == 0), stop=(k == KD - 1))
            P.op('act', 'activation', out=qT[:, j, :], in_=pq[:, 0:n], func=AF.Copy)
        for sub in range(2 if 'topk' in self.peer_parts else 0):
            tsl = slice(sub * 128, (sub + 1) * 128)
            for j4 in range(4):
                pscr = Wps.get()
                for jj in range(4):
                    j = j4 * 4 + jj
                    P.mm(pscr[:, jj * 128:(jj + 1) * 128], qT[:, j, tsl], keysT[:, j % 2, :])
                P.op('act', 'activation', out=S[:, j4 * 4:j4 * 4 + 4, :], in_=pscr.a.re("p (a b) -> p a b", b=128), func=AF.Copy)
            for j in range(16):
                P.op('dve', 'max', out=V1s[j][:, j, 0:8], in_=S[:, j, :])
            for j in range(16):
                P.op('dve', 'max_index', out=I1s[j][:, j, 0:8], in_max=V1s[j][:, j, 0:8], in_values=S[:, j, :])
            for j in range(16):
                P.op('dve', 'match_replace', out=S2s[j][:, j, :], in_to_replace=V1s[j][:, j, 0:8], in_values=S[:, j, :], imm_value=NEG)
            for j in range(16):
                P.op('dve', 'max', out=V1s[j][:, j, 8:16], in_=S2s[j][:, j, :])
            for j in range(16):
                P.op('dve', 'max_index', out=I1s[j][:, j, 8:16], in_max=V1s[j][:, j, 8:16], in_values=S2s[j][:, j, :])
            P.op('dve', 'tensor_copy', out=I1f.a, in_=I1s[0].a, xr=I1s[1:])
            V1v = V1s[0].a.re("q (h p) i -> q h p i", p=2)
            P.op('dve', 'tensor_tensor', out=cands[0].a.re("q h (i j) -> q h i j", j=16), in0=V1v[:, :, 0, :].unsq(3).bc([128, 8, 16, 16]),
                 in1=V1v[:, :, 1, :].unsq(2).bc([128, 8, 16, 16]), op=ALU.add, xr=V1s[1:], xw=cands[1:])
            c2v = S2.a.re("q a b -> q (a b)").re("q (h c) -> q h c", h=8)
            ohv = S.a.re("q a b -> q (a b)").re("q (j i) -> q j i", i=16)
            def c2(h):
                return V(S2s[2 * h], c2v.ap[:, h, :])
            for h in range(8):
                P.op('dve', 'max', out=SCs[h][:, h, 0:8], in_=cands[h][:, h, :])
            for h in range(8):
                P.op('dve', 'max_index', out=POSs[h][:, h, 0:8], in_max=SCs[h][:, h, 0:8], in_values=cands[h][:, h, :])
            for h in range(8):
                P.op('dve', 'match_replace', out=c2(h), in_to_replace=SCs[h][:, h, 0:8], in_values=cands[h][:, h, :], imm_value=NEG, xw=[S2s[2 * h + 1]])
            for h in range(8):
                P.op('dve', 'max', out=SCs[h][:, h, 8:16], in_=c2(h), xr=[S2s[2 * h + 1]])
            for h in range(8):
                P.op('dve', 'max_index', out=POSs[h][:, h, 8:16], in_max=SCs[h][:, h, 8:16], in_values=c2(h), xr=[S2s[2 * h + 1]])
            P.op('dve', 'tensor_single_scalar', out=PIu.a, in_=POSs[0].a, scalar=4, op=ALU.logical_shift_right, xr=POSs[1:])
            P.op('dve', 'tensor_single_scalar', out=PJu.a, in_=POSs[0].a, scalar=15, op=ALU.bitwise_and, xr=POSs[1:])
            P.op('dve', 'tensor_copy', out=PIf.a, in_=PIu.a.re("q h k -> q (h k)"))
            P.op('dve', 'tensor_copy', out=PJf.a, in_=PJu.a.re("q h k -> q (h k)"))
            I1v = I1f.a.re("q (h p) i -> q h p i", p=2)
            for (Pf, pp, Eo) in ((PIf, 0, E1), (PJf, 1, E2)):
                P.op('dve', 'tensor_tensor', out=ohv, in0=i16.unsq(1).bc([128, 128, 16]), in1=Pf.a.unsq(2).bc([128, 128, 16]), op=ALU.is_equal)
                P.op('dve', 'tensor_tensor', out=ohv.re("q (h k) i -> q h k i", h=8), in0=ohv.re("q (h k) i -> q h k i", h=8),
                     in1=I1v[:, :, pp, :].unsq(2).bc([128, 8, 16, 16]), op=ALU.mult)
                P.op('dve', 'tensor_reduce', out=Eo.a, in_=ohv, axis=AX.X, op=ALU.add)
            Gv = G.a.re("q (h k) -> q h k", h=8)
            P.op('dve', 'tensor_tensor', out=Gv, in0=SC.a, in1=SC[:, :, 0:1].bc([128, 8, 16]), op=ALU.subtract, xr=SCs[1:])
            P.op('act', 'activation', out=G.a, in_=G.a, func=AF.Exp)
            P.op('dve', 'tensor_reduce', out=sm[:, 0:8], in_=Gv, axis=AX.X, op=ALU.add)
            P.op('dve', 'reciprocal', out=sm[:, 8:16], in_=sm[:, 0:8])
            P.op('dve', 'tensor_tensor', out=Gv, in0=Gv, in1=sm[:, 8:16].unsq(2).bc([128, 8, 16]), op=ALU.mult)
            for (src, dst) in ((E1, E1T), (E2, E2T), (G, GT)):
                ptr = Wps.get()
                P.op('pe', 'transpose', out=ptr[:, 0:128], in_=src.a, identity=self.identf)
                P.op('act', 'activation', out=dst[:, tsl], in_=ptr[:, 0:128], func=AF.Copy)
    def run_front(gi):
        lst = []
        P.deferred = lst
        front(gi)
        P.deferred = None
        return lst

    P.run_deferred(run_front(0))
    for gi, (n0, n, seg) in enumerate(glist):
        x = xgs[gi % 2]
        h2 = h2s[gi % 2]
        for t4 in range(n // 4 if 'wb' in self.peer_parts else 0):
            wp = Wps.get()
            if self.wb4:
                a_ = An4.get()
                b_ = Bn4.get()
                t0_ = t4 * 4
                P.op('dve', 'tensor_tensor', out=a_.a, in0=iota4.a, in1=E1T[:, t0_:t0_ + 4].unsq(2).bc([128, 4, 128]), op=ALU.is_equal)
                P.op('dve', 'tensor_tensor', out=a_.a, in0=a_.a, in1=GT[:, t0_:t0_ + 4].unsq(2).bc([128, 4, 128]), op=ALU.mult)
                P.op('dve', 'tensor_tensor', out=b_.a, in0=iota4.a, in1=E2T[:, t0_:t0_ + 4].unsq(2).bc([128, 4, 128]), op=ALU.is_equal)
                for tt in range(4):
                    P.mm(wp[:, tt * 128:(tt + 1) * 128], a_[:, tt, :], b_[:, tt, :])
            for tt in range(0 if self.wb4 else 4):
                t = t4 * 4 + tt
                a_ = An.get()
                b_ = Bn.get()
                P.op('dve', 'tensor_scalar', out=a_.a, in0=self.iota128b_t.a, scalar1=E1T[:, t:t + 1], scalar2=GT[:, t:t + 1], op0=ALU.is_equal, op1=ALU.mult)
                P.op('dve', 'tensor_scalar', out=b_.a, in0=self.iota128b_t.a, scalar1=E2T[:, t:t + 1], scalar2=None, op0=ALU.is_equal)
                P.mm(wp[:, tt * 128:(tt + 1) * 128], a_.a, b_.a)
            P.op('act', 'activation', out=Wbuf[:, t4 * 4:t4 * 4 + 4, :], in_=wp.a.re("p (a b) -> p a b", b=128), func=AF.Copy)
        for bnk in range(4):
            P.mm(acc[bnk].a.re("p a b -> p (a b)"), zlhs.a, zrhs.a, start=True, stop=False)
        NE = 128 if 'ex' in self.peer_parts else 0

        def stage_a(e2):
            u = ut.get()
            v = vt.get()
            P.dma(u.a, self.UTs[l % 2][e2].re("p (k e) -> p k e", e=128), q='sp')
            P.dma(v.a, self.VBs[l % 2][e2], q='sp')
            pa = psA.get()
            for k in range(KD):
                P.mm(pa[:, 0:n], u[:, k, :], h2[:, k, :], start=(k == 0), stop=(k == KD - 1))
            ab = Ab.get()
            P.op('act', 'activation', out=ab.a, in_=pa[:, 0:n], func=AF.Gelu_apprx_tanh)
            aw = AW.get()
            P.op(self.aw_eng, 'tensor_tensor', out=aw.a, in0=ab.a, in1=Wbuf[:, :, e2], op=ALU.mult)
            return v, aw

        nxt = run_front(gi + 1) if gi + 1 < len(glist) else []
        if not getattr(self, 'peer_pipe', True):
            pre_nxt, nxt = nxt, []
        per_chunk = (len(nxt) + 119) // 120 if NE else len(nxt)
        pend = stage_a(0) if NE else None
        for e2 in range(NE):
            v, aw = pend
            if e2 + 1 < NE:
                pend = stage_a(e2 + 1)
            P.run_deferred(nxt, per_chunk)
            for dc in range(KD):
                P.mm(acc[dc // 2][:, dc % 2, :], v[:, dc * 128:(dc + 1) * 128], aw.a, start=False, stop=(e2 == 127 and dc % 2 == 1))
        P.run_deferred(nxt)
        if not getattr(self, 'peer_pipe', True):
            P.run_deferred(pre_nxt)
        for dc in range(KD):
            P.op('dve', 'scalar_tensor_tensor', out=x[:, dc, :], in0=acc[dc // 2][:, dc % 2, :], scalar=gate2[:, dc, seg:seg + 1], in1=x[:, dc, :], op0=ALU.mult, op1=ALU.add)
        P.dma(self.xT[:, :, n0:n0 + n].re("k p t -> p k t"), x.a, q='pool')
    es.close()
    P.barrier()


MK.phase_peer = _peer


def build_program(cfg, debug=False, phases=None, **opts):
    mk = MK(cfg, debug=debug)
    mk.skip_ctx = False
    mk.prep_done = {}
    mk.prep_in_mlstm = (phases is None) and opts.get('prep_in_mlstm', False)
    mk.peer_pipe = opts.get('peer_pipe', True)
    mk.wb4 = opts.get('wb4', False) or (phases is not None and 'wb4' in phases)
    mk.aw_eng = 'pool' if (opts.get('aw_pool', True) or (phases is not None and 'aw_pool' in phases)) else 'dve'
    mk.scan_eng = 'dve' if (opts.get('scan_dve', True) and not (phases is not None and 'scan_pool' in phases)) else 'pool'
    mk.merge_eng = 'dve' if (opts.get('merge_dve', True) and not (phases is not None and 'merge_pool' in phases)) else 'pool'
    mk.wb_pool = opts.get('wb_pool', False) or (phases is not None and 'wb_pool' in phases)
    mk.peer_parts = set(['topk', 'wb', 'ex']) if (phases is None or not any(p.startswith('pp_') for p in phases)) else set(p[3:] for p in phases if p.startswith('pp_'))
    on = lambda p: phases is None or p in phases
    mk.phase_init()
    for l in range(cfg.depth):
        mk.skip_ctx = False
        if on('mod'):
            mk.phase_mod(l)
        if on('norm1'):
            mk.phase_norm1(l)
        if on('mlstm'):
            mk.phase_mlstm(l)
        mk.skip_ctx = (l == cfg.depth - 1) and phases is None
        if on('gmlp'):
            mk.phase_gmlp(l)
        if on('conv'):
            mk.phase_conv(l)
        if on('fnet'):
            mk.phase_fnet(l)
        if on('merge'):
            mk.phase_merge(l)
        if on('prep'):
            mk.phase_peer_prep(l)
        if on('peer'):
            mk.phase_peer(l)
    mk.phase_final()
    mk.P.finish()
    return mk


_CACHE = {}


def make_in_maps(cfg, inp):
    dftc, dfts, cd, cst = host_consts(cfg)
    pos = grid_sincos(cfg.t_lat, D)
    L = cfg.depth
    shared = {"pos": pos, "dftc": dftc, "dfts": dfts, "cd": cd, "cst": cst}
    for k in ["w_ada", "b_ada", "norm1_g", "norm2_g", "w_in", "ml_conv_w", "ml_conv_b", "ml_gate_b", "gm_ln_g", "gm_ln_b",
              "gm_w_s", "cv_dw_w", "cv_dw_b", "cv_ln_g", "cv_ln_b", "w_branch", "w_out", "peer_w_q", "peer_keys",
              "peer_u", "peer_v", "final_norm_g"]:
        shared[k] = np.ascontiguousarray(np.asarray(inp[k], dtype=np.float32))
    shared["ml_norm_g"] = np.ascontiguousarray(np.asarray(inp["ml_norm_g"], np.float32).reshape(L, 256))
    shared["gm_b_s"] = np.ascontiguousarray(np.asarray(inp["gm_b_s"], np.float32).reshape(L, 512))
    x = np.asarray(inp["x"], np.float32)
    ctx = np.asarray(inp["ctx"], np.float32)
    c = np.asarray(inp["c"], np.float32)
    c_ctx = np.asarray(inp["c_ctx"], np.float32)
    maps = []
    for b in range(x.shape[0]):
        m = dict(shared)
        m["xin"] = np.ascontiguousarray(np.concatenate([ctx[b], x[b]], 0))
        m["cvec"] = np.ascontiguousarray(np.stack([c[b], c_ctx], 0))
        maps.append(m)
    return maps


def kernel(**inputs):
    x = np.asarray(inputs["x"])
    B, T, _ = x.shape
    depth = np.asarray(inputs["w_ada"]).shape[0]
    cfg = Cfg(depth=depth, t_lat=T, t_ctx=np.asarray(inputs["ctx"]).shape[1])
    key = (depth, T, cfg.t_ctx)
    if key not in _CACHE:
        _CACHE[key] = build_program(cfg)
    mk = _CACHE[key]
    maps = make_in_maps(cfg, inputs)
    res = run_bass_kernel_spmd(mk.nc, maps, core_ids=list(range(B)))
    return np.stack([np.asarray(r["out"], dtype=np.float32) for r in res.results], 0)
```

```python
import contextlib
import numpy as np
import ml_dtypes
import concourse.bass as bass
import concourse.mybir as mybir
from concourse.bass_utils import run_bass_kernel_spmd

F32 = mybir.dt.float32
BF16 = mybir.dt.bfloat16
I32 = mybir.dt.int32
U32 = mybir.dt.uint32
AF = mybir.ActivationFunctionType
ALU = mybir.AluOpType
AX = mybir.AxisListType

COMPUTE = ('pe', 'act', 'dve', 'pool')
NRING = 12
WRITE_KW = ('out', 'accum_out', 'ap')
EPS = 1e-6
NEG = -1.0e30


class V:
    __slots__ = ('buf', 'ap')

    def __init__(self, buf, ap):
        self.buf = buf
        self.ap = ap

    def __getitem__(self, k):
        return V(self.buf, self.ap[k])

    def re(self, pat, **kw):
        return V(self.buf, self.ap.rearrange(pat, **kw))

    def bc(self, shape):
        return V(self.buf, self.ap.to_broadcast(list(shape)))

    def unsq(self, ax):
        return V(self.buf, self.ap.unsqueeze(ax))

    def pbc(self, n):
        return V(self.buf, self.ap.partition_broadcast(n))


class Buf:
    __slots__ = ('t', 'lw', 'lwd', 'rd', 'name')

    def __init__(self, t, name=''):
        self.t = t
        self.lw = None
        self.lwd = {}
        self.rd = {}
        self.name = name

    def __getitem__(self, k):
        return V(self, self.t[k])

    @property
    def a(self):
        return V(self, self.t[:])

    def sub(self):
        return Buf(self.t, self.name + '_s')


class Prog:
    def __init__(self, nc):
        self.nc = nc
        self.ops = {e: [] for e in ('pe', 'act', 'dve', 'pool', 'sp')}
        self.count = {e: 0 for e in COMPUTE}
        self.known = {e: {} for e in self.ops}
        self.ndma = {'sp': 0, 'pool': 0, 'act': 0}
        self.es = contextlib.ExitStack()
        self.sems = {}
        for e in COMPUTE:
            self.sems[('c', e)] = self.es.enter_context(nc.semaphore('s_' + e))
        for q in ('sp', 'pool', 'act'):
            for i in range(NRING):
                self.sems[('d', q, i)] = self.es.enter_context(nc.semaphore('d_%s_%d' % (q, i)))
        self.nbuf = 0
        self.ninst = 0
        self.deferred = None

    def sb(self, shape, dt, name=None, es=None):
        self.nbuf += 1
        name = '%s_%d' % (name or 'sb', self.nbuf)
        t = (es or self.es).enter_context(self.nc.sbuf_tensor(name, list(shape), dt))
        return Buf(t, name)

    def ps(self, shape, dt, name=None, es=None):
        self.nbuf += 1
        name = '%s_%d' % (name or 'ps', self.nbuf)
        t = (es or self.es).enter_context(self.nc.psum_tensor(name, list(shape), dt))
        return Buf(t, name)

    def dram(self, name, shape, dt, kind='Internal'):
        t = self.nc.dram_tensor(name, list(shape), dt, kind=kind)
        return Buf(t, name)

    def _need(self, eng, tok, waits):
        if tok is None:
            return
        k, v = tok
        if self.known[eng].get(k, 0) >= v:
            return
        if waits.get(k, 0) < v:
            waits[k] = v

    def emit(self, eng, fn, reads=(), writes=(), dma=False):
        waits = {}
        own = ('c', eng) if (eng in COMPUTE and not dma) else None
        for b in reads:
            for k, v in b.lwd.items():
                self._need(eng, (k, v), waits)
            if b.lw is not None:
                if own is not None and b.lw[0] == own and eng == 'pe':
                    continue
                self._need(eng, b.lw, waits)
        for b in writes:
            if not dma:
                for k, v in b.lwd.items():
                    self._need(eng, (k, v), waits)
            if b.lw is not None:
                if not (own is not None and b.lw[0] == own and eng == 'pe'):
                    self._need(eng, b.lw, waits)
            for k, v in b.rd.items():
                if own is not None and k == own and eng == 'pe':
                    continue
                self._need(eng, (k, v), waits)
        if dma:
            i = self.ndma[eng]
            self.ndma[eng] = i + 1
            slot, gen = i % NRING, i // NRING
            key = ('d', eng, slot)
            if gen > 0:
                self._need(eng, (key, 16 * gen), waits)
            tok = (key, 16 * (gen + 1))
            inc = (key, 16)
        else:
            self.count[eng] += 1
            tok = (own, self.count[eng])
            inc = (own, 1)
        for k, v in waits.items():
            self.known[eng][k] = v
        self.ops[eng].append((fn, list(waits.items()), inc))
        self.ninst += 1
        for b in writes:
            if dma:
                b.lwd[tok[0]] = tok[1]
            else:
                b.lw = tok
                b.lwd = {}
                b.rd = {}
        for b in reads:
            if b in writes:
                continue
            if b.rd.get(tok[0], 0) < tok[1]:
                b.rd[tok[0]] = tok[1]
        return tok

    def run_deferred(self, lst, k=None):
        k = len(lst) if k is None else min(k, len(lst))
        for _ in range(k):
            eng, name, xr, xw, kw = lst.pop(0)
            self.op(eng, name, xr=xr, xw=xw, **kw)

    def op(self, eng, name, *, xr=(), xw=(), **kw):
        if self.deferred is not None:
            self.deferred.append((eng, name, xr, xw, kw))
            return None
        reads, writes, real = list(xr), list(xw), {}
        for k, v in kw.items():
            if isinstance(v, V):
                (writes if k in WRITE_KW else reads).append(v.buf)
                real[k] = v.ap
            else:
                real[k] = v
        isdma = name == 'dma_start'

        def fn(e, name=name, real=real):
            return getattr(e, name)(**real)
        return self.emit(eng, fn, reads, writes, dma=isdma)

    def dma(self, out, in_, q='sp', **kw):
        return self.op(q, 'dma_start', out=out, in_=in_, **kw)

    def mm(self, out, lhsT, rhs, start=True, stop=True):
        return self.op('pe', 'matmul', out=out, lhsT=lhsT, rhs=rhs, start=start, stop=stop)

    def barrier(self):
        toks = []
        for e in COMPUTE:
            if self.count[e] > 0:
                toks.append((('c', e), self.count[e]))
        for q, n in self.ndma.items():
            for i in range(max(0, n - NRING), n):
                toks.append((('d', q, i % NRING), 16 * (i // NRING + 1)))
        for e in self.ops:
            waits = {}
            for tok in toks:
                if tok[0] == ('c', e):
                    continue
                self._need(e, tok, waits)
            for k, v in waits.items():
                self.known[e][k] = v
            if waits:
                self.ops[e].append((None, list(waits.items()), None))

    def finish(self):
        self.barrier()
        nc = self.nc
        sems = self.sems
        ops = self.ops

        def replay(name, e):
            for fn, waits, inc in ops[name]:
                for k, v in waits:
                    e.wait_ge(sems[k], v)
                if fn is None:
                    continue
                ins = fn(e)
                if inc is not None:
                    ins.then_inc(sems[inc[0]], inc[1])

        with nc.Block() as block:
            @block.sync
            def _(e):
                replay('sp', e)

            @block.tensor
            def _(e):
                replay('pe', e)

            @block.scalar
            def _(e):
                replay('act', e)

            @block.vector
            def _(e):
                replay('dve', e)

            @block.gpsimd
            def _(e):
                replay('pool', e)
        self.es.close()


class Rot:
    def __init__(self, bufs):
        self.bufs = bufs
        self.i = 0

    def get(self):
        b = self.bufs[self.i % len(self.bufs)]
        self.i += 1
        return b


D = 1024
KD = 8
IN_COLS = 6416
GM_OFF = 1040
CV_OFF = 1552
FT_OFF = 2064
GATE_OFF = 2320
NEXP = 16384


class Cfg:
    def __init__(self, depth=4, t_lat=4096, t_ctx=256):
        self.depth = depth
        self.t_lat = t_lat
        self.t_ctx = t_ctx
        self.nt = t_lat + t_ctx
        self.groups = [(0, t_ctx, 1)] + [(t_ctx + i * 512, 512, 0) for i in range(t_lat // 512)]
        self.groups256 = [(i * 256, 256, 1 if i * 256 < t_ctx else 0) for i in range(self.nt // 256)]
        self.segs = [(0, t_ctx, 1), (t_ctx, t_lat, 0)]
        self.nch = self.nt // 64


def host_consts(cfg):
    T = cfg.t_lat
    k = np.arange(T, dtype=np.float64)
    ang = 2.0 * np.pi * ((k[:, None] * k[None, :]) % T) / T
    dftc = (np.cos(ang) / np.sqrt(T)).astype(ml_dtypes.bfloat16)
    dfts = (-np.sin(ang) / np.sqrt(T)).astype(ml_dtypes.bfloat16)
    c = np.arange(64, dtype=np.float64)
    a64 = 2.0 * np.pi * ((c[:, None] * c[None, :]) % 64) / 64
    cd = np.zeros((256, 512), np.float64)
    for g in range(4):
        cd[g * 64:(g + 1) * 64, g * 64:(g + 1) * 64] = np.cos(a64) / 8.0
        cd[g * 64:(g + 1) * 64, 256 + g * 64:256 + (g + 1) * 64] = np.sin(a64) / 8.0
    cd = cd.astype(ml_dtypes.bfloat16)
    cst = np.zeros((128, 1024), np.float32)
    cst[:, 0:128] = np.eye(128, dtype=np.float32)
    cst[:, 128:256] = np.arange(128, dtype=np.float32)[None, :]
    s = np.arange(64)
    cst[0:64, 256:320] = (s[:, None] <= s[None, :]).astype(np.float32)
    cst[0:64, 320:384] = (s[:, None] >= s[None, :]).astype(np.float32)
    for g in range(8):
        row = g if g < 4 else 32 + (g - 4)
        cst[row, 384 + g * 64:384 + (g + 1) * 64] = 1.0
    cst[:, 896:912] = np.arange(16, dtype=np.float32)[None, :]
    cst[:, 912] = EPS
    cst[:, 913] = 1.0
    return dftc, dfts, cd, cst


def grid_sincos(n_tok, d):
    rows = n_tok // 64
    n_freq = d // 4
    freq = (1.0 / (10000.0 ** (np.arange(n_freq, dtype=np.float32) / np.float32(n_freq)))).astype(np.float32)
    r = np.repeat(np.arange(rows, dtype=np.float32), 64)
    cc = np.tile(np.arange(64, dtype=np.float32), rows)
    ar = r[:, None] * freq[None, :]
    ac = cc[:, None] * freq[None, :]
    return np.concatenate([np.sin(ar), np.cos(ar), np.sin(ac), np.cos(ac)], axis=-1).astype(np.float32)


class MK:
    def __init__(self, cfg, debug=False):
        self.cfg = cfg
        self.nc = bass.Bass("TRN2", target_bir_lowering=False)
        self.P = Prog(self.nc)
        self.debug = debug
        P = self.P
        L = cfg.depth
        NT = cfg.nt
        din = lambda name, shape, dt=F32: P.dram(name, shape, dt, kind="ExternalInput")
        self.xin = din("xin", [NT, D])
        self.pos = din("pos", [cfg.t_lat, D])
        self.cvec = din("cvec", [2, D])
        self.w_ada = din("w_ada", [L, D, 6 * D])
        self.b_ada = din("b_ada", [L, 6 * D])
        self.norm1_g = din("norm1_g", [L, D])
        self.norm2_g = din("norm2_g", [L, D])
        self.w_in = din("w_in", [L, D, IN_COLS])
        self.ml_conv_w = din("ml_conv_w", [L, 3, 512])
        self.ml_conv_b = din("ml_conv_b", [L, 512])
        self.ml_gate_b = din("ml_gate_b", [L, 16])
        self.ml_norm_g = din("ml_norm_g", [L, 256])
        self.gm_ln_g = din("gm_ln_g", [L, 256])
        self.gm_ln_b = din("gm_ln_b", [L, 256])
        self.gm_w_s = din("gm_w_s", [L, 4, 128, 128])
        self.gm_b_s = din("gm_b_s", [L, 512])
        self.cv_dw_w = din("cv_dw_w", [L, 31, 256])
        self.cv_dw_b = din("cv_dw_b", [L, 256])
        self.cv_ln_g = din("cv_ln_g", [L, 256])
        self.cv_ln_b = din("cv_ln_b", [L, 256])
        self.w_branch = din("w_branch", [L, 4, 256, D])
        self.w_out = din("w_out", [L, D, D])
        self.peer_w_q = din("peer_w_q", [L, D, 2048])
        self.peer_keys = din("peer_keys", [L, 2, 128, 128])
        self.peer_u = din("peer_u", [L, NEXP, D])
        self.peer_v = din("peer_v", [L, NEXP, D])
        self.final_norm_g = din("final_norm_g", [D])
        self.dftc = din("dftc", [cfg.t_lat, cfg.t_lat], BF16)
        self.dfts = din("dfts", [cfg.t_lat, cfg.t_lat], BF16)
        self.cd = din("cd", [256, 512], BF16)
        self.cst_d = din("cst", [128, 1024])
        self.out = P.dram("out", [cfg.t_lat, D], F32, kind="ExternalOutput")
        sk = "ExternalOutput" if debug else "Internal"
        self.xT = P.dram("xT", [KD, 128, NT], F32, kind=sk)
        self.hT = P.dram("hT", [KD, 128, NT], BF16, kind=sk)
        self.Z = P.dram("Z", [4, 256, NT], BF16, kind=sk)
        self.Hf = P.dram("Hf", [cfg.nch, 64, 256], F32, kind=sk)
        self.Hb = P.dram("Hb", [cfg.nch, 64, 256], F32, kind=sk)
        self.UTs = [P.dram("UT%d" % i, [128, 128, KD * 128], BF16) for i in range(2)]
        self.VBs = [P.dram("VB%d" % i, [128, 128, D], BF16) for i in range(2)]
        self.cst = P.sb([128, 1024], F32, "cst")
        P.dma(self.cst.a, self.cst_d.a)
        c = self.cst
        self.identf = c[:, 0:128]
        self.iota128 = c[:, 128:256]
        self.iota16 = c[:, 896:912]
        self.eps = c[:, 912:913]
        self.identb_t = P.sb([128, 128], BF16, "identb")
        P.op('dve', 'tensor_copy', out=self.identb_t.a, in_=self.identf)
        self.identb = self.identb_t.a
        self.onesb_t = P.sb([128, 128], BF16, "onesb")
        P.op('pool', 'memset', ap=self.onesb_t.a, constant=1.0)
        self.onesb = self.onesb_t.a
        self.iota128b_t = P.sb([128, 128], BF16, "iota128b")
        P.op('dve', 'tensor_copy', out=self.iota128b_t.a, in_=self.iota128)
        self.onesf_t = P.sb([128, 128], F32, "onesf")
        P.op('pool', 'memset', ap=self.onesf_t.a, constant=1.0)
        self.onesf = self.onesf_t.a
        self.maskb_t = P.sb([64, 2, 4, 64], BF16, "maskb")
        for d_ in range(2):
            for h in range(4):
                P.op('dve', 'tensor_copy', out=self.maskb_t[:, d_, h, :], in_=c[0:64, 256 + 64 * d_:320 + 64 * d_])
        self.modT = P.sb([128, 48, 2], F32, "modT")
        cT = P.sb([2, D], F32, "cT")
        P.dma(cT.a, self.cvec.a)
        self.sT = P.sb([128, 2, KD], BF16, "sT")
        es = contextlib.ExitStack()
        ps = P.ps([128, 512], F32, es=es)
        for k in range(KD):
            P.op('pe', 'transpose', out=ps[:, 2 * k:2 * k + 2], in_=cT[:, k * 128:(k + 1) * 128], identity=self.identf[0:2, 0:2])
        P.op('act', 'activation', out=self.sT.a, in_=ps[:, 0:16].re("p (k s) -> p s k", s=2), func=AF.Silu)
        es.close()
        P.barrier()

    def load_rows_T(self, rows, es):
        P = self.P
        R = sum(v.ap.shape[0] for v in rows)
        w = rows[0].ap.shape[1]
        assert R <= 128
        rt = P.sb([128, 128], F32, "rows", es=es)
        r0 = 0
        for v in rows:
            r = v.ap.shape[0]
            P.dma(rt[r0:r0 + r, 0:w], v)
            r0 += r
        es_ = contextlib.ExitStack()
        ps = P.ps([128, 512], F32, es=es_)
        P.op('pe', 'transpose', out=ps[0:w, 0:R], in_=rt[0:R, 0:w], identity=self.identf[0:R, 0:R])
        ct = P.sb([128, R], F32, "colsT", es=es)
        P.op('dve', 'tensor_copy', out=ct[0:w, :], in_=ps[0:w, 0:R])
        P.barrier()
        es_.close()
        return ct

    def load_w(self, dst, src, q='pool'):
        self.P.dma(dst, src, q=q)

    def phase_init(self):
        P, cfg = self.P, self.cfg
        es = contextlib.ExitStack()
        xt = Rot([P.sb([128, D], F32, "xt", es=es) for _ in range(2)])
        pt = Rot([P.sb([128, D], F32, "pt", es=es) for _ in range(2)])
        pss = Rot([P.ps([128, 512], F32, es=es) for _ in range(4)])
        st = Rot([P.sb([128, KD, 128], F32, "xst", es=es) for _ in range(2)])
        for ti in range(cfg.nt // 128):
            x = xt.get()
            P.dma(x.a, self.xin[ti * 128:(ti + 1) * 128, :])
            if ti * 128 >= cfg.t_ctx:
                p = pt.get()
                r0 = ti * 128 - cfg.t_ctx
                P.dma(p.a, self.pos[r0:r0 + 128, :])
                P.op('dve', 'tensor_tensor', out=x.a, in0=x.a, in1=p.a, op=ALU.add)
            s = st.get()
            for half in range(2):
                ps = pss.get()
                for j in range(4):
                    k = half * 4 + j
                    P.op('pe', 'transpose', out=ps[:, j * 128:(j + 1) * 128], in_=x[:, k * 128:(k + 1) * 128], identity=self.identf)
                P.op('act', 'activation', out=s[:, half * 4:half * 4 + 4, :], in_=ps.a.re("p (j t) -> p j t", j=4), func=AF.Copy)
            P.dma(self.xT[:, :, ti * 128:(ti + 1) * 128].re("k p t -> p k t"), s.a)
        es.close()
        P.barrier()

    def phase_mod(self, l):
        P = self.P
        es = contextlib.ExitStack()
        wt = Rot([P.sb([128, KD, 1536], BF16, "wada", es=es) for _ in range(2)])
        ps = P.ps([128, 48, 2], F32, es=es)
        bT = self.load_rows_T([self.b_ada[l].re("(j p) -> j p", p=128)], es)
        for cg in range(4):
            w = wt.get()
            for k in range(KD):
                self.load_w(w[:, k, :], self.w_ada[l, k * 128:(k + 1) * 128, cg * 1536:(cg + 1) * 1536])
            for jj in range(12):
                j = cg * 12 + jj
                for k in range(KD):
                    P.mm(ps[:, j, :], w[:, k, jj * 128:(jj + 1) * 128], self.sT[:, :, k], start=(k == 0), stop=(k == KD - 1))
        P.op('dve', 'tensor_tensor', out=self.modT.a, in0=ps.a, in1=bT.a.unsq(2).bc([128, 48, 2]), op=ALU.add)
        es.close()
        P.barrier()

    def mod_scale_shift(self, l, which, es):
        P = self.P
        g = self.norm1_g if which == 0 else self.norm2_g
        gT = self.load_rows_T([g[l].re("(j p) -> j p", p=128)], es)
        base = 0 if which == 0 else 24
        gs = P.sb([128, KD, 2], F32, "gs", es=es)
        P.op('dve', 'tensor_scalar', out=gs.a, in0=self.modT[:, base + 8:base + 16, :], scalar1=1.0, scalar2=None, op0=ALU.add)
        P.op('dve', 'tensor_tensor', out=gs.a, in0=gs.a, in1=gT.a.unsq(2).bc([128, KD, 2]), op=ALU.mult)
        return gs, self.modT[:, base:base + 8, :], self.modT[:, base + 16:base + 24, :]

    def norm_group(self, xg, n, gs, sh, seg, hout, pss, tmps, sq, rsb):
        P = self.P
        P.op('act', 'activation', out=sq[:, :, 0:n], in_=xg, func=AF.Square)
        ps = pss.get()
        for k in range(KD):
            P.mm(ps[:, 0:n], self.onesb, sq[:, k, 0:n], start=(k == 0), stop=(k == KD - 1))
        rs = rsb
        P.op('act', 'activation', out=rs[:, 0:n], in_=ps[:, 0:n], func=AF.Sqrt, scale=1.0 / D, bias=self.eps)
        P.op('dve', 'reciprocal', out=rs[:, 0:n], in_=rs[:, 0:n])
        for k in range(KD):
            t = tmps.get()
            P.op('dve', 'scalar_tensor_tensor', out=t[:, 0:n], in0=xg[:, k, :], scalar=gs[:, k, seg:seg + 1], in1=rs[:, 0:n], op0=ALU.mult, op1=ALU.mult)
            if sh is None:
                P.op('act', 'activation', out=hout[:, k, :], in_=t[:, 0:n], func=AF.Copy)
            else:
                P.op('act', 'activation', out=hout[:, k, :], in_=t[:, 0:n], func=AF.Identity, bias=sh[:, k, seg:seg + 1])

    def phase_norm1(self, l):
        P, cfg = self.P, self.cfg
        es = contextlib.ExitStack()
        gs, sh, _ = self.mod_scale_shift(l, 0, es)
        xg = Rot([P.sb([128, KD, 512], F32, "xg", es=es) for _ in range(2)])
        hg = Rot([P.sb([128, KD, 512], BF16, "hg", es=es) for _ in range(2)])
        sq = P.sb([128, KD, 512], BF16, "sq", es=es)
        pss = Rot([P.ps([128, 512], F32, es=es) for _ in range(2)])
        tmps = Rot([P.sb([128, 512], F32, "nt", es=es) for _ in range(4)])
        rsb = P.sb([128, 512], F32, "rsb", es=es)
        for (n0, n, seg) in cfg.groups:
            x = xg.get()
            h = hg.get()
            P.dma(x[:, :, 0:n], self.xT[:, :, n0:n0 + n].re("k p t -> p k t"))
            self.norm_group(x[:, :, 0:n], n, gs, sh, seg, h[:, :, 0:n], pss, tmps, sq, rsb)
            P.dma(self.hT[:, :, n0:n0 + n].re("k p t -> p k t"), h[:, :, 0:n], q='pool')
        es.close()
        P.barrier()


def _gm(self, l):
    P, cfg = self.P, self.cfg
    es = contextlib.ExitStack()
    W = P.sb([128, KD, 512], BF16, "wgm", es=es)
    for k in range(KD):
        self.load_w(W[:, k, :], self.w_in[l, k * 128:(k + 1) * 128, GM_OFF:GM_OFF + 512])
    lng = P.sb([128, 256], F32, "lng", es=es)
    lnb = P.sb([128, 256], F32, "lnb", es=es)
    P.dma(lng.a, self.gm_ln_g[l:l + 1, :].pbc(128))
    P.dma(lnb.a, self.gm_ln_b[l:l + 1, :].pbc(128))
    bsr = P.sb([1, 512], F32, "bsr", es=es)
    P.dma(bsr.a, self.gm_b_s[l:l + 1, :])
    wsT = P.sb([128, 4, 128], BF16, "wsT", es=es)
    wsl = P.sb([128, 4, 128], BF16, "wsl", es=es)
    self.load_w(wsl.a, self.gm_w_s[l].re("g t s -> t g s"))
    pst = P.ps([128, 4, 128], BF16, es=es)
    for g in range(4):
        P.op('pe', 'transpose', out=pst[:, g, :], in_=wsl[:, g, :], identity=self.identb)
    P.op('dve', 'tensor_copy', out=wsT.a, in_=pst.a)
    hg = Rot([P.sb([128, KD, 512], BF16, "hg", es=es) for _ in range(2)])
    u64 = Rot([P.sb([64, 4, 512], BF16, "u64", es=es) for _ in range(2)])
    zst = Rot([P.sb([64, 4, 512], BF16, "zst", es=es) for _ in range(2)])
    psu = Rot([P.ps([128, 512], F32, es=es) for _ in range(1)])
    psv = Rot([P.ps([128, 512], F32, es=es) for _ in range(4)])
    pss = Rot([P.ps([64, 4, 128], F32, es=es) for _ in range(2)])
    vt = Rot([P.sb([128, 256], F32, "vt", es=es) for _ in range(4)])
    vn = Rot([P.sb([128, 256], BF16, "vn", es=es) for _ in range(4)])
    st6 = Rot([P.sb([128, 8], F32, "st6", es=es) for _ in range(4)])
    for (n0, n, seg) in cfg.groups:
        if self.skip_ctx and seg == 1:
            continue
        h = hg.get()
        P.dma(h[:, :, 0:n], self.hT[:, :, n0:n0 + n].re("k p t -> p k t"))
        u = u64.get()
        for g in range(4):
            ps = psu.get()
            for k in range(KD):
                P.mm(ps[0:64, 0:n], W[:, k, g * 64:(g + 1) * 64], h[:, k, 0:n], start=(k == 0), stop=(k == KD - 1))
            P.op('act', 'activation', out=u[:, g, 0:n], in_=ps[0:64, 0:n], func=AF.Gelu_apprx_tanh)
        z = zst.get()
        subs = list(range(n // 128))
        pv_ = [psv.get() for _ in subs]
        v_ = [vt.get() for _ in subs]
        s6_ = [st6.get() for _ in subs]
        vb_ = [vn.get() for _ in subs]
        for sub in subs:
            for k in range(KD):
                P.mm(pv_[sub][:, 0:256], h[:, k, sub * 128:(sub + 1) * 128], W[:, k, 256:512], start=(k == 0), stop=(k == KD - 1))
        for sub in subs:
            P.op('act', 'activation', out=v_[sub].a, in_=pv_[sub][:, 0:256], func=AF.Gelu_apprx_tanh)
        for sub in subs:
            P.op('dve', 'bn_stats', out=s6_[sub][:, 0:6], in_=v_[sub].a)
        for sub in subs:
            P.op('dve', 'bn_aggr', out=s6_[sub][:, 6:8], in_=s6_[sub][:, 0:6])
        for sub in subs:
            P.op('act', 'activation', out=s6_[sub][:, 7:8], in_=s6_[sub][:, 7:8], func=AF.Sqrt, bias=self.eps)
        for sub in subs:
            P.op('dve', 'reciprocal', out=s6_[sub][:, 7:8], in_=s6_[sub][:, 7:8])
        for sub in subs:
            P.op('dve', 'tensor_scalar', out=v_[sub].a, in0=v_[sub].a, scalar1=s6_[sub][:, 6:7], scalar2=s6_[sub][:, 7:8], op0=ALU.subtract, op1=ALU.mult)
        for sub in subs:
            P.op('dve', 'tensor_tensor', out=v_[sub].a, in0=v_[sub].a, in1=lng.a, op=ALU.mult)
        for sub in subs:
            P.op('dve', 'tensor_tensor', out=vb_[sub].a, in0=v_[sub].a, in1=lnb.a, op=ALU.add)
        for sub in subs:
            pg = pss.get()
            for g in range(4):
                P.mm(pg[:, g, :], vb_[sub][:, g * 64:(g + 1) * 64], wsT[:, g, :], start=True, stop=False)
                P.mm(pg[:, g, :], self.onesf[0:1, 0:64], bsr[0:1, g * 128:(g + 1) * 128], start=False, stop=True)
            P.op('dve', 'tensor_tensor', out=z[:, :, sub * 128:(sub + 1) * 128], in0=pg.a, in1=u[:, :, sub * 128:(sub + 1) * 128], op=ALU.mult)
        P.dma(self.Z[1, :, n0:n0 + n].re("(g p) t -> p g t", p=64), z[:, :, 0:n], q='pool')
    es.close()
    P.barrier()


MK.phase_gmlp = _gm


def _cv(self, l):
    P, cfg = self.P, self.cfg
    es = contextlib.ExitStack()
    PAD = 15
    W = P.sb([128, KD, 512], BF16, "wcv", es=es)
    for k in range(KD):
        self.load_w(W[:, k, :], self.w_in[l, k * 128:(k + 1) * 128, CV_OFF:CV_OFF + 512])
    cols = self.load_rows_T([self.cv_dw_w[l].re("j (c p) -> (j c) p", p=128), self.cv_dw_b[l].re("(c p) -> c p", p=128),
                             self.cv_ln_g[l].re("(c p) -> c p", p=128), self.cv_ln_b[l].re("(c p) -> c p", p=128)], es)
    diag = P.sb([128, 62, 128], BF16, "diag", es=es)
    for jc in range(62):
        P.op('dve', 'tensor_scalar', out=diag[:, jc, :], in0=self.identf, scalar1=cols[:, jc:jc + 1], scalar2=None, op0=ALU.mult)
    hg = Rot([P.sb([128, KD, 512], BF16, "hg", es=es) for _ in range(2)])
    psa = Rot([P.ps([128, 512], F32, es=es) for _ in range(2)])
    psb = Rot([P.ps([128, 512], F32, es=es) for _ in range(2)])
    psc = Rot([P.ps([128, 512], F32, es=es) for _ in range(2)])
    sg = Rot([P.sb([128, 512], F32, "sg", es=es) for _ in range(2)])
    for (s0, sn, seg) in cfg.segs:
        if self.skip_ctx and seg == 1:
            continue
        es2 = contextlib.ExitStack()
        zp = P.sb([128, 2, sn + 2 * PAD], BF16, "zp", es=es2)
        P.op('pool', 'memset', ap=zp.a, constant=0.0)
        y = P.sb([128, 2, 512], F32, "ycv", es=es2)
        y2 = P.sb([128, 2, 512], F32, "ycv2", es=es2)
        mean = P.sb([128, 512], F32, "mean", es=es2)
        rstd = P.sb([128, 512], F32, "rstd", es=es2)
        zo = Rot([P.sb([128, 2, 512], BF16, "zo", es=es2) for _ in range(2)])
        grp = [(a, n) for (a, n, sg_) in cfg.groups if sg_ == seg]
        for (n0, n) in grp:
            h = hg.get()
            P.dma(h[:, :, 0:n], self.hT[:, :, n0:n0 + n].re("k p t -> p k t"))
            for c in range(2):
                pa = psa.get()
                pb = psb.get()
                for k in range(KD):
                    P.mm(pa[:, 0:n], W[:, k, c * 128:(c + 1) * 128], h[:, k, 0:n], start=(k == 0), stop=(k == KD - 1))
                for k in range(KD):
                    P.mm(pb[:, 0:n], W[:, k, 256 + c * 128:256 + (c + 1) * 128], h[:, k, 0:n], start=(k == 0), stop=(k == KD - 1))
                s = sg.get()
                P.op('act', 'activation', out=s[:, 0:n], in_=pb[:, 0:n], func=AF.Sigmoid)
                o0 = PAD + n0 - s0
                P.op('dve', 'tensor_tensor', out=zp[:, c, o0:o0 + n], in0=pa[:, 0:n], in1=s[:, 0:n], op=ALU.mult)
        for (n0, n) in grp:
            o0 = n0 - s0
            for c in range(2):
                pc = psc.get()
                for j in range(31):
                    P.mm(pc[:, 0:n], diag[:, j * 2 + c, :], zp[:, c, o0 + j:o0 + j + n], start=(j == 0), stop=(j == 30))
                P.op('act', 'activation', out=y[:, c, 0:n], in_=pc[:, 0:n], func=AF.Identity, bias=cols[:, 62 + c:63 + c])
                P.op('act', 'activation', out=y2[:, c, 0:n], in_=y[:, c, 0:n], func=AF.Square)
            p1 = psa.get()
            p2 = psb.get()
            for c in range(2):
                P.mm(p1[:, 0:n], self.onesf, y[:, c, 0:n], start=(c == 0), stop=(c == 1))
            for c in range(2):
                P.mm(p2[:, 0:n], self.onesf, y2[:, c, 0:n], start=(c == 0), stop=(c == 1))
            P.op('act', 'activation', out=mean[:, 0:n], in_=p1[:, 0:n], func=AF.Identity, scale=1.0 / 256)
            P.op('dve', 'tensor_tensor', out=rstd[:, 0:n], in0=mean[:, 0:n], in1=mean[:, 0:n], op=ALU.mult)
            P.op('dve', 'scalar_tensor_tensor', out=rstd[:, 0:n], in0=p2[:, 0:n], scalar=1.0 / 256, in1=rstd[:, 0:n], op0=ALU.mult, op1=ALU.subtract)
            P.op('act', 'activation', out=rstd[:, 0:n], in_=rstd[:, 0:n], func=AF.Sqrt, bias=self.eps)
            P.op('dve', 'reciprocal', out=rstd[:, 0:n], in_=rstd[:, 0:n])
            z = zo.get()
            for c in range(2):
                P.op('dve', 'tensor_tensor', out=y[:, c, 0:n], in0=y[:, c, 0:n], in1=mean[:, 0:n], op=ALU.subtract)
                P.op('dve', 'tensor_tensor', out=y[:, c, 0:n], in0=y[:, c, 0:n], in1=rstd[:, 0:n], op=ALU.mult)
                P.op('act', 'activation', out=z[:, c, 0:n], in_=y[:, c, 0:n], func=AF.Silu, scale=cols[:, 64 + c:65 + c], bias=cols[:, 66 + c:67 + c])
            P.dma(self.Z[2, :, n0:n0 + n].re("(c p) t -> p c t", p=128), z[:, :, 0:n], q='pool')
        es2.close()
        P.barrier()
    es.close()
    P.barrier()


MK.phase_conv = _cv


def _ft(self, l):
    P, cfg = self.P, self.cfg
    es = contextlib.ExitStack()
    W = P.sb([128, KD, 256], BF16, "wft", es=es)
    for k in range(KD):
        self.load_w(W[:, k, :], self.w_in[l, k * 128:(k + 1) * 128, FT_OFF:FT_OFF + 256])
    CD = P.sb([128, 2, 512], BF16, "cdt", es=es)
    P.dma(CD.a, self.cd.a.re("(c p) n -> p c n", p=128))
    hg = Rot([P.sb([128, KD, 512], BF16, "hg", es=es) for _ in range(2)])
    zf = Rot([P.sb([128, 2, 512], BF16, "zf", es=es) for _ in range(2)])
    psa = Rot([P.ps([128, 512], F32, es=es) for _ in range(2)])
    psd = Rot([P.ps([128, 512], F32, es=es) for _ in range(4)])
    for (s0, sn, seg) in cfg.segs:
        if self.skip_ctx and seg == 1:
            continue
        es2 = contextlib.ExitStack()
        ntile = sn // 128
        zcs = P.sb([128, ntile, 512], BF16, "zcs", es=es2)
        grp = [(a, n) for (a, n, sg_) in cfg.groups if sg_ == seg]
        for (n0, n) in grp:
            h = hg.get()
            P.dma(h[:, :, 0:n], self.hT[:, :, n0:n0 + n].re("k p t -> p k t"))
            z = zf.get()
            for c in range(2):
                pa = psa.get()
                for k in range(KD):
                    P.mm(pa[:, 0:n], W[:, k, c * 128:(c + 1) * 128], h[:, k, 0:n], start=(k == 0), stop=(k == KD - 1))
                P.op('act', 'activation', out=z[:, c, 0:n], in_=pa[:, 0:n], func=AF.Copy)
            for sub in range(n // 128):
                pd = psd.get()
                for c in range(2):
                    P.mm(pd.a, z[:, c, sub * 128:(sub + 1) * 128], CD[:, c, :], start=(c == 0), stop=(c == 1))
                ti = (n0 - s0) // 128 + sub
                P.op('dve', 'tensor_copy', out=zcs[:, ti, :], in_=pd.a)
        kb_n = min(512, sn)
        nkb = sn // kb_n
        tcs = Rot([P.sb([128, ntile, kb_n], BF16, "tc", es=es2) for _ in range(2)])
        tss = Rot([P.sb([128, ntile, kb_n], BF16, "ts", es=es2) for _ in range(2)])
        zo = Rot([P.sb([128, 2, 512], BF16, "zo", es=es2) for _ in range(2)])
        rstride = cfg.t_lat // sn
        for kb in range(nkb):
            tc_ = tcs.get()
            ts_ = tss.get()
            if rstride == 1:
                P.dma(tc_.a, self.dftc[:, kb * kb_n:(kb + 1) * kb_n].re("(t p) n -> p t n", p=128))
                P.dma(ts_.a, self.dfts[:, kb * kb_n:(kb + 1) * kb_n].re("(t p) n -> p t n", p=128))
            else:
                P.dma(tc_.a, self.dftc.a.re("(r s) n -> r s n", s=rstride)[:, 0, kb * kb_n:(kb + 1) * kb_n].re("(t p) n -> p t n", p=128))
                P.dma(ts_.a, self.dfts.a.re("(r s) n -> r s n", s=rstride)[:, 0, kb * kb_n:(kb + 1) * kb_n].re("(t p) n -> p t n", p=128))
            z = zo.get()
            for c in range(2):
                pd = psd.get()
                for ti in range(ntile):
                    P.mm(pd[:, 0:kb_n], zcs[:, ti, c * 128:(c + 1) * 128], tc_[:, ti, :], start=(ti == 0), stop=False)
                    P.mm(pd[:, 0:kb_n], zcs[:, ti, 256 + c * 128:256 + (c + 1) * 128], ts_[:, ti, :], start=False, stop=(ti == ntile - 1))
                P.op('act', 'activation', out=z[:, c, 0:kb_n], in_=pd[:, 0:kb_n], func=AF.Identity, scale=float(np.sqrt(rstride)))
            P.dma(self.Z[3, :, s0 + kb * kb_n:s0 + (kb + 1) * kb_n].re("(c p) t -> p c t", p=128), z[:, :, 0:kb_n], q='pool')
        es2.close()
        P.barrier()
    es.close()
    P.barrier()


MK.phase_fnet = _ft


def _merge(self, l):
    P, cfg = self.P, self.cfg
    es = contextlib.ExitStack()
    Wg = P.sb([128, KD, 4096], BF16, "wgate", es=es)
    for k in range(KD):
        for i in range(4):
            self.load_w(Wg[:, k, i * 1024:(i + 1) * 1024], self.w_in[l, k * 128:(k + 1) * 128, GATE_OFF + i * 1024:GATE_OFF + (i + 1) * 1024])
    Wb = P.sb([128, 4, 2, 1024], BF16, "wbr", es=es)
    for i in range(4):
        for c in range(2):
            self.load_w(Wb[:, i, c, :], self.w_branch[l, i, c * 128:(c + 1) * 128, :])
    Wo = P.sb([128, KD, 1024], BF16, "wout", es=es)
    for k in range(KD):
        self.load_w(Wo[:, k, :], self.w_out[l, k * 128:(k + 1) * 128, :])
    gate1 = self.modT[:, 16:24, :]
    hg = Rot([P.sb([128, KD, 512], BF16, "hg", es=es) for _ in range(2)])
    zg = Rot([P.sb([128, 4, 2, 512], BF16, "zg", es=es) for _ in range(2)])
    xg = Rot([P.sb([128, KD, 512], F32, "xg", es=es) for _ in range(2)])
    yT = P.sb([128, KD, 512], BF16, "yT", es=es)
    psg = Rot([P.ps([128, 512], F32, es=es) for _ in range(3)])
    psl = Rot([P.ps([128, 512], F32, es=es) for _ in range(3)])
    pso = Rot([P.ps([128, 512], F32, es=es) for _ in range(2)])
    sg = Rot([P.sb([128, 512], F32, "sg", es=es) for _ in range(3)])
    acc = Rot([P.sb([128, 512], F32, "acc", es=es) for _ in range(2)])
    for (n0, n, seg) in cfg.groups:
        if self.skip_ctx and seg == 1:
            continue
        h = hg.get()
        P.dma(h[:, :, 0:n], self.hT[:, :, n0:n0 + n].re("k p t -> p k t"))
        z = zg.get()
        for i in range(4):
            P.dma(z[:, i, :, 0:n], self.Z[i, :, n0:n0 + n].re("(c p) t -> p c t", p=128))
        x = xg.get()
        P.dma(x[:, :, 0:n], self.xT[:, :, n0:n0 + n].re("k p t -> p k t"))
        for dc in range(KD):
            a = acc.get()
            for i in range(4):
                pg = psg.get()
                for k in range(KD):
                    P.mm(pg[:, 0:n], Wg[:, k, i * 1024 + dc * 128:i * 1024 + (dc + 1) * 128], h[:, k, 0:n], start=(k == 0), stop=(k == KD - 1))
                s = sg.get()
                P.op('act', 'activation', out=s[:, 0:n], in_=pg[:, 0:n], func=AF.Sigmoid)
                pl = psl.get()
                for c in range(2):
                    P.mm(pl[:, 0:n], Wb[:, i, c, dc * 128:(dc + 1) * 128], z[:, i, c, 0:n], start=(c == 0), stop=(c == 1))
                if i == 0:
                    P.op('dve', 'tensor_tensor', out=a[:, 0:n], in0=pl[:, 0:n], in1=s[:, 0:n], op=ALU.mult)
                else:
                    P.op('dve', 'tensor_tensor', out=s[:, 0:n], in0=pl[:, 0:n], in1=s[:, 0:n], op=ALU.mult)
                    if i < 3:
                        P.op(self.merge_eng, 'tensor_tensor', out=a[:, 0:n], in0=a[:, 0:n], in1=s[:, 0:n], op=ALU.add)
                    else:
                        P.op(self.merge_eng, 'tensor_tensor', out=yT[:, dc, 0:n], in0=a[:, 0:n], in1=s[:, 0:n], op=ALU.add)
        for dc in range(KD):
            po = pso.get()
            for k in range(KD):
                P.mm(po[:, 0:n], Wo[:, k, dc * 128:(dc + 1) * 128], yT[:, k, 0:n], start=(k == 0), stop=(k == KD - 1))
            P.op('dve', 'scalar_tensor_tensor', out=x[:, dc, 0:n], in0=po[:, 0:n], scalar=gate1[:, dc, seg:seg + 1], in1=x[:, dc, 0:n], op0=ALU.mult, op1=ALU.add)
        P.dma(self.xT[:, :, n0:n0 + n].re("k p t -> p k t"), x[:, :, 0:n], q='pool')
    es.close()
    P.barrier()


MK.phase_merge = _merge


def _final(self):
    P, cfg = self.P, self.cfg
    es = contextlib.ExitStack()
    gT = self.load_rows_T([self.final_norm_g.a.re("(j p) -> j p", p=128)], es)
    gs = P.sb([128, KD, 2], F32, "gsf", es=es)
    P.op('dve', 'tensor_copy', out=gs.a, in_=gT.a.unsq(2).bc([128, KD, 2]))
    xg = Rot([P.sb([128, KD, 512], F32, "xg", es=es) for _ in range(2)])
    yg = Rot([P.sb([128, KD, 512], F32, "yg", es=es) for _ in range(2)])
    sq = P.sb([128, KD, 512], BF16, "sq", es=es)
    pss = Rot([P.ps([128, 512], F32, es=es) for _ in range(2)])
    pst = Rot([P.ps([128, 512], F32, es=es) for _ in range(4)])
    tmps = Rot([P.sb([128, 512], F32, "nt", es=es) for _ in range(4)])
    rsb = P.sb([128, 512], F32, "rsb", es=es)
    ot = Rot([P.sb([128, D], F32, "ot", es=es) for _ in range(3)])
    for (n0, n, seg) in cfg.groups:
        if seg == 1:
            continue
        x = xg.get()
        y = yg.get()
        P.dma(x[:, :, 0:n], self.xT[:, :, n0:n0 + n].re("k p t -> p k t"))
        self.norm_group(x[:, :, 0:n], n, gs, None, 0, y[:, :, 0:n], pss, tmps, sq, rsb)
        for sub in range(n // 128):
            o = ot.get()
            for half in range(2):
                ps = pst.get()
                for j in range(4):
                    k = half * 4 + j
                    P.op('pe', 'transpose', out=ps[:, j * 128:(j + 1) * 128], in_=y[:, k, sub * 128:(sub + 1) * 128], identity=self.identf)
                P.op('act', 'activation', out=o[:, half * 512:(half + 1) * 512], in_=ps.a, func=AF.Copy)
            r0 = n0 - cfg.t_ctx + sub * 128
            P.dma(self.out[r0:r0 + 128, :], o.a, q='pool')
    es.close()
    P.barrier()


MK.phase_final = _final


def _ml(self, l):
    P, cfg = self.P, self.cfg
    NT, NCH = cfg.nt, cfg.nch
    nctx = cfg.t_ctx // 64
    order_b = list(range(nctx - 1, -1, -1)) + list(range(NCH - 1, nctx - 1, -1))
    order_f = list(range(NCH))
    es = contextlib.ExitStack()
    biasA = P.sb([64, 1], F32, "biasA", es=es)
    biasB = P.sb([64, 1], F32, "biasB", es=es)
    P.op('pool', 'memset', ap=biasA.a, constant=0.0)
    P.op('pool', 'memset', ap=biasB.a, constant=0.0)
    gb = self.ml_gate_b
    P.dma(biasA[0:4, :], gb[l, 0:4].re("(a b) -> a b", b=1))
    P.dma(biasA[32:36, :], gb[l, 4:8].re("(a b) -> a b", b=1))
    P.dma(biasB[0:4, :], gb[l, 8:12].re("(a b) -> a b", b=1))
    P.dma(biasB[32:36, :], gb[l, 12:16].re("(a b) -> a b", b=1))
    cols = self.load_rows_T([self.ml_conv_w[l].re("j (c p) -> (j c) p", p=128), self.ml_conv_b[l].re("(c p) -> c p", p=128)], es)
    colsB = self.load_rows_T([self.ml_conv_b[l].re("(c p) -> c p", p=64)], es)
    diag = P.sb([128, 12, 128], BF16, "diag3", es=es)
    for jc in range(12):
        P.op('dve', 'tensor_scalar', out=diag[:, jc, :], in0=self.identf, scalar1=cols[:, jc:jc + 1], scalar2=None, op0=ALU.mult)
    qk8 = [P.sb([64, NT], BF16, "qk8_%d" % i, es=es) for i in range(8)]
    Atok = P.sb([64, NCH, 8], F32, "Atok", es=es)
    Btok = P.sb([64, NCH, 8], F32, "Btok", es=es)
    Ctok = P.sb([64, NCH, 8], F32, "Ctok", es=es)
    decb = P.sb([64, 8, NCH], F32, "decb", es=es)
    emb = P.sb([64, 8, NCH], F32, "emb", es=es)
    es1 = contextlib.ExitStack()
    Wqk = P.sb([128, KD, 512], BF16, "wqk", es=es1)
    for k in range(KD):
        self.load_w(Wqk[:, k, :], self.w_in[l, k * 128:(k + 1) * 128, 0:512])
    pre = [P.sb([128, NT + 4], BF16, "pre%d" % i, es=es1) for i in range(4)]
    for i in range(4):
        P.op('pool', 'memset', ap=pre[i].a, constant=0.0)
    hg = Rot([P.sb([128, KD, 512], BF16, "hg", es=es1) for _ in range(2)])
    psQ = Rot([P.ps([128, 512], F32, es=es1) for _ in range(2)])

    def poff(seg):
        return 1 if seg == 1 else cfg.t_ctx + 3

    for (n0, n, seg) in cfg.groups:
        s0 = 0 if seg == 1 else cfg.t_ctx
        h = hg.get()
        P.dma(h[:, :, 0:n], self.hT[:, :, n0:n0 + n].re("k p t -> p k t"))
        for ci in range(4):
            pq = psQ.get()
            for k in range(KD):
                P.mm(pq[:, 0:n], Wqk[:, k, ci * 128:(ci + 1) * 128], h[:, k, 0:n], start=(k == 0), stop=(k == KD - 1))
            o0 = poff(seg) + n0 - s0
            P.op('dve', 'tensor_copy', out=pre[ci][:, o0:o0 + n], in_=pq[:, 0:n])
    for (n0, n, seg) in cfg.groups:
        s0 = 0 if seg == 1 else cfg.t_ctx
        for ci in range(4):
            for hp in range(2):
                pq = psQ.get()
                o0 = poff(seg) + n0 - s0 - 1
                for j in range(3):
                    P.mm(pq[0:64, 0:n], diag[:, j * 4 + ci, hp * 64:(hp + 1) * 64], pre[ci][:, o0 + j:o0 + j + n], start=(j == 0), stop=(j == 2))
                P.op('act', 'activation', out=qk8[ci * 2 + hp][:, n0:n0 + n], in_=pq[0:64, 0:n], func=AF.Silu, bias=colsB[0:64, ci * 2 + hp:ci * 2 + hp + 1])
    for h8 in range(4, 8):
        P.op('dve', 'tensor_scalar', out=qk8[h8].a, in0=qk8[h8].a, scalar1=0.125, scalar2=None, op0=ALU.mult)
    es1.close()
    P.barrier()
    esA = contextlib.ExitStack()
    LI = P.sb([64, NT], F32, "LI", es=esA)
    LF = P.sb([64, NT], F32, "LF", es=esA)
    es1 = contextlib.ExitStack()
    WgA = P.sb([128, KD, 64], BF16, "wga", es=es1)
    WgB = P.sb([128, KD, 64], BF16, "wgb", es=es1)
    P.op('pool', 'memset', ap=WgA.a, constant=0.0)
    P.op('pool', 'memset', ap=WgB.a, constant=0.0)
    for k in range(KD):
        rows = slice(k * 128, (k + 1) * 128)
        self.load_w(WgA[:, k, 0:4], self.w_in[l, rows, 768:772])
        self.load_w(WgA[:, k, 32:36], self.w_in[l, rows, 772:776])
        self.load_w(WgB[:, k, 0:4], self.w_in[l, rows, 776:780])
        self.load_w(WgB[:, k, 32:36], self.w_in[l, rows, 780:784])
    hg = Rot([P.sb([128, KD, 512], BF16, "hg", es=es1) for _ in range(2)])
    psA = Rot([P.ps([128, 512], F32, es=es1) for _ in range(2)])
    sgt = Rot([P.sb([64, 512], F32, "sgt", es=es1) for _ in range(2)])
    for (n0, n, seg) in cfg.groups:
        h = hg.get()
        P.dma(h[:, :, 0:n], self.hT[:, :, n0:n0 + n].re("k p t -> p k t"))
        pa = psA.get()
        for k in range(KD):
            P.mm(pa[0:64, 0:n], WgA[:, k, :], h[:, k, 0:n], start=(k == 0), stop=(k == KD - 1))
        P.op('act', 'activation', out=LI[:, n0:n0 + n], in_=pa[0:64, 0:n], func=AF.Identity, bias=biasA.a)
        pb = psA.get()
        for k in range(KD):
            P.mm(pb[0:64, 0:n], WgB[:, k, :], h[:, k, 0:n], start=(k == 0), stop=(k == KD - 1))
        s = sgt.get()
        P.op('act', 'activation', out=s[:, 0:n], in_=pb[0:64, 0:n], func=AF.Sigmoid, bias=biasB.a)
        P.op('act', 'activation', out=LF[:, n0:n0 + n], in_=s[:, 0:n], func=AF.Ln)
    es1.close()
    P.barrier()
    es1 = contextlib.ExitStack()
    ones = P.sb([64, NT], BF16, "ones", es=es1)
    P.op('pool', 'memset', ap=ones.a, constant=1.0)
    cs = P.sb([64, NT], F32, "cs", es=es1)
    P.op('dve', 'tensor_tensor_scan', out=cs.a, data0=ones.a, data1=LF.a, initial=0.0, op0=ALU.mult, op1=ALU.add)
    j3 = lambda b: b.a.re("p (c j) -> p c j", j=64)
    sm = lambda nm: P.sb([64, NCH], F32, nm, es=es1)
    base, tot, wmax, cmax, m_in, m_new, Mx, dec, em = [sm(x) for x in ("base", "tot", "wmax", "cmax", "m_in", "m_new", "Mx", "dec", "em")]
    bc3 = lambda b: b.a.unsq(2).bc([64, NCH, 64])
    P.op('dve', 'tensor_tensor', out=base.a, in0=j3(cs)[:, :, 0], in1=j3(LF)[:, :, 0], op=ALU.subtract)
    BC = cs
    P.op('dve', 'tensor_tensor', out=j3(BC), in0=j3(cs), in1=bc3(base), op=ALU.subtract)
    P.op('dve', 'tensor_copy', out=tot.a, in_=j3(BC)[:, :, 63])
    P.op('dve', 'tensor_tensor', out=j3(BC)[32:64], in0=bc3(tot)[32:64], in1=j3(BC)[32:64], op=ALU.subtract)
    P.op('dve', 'tensor_tensor', out=BC[32:64, :], in0=BC[32:64, :], in1=LF[32:64, :], op=ALU.add)
    Wt = LF
    Cq = LI
    P.op('dve', 'tensor_tensor', out=Cq.a, in0=LI.a, in1=BC.a, op=ALU.subtract)
    P.op('dve', 'tensor_tensor', out=j3(Wt), in0=j3(Cq), in1=bc3(tot), op=ALU.add)
    P.op('dve', 'tensor_reduce', out=wmax.a, in_=j3(Wt), axis=AX.X, op=ALU.max)
    P.op('dve', 'tensor_reduce', out=cmax.a, in_=j3(Cq), axis=AX.X, op=ALU.max)
    zero = P.sb([64, 1], F32, "zero", es=es1)
    P.op('pool', 'memset', ap=zero.a, constant=0.0)
    P.op('pool', 'memset', ap=m_in.a, constant=0.0)
    for (rows, order) in ((slice(0, 32), order_f), (slice(32, 64), order_b)):
        prev = None
        for i, c in enumerate(order):
            pv_ = zero[rows, 0:1] if prev is None else m_new[rows, prev:prev + 1]
            if prev is not None:
                P.op('dve', 'tensor_copy', out=m_in[rows, c:c + 1], in_=pv_)
            P.op('dve', 'tensor_scalar', out=m_new[rows, c:c + 1], in0=pv_, scalar1=tot[rows, c:c + 1], scalar2=wmax[rows, c:c + 1], op0=ALU.add, op1=ALU.max)
            prev = c
    P.op('dve', 'tensor_tensor', out=Mx.a, in0=m_in.a, in1=cmax.a, op=ALU.max)
    P.op('dve', 'tensor_tensor', out=j3(Cq), in0=j3(Cq), in1=bc3(Mx), op=ALU.subtract)
    P.op('act', 'activation', out=Cq.a, in_=Cq.a, func=AF.Exp)
    P.op('dve', 'tensor_tensor', out=j3(Wt), in0=j3(Wt), in1=bc3(m_new), op=ALU.subtract)
    P.op('act', 'activation', out=Wt.a, in_=Wt.a, func=AF.Exp)
    P.op('dve', 'tensor_tensor', out=j3(BC), in0=j3(BC), in1=bc3(Mx), op=ALU.add)
    P.op('act', 'activation', out=BC.a, in_=BC.a, func=AF.Exp, scale=-1.0)
    P.op('dve', 'tensor_tensor', out=dec.a, in0=tot.a, in1=m_in.a, op=ALU.add)
    P.op('dve', 'tensor_tensor', out=dec.a, in0=dec.a, in1=m_new.a, op=ALU.subtract)
    P.op('act', 'activation', out=dec.a, in_=dec.a, func=AF.Exp)
    P.op('dve', 'tensor_tensor', out=em.a, in0=m_in.a, in1=Mx.a, op=ALU.subtract)
    P.op('act', 'activation', out=em.a, in_=em.a, func=AF.Exp)
    pt = Rot([P.ps([64, 8, 64], F32, es=es1) for _ in range(2)])
    for (src, dst) in ((Cq, Atok), (Wt, Btok), (BC, Ctok)):
        for c0 in range(0, NCH, 8):
            nb = min(8, NCH - c0)
            p_ = pt.get()
            for cc in range(nb):
                P.op('pe', 'transpose', out=p_[:, cc, :], in_=src[:, (c0 + cc) * 64:(c0 + cc + 1) * 64], identity=self.identf[0:64, 0:64])
            P.op('dve', 'tensor_copy', out=dst[:, c0:c0 + nb, 0:4], in_=p_[:, 0:nb, 0:4])
            P.op('dve', 'tensor_copy', out=dst[:, c0:c0 + nb, 4:8], in_=p_[:, 0:nb, 32:36])
    pbq = Rot([P.ps([64, 4, NCH], F32, es=es1) for _ in range(2)])
    for (src, dst) in ((dec, decb), (em, emb)):
        for half in range(2):
            p_ = pbq.get()
            for gg in range(4):
                g = half * 4 + gg
                P.mm(p_[:, gg, :], self.cst[0:64, 384 + g * 64:384 + (g + 1) * 64], src.a, start=True, stop=True)
            P.op('dve', 'tensor_copy', out=dst[:, half * 4:half * 4 + 4, :], in_=p_.a)
    es1.close()
    esA.close()
    P.barrier()
    es3 = contextlib.ExitStack()
    v1 = P.sb([64, NCH, 4, 65], BF16, "v1", es=es3)
    P.op('pool', 'memset', ap=v1.a, constant=1.0)
    ktok = P.sb([64, NCH, 256], BF16, "ktok", es=es3)
    es3a = contextlib.ExitStack()
    Wv = P.sb([128, KD, 256], BF16, "wv", es=es3a)
    for k in range(KD):
        self.load_w(Wv[:, k, :], self.w_in[l, k * 128:(k + 1) * 128, 512:768])
    hg = Rot([P.sb([128, KD, 512], BF16, "hg", es=es3a) for _ in range(2)])
    psV = Rot([P.ps([128, 512], F32, es=es3a) for _ in range(2)])
    pk = Rot([P.ps([64, 256], BF16, es=es3a) for _ in range(2)])
    for (n0, n, seg) in cfg.groups:
        h = hg.get()
        P.dma(h[:, :, 0:n], self.hT[:, :, n0:n0 + n].re("k p t -> p k t"))
        for cc in range(n // 64):
            c = n0 // 64 + cc
            pv = psV.get()
            for k in range(KD):
                P.mm(pv[0:64, 0:256], h[:, k, cc * 64:(cc + 1) * 64], Wv[:, k, :], start=(k == 0), stop=(k == KD - 1))
            P.op('act', 'activation', out=v1[:, c, :, 0:64], in_=pv[0:64, 0:256].re("p (h d) -> p h d", h=4), func=AF.Copy)
    for c in range(NCH):
        p_ = pk.get()
        for hh in range(4):
            P.op('pe', 'transpose', out=p_[:, hh * 64:(hh + 1) * 64], in_=qk8[4 + hh][:, c * 64:(c + 1) * 64], identity=self.identb[0:64, 0:64])
        P.op('act', 'activation', out=ktok[:, c, :], in_=p_.a, func=AF.Copy)
    es3a.close()
    P.barrier()
    PS1 = [P.ps([64, 4, 64], F32, es=es3) for _ in range(2)]
    PS2 = [P.ps([64, 4, 65], F32, es=es3) for _ in range(2)]
    PS3 = [P.ps([64, 4, 65], F32, es=es3) for _ in range(2)]
    Cst = [P.sb([64, 4, 65], F32, "Cst", es=es3) for _ in range(2)]
    CnS = [P.sb([64, 4, 65], BF16, "CnS", es=es3) for _ in range(2)]
    for d_ in range(2):
        P.op('pool', 'memset', ap=Cst[d_].a, constant=0.0)
        P.op('pool', 'memset', ap=CnS[d_].a, constant=0.0)
    tmp = [Rot([P.sb([64, 4, 64], F32, "stmp", es=es3) for _ in range(2)]) for _ in range(2)]
    SD = [Rot([P.sb([64, 4, 64], BF16, "SD", es=es3) for _ in range(2)]) for _ in range(2)]
    kw = [Rot([P.sb([64, 4, 64], BF16, "kw", es=es3) for _ in range(2)]) for _ in range(2)]
    dn = [Rot([P.sb([64, 8], F32, "dn", es=es3) for _ in range(2)]) for _ in range(2)]
    hb = [Rot([P.sb([64, 4, 64], F32, "hbuf", es=es3) for _ in range(3)]) for _ in range(2)]
    Hd = [self.Hf, self.Hb]
    orders = [order_f, order_b]
    prep_lst = []
    prep_es = contextlib.ExitStack()
    if self.prep_in_mlstm:
        prep_lst = self.phase_peer_prep(l, es=prep_es, defer=True) or []
    per_step = (len(prep_lst) + NCH - 1) // NCH + 1
    for i in range(NCH):
        cs_ = [orders[d_][i] for d_ in range(2)]
        tks = [slice(c * 64, (c + 1) * 64) for c in cs_]
        qh = [[qk8[hh][:, tks[d_]] for hh in range(4)] for d_ in range(2)]
        kh = [[qk8[4 + hh][:, tks[d_]] for hh in range(4)] for d_ in range(2)]
        for d_ in range(2):
            for hh in range(4):
                P.mm(PS1[d_][:, hh, :], kh[d_][hh], qh[d_][hh])
        t_ = [tmp[d_].get() for d_ in range(2)]
        for d_ in range(2):
            P.op('dve', 'tensor_tensor', out=t_[d_].a, in0=PS1[d_].a, in1=Atok[:, cs_[d_], 4 * d_:4 * d_ + 4].unsq(2).bc([64, 4, 64]), op=ALU.mult)
        sd = [SD[d_].get() for d_ in range(2)]
        for d_ in range(2):
            P.op(self.scan_eng, 'tensor_tensor', out=sd[d_].a, in0=t_[d_].a, in1=self.maskb_t[:, d_, :, :], op=ALU.mult)
        kw_ = [kw[d_].get() for d_ in range(2)]
        for d_ in range(2):
            P.op(self.scan_eng, 'tensor_tensor', out=kw_[d_].a, in0=ktok[:, cs_[d_], :].re("p (h d) -> p h d", h=4), in1=Btok[:, cs_[d_], 4 * d_:4 * d_ + 4].unsq(2).bc([64, 4, 64]), op=ALU.mult)
        for d_ in range(2):
            for hh in range(4):
                P.mm(PS2[d_][:, hh, :], qh[d_][hh], CnS[d_][:, hh, :], start=True, stop=False)
                P.mm(PS2[d_][:, hh, :], sd[d_][:, hh, :], v1[:, cs_[d_], hh, :], start=False, stop=True)
        for d_ in range(2):
            for hh in range(4):
                P.mm(PS3[d_][:, hh, :], kw_[d_][:, hh, :], v1[:, cs_[d_], hh, :])
        for d_ in range(2):
            P.op(self.scan_eng, 'tensor_tensor', out=Cst[d_].a, in0=Cst[d_].a, in1=decb[:, 4 * d_:4 * d_ + 4, cs_[d_]].unsq(2).bc([64, 4, 65]), op=ALU.mult)
        for d_ in range(2):
            P.op('dve', 'tensor_tensor', out=Cst[d_].a, in0=Cst[d_].a, in1=PS3[d_].a, op=ALU.add)
        if i + 1 < NCH:
            for d_ in range(2):
                cn = orders[d_][i + 1]
                P.op(self.scan_eng, 'tensor_tensor', out=CnS[d_].a, in0=Cst[d_].a, in1=emb[:, 4 * d_:4 * d_ + 4, cn].unsq(2).bc([64, 4, 65]), op=ALU.mult)
        dd = [dn[d_].get() for d_ in range(2)]
        for d_ in range(2):
            P.op('act', 'activation', out=dd[d_][:, 0:4], in_=PS2[d_][:, :, 64], func=AF.Abs)
        for d_ in range(2):
            P.op('dve', 'tensor_tensor', out=dd[d_][:, 0:4], in0=dd[d_][:, 0:4], in1=Ctok[:, cs_[d_], 4 * d_:4 * d_ + 4], op=ALU.max)
        for d_ in range(2):
            P.op('dve', 'reciprocal', out=dd[d_][:, 4:8], in_=dd[d_][:, 0:4])
        for d_ in range(2):
            hbuf = hb[d_].get()
            P.op('dve', 'tensor_tensor', out=hbuf.a, in0=PS2[d_][:, :, 0:64], in1=dd[d_][:, 4:8].unsq(2).bc([64, 4, 64]), op=ALU.mult)
            P.dma(Hd[d_][cs_[d_]].re("p (h d) -> p h d", h=4), hbuf.a, q='sp')
        P.run_deferred(prep_lst, per_step)
    P.run_deferred(prep_lst)
    P.barrier()
    prep_es.close()
    es3.close()
    P.barrier()
    es4 = contextlib.ExitStack()
    Wo = P.sb([128, KD, 256], BF16, "wo", es=es4)
    for k in range(KD):
        self.load_w(Wo[:, k, :], self.w_in[l, k * 128:(k + 1) * 128, 784:1040])
    ng = P.sb([64, 256], F32, "mlng", es=es4)
    P.dma(ng.a, self.ml_norm_g[l:l + 1, :].pbc(64))
    hg = Rot([P.sb([128, KD, 512], BF16, "hg", es=es4) for _ in range(2)])
    hf_ = Rot([P.sb([64, 8, 256], F32, "hf", es=es4) for _ in range(2)])
    hb_ = Rot([P.sb([64, 8, 256], F32, "hb", es=es4) for _ in range(2)])
    sq = P.sb([64, 8, 256], F32, "sq4", es=es4)
    ss = P.sb([64, 32], F32, "ss4", es=es4)
    ob = Rot([P.sb([64, 8, 256], F32, "ob", es=es4) for _ in range(2)])
    hz = Rot([P.sb([64, 8, 256], BF16, "hz", es=es4) for _ in range(2)])
    zst = Rot([P.sb([128, 2, 512], BF16, "zst", es=es4) for _ in range(2)])
    pso = Rot([P.ps([128, 512], F32, es=es4) for _ in range(2)])
    pst = Rot([P.ps([128, 8, 64], BF16, es=es4) for _ in range(2)])
    for (n0, n, seg) in cfg.groups:
        if self.skip_ctx and seg == 1:
            continue
        nc_ = n // 64
        c0 = n0 // 64
        h = hg.get()
        P.dma(h[:, :, 0:n], self.hT[:, :, n0:n0 + n].re("k p t -> p k t"))
        a = hf_.get()
        b = hb_.get()
        P.dma(a[:, 0:nc_, :], self.Hf[c0:c0 + nc_].re("c p f -> p c f"))
        P.dma(b[:, 0:nc_, :], self.Hb[c0:c0 + nc_].re("c p f -> p c f"))
        P.op('dve', 'tensor_tensor', out=a[:, 0:nc_, :], in0=a[:, 0:nc_, :], in1=b[:, 0:nc_, :], op=ALU.add)
        P.op('act', 'activation', out=sq[:, 0:nc_, :], in_=a[:, 0:nc_, :], func=AF.Square)
        P.op('dve', 'tensor_reduce', out=ss[:, 0:nc_ * 4], in_=sq[:, 0:nc_, :].re("p c (h d) -> p (c h) d", h=4), axis=AX.X, op=ALU.add)
        P.op('act', 'activation', out=ss[:, 0:nc_ * 4], in_=ss[:, 0:nc_ * 4], func=AF.Sqrt, scale=1.0 / 64, bias=self.eps[0:64, :])
        P.op('dve', 'reciprocal', out=ss[:, 0:nc_ * 4], in_=ss[:, 0:nc_ * 4])
        P.op('dve', 'tensor_tensor', out=a[:, 0:nc_, :].re("p c (h d) -> p (c h) d", h=4), in0=a[:, 0:nc_, :].re("p c (h d) -> p (c h) d", h=4),
             in1=ss[:, 0:nc_ * 4].unsq(2).bc([64, nc_ * 4, 64]), op=ALU.mult)
        P.op('dve', 'tensor_tensor', out=a[:, 0:nc_, :], in0=a[:, 0:nc_, :], in1=ng.a.unsq(1).bc([64, nc_, 256]), op=ALU.mult)
        o = ob.get()
        for cc in range(nc_):
            po = pso.get()
            for k in range(KD):
                P.mm(po[0:64, 0:256], h[:, k, cc * 64:(cc + 1) * 64], Wo[:, k, :], start=(k == 0), stop=(k == KD - 1))
            P.op('act', 'activation', out=o[:, cc, :], in_=po[0:64, 0:256], func=AF.Sigmoid)
        z = hz.get()
        P.op('dve', 'tensor_tensor', out=z[:, 0:nc_, :], in0=a[:, 0:nc_, :], in1=o[:, 0:nc_, :], op=ALU.mult)
        zs = zst.get()
        for half in range(2):
            p_ = pst.get()
            for cc in range(nc_):
                P.op('pe', 'transpose', out=p_[:, cc, :], in_=z[:, cc, half * 128:(half + 1) * 128], identity=self.identb[0:64, 0:64])
            P.op('act', 'activation', out=zs[:, half, 0:n], in_=p_[:, 0:nc_, :].re("p c t -> p (c t)"), func=AF.Copy)
        P.dma(self.Z[0, :, n0:n0 + n].re("(c p) t -> p c t", p=128), zs[:, :, 0:n], q='pool')
    es4.close()
    es.close()
    P.barrier()


MK.phase_mlstm = _ml


def _peer_prep(self, l, es=None, defer=False):
    P = self.P
    if self.prep_done.get(l):
        return None
    self.prep_done[l] = True
    own_es = es is None
    es = es or contextlib.ExitStack()
    lst = []
    if defer:
        P.deferred = lst
    ub = Rot([P.sb([128, D], BF16, "ub", es=es) for _ in range(3)])
    vb = Rot([P.sb([128, D], BF16, "vb", es=es) for _ in range(3)])
    uo = Rot([P.sb([128, D], BF16, "uo", es=es) for _ in range(3)])
    pst = Rot([P.ps([128, KD, 128], BF16, es=es) for _ in range(2 if defer else 3)])
    Uv = self.peer_u[l].re("(a b) d -> b a d", b=128)
    Vv = self.peer_v[l].re("(a b) d -> b a d", b=128)
    for e2 in range(128):
        u = ub.get()
        P.dma(u.a, Uv[e2], q='pool')
        p_ = pst.get()
        for k in range(KD):
            P.op('pe', 'transpose', out=p_[:, k, :], in_=u[:, k * 128:(k + 1) * 128], identity=self.identb)
        o = uo.get()
        if e2 % 2:
            P.op('act', 'activation', out=o.a, in_=p_.a.re("p k e -> p (k e)"), func=AF.Copy)
        else:
            P.op('dve', 'tensor_copy', out=o.a, in_=p_.a.re("p k e -> p (k e)"))
        P.dma(self.UTs[l % 2][e2], o.a, q='sp')
        v = vb.get()
        P.dma(v.a, Vv[e2], q='pool')
        P.dma(self.VBs[l % 2][e2], v.a, q='sp')
    P.deferred = None
    if own_es:
        es.close()
        P.barrier()
    return lst


MK.phase_peer_prep = _peer_prep


def _peer(self, l):
    P, cfg = self.P, self.cfg
    es = contextlib.ExitStack()
    gs, sh, gate2 = self.mod_scale_shift(l, 1, es)
    Wq = P.sb([128, KD, 2048], BF16, "wq", es=es)
    for k in range(KD):
        self.load_w(Wq[:, k, :], self.peer_w_q[l, k * 128:(k + 1) * 128, :])
    psA = Rot([P.ps([128, 512], F32, es=es) for _ in range(2)])
    Wps = Rot([P.ps([128, 512], F32, es=es) for _ in range(2)])
    acc = [P.ps([128, 2, 256], F32, es=es) for _ in range(4)]
    kl = P.sb([128, 2, 128], BF16, "kl", es=es)
    self.load_w(kl.a, self.peer_keys[l].re("p e k -> e p k"))
    keysT = P.sb([128, 2, 128], BF16, "keysT", es=es)
    for p in range(2):
        pk_ = Wps.get()
        pkb = pk_.a.re("q (a b) -> q a b", b=128)
        klf = P.sb([128, 128], F32, "klf", es=es)
        P.op('dve', 'tensor_copy', out=klf.a, in_=kl[:, p, :])
        P.op('pe', 'transpose', out=pk_[:, 0:128], in_=klf.a, identity=self.identf)
        P.op('dve', 'tensor_copy', out=keysT[:, p, :], in_=pk_[:, 0:128])
    xgs = [P.sb([128, KD, 256], F32, "xg", es=es) for _ in range(2)]
    h2s = [P.sb([128, KD, 256], BF16, "h2", es=es) for _ in range(2)]
    qT = P.sb([128, 16, 256], BF16, "qT", es=es)
    S = P.sb([128, 16, 128], F32, "S", es=es)
    S2 = P.sb([128, 16, 128], F32, "S2", es=es)
    V1 = P.sb([128, 16, 16], F32, "V1", es=es)
    I1u = P.sb([128, 16, 16], U32, "I1u", es=es)
    I1f = P.sb([128, 16, 16], F32, "I1f", es=es)
    cand = P.sb([128, 8, 256], F32, "cand", es=es)
    cand2 = S2
    SC = P.sb([128, 8, 16], F32, "SC", es=es)
    POSu = P.sb([128, 8, 16], U32, "POSu", es=es)
    PIu = P.sb([128, 8, 16], U32, "PIu", es=es)
    PJu = P.sb([128, 8, 16], U32, "PJu", es=es)
    PIf = P.sb([128, 128], F32, "PIf", es=es)
    PJf = P.sb([128, 128], F32, "PJf", es=es)
    OH = S
    E1 = P.sb([128, 128], F32, "E1", es=es)
    E2 = P.sb([128, 128], F32, "E2", es=es)
    G = P.sb([128, 128], F32, "G", es=es)
    sm = P.sb([128, 16], F32, "sm", es=es)
    E1T = P.sb([128, 256], BF16, "E1T", es=es)
    E2T = P.sb([128, 256], BF16, "E2T", es=es)
    GT = P.sb([128, 256], BF16, "GT", es=es)
    if self.wb4:
        An4 = Rot([P.sb([128, 4, 128], BF16, "An", es=es) for _ in range(2)])
        Bn4 = Rot([P.sb([128, 4, 128], BF16, "Bn", es=es) for _ in range(2)])
        iota4 = P.sb([128, 4, 128], BF16, "iota4", es=es)
        for tt in range(4):
            P.op('dve', 'tensor_copy', out=iota4[:, tt, :], in_=self.iota128b_t.a)
    else:
        An = Rot([P.sb([128, 128], BF16, "An", es=es) for _ in range(6)])
        Bn = Rot([P.sb([128, 128], BF16, "Bn", es=es) for _ in range(6)])
    Wbuf = P.sb([128, 256, 128], BF16, "Wbuf", es=es)
    ut = Rot([P.sb([128, KD, 128], BF16, "ut", es=es) for _ in range(4)])
    vt = Rot([P.sb([128, D], BF16, "vtb", es=es) for _ in range(4)])
    Ab = Rot([P.sb([128, 256], BF16, "Ab", es=es) for _ in range(3)])
    AW = Rot([P.sb([128, 256], BF16, "AW", es=es) for _ in range(3)])
    i16 = self.iota16
    V1s = [V1] + [V1.sub() for _ in range(15)]
    I1s = [I1u] + [I1u.sub() for _ in range(15)]
    S2s = [S2] + [S2.sub() for _ in range(15)]
    cands = [cand] + [cand.sub() for _ in range(7)]
    SCs = [SC] + [SC.sub() for _ in range(7)]
    POSs = [POSu] + [POSu.sub() for _ in range(7)]
    zlhs = P.sb([128, 128], BF16, "zlhs", es=es)
    zrhs = P.sb([128, 512], BF16, "zrhs", es=es)
    P.op('pool', 'memset', ap=zlhs.a, constant=0.0)
    P.op('pool', 'memset', ap=zrhs.a, constant=0.0)
    glist = [g for g in cfg.groups256 if not (self.skip_ctx and g[2] == 1)]
    sq = qT[:, 0:8, :]
    tmps = Rot([cands[i][:, i, :] for i in range(3)])
    rsb = cands[3][:, 3, :]

    def front(gi):
        (n0, n, seg) = glist[gi]
        x = xgs[gi % 2]
        h2 = h2s[gi % 2]
        P.dma(x.a, self.xT[:, :, n0:n0 + n].re("k p t -> p k t"))
        self.norm_group(x.a, n, gs, sh, seg, h2.a, Wps, tmps, sq, rsb)
        for j in range(16):
            pq = Wps.get()
            for k in range(KD):
                P.mm(pq[:, 0:n], Wq[:, k, j * 128:(j + 1) * 128], h2[:, k, :], start=(k == 0), stop=(k == KD - 1))
            P.op('act', 'activation', out=qT[:, j, :], in_=pq[:, 0:n], func=AF.Copy)
        for sub in range(2 if 'topk' in self.peer_parts else 0):
            tsl = slice(sub * 128, (sub + 1) * 128)
            for j4 in range(4):
                pscr = Wps.get()
                for jj in range(4):
                    j = j4 * 4 + jj
                    P.mm(pscr[:, jj * 128:(jj + 1) * 128], qT[:, j, tsl], keysT[:, j % 2, :])
                P.op('act', 'activation', out=S[:, j4 * 4:j4 * 4 + 4, :], in_=pscr.a.re("p (a b) -> p a b", b=128), func=AF.Copy)
            for j in range(16):
                P.op('dve', 'max', out=V1s[j][:, j, 0:8], in_=S[:, j, :])
            for j in range(16):
                P.op('dve', 'max_index', out=I1s[j][:, j, 0:8], in_max=V1s[j][:, j, 0:8], in_values=S[:, j, :])
            for j in range(16):
                P.op('dve', 'match_replace', out=S2s[j][:, j, :], in_to_replace=V1s[j][:, j, 0:8], in_values=S[:, j, :], imm_value=NEG)
            for j in range(16):
                P.op('dve', 'max', out=V1s[j][:, j, 8:16], in_=S2s[j][:, j, :])
            for j in range(16):
                P.op('dve', 'max_index', out=I1s[j][:, j, 8:16], in_max=V1s[j][:, j, 8:16], in_values=S2s[j][:, j, :])
            P.op('dve', 'tensor_copy', out=I1f.a, in_=I1s[0].a, xr=I1s[1:])
            V1v = V1s[0].a.re("q (h p) i -> q h p i", p=2)
            P.op('dve', 'tensor_tensor', out=cands[0].a.re("q h (i j) -> q h i j", j=16), in0=V1v[:, :, 0, :].unsq(3).bc([128, 8, 16, 16]),
                 in1=V1v[:, :, 1, :].unsq(2).bc([128, 8, 16, 16]), op=ALU.add, xr=V1s[1:], xw=cands[1:])
            c2v = S2.a.re("q a b -> q (a b)").re("q (h c) -> q h c", h=8)
            ohv = S.a.re("q a b -> q (a b)").re("q (j i) -> q j i", i=16)
            def c2(h):
                return V(S2s[2 * h], c2v.ap[:, h, :])
            for h in range(8):
                P.op('dve', 'max', out=SCs[h][:, h, 0:8], in_=cands[h][:, h, :])
            for h in range(8):
                P.op('dve', 'max_index', out=POSs[h][:, h, 0:8], in_max=SCs[h][:, h, 0:8], in_values=cands[h][:, h, :])
            for h in range(8):
                P.op('dve', 'match_replace', out=c2(h), in_to_replace=SCs[h][:, h, 0:8], in_values=cands[h][:, h, :], imm_value=NEG, xw=[S2s[2 * h + 1]])
            for h in range(8):
                P.op('dve', 'max', out=SCs[h][:, h, 8:16], in_=c2(h), xr=[S2s[2 * h + 1]])
            for h in range(8):
                P.op('dve', 'max_index', out=POSs[h][:, h, 8:16], in_max=SCs[h][:, h, 8:16], in_values=c2(h), xr=[S2s[2 * h + 1]])
            P.op('dve', 'tensor_single_scalar', out=PIu.a, in_=POSs[0].a, scalar=4, op=ALU.logical_shift_right, xr=POSs[1:])
            P.op('dve', 'tensor_single_scalar', out=PJu.a, in_=POSs[0].a, scalar=15, op=ALU.bitwise_and, xr=POSs[1:])
            P.op('dve', 'tensor_copy', out=PIf.a, in_=PIu.a.re("q h k -> q (h k)"))
            P.op('dve', 'tensor_copy', out=PJf.a, in_=PJu.a.re("q h k -> q (h k)"))
            I1v = I1f.a.re("q (h p) i -> q h p i", p=2)
            for (Pf, pp, Eo) in ((PIf, 0, E1), (PJf, 1, E2)):
                P.op('dve', 'tensor_tensor', out=ohv, in0=i16.unsq(1).bc([128, 128, 16]), in1=Pf.a.unsq(2).bc([128, 128, 16]), op=ALU.is_equal)
                P.op('dve', 'tensor_tensor', out=ohv.re("q (h k) i -> q h k i", h=8), in0=ohv.re("q (h k) i -> q h k i", h=8),
                     in1=I1v[:, :, pp, :].unsq(2).bc([128, 8, 16, 16]), op=ALU.mult)
                P.op('dve', 'tensor_reduce', out=Eo.a, in_=ohv, axis=AX.X, op=ALU.add)
            Gv = G.a.re("q (h k) -> q h k", h=8)
            P.op('dve', 'tensor_tensor', out=Gv, in0=SC.a, in1=SC[:, :, 0:1].bc([128, 8, 16]), op=ALU.subtract, xr=SCs[1:])
            P.op('act', 'activation', out=G.a, in_=G.a, func=AF.Exp)
            P.op('dve', 'tensor_reduce', out=sm[:, 0:8], in_=Gv, axis=AX.X, op=ALU.add)
            P.op('dve', 'reciprocal', out=sm[:, 8:16], in_=sm[:, 0:8])
            P.op('dve', 'tensor_tensor', out=Gv, in0=Gv, in1=sm[:, 8:16].unsq(2).bc([128, 8, 16]), op=ALU.mult)
            for (src, dst) in ((E1, E1T), (E2, E2T), (G, GT)):
                ptr = Wps.get()
                P.op('pe', 'transpose', out=ptr[:, 0:128], in_=src.a, identity=self.identf)
                P.op('act', 'activation', out=dst[:, tsl], in_=ptr[:, 0:128], func=AF.Copy)
    def run_front(gi):
        lst = []
        P.deferred = lst
        front(gi)
        P.deferred = None
        return lst

    P.run_deferred(run_front(0))
    for gi, (n0, n, seg) in enumerate(glist):
        x = xgs[gi % 2]
        h2 = h2s[gi % 2]
        for t4 in range(n // 4 if 'wb' in self.peer_parts else 0):
            wp = Wps.get()
            if self.wb4:
                a_ = An4.get()
                b_ = Bn4.get()
                t0_ = t4 * 4
                P.op('dve', 'tensor_tensor', out=a_.a, in0=iota4.a, in1=E1T[:, t0_:t0_ + 4].unsq(2).bc([128, 4, 128]), op=ALU.is_equal)
                P.op('dve', 'tensor_tensor', out=a_.a, in0=a_.a, in1=GT[:, t0_:t0_ + 4].unsq(2).bc([128, 4, 128]), op=ALU.mult)
                P.op('dve', 'tensor_tensor', out=b_.a, in0=iota4.a, in1=E2T[:, t0_:t0_ + 4].unsq(2).bc([128, 4, 128]), op=ALU.is_equal)
                for tt in range(4):
                    P.mm(wp[:, tt * 128:(tt + 1) * 128], a_[:, tt, :], b_[:, tt, :])
            for tt in range(0 if self.wb4 else 4):
                t = t4 * 4 + tt
                a_ = An.get()
                b_ = Bn.get()
                P.op('dve', 'tensor_scalar', out=a_.a, in0=self.iota128b_t.a, scalar1=E1T[:, t:t + 1], scalar2=GT[:, t:t + 1], op0=ALU.is_equal, op1=ALU.mult)
                P.op('dve', 'tensor_scalar', out=b_.a, in0=self.iota128b_t.a, scalar1=E2T[:, t:t + 1], scalar2=None, op0=ALU.is_equal)
                P.mm(wp[:, tt * 128:(tt + 1) * 128], a_.a, b_.a)
            P.op('act', 'activation', out=Wbuf[:, t4 * 4:t4 * 4 + 4, :], in_=wp.a.re("p (a b) -> p a b", b=128), func=AF.Copy)
        for bnk in range(4):
            P.mm(acc[bnk].a.re("p a b -> p (a b)"), zlhs.a, zrhs.a, start=True, stop=False)
        NE = 128 if 'ex' in self.peer_parts else 0

        def stage_a(e2):
            u = ut.get()
            v = vt.get()
            P.dma(u.a, self.UTs[l % 2][e2].re("p (k e) -> p k e", e=128), q='sp')
            P.dma(v.a, self.VBs[l % 2][e2], q='sp')
            pa = psA.get()
            for k in range(KD):
                P.mm(pa[:, 0:n], u[:, k, :], h2[:, k, :], start=(k == 0), stop=(k == KD - 1))
            ab = Ab.get()
            P.op('act', 'activation', out=ab.a, in_=pa[:, 0:n], func=AF.Gelu_apprx_tanh)
            aw = AW.get()
            P.op(self.aw_eng, 'tensor_tensor', out=aw.a, in0=ab.a, in1=Wbuf[:, :, e2], op=ALU.mult)
            return v, aw

        nxt = run_front(gi + 1) if gi + 1 < len(glist) else []
        if not getattr(self, 'peer_pipe', True):
            pre_nxt, nxt = nxt, []
        per_chunk = (len(nxt) + 119) // 120 if NE else len(nxt)
        pend = stage_a(0) if NE else None
        for e2 in range(NE):
            v, aw = pend
            if e2 + 1 < NE:
                pend = stage_a(e2 + 1)
            P.run_deferred(nxt, per_chunk)
            for dc in range(KD):
                P.mm(acc[dc // 2][:, dc % 2, :], v[:, dc * 128:(dc + 1) * 128], aw.a, start=False, stop=(e2 == 127 and dc % 2 == 1))
        P.run_deferred(nxt)
        if not getattr(self, 'peer_pipe', True):
            P.run_deferred(pre_nxt)
        for dc in range(KD):
            P.op('dve', 'scalar_tensor_tensor', out=x[:, dc, :], in0=acc[dc // 2][:, dc % 2, :], scalar=gate2[:, dc, seg:seg + 1], in1=x[:, dc, :], op0=ALU.mult, op1=ALU.add)
        P.dma(self.xT[:, :, n0:n0 + n].re("k p t -> p k t"), x.a, q='pool')
    es.close()
    P.barrier()


MK.phase_peer = _peer


def build_program(cfg, debug=False, phases=None, **opts):
    mk = MK(cfg, debug=debug)
    mk.skip_ctx = False
    mk.prep_done = {}
    mk.prep_in_mlstm = (phases is None) and opts.get('prep_in_mlstm', False)
    mk.peer_pipe = opts.get('peer_pipe', True)
    mk.wb4 = opts.get('wb4', False) or (phases is not None and 'wb4' in phases)
    mk.aw_eng = 'pool' if (opts.get('aw_pool', True) or (phases is not None and 'aw_pool' in phases)) else 'dve'
    mk.scan_eng = 'dve' if (opts.get('scan_dve', True) and not (phases is not None and 'scan_pool' in phases)) else 'pool'
    mk.merge_eng = 'dve' if (opts.get('merge_dve', True) and not (phases is not None and 'merge_pool' in phases)) else 'pool'
    mk.wb_pool = opts.get('wb_pool', False) or (phases is not None and 'wb_pool' in phases)
    mk.peer_parts = set(['topk', 'wb', 'ex']) if (phases is None or not any(p.startswith('pp_') for p in phases)) else set(p[3:] for p in phases if p.startswith('pp_'))
    on = lambda p: phases is None or p in phases
    mk.phase_init()
    for l in range(cfg.depth):
        mk.skip_ctx = False
        if on('mod'):
            mk.phase_mod(l)
        if on('norm1'):
            mk.phase_norm1(l)
        if on('mlstm'):
            mk.phase_mlstm(l)
        mk.skip_ctx = (l == cfg.depth - 1) and phases is None
        if on('gmlp'):
            mk.phase_gmlp(l)
        if on('conv'):
            mk.phase_conv(l)
        if on('fnet'):
            mk.phase_fnet(l)
        if on('merge'):
            mk.phase_merge(l)
        if on('prep'):
            mk.phase_peer_prep(l)
        if on('peer'):
            mk.phase_peer(l)
    mk.phase_final()
    mk.P.finish()
    return mk


_CACHE = {}


def make_in_maps(cfg, inp):
    dftc, dfts, cd, cst = host_consts(cfg)
    pos = grid_sincos(cfg.t_lat, D)
    L = cfg.depth
    shared = {"pos": pos, "dftc": dftc, "dfts": dfts, "cd": cd, "cst": cst}
    for k in ["w_ada", "b_ada", "norm1_g", "norm2_g", "w_in", "ml_conv_w", "ml_conv_b", "ml_gate_b", "gm_ln_g", "gm_ln_b",
              "gm_w_s", "cv_dw_w", "cv_dw_b", "cv_ln_g", "cv_ln_b", "w_branch", "w_out", "peer_w_q", "peer_keys",
              "peer_u", "peer_v", "final_norm_g"]:
        shared[k] = np.ascontiguousarray(np.asarray(inp[k], dtype=np.float32))
    shared["ml_norm_g"] = np.ascontiguousarray(np.asarray(inp["ml_norm_g"], np.float32).reshape(L, 256))
    shared["gm_b_s"] = np.ascontiguousarray(np.asarray(inp["gm_b_s"], np.float32).reshape(L, 512))
    x = np.asarray(inp["x"], np.float32)
    ctx = np.asarray(inp["ctx"], np.float32)
    c = np.asarray(inp["c"], np.float32)
    c_ctx = np.asarray(inp["c_ctx"], np.float32)
    maps = []
    for b in range(x.shape[0]):
        m = dict(shared)
        m["xin"] = np.ascontiguousarray(np.concatenate([ctx[b], x[b]], 0))
        m["cvec"] = np.ascontiguousarray(np.stack([c[b], c_ctx], 0))
        maps.append(m)
    return maps


def kernel(**inputs):
    x = np.asarray(inputs["x"])
    B, T, _ = x.shape
    depth = np.asarray(inputs["w_ada"]).shape[0]
    cfg = Cfg(depth=depth, t_lat=T, t_ctx=np.asarray(inputs["ctx"]).shape[1])
    key = (depth, T, cfg.t_ctx)
    if key not in _CACHE:
        _CACHE[key] = build_program(cfg)
    mk = _CACHE[key]
    maps = make_in_maps(cfg, inputs)
    res = run_bass_kernel_spmd(mk.nc, maps, core_ids=list(range(B)))
    return np.stack([np.asarray(r["out"], dtype=np.float32) for r in res.results], 0)
```

```python
import contextlib
import numpy as np
import ml_dtypes
import concourse.bass as bass
import concourse.mybir as mybir
from concourse.bass_utils import run_bass_kernel_spmd

F32 = mybir.dt.float32
BF16 = mybir.dt.bfloat16
I32 = mybir.dt.int32
U32 = mybir.dt.uint32
AF = mybir.ActivationFunctionType
ALU = mybir.AluOpType
AX = mybir.AxisListType

COMPUTE = ('pe', 'act', 'dve', 'pool')
NRING = 12
WRITE_KW = ('out', 'accum_out', 'ap')
EPS = 1e-6
NEG = -1.0e30


class V:
    __slots__ = ('buf', 'ap')

    def __init__(self, buf, ap):
        self.buf = buf
        self.ap = ap

    def __getitem__(self, k):
        return V(self.buf, self.ap[k])

    def re(self, pat, **kw):
        return V(self.buf, self.ap.rearrange(pat, **kw))

    def bc(self, shape):
        return V(self.buf, self.ap.to_broadcast(list(shape)))

    def unsq(self, ax):
        return V(self.buf, self.ap.unsqueeze(ax))

    def pbc(self, n):
        return V(self.buf, self.ap.partition_broadcast(n))


class Buf:
    __slots__ = ('t', 'lw', 'lwd', 'rd', 'name')

    def __init__(self, t, name=''):
        self.t = t
        self.lw = None
        self.lwd = {}
        self.rd = {}
        self.name = name

    def __getitem__(self, k):
        return V(self, self.t[k])

    @property
    def a(self):
        return V(self, self.t[:])

    def sub(self):
        return Buf(self.t, self.name + '_s')


class Prog:
    def __init__(self, nc):
        self.nc = nc
        self.ops = {e: [] for e in ('pe', 'act', 'dve', 'pool', 'sp')}
        self.count = {e: 0 for e in COMPUTE}
        self.known = {e: {} for e in self.ops}
        self.ndma = {'sp': 0, 'pool': 0, 'act': 0}
        self.es = contextlib.ExitStack()
        self.sems = {}
        for e in COMPUTE:
            self.sems[('c', e)] = self.es.enter_context(nc.semaphore('s_' + e))
        for q in ('sp', 'pool', 'act'):
            for i in range(NRING):
                self.sems[('d', q, i)] = self.es.enter_context(nc.semaphore('d_%s_%d' % (q, i)))
        self.nbuf = 0
        self.ninst = 0
        self.deferred = None

    def sb(self, shape, dt, name=None, es=None):
        self.nbuf += 1
        name = '%s_%d' % (name or 'sb', self.nbuf)
        t = (es or self.es).enter_context(self.nc.sbuf_tensor(name, list(shape), dt))
        return Buf(t, name)

    def ps(self, shape, dt, name=None, es=None):
        self.nbuf += 1
        name = '%s_%d' % (name or 'ps', self.nbuf)
        t = (es or self.es).enter_context(self.nc.psum_tensor(name, list(shape), dt))
        return Buf(t, name)

    def dram(self, name, shape, dt, kind='Internal'):
        t = self.nc.dram_tensor(name, list(shape), dt, kind=kind)
        return Buf(t, name)

    def _need(self, eng, tok, waits):
        if tok is None:
            return
        k, v = tok
        if self.known[eng].get(k, 0) >= v:
            return
        if waits.get(k, 0) < v:
            waits[k] = v

    def emit(self, eng, fn, reads=(), writes=(), dma=False):
        waits = {}
        own = ('c', eng) if (eng in COMPUTE and not dma) else None
        for b in reads:
            for k, v in b.lwd.items():
                self._need(eng, (k, v), waits)
            if b.lw is not None:
                if own is not None and b.lw[0] == own and eng == 'pe':
                    continue
                self._need(eng, b.lw, waits)
        for b in writes:
            if not dma:
                for k, v in b.lwd.items():
                    self._need(eng, (k, v), waits)
            if b.lw is not None:
                if not (own is not None and b.lw[0] == own and eng == 'pe'):
                    self._need(eng, b.lw, waits)
            for k, v in b.rd.items():
                if own is not None and k == own and eng == 'pe':
                    continue
                self._need(eng, (k, v), waits)
        if dma:
            i = self.ndma[eng]
            self.ndma[eng] = i + 1
            slot, gen = i % NRING, i // NRING
            key = ('d', eng, slot)
            if gen > 0:
                self._need(eng, (key, 16 * gen), waits)
            tok = (key, 16 * (gen + 1))
            inc = (key, 16)
        else:
            self.count[eng] += 1
            tok = (own, self.count[eng])
            inc = (own, 1)
        for k, v in waits.items():
            self.known[eng][k] = v
        self.ops[eng].append((fn, list(waits.items()), inc))
        self.ninst += 1
        for b in writes:
            if dma:
                b.lwd[tok[0]] = tok[1]
            else:
                b.lw = tok
                b.lwd = {}
                b.rd = {}
        for b in reads:
            if b in writes:
                continue
            if b.rd.get(tok[0], 0) < tok[1]:
                b.rd[tok[0]] = tok[1]
        return tok

    def run_deferred(self, lst, k=None):
        k = len(lst) if k is None else min(k, len(lst))
        for _ in range(k):
            eng, name, xr, xw, kw = lst.pop(0)
            self.op(eng, name, xr=xr, xw=xw, **kw)

    def op(self, eng, name, *, xr=(), xw=(), **kw):
        if self.deferred is not None:
            self.deferred.append((eng, name, xr, xw, kw))
            return None
        reads, writes, real = list(xr), list(xw), {}
        for k, v in kw.items():
            if isinstance(v, V):
                (writes if k in WRITE_KW else reads).append(v.buf)
                real[k] = v.ap
            else:
                real[k] = v
        isdma = name == 'dma_start'

        def fn(e, name=name, real=real):
            return getattr(e, name)(**real)
        return self.emit(eng, fn, reads, writes, dma=isdma)

    def dma(self, out, in_, q='sp', **kw):
        return self.op(q, 'dma_start', out=out, in_=in_, **kw)

    def mm(self, out, lhsT, rhs, start=True, stop=True):
        return self.op('pe', 'matmul', out=out, lhsT=lhsT, rhs=rhs, start=start, stop=stop)

    def barrier(self):
        toks = []
        for e in COMPUTE:
            if self.count[e] > 0:
                toks.append((('c', e), self.count[e]))
        for q, n in self.ndma.items():
            for i in range(max(0, n - NRING), n):
                toks.append((('d', q, i % NRING), 16 * (i // NRING + 1)))
        for e in self.ops:
            waits = {}
            for tok in toks:
                if tok[0] == ('c', e):
                    continue
                self._need(e, tok, waits)
            for k, v in waits.items():
                self.known[e][k] = v
            if waits:
                self.ops[e].append((None, list(waits.items()), None))

    def finish(self):
        self.barrier()
        nc = self.nc
        sems = self.sems
        ops = self.ops

        def replay(name, e):
            for fn, waits, inc in ops[name]:
                for k, v in waits:
                    e.wait_ge(sems[k], v)
                if fn is None:
                    continue
                ins = fn(e)
                if inc is not None:
                    ins.then_inc(sems[inc[0]], inc[1])

        with nc.Block() as block:
            @block.sync
            def _(e):
                replay('sp', e)

            @block.tensor
            def _(e):
                replay('pe', e)

            @block.scalar
            def _(e):
                replay('act', e)

            @block.vector
            def _(e):
                replay('dve', e)

            @block.gpsimd
            def _(e):
                replay('pool', e)
        self.es.close()


class Rot:
    def __init__(self, bufs):
        self.bufs = bufs
        self.i = 0

    def get(self):
        b = self.bufs[self.i % len(self.bufs)]
        self.i += 1
        return b


D = 1024
KD = 8
IN_COLS = 6416
GM_OFF = 1040
CV_OFF = 1552
FT_OFF = 2064
GATE_OFF = 2320
NEXP = 16384


class Cfg:
    def __init__(self, depth=4, t_lat=4096, t_ctx=256):
        self.depth = depth
        self.t_lat = t_lat
        self.t_ctx = t_ctx
        self.nt = t_lat + t_ctx
        self.groups = [(0, t_ctx, 1)] + [(t_ctx + i * 512, 512, 0) for i in range(t_lat // 512)]
        self.groups256 = [(i * 256, 256, 1 if i * 256 < t_ctx else 0) for i in range(self.nt // 256)]
        self.segs = [(0, t_ctx, 1), (t_ctx, t_lat, 0)]
        self.nch = self.nt // 64


def host_consts(cfg):
    T = cfg.t_lat
    k = np.arange(T, dtype=np.float64)
    ang = 2.0 * np.pi * ((k[:, None] * k[None, :]) % T) / T
    dftc = (np.cos(ang) / np.sqrt(T)).astype(ml_dtypes.bfloat16)
    dfts = (-np.sin(ang) / np.sqrt(T)).astype(ml_dtypes.bfloat16)
    c = np.arange(64, dtype=np.float64)
    a64 = 2.0 * np.pi * ((c[:, None] * c[None, :]) % 64) / 64
    cd = np.zeros((256, 512), np.float64)
    for g in range(4):
        cd[g * 64:(g + 1) * 64, g * 64:(g + 1) * 64] = np.cos(a64) / 8.0
        cd[g * 64:(g + 1) * 64, 256 + g * 64:256 + (g + 1) * 64] = np.sin(a64) / 8.0
    cd = cd.astype(ml_dtypes.bfloat16)
    cst = np.zeros((128, 1024), np.float32)
    cst[:, 0:128] = np.eye(128, dtype=np.float32)
    cst[:, 128:256] = np.arange(128, dtype=np.float32)[None, :]
    s = np.arange(64)
    cst[0:64, 256:320] = (s[:, None] <= s[None, :]).astype(np.float32)
    cst[0:64, 320:384] = (s[:, None] >= s[None, :]).astype(np.float32)
    for g in range(8):
        row = g if g < 4 else 32 + (g - 4)
        cst[row, 384 + g * 64:384 + (g + 1) * 64] = 1.0
    cst[:, 896:912] = np.arange(16, dtype=np.float32)[None, :]
    cst[:, 912] = EPS
    cst[:, 913] = 1.0
    return dftc, dfts, cd, cst


def grid_sincos(n_tok, d):
    rows = n_tok // 64
    n_freq = d // 4
    freq = (1.0 / (10000.0 ** (np.arange(n_freq, dtype=np.float32) / np.float32(n_freq)))).astype(np.float32)
    r = np.repeat(np.arange(rows, dtype=np.float32), 64)
    cc = np.tile(np.arange(64, dtype=np.float32), rows)
    ar = r[:, None] * freq[None, :]
    ac = cc[:, None] * freq[None, :]
    return np.concatenate([np.sin(ar), np.cos(ar), np.sin(ac), np.cos(ac)], axis=-1).astype(np.float32)


class MK:
    def __init__(self, cfg, debug=False):
        self.cfg = cfg
        self.nc = bass.Bass("TRN2", target_bir_lowering=False)
        self.P = Prog(self.nc)
        self.debug = debug
        P = self.P
        L = cfg.depth
        NT = cfg.nt
        din = lambda name, shape, dt=F32: P.dram(name, shape, dt, kind="ExternalInput")
        self.xin = din("xin", [NT, D])
        self.pos = din("pos", [cfg.t_lat, D])
        self.cvec = din("cvec", [2, D])
        self.w_ada = din("w_ada", [L, D, 6 * D])
        self.b_ada = din("b_ada", [L, 6 * D])
        self.norm1_g = din("norm1_g", [L, D])
        self.norm2_g = din("norm2_g", [L, D])
        self.w_in = din("w_in", [L, D, IN_COLS])
        self.ml_conv_w = din("ml_conv_w", [L, 3, 512])
        self.ml_conv_b = din("ml_conv_b", [L, 512])
        self.ml_gate_b = din("ml_gate_b", [L, 16])
        self.ml_norm_g = din("ml_norm_g", [L, 256])
        self.gm_ln_g = din("gm_ln_g", [L, 256])
        self.gm_ln_b = din("gm_ln_b", [L, 256])
        self.gm_w_s = din("gm_w_s", [L, 4, 128, 128])
        self.gm_b_s = din("gm_b_s", [L, 512])
        self.cv_dw_w = din("cv_dw_w", [L, 31, 256])
        self.cv_dw_b = din("cv_dw_b", [L, 256])
        self.cv_ln_g = din("cv_ln_g", [L, 256])
        self.cv_ln_b = din("cv_ln_b", [L, 256])
        self.w_branch = din("w_branch", [L, 4, 256, D])
        self.w_out = din("w_out", [L, D, D])
        self.peer_w_q = din("peer_w_q", [L, D, 2048])
        self.peer_keys = din("peer_keys", [L, 2, 128, 128])
        self.peer_u = din("peer_u", [L, NEXP, D])
        self.peer_v = din("peer_v", [L, NEXP, D])
        self.final_norm_g = din("final_norm_g", [D])
        self.dftc = din("dftc", [cfg.t_lat, cfg.t_lat], BF16)
        self.dfts = din("dfts", [cfg.t_lat, cfg.t_lat], BF16)
        self.cd = din("cd", [256, 512], BF16)
        self.cst_d = din("cst", [128, 1024])
        self.out = P.dram("out", [cfg.t_lat, D], F32, kind="ExternalOutput")
        sk = "ExternalOutput" if debug else "Internal"
        self.xT = P.dram("xT", [KD, 128, NT], F32, kind=sk)
        self.hT = P.dram("hT", [KD, 128, NT], BF16, kind=sk)
        self.Z = P.dram("Z", [4, 256, NT], BF16, kind=sk)
        self.Hf = P.dram("Hf", [cfg.nch, 64, 256], F32, kind=sk)
        self.Hb = P.dram("Hb", [cfg.nch, 64, 256], F32, kind=sk)
        self.UTs = [P.dram("UT%d" % i, [128, 128, KD * 128], BF16) for i in range(2)]
        self.VBs = [P.dram("VB%d" % i, [128, 128, D], BF16) for i in range(2)]
        self.cst = P.sb([128, 1024], F32, "cst")
        P.dma(self.cst.a, self.cst_d.a)
        c = self.cst
        self.identf = c[:, 0:128]
        self.iota128 = c[:, 128:256]
        self.iota16 = c[:, 896:912]
        self.eps = c[:, 912:913]
        self.identb_t = P.sb([128, 128], BF16, "identb")
        P.op('dve', 'tensor_copy', out=self.identb_t.a, in_=self.identf)
        self.identb = self.identb_t.a
        self.onesb_t = P.sb([128, 128], BF16, "onesb")
        P.op('pool', 'memset', ap=self.onesb_t.a, constant=1.0)
        self.onesb = self.onesb_t.a
        self.iota128b_t = P.sb([128, 128], BF16, "iota128b")
        P.op('dve', 'tensor_copy', out=self.iota128b_t.a, in_=self.iota128)
        self.onesf_t = P.sb([128, 128], F32, "onesf")
        P.op('pool', 'memset', ap=self.onesf_t.a, constant=1.0)
        self.onesf = self.onesf_t.a
        self.maskb_t = P.sb([64, 2, 4, 64], BF16, "maskb")
        for d_ in range(2):
            for h in range(4):
                P.op('dve', 'tensor_copy', out=self.maskb_t[:, d_, h, :], in_=c[0:64, 256 + 64 * d_:320 + 64 * d_])
        self.modT = P.sb([128, 48, 2], F32, "modT")
        cT = P.sb([2, D], F32, "cT")
        P.dma(cT.a, self.cvec.a)
        self.sT = P.sb([128, 2, KD], BF16, "sT")
        es = contextlib.ExitStack()
        ps = P.ps([128, 512], F32, es=es)
        for k in range(KD):
            P.op('pe', 'transpose', out=ps[:, 2 * k:2 * k + 2], in_=cT[:, k * 128:(k + 1) * 128], identity=self.identf[0:2, 0:2])
        P.op('act', 'activation', out=self.sT.a, in_=ps[:, 0:16].re("p (k s) -> p s k", s=2), func=AF.Silu)
        es.close()
        P.barrier()

    def load_rows_T(self, rows, es):
        P = self.P
        R = sum(v.ap.shape[0] for v in rows)
        w = rows[0].ap.shape[1]
        assert R <= 128
        rt = P.sb([128, 128], F32, "rows", es=es)
        r0 = 0
        for v in rows:
            r = v.ap.shape[0]
            P.dma(rt[r0:r0 + r, 0:w], v)
            r0 += r
        es_ = contextlib.ExitStack()
        ps = P.ps([128, 512], F32, es=es_)
        P.op('pe', 'transpose', out=ps[0:w, 0:R], in_=rt[0:R, 0:w], identity=self.identf[0:R, 0:R])
        ct = P.sb([128, R], F32, "colsT", es=es)
        P.op('dve', 'tensor_copy', out=ct[0:w, :], in_=ps[0:w, 0:R])
        P.barrier()
        es_.close()
        return ct

    def load_w(self, dst, src, q='pool'):
        self.P.dma(dst, src, q=q)

    def phase_init(self):
        P, cfg = self.P, self.cfg
        es = contextlib.ExitStack()
        xt = Rot([P.sb([128, D], F32, "xt", es=es) for _ in range(2)])
        pt = Rot([P.sb([128, D], F32, "pt", es=es) for _ in range(2)])
        pss = Rot([P.ps([128, 512], F32, es=es) for _ in range(4)])
        st = Rot([P.sb([128, KD, 128], F32, "xst", es=es) for _ in range(2)])
        for ti in range(cfg.nt // 128):
            x = xt.get()
            P.dma(x.a, self.xin[ti * 128:(ti + 1) * 128, :])
            if ti * 128 >= cfg.t_ctx:
                p = pt.get()
                r0 = ti * 128 - cfg.t_ctx
                P.dma(p.a, self.pos[r0:r0 + 128, :])
                P.op('dve', 'tensor_tensor', out=x.a, in0=x.a, in1=p.a, op=ALU.add)
            s = st.get()
            for half in range(2):
                ps = pss.get()
                for j in range(4):
                    k = half * 4 + j
                    P.op('pe', 'transpose', out=ps[:, j * 128:(j + 1) * 128], in_=x[:, k * 128:(k + 1) * 128], identity=self.identf)
                P.op('act', 'activation', out=s[:, half * 4:half * 4 + 4, :], in_=ps.a.re("p (j t) -> p j t", j=4), func=AF.Copy)
            P.dma(self.xT[:, :, ti * 128:(ti + 1) * 128].re("k p t -> p k t"), s.a)
        es.close()
        P.barrier()

    def phase_mod(self, l):
        P = self.P
        es = contextlib.ExitStack()
        wt = Rot([P.sb([128, KD, 1536], BF16, "wada", es=es) for _ in range(2)])
        ps = P.ps([128, 48, 2], F32, es=es)
        bT = self.load_rows_T([self.b_ada[l].re("(j p) -> j p", p=128)], es)
        for cg in range(4):
            w = wt.get()
            for k in range(KD):
                self.load_w(w[:, k, :], self.w_ada[l, k * 128:(k + 1) * 128, cg * 1536:(cg + 1) * 1536])
            for jj in range(12):
                j = cg * 12 + jj
                for k in range(KD):
                    P.mm(ps[:, j, :], w[:, k, jj * 128:(jj + 1) * 128], self.sT[:, :, k], start=(k == 0), stop=(k == KD - 1))
        P.op('dve', 'tensor_tensor', out=self.modT.a, in0=ps.a, in1=bT.a.unsq(2).bc([128, 48, 2]), op=ALU.add)
        es.close()
        P.barrier()

    def mod_scale_shift(self, l, which, es):
        P = self.P
        g = self.norm1_g if which == 0 else self.norm2_g
        gT = self.load_rows_T([g[l].re("(j p) -> j p", p=128)], es)
        base = 0 if which == 0 else 24
        gs = P.sb([128, KD, 2], F32, "gs", es=es)
        P.op('dve', 'tensor_scalar', out=gs.a, in0=self.modT[:, base + 8:base + 16, :], scalar1=1.0, scalar2=None, op0=ALU.add)
        P.op('dve', 'tensor_tensor', out=gs.a, in0=gs.a, in1=gT.a.unsq(2).bc([128, KD, 2]), op=ALU.mult)
        return gs, self.modT[:, base:base + 8, :], self.modT[:, base + 16:base + 24, :]

    def norm_group(self, xg, n, gs, sh, seg, hout, pss, tmps, sq, rsb):
        P = self.P
        P.op('act', 'activation', out=sq[:, :, 0:n], in_=xg, func=AF.Square)
        ps = pss.get()
        for k in range(KD):
            P.mm(ps[:, 0:n], self.onesb, sq[:, k, 0:n], start=(k == 0), stop=(k == KD - 1))
        rs = rsb
        P.op('act', 'activation', out=rs[:, 0:n], in_=ps[:, 0:n], func=AF.Sqrt, scale=1.0 / D, bias=self.eps)
        P.op('dve', 'reciprocal', out=rs[:, 0:n], in_=rs[:, 0:n])
        for k in range(KD):
            t = tmps.get()
            P.op('dve', 'scalar_tensor_tensor', out=t[:, 0:n], in0=xg[:, k, :], scalar=gs[:, k, seg:seg + 1], in1=rs[:, 0:n], op0=ALU.mult, op1=ALU.mult)
            if sh is None:
                P.op('act', 'activation', out=hout[:, k, :], in_=t[:, 0:n], func=AF.Copy)
            else:
                P.op('act', 'activation', out=hout[:, k, :], in_=t[:, 0:n], func=AF.Identity, bias=sh[:, k, seg:seg + 1])

    def phase_norm1(self, l):
        P, cfg = self.P, self.cfg
        es = contextlib.ExitStack()
        gs, sh, _ = self.mod_scale_shift(l, 0, es)
        xg = Rot([P.sb([128, KD, 512], F32, "xg", es=es) for _ in range(2)])
        hg = Rot([P.sb([128, KD, 512], BF16, "hg", es=es) for _ in range(2)])
        sq = P.sb([128, KD, 512], BF16, "sq", es=es)
        pss = Rot([P.ps([128, 512], F32, es=es) for _ in range(2)])
        tmps = Rot([P.sb([128, 512], F32, "nt", es=es) for _ in range(4)])
        rsb = P.sb([128, 512], F32, "rsb", es=es)
        for (n0, n, seg) in cfg.groups:
            x = xg.get()
            h = hg.get()
            P.dma(x[:, :, 0:n], self.xT[:, :, n0:n0 + n].re("k p t -> p k t"))
            self.norm_group(x[:, :, 0:n], n, gs, sh, seg, h[:, :, 0:n], pss, tmps, sq, rsb)
            P.dma(self.hT[:, :, n0:n0 + n].re("k p t -> p k t"), h[:, :, 0:n], q='pool')
        es.close()
        P.barrier()


def _gm(self, l):
    P, cfg = self.P, self.cfg
    es = contextlib.ExitStack()
    W = P.sb([128, KD, 512], BF16, "wgm", es=es)
    for k in range(KD):
        self.load_w(W[:, k, :], self.w_in[l, k * 128:(k + 1) * 128, GM_OFF:GM_OFF + 512])
    lng = P.sb([128, 256], F32, "lng", es=es)
    lnb = P.sb([128, 256], F32, "lnb", es=es)
    P.dma(lng.a, self.gm_ln_g[l:l + 1, :].pbc(128))
    P.dma(lnb.a, self.gm_ln_b[l:l + 1, :].pbc(128))
    bsr = P.sb([1, 512], F32, "bsr", es=es)
    P.dma(bsr.a, self.gm_b_s[l:l + 1, :])
    wsT = P.sb([128, 4, 128], BF16, "wsT", es=es)
    wsl = P.sb([128, 4, 128], BF16, "wsl", es=es)
    self.load_w(wsl.a, self.gm_w_s[l].re("g t s -> t g s"))
    pst = P.ps([128, 4, 128], BF16, es=es)
    for g in range(4):
        P.op('pe', 'transpose', out=pst[:, g, :], in_=wsl[:, g, :], identity=self.identb)
    P.op('dve', 'tensor_copy', out=wsT.a, in_=pst.a)
    hg = Rot([P.sb([128, KD, 512], BF16, "hg", es=es) for _ in range(2)])
    u64 = Rot([P.sb([64, 4, 512], BF16, "u64", es=es) for _ in range(2)])
    zst = Rot([P.sb([64, 4, 512], BF16, "zst", es=es) for _ in range(2)])
    psu = Rot([P.ps([128, 512], F32, es=es) for _ in range(1)])
    psv = Rot([P.ps([128, 512], F32, es=es) for _ in range(4)])
    pss = Rot([P.ps([64, 4, 128], F32, es=es) for _ in range(2)])
    vt = Rot([P.sb([128, 256], F32, "vt", es=es) for _ in range(4)])
    vn = Rot([P.sb([128, 256], BF16, "vn", es=es) for _ in range(4)])
    st6 = Rot([P.sb([128, 8], F32, "st6", es=es) for _ in range(4)])
    for (n0, n, seg) in cfg.groups:
        if self.skip_ctx and seg == 1:
            continue
        h = hg.get()
        P.dma(h[:, :, 0:n], self.hT[:, :, n0:n0 + n].re("k p t -> p k t"))
        u = u64.get()
        for g in range(4):
            ps = psu.get()
            for k in range(KD):
                P.mm(ps[0:64, 0:n], W[:, k, g * 64:(g + 1) * 64], h[:, k, 0:n], start=(k == 0), stop=(k == KD - 1))
            P.op('act', 'activation', out=u[:, g, 0:n], in_=ps[0:64, 0:n], func=AF.Gelu_apprx_tanh)
        z = zst.get()
        subs = list(range(n // 128))
        pv_ = [psv.get() for _ in subs]
        v_ = [vt.get() for _ in subs]
        s6_ = [st6.get() for _ in subs]
        vb_ = [vn.get() for _ in subs]
        for sub in subs:
            for k in range(KD):
                P.mm(pv_[sub][:, 0:256], h[:, k, sub * 128:(sub + 1) * 128], W[:, k, 256:512], start=(k == 0), stop=(k == KD - 1))
        for sub in subs:
            P.op('act', 'activation', out=v_[sub].a, in_=pv_[sub][:, 0:256], func=AF.Gelu_apprx_tanh)
        for sub in subs:
            P.op('dve', 'bn_stats', out=s6_[sub][:, 0:6], in_=v_[sub].a)
        for sub in subs:
            P.op('dve', 'bn_aggr', out=s6_[sub][:, 6:8], in_=s6_[sub][:, 0:6])
        for sub in subs:
            P.op('act', 'activation', out=s6_[sub][:, 7:8], in_=s6_[sub][:, 7:8], func=AF.Sqrt, bias=self.eps)
        for sub in subs:
            P.op('dve', 'reciprocal', out=s6_[sub][:, 7:8], in_=s6_[sub][:, 7:8])
        for sub in subs:
            P.op('dve', 'tensor_scalar', out=v_[sub].a, in0=v_[sub].a, scalar1=s6_[sub][:, 6:7], scalar2=s6_[sub][:, 7:8], op0=ALU.subtract, op1=ALU.mult)
        for sub in subs:
            P.op('dve', 'tensor_tensor', out=v_[sub].a, in0=v_[sub].a, in1=lng.a, op=ALU.mult)
        for sub in subs:
            P.op('dve', 'tensor_tensor', out=vb_[sub].a, in0=v_[sub].a, in1=lnb.a, op=ALU.add)
        for sub in subs:
            pg = pss.get()
            for g in range(4):
                P.mm(pg[:, g, :], vb_[sub][:, g * 64:(g + 1) * 64], wsT[:, g, :], start=True, stop=False)
                P.mm(pg[:, g, :], self.onesf[0:1, 0:64], bsr[0:1, g * 128:(g + 1) * 128], start=False, stop=True)
            P.op('dve', 'tensor_tensor', out=z[:, :, sub * 128:(sub + 1) * 128], in0=pg.a, in1=u[:, :, sub * 128:(sub + 1) * 128], op=ALU.mult)
        P.dma(self.Z[1, :, n0:n0 + n].re("(g p) t -> p g t", p=64), z[:, :, 0:n], q='pool')
    es.close()
    P.barrier()


MK.phase_gmlp = _gm


def _cv(self, l):
    P, cfg = self.P, self.cfg
    es = contextlib.ExitStack()
    PAD = 15
    W = P.sb([128, KD, 512], BF16, "wcv", es=es)
    for k in range(KD):
        self.load_w(W[:, k, :], self.w_in[l, k * 128:(k + 1) * 128, CV_OFF:CV_OFF + 512])
    cols = self.load_rows_T([self.cv_dw_w[l].re("j (c p) -> (j c) p", p=128), self.cv_dw_b[l].re("(c p) -> c p", p=128),
                             self.cv_ln_g[l].re("(c p) -> c p", p=128), self.cv_ln_b[l].re("(c p) -> c p", p=128)], es)
    diag = P.sb([128, 62, 128], BF16, "diag", es=es)
    for jc in range(62):
        P.op('dve', 'tensor_scalar', out=diag[:, jc, :], in0=self.identf, scalar1=cols[:, jc:jc + 1], scalar2=None, op0=ALU.mult)
    hg = Rot([P.sb([128, KD, 512], BF16, "hg", es=es) for _ in range(2)])
    psa = Rot([P.ps([128, 512], F32, es=es) for _ in range(2)])
    psb = Rot([P.ps([128, 512], F32, es=es) for _ in range(2)])
    psc = Rot([P.ps([128, 512], F32, es=es) for _ in range(2)])
    sg = Rot([P.sb([128, 512], F32, "sg", es=es) for _ in range(2)])
    for (s0, sn, seg) in cfg.segs:
        if self.skip_ctx and seg == 1:
            continue
        es2 = contextlib.ExitStack()
        zp = P.sb([128, 2, sn + 2 * PAD], BF16, "zp", es=es2)
        P.op('pool', 'memset', ap=zp.a, constant=0.0)
        y = P.sb([128, 2, 512], F32, "ycv", es=es2)
        y2 = P.sb([128, 2, 512], F32, "ycv2", es=es2)
        mean = P.sb([128, 512], F32, "mean", es=es2)
        rstd = P.sb([128, 512], F32, "rstd", es=es2)
        zo = Rot([P.sb([128, 2, 512], BF16, "zo", es=es2) for _ in range(2)])
        grp = [(a, n) for (a, n, sg_) in cfg.groups if sg_ == seg]
        for (n0, n) in grp:
            h = hg.get()
            P.dma(h[:, :, 0:n], self.hT[:, :, n0:n0 + n].re("k p t -> p k t"))
            for c in range(2):
                pa = psa.get()
                pb = psb.get()
                for k in range(KD):
                    P.mm(pa[:, 0:n], W[:, k, c * 128:(c + 1) * 128], h[:, k, 0:n], start=(k == 0), stop=(k == KD - 1))
                for k in range(KD):
                    P.mm(pb[:, 0:n], W[:, k, 256 + c * 128:256 + (c + 1) * 128], h[:, k, 0:n], start=(k == 0), stop=(k == KD - 1))
                s = sg.get()
                P.op('act', 'activation', out=s[:, 0:n], in_=pb[:, 0:n], func=AF.Sigmoid)
                o0 = PAD + n0 - s0
                P.op('dve', 'tensor_tensor', out=zp[:, c, o0:o0 + n], in0=pa[:, 0:n], in1=s[:, 0:n], op=ALU.mult)
        for (n0, n) in grp:
            o0 = n0 - s0
            for c in range(2):
                pc = psc.get()
                for j in range(31):
                    P.mm(pc[:, 0:n], diag[:, j * 2 + c, :], zp[:, c, o0 + j:o0 + j + n], start=(j == 0), stop=(j == 30))
                P.op('act', 'activation', out=y[:, c, 0:n], in_=pc[:, 0:n], func=AF.Identity, bias=cols[:, 62 + c:63 + c])
                P.op('act', 'activation', out=y2[:, c, 0:n], in_=y[:, c, 0:n], func=AF.Square)
            p1 = psa.get()
            p2 = psb.get()
            for c in range(2):
                P.mm(p1[:, 0:n], self.onesf, y[:, c, 0:n], start=(c == 0), stop=(c == 1))
            for c in range(2):
                P.mm(p2[:, 0:n], self.onesf, y2[:, c, 0:n], start=(c == 0), stop=(c == 1))
            P.op('act', 'activation', out=mean[:, 0:n], in_=p1[:, 0:n], func=AF.Identity, scale=1.0 / 256)
            P.op('dve', 'tensor_tensor', out=rstd[:, 0:n], in0=mean[:, 0:n], in1=mean[:, 0:n], op=ALU.mult)
            P.op('dve', 'scalar_tensor_tensor', out=rstd[:, 0:n], in0=p2[:, 0:n], scalar=1.0 / 256, in1=rstd[:, 0:n], op0=ALU.mult, op1=ALU.subtract)
            P.op('act', 'activation', out=rstd[:, 0:n], in_=rstd[:, 0:n], func=AF.Sqrt, bias=self.eps)
            P.op('dve', 'reciprocal', out=rstd[:, 0:n], in_=rstd[:, 0:n])
            z = zo.get()
            for c in range(2):
                P.op('dve', 'tensor_tensor', out=y[:, c, 0:n], in0=y[:, c, 0:n], in1=mean[:, 0:n], op=ALU.subtract)
                P.op('dve', 'tensor_tensor', out=y[:, c, 0:n], in0=y[:, c, 0:n], in1=rstd[:, 0:n], op=ALU.mult)
                P.op('act', 'activation', out=z[:, c, 0:n], in_=y[:, c, 0:n], func=AF.Silu, scale=cols[:, 64 + c:65 + c], bias=cols[:, 66 + c:67 + c])
            P.dma(self.Z[2, :, n0:n0 + n].re("(c p) t -> p c t", p=128), z[:, :, 0:n], q='pool')
        es2.close()
        P.barrier()
    es.close()
    P.barrier()


MK.phase_conv = _cv


def _ft(self, l):
    P, cfg = self.P, self.cfg
    es = contextlib.ExitStack()
    W = P.sb([128, KD, 256], BF16, "wft", es=es)
    for k in range(KD):
        self.load_w(W[:, k, :], self.w_in[l, k * 128:(k + 1) * 128, FT_OFF:FT_OFF + 256])
    CD = P.sb([128, 2, 512], BF16, "cdt", es=es)
    P.dma(CD.a, self.cd.a.re("(c p) n -> p c n", p=128))
    hg = Rot([P.sb([128, KD, 512], BF16, "hg", es=es) for _ in range(2)])
    zf = Rot([P.sb([128, 2, 512], BF16, "zf", es=es) for _ in range(2)])
    psa = Rot([P.ps([128, 512], F32, es=es) for _ in range(2)])
    psd = Rot([P.ps([128, 512], F32, es=es) for _ in range(4)])
    for (s0, sn, seg) in cfg.segs:
        if self.skip_ctx and seg == 1:
            continue
        es2 = contextlib.ExitStack()
        ntile = sn // 128
        zcs = P.sb([128, ntile, 512], BF16, "zcs", es=es2)
        grp = [(a, n) for (a, n, sg_) in cfg.groups if sg_ == seg]
        for (n0, n) in grp:
            h = hg.get()
            P.dma(h[:, :, 0:n], self.hT[:, :, n0:n0 + n].re("k p t -> p k t"))
            z = zf.get()
            for c in range(2):
                pa = psa.get()
                for k in range(KD):
                    P.mm(pa[:, 0:n], W[:, k, c * 128:(c + 1) * 128], h[:, k, 0:n], start=(k == 0), stop=(k == KD - 1))
                P.op('act', 'activation', out=z[:, c, 0:n], in_=pa[:, 0:n], func=AF.Copy)
            for sub in range(n // 128):
                pd = psd.get()
                for c in range(2):
                    P.mm(pd.a, z[:, c, sub * 128:(sub + 1) * 128], CD[:, c, :], start=(c == 0), stop=(c == 1))
                ti = (n0 - s0) // 128 + sub
                P.op('dve', 'tensor_copy', out=zcs[:, ti, :], in_=pd.a)
        kb_n = min(512, sn)
        nkb = sn // kb_n
        tcs = Rot([P.sb([128, ntile, kb_n], BF16, "tc", es=es2) for _ in range(2)])
        tss = Rot([P.sb([128, ntile, kb_n], BF16, "ts", es=es2) for _ in range(2)])
        zo = Rot([P.sb([128, 2, 512], BF16, "zo", es=es2) for _ in range(2)])
        rstride = cfg.t_lat // sn
        for kb in range(nkb):
            tc_ = tcs.get()
            ts_ = tss.get()
            if rstride == 1:
                P.dma(tc_.a, self.dftc[:, kb * kb_n:(kb + 1) * kb_n].re("(t p) n -> p t n", p=128))
                P.dma(ts_.a, self.dfts[:, kb * kb_n:(kb + 1) * kb_n].re("(t p) n -> p t n", p=128))
            else:
                P.dma(tc_.a, self.dftc.a.re("(r s) n -> r s n", s=rstride)[:, 0, kb * kb_n:(kb + 1) * kb_n].re("(t p) n -> p t n", p=128))
                P.dma(ts_.a, self.dfts.a.re("(r s) n -> r s n", s=rstride)[:, 0, kb * kb_n:(kb + 1) * kb_n].re("(t p) n -> p t n", p=128))
            z = zo.get()
            for c in range(2):
                pd = psd.get()
                for ti in range(ntile):
                    P.mm(pd[:, 0:kb_n], zcs[:, ti, c * 128:(c + 1) * 128], tc_[:, ti, :], start=(ti == 0), stop=False)
                    P.mm(pd[:, 0:kb_n], zcs[:, ti, 256 + c * 128:256 + (c + 1) * 128], ts_[:, ti, :], start=False, stop=(ti == ntile - 1))
                P.op('act', 'activation', out=z[:, c, 0:kb_n], in_=pd[:, 0:kb_n], func=AF.Identity, scale=float(np.sqrt(rstride)))
            P.dma(self.Z[3, :, s0 + kb * kb_n:s0 + (kb + 1) * kb_n].re("(c p) t -> p c t", p=128), z[:, :, 0:kb_n], q='pool')
        es2.close()
        P.barrier()
    es.close()
    P.barrier()


MK.phase_fnet = _ft


def _merge(self, l):
    P, cfg = self.P, self.cfg
    es = contextlib.ExitStack()
    Wg = P.sb([128, KD, 4096], BF16, "wgate", es=es)
    for k in range(KD):
        for i in range(4):
            self.load_w(Wg[:, k, i * 1024:(i + 1) * 1024], self.w_in[l, k * 128:(k + 1) * 128, GATE_OFF + i * 1024:GATE_OFF + (i + 1) * 1024])
    Wb = P.sb([128, 4, 2, 1024], BF16, "wbr", es=es)
    for i in range(4):
        for c in range(2):
            self.load_w(Wb[:, i, c, :], self.w_branch[l, i, c * 128:(c + 1) * 128, :])
    Wo = P.sb([128, KD, 1024], BF16, "wout", es=es)
    for k in range(KD):
        self.load_w(Wo[:, k, :], self.w_out[l, k * 128:(k + 1) * 128, :])
    gate1 = self.modT[:, 16:24, :]
    hg = Rot([P.sb([128, KD, 512], BF16, "hg", es=es) for _ in range(2)])
    zg = Rot([P.sb([128, 4, 2, 512], BF16, "zg", es=es) for _ in range(2)])
    xg = Rot([P.sb([128, KD, 512], F32, "xg", es=es) for _ in range(2)])
    yT = P.sb([128, KD, 512], BF16, "yT", es=es)
    psg = Rot([P.ps([128, 512], F32, es=es) for _ in range(3)])
    psl = Rot([P.ps([128, 512], F32, es=es) for _ in range(3)])
    pso = Rot([P.ps([128, 512], F32, es=es) for _ in range(2)])
    sg = Rot([P.sb([128, 512], F32, "sg", es=es) for _ in range(3)])
    acc = Rot([P.sb([128, 512], F32, "acc", es=es) for _ in range(2)])
    for (n0, n, seg) in cfg.groups:
        if self.skip_ctx and seg == 1:
            continue
        h = hg.get()
        P.dma(h[:, :, 0:n], self.hT[:, :, n0:n0 + n].re("k p t -> p k t"))
        z = zg.get()
        for i in range(4):
            P.dma(z[:, i, :, 0:n], self.Z[i, :, n0:n0 + n].re("(c p) t -> p c t", p=128))
        x = xg.get()
        P.dma(x[:, :, 0:n], self.xT[:, :, n0:n0 + n].re("k p t -> p k t"))
        for dc in range(KD):
            a = acc.get()
            for i in range(4):
                pg = psg.get()
                for k in range(KD):
                    P.mm(pg[:, 0:n], Wg[:, k, i * 1024 + dc * 128:i * 1024 + (dc + 1) * 128], h[:, k, 0:n], start=(k == 0), stop=(k == KD - 1))
                s = sg.get()
                P.op('act', 'activation', out=s[:, 0:n], in_=pg[:, 0:n], func=AF.Sigmoid)
                pl = psl.get()
                for c in range(2):
                    P.mm(pl[:, 0:n], Wb[:, i, c, dc * 128:(dc + 1) * 128], z[:, i, c, 0:n], start=(c == 0), stop=(c == 1))
                if i == 0:
                    P.op('dve', 'tensor_tensor', out=a[:, 0:n], in0=pl[:, 0:n], in1=s[:, 0:n], op=ALU.mult)
                else:
                    P.op('dve', 'tensor_tensor', out=s[:, 0:n], in0=pl[:, 0:n], in1=s[:, 0:n], op=ALU.mult)
                    if i < 3:
                        P.op(self.merge_eng, 'tensor_tensor', out=a[:, 0:n], in0=a[:, 0:n], in1=s[:, 0:n], op=ALU.add)
                    else:
                        P.op(self.merge_eng, 'tensor_tensor', out=yT[:, dc, 0:n], in0=a[:, 0:n], in1=s[:, 0:n], op=ALU.add)
        for dc in range(KD):
            po = pso.get()
            for k in range(KD):
                P.mm(po[:, 0:n], Wo[:, k, dc * 128:(dc + 1) * 128], yT[:, k, 0:n], start=(k == 0), stop=(k == KD - 1))
            P.op('dve', 'scalar_tensor_tensor', out=x[:, dc, 0:n], in0=po[:, 0:n], scalar=gate1[:, dc, seg:seg + 1], in1=x[:, dc, 0:n], op0=ALU.mult, op1=ALU.add)
        P.dma(self.xT[:, :, n0:n0 + n].re("k p t -> p k t"), x[:, :, 0:n], q='pool')
    es.close()
    P.barrier()


MK.phase_merge = _merge


def _final(self):
    P, cfg = self.P, self.cfg
    es = contextlib.ExitStack()
    gT = self.load_rows_T([self.final_norm_g.a.re("(j p) -> j p", p=128)], es)
    gs = P.sb([128, KD, 2], F32, "gsf", es=es)
    P.op('dve', 'tensor_copy', out=gs.a, in_=gT.a.unsq(2).bc([128, KD, 2]))
    xg = Rot([P.sb([128, KD, 512], F32, "xg", es=es) for _ in range(2)])
    yg = Rot([P.sb([128, KD, 512], F32, "yg", es=es) for _ in range(2)])
    sq = P.sb([128, KD, 512], BF16, "sq", es=es)
    pss = Rot([P.ps([128, 512], F32, es=es) for _ in range(2)])
    pst = Rot([P.ps([128, 512], F32, es=es) for _ in range(4)])
    tmps = Rot([P.sb([128, 512], F32, "nt", es=es) for _ in range(4)])
    rsb = P.sb([128, 512], F32, "rsb", es=es)
    ot = Rot([P.sb([128, D], F32, "ot", es=es) for _ in range(3)])
    for (n0, n, seg) in cfg.groups:
        if seg == 1:
            continue
        x = xg.get()
        y = yg.get()
        P.dma(x[:, :, 0:n], self.xT[:, :, n0:n0 + n].re("k p t -> p k t"))
        self.norm_group(x[:, :, 0:n], n, gs, None, 0, y[:, :, 0:n], pss, tmps, sq, rsb)
        for sub in range(n // 128):
            o = ot.get()
            for half in range(2):
                ps = pst.get()
                for j in range(4):
                    k = half * 4 + j
                    P.op('pe', 'transpose', out=ps[:, j * 128:(j + 1) * 128], in_=y[:, k, sub * 128:(sub + 1) * 128], identity=self.identf)
                P.op('act', 'activation', out=o[:, half * 512:(half + 1) * 512], in_=ps.a, func=AF.Copy)
            r0 = n0 - cfg.t_ctx + sub * 128
            P.dma(self.out[r0:r0 + 128, :], o.a, q='pool')
    es.close()
    P.barrier()


MK.phase_final = _final


def _ml(self, l):
    P, cfg = self.P, self.cfg
    NT, NCH = cfg.nt, cfg.nch
    nctx = cfg.t_ctx // 64
    order_b = list(range(nctx - 1, -1, -1)) + list(range(NCH - 1, nctx - 1, -1))
    order_f = list(range(NCH))
    es = contextlib.ExitStack()
    biasA = P.sb([64, 1], F32, "biasA", es=es)
    biasB = P.sb([64, 1], F32, "biasB", es=es)
    P.op('pool', 'memset', ap=biasA.a, constant=0.0)
    P.op('pool', 'memset', ap=biasB.a, constant=0.0)
    gb = self.ml_gate_b
    P.dma(biasA[0:4, :], gb[l, 0:4].re("(a b) -> a b", b=1))
    P.dma(biasA[32:36, :], gb[l, 4:8].re("(a b) -> a b", b=1))
    P.dma(biasB[0:4, :], gb[l, 8:12].re("(a b) -> a b", b=1))
    P.dma(biasB[32:36, :], gb[l, 12:16].re("(a b) -> a b", b=1))
    cols = self.load_rows_T([self.ml_conv_w[l].re("j (c p) -> (j c) p", p=128), self.ml_conv_b[l].re("(c p) -> c p", p=128)], es)
    colsB = self.load_rows_T([self.ml_conv_b[l].re("(c p) -> c p", p=64)], es)
    diag = P.sb([128, 12, 128], BF16, "diag3", es=es)
    for jc in range(12):
        P.op('dve', 'tensor_scalar', out=diag[:, jc, :], in0=self.identf, scalar1=cols[:, jc:jc + 1], scalar2=None, op0=ALU.mult)
    qk8 = [P.sb([64, NT], BF16, "qk8_%d" % i, es=es) for i in range(8)]
    Atok = P.sb([64, NCH, 8], F32, "Atok", es=es)
    Btok = P.sb([64, NCH, 8], F32, "Btok", es=es)
    Ctok = P.sb([64, NCH, 8], F32, "Ctok", es=es)
    decb = P.sb([64, 8, NCH], F32, "decb", es=es)
    emb = P.sb([64, 8, NCH], F32, "emb", es=es)
    es1 = contextlib.ExitStack()
    Wqk = P.sb([128, KD, 512], BF16, "wqk", es=es1)
    for k in range(KD):
        self.load_w(Wqk[:, k, :], self.w_in[l, k * 128:(k + 1) * 128, 0:512])
    pre = [P.sb([128, NT + 4], BF16, "pre%d" % i, es=es1) for i in range(4)]
    for i in range(4):
        P.op('pool', 'memset', ap=pre[i].a, constant=0.0)
    hg = Rot([P.sb([128, KD, 512], BF16, "hg", es=es1) for _ in range(2)])
    psQ = Rot([P.ps([128, 512], F32, es=es1) for _ in range(2)])

    def poff(seg):
        return 1 if seg == 1 else cfg.t_ctx + 3

    for (n0, n, seg) in cfg.groups:
        s0 = 0 if seg == 1 else cfg.t_ctx
        h = hg.get()
        P.dma(h[:, :, 0:n], self.hT[:, :, n0:n0 + n].re("k p t -> p k t"))
        for ci in range(4):
            pq = psQ.get()
            for k in range(KD):
                P.mm(pq[:, 0:n], Wqk[:, k, ci * 128:(ci + 1) * 128], h[:, k, 0:n], start=(k == 0), stop=(k == KD - 1))
            o0 = poff(seg) + n0 - s0
            P.op('dve', 'tensor_copy', out=pre[ci][:, o0:o0 + n], in_=pq[:, 0:n])
    for (n0, n, seg) in cfg.groups:
        s0 = 0 if seg == 1 else cfg.t_ctx
        for ci in range(4):
            for hp in range(2):
                pq = psQ.get()
                o0 = poff(seg) + n0 - s0 - 1
                for j in range(3):
                    P.mm(pq[0:64, 0:n], diag[:, j * 4 + ci, hp * 64:(hp + 1) * 64], pre[ci][:, o0 + j:o0 + j + n], start=(j == 0), stop=(j == 2))
                P.op('act', 'activation', out=qk8[ci * 2 + hp][:, n0:n0 + n], in_=pq[0:64, 0:n], func=AF.Silu, bias=colsB[0:64, ci * 2 + hp:ci * 2 + hp + 1])
    for h8 in range(4, 8):
        P.op('dve', 'tensor_scalar', out=qk8[h8].a, in0=qk8[h8].a, scalar1=0.125, scalar2=None, op0=ALU.mult)
    es1.close()
    P.barrier()
    esA = contextlib.ExitStack()
    LI = P.sb([64, NT], F32, "LI", es=esA)
    LF = P.sb([64, NT], F32, "LF", es=esA)
    es1 = contextlib.ExitStack()
    WgA = P.sb([128, KD, 64], BF16, "wga", es=es1)
    WgB = P.sb([128, KD, 64], BF16, "wgb", es=es1)
    P.op('pool', 'memset', ap=WgA.a, constant=0.0)
    P.op('pool', 'memset', ap=WgB.a, constant=0.0)
    for k in range(KD):
        rows = slice(k * 128, (k + 1) * 128)
        self.load_w(WgA[:, k, 0:4], self.w_in[l, rows, 768:772])
        self.load_w(WgA[:, k, 32:36], self.w_in[l, rows, 772:776])
        self.load_w(WgB[:, k, 0:4], self.w_in[l, rows, 776:780])
        self.load_w(WgB[:, k, 32:36], self.w_in[l, rows, 780:784])
    hg = Rot([P.sb([128, KD, 512], BF16, "hg", es=es1) for _ in range(2)])
    psA = Rot([P.ps([128, 512], F32, es=es1) for _ in range(2)])
    sgt = Rot([P.sb([64, 512], F32, "sgt", es=es1) for _ in range(2)])
    for (n0, n, seg) in cfg.groups:
        h = hg.get()
        P.dma(h[:, :, 0:n], self.hT[:, :, n0:n0 + n].re("k p t -> p k t"))
        pa = psA.get()
        for k in range(KD):
            P.mm(pa[0:64, 0:n], WgA[:, k, :], h[:, k, 0:n], start=(k == 0), stop=(k == KD - 1))
        P.op('act', 'activation', out=LI[:, n0:n0 + n], in_=pa[0:64, 0:n], func=AF.Identity, bias=biasA.a)
        pb = psA.get()
        for k in range(KD):
            P.mm(pb[0:64, 0:n], WgB[:, k, :], h[:, k, 0:n], start=(k == 0), stop=(k == KD - 1))
        s = sgt.get()
        P.op('act', 'activation', out=s[:, 0:n], in_=pb[0:64, 0:n], func=AF.Sigmoid, bias=biasB.a)
        P.op('act', 'activation', out=LF[:, n0:n0 + n], in_=s[:, 0:n], func=AF.Ln)
    es1.close()
    P.barrier()
    es1 = contextlib.ExitStack()
    ones = P.sb([64, NT], BF16, "ones", es=es1)
    P.op('pool', 'memset', ap=ones.a, constant=1.0)
    cs = P.sb([64, NT], F32, "cs", es=es1)
    P.op('dve', 'tensor_tensor_scan', out=cs.a, data0=ones.a, data1=LF.a, initial=0.0, op0=ALU.mult, op1=ALU.add)
    j3 = lambda b: b.a.re("p (c j) -> p c j", j=64)
    sm = lambda nm: P.sb([64, NCH], F32, nm, es=es1)
    base, tot, wmax, cmax, m_in, m_new, Mx, dec, em = [sm(x) for x in ("base", "tot", "wmax", "cmax", "m_in", "m_new", "Mx", "dec", "em")]
    bc3 = lambda b: b.a.unsq(2).bc([64, NCH, 64])
    P.op('dve', 'tensor_tensor', out=base.a, in0=j3(cs)[:, :, 0], in1=j3(LF)[:, :, 0], op=ALU.subtract)
    BC = cs
    P.op('dve', 'tensor_tensor', out=j3(BC), in0=j3(cs), in1=bc3(base), op=ALU.subtract)
    P.op('dve', 'tensor_copy', out=tot.a, in_=j3(BC)[:, :, 63])
    P.op('dve', 'tensor_tensor', out=j3(BC)[32:64], in0=bc3(tot)[32:64], in1=j3(BC)[32:64], op=ALU.subtract)
    P.op('dve', 'tensor_tensor', out=BC[32:64, :], in0=BC[32:64, :], in1=LF[32:64, :], op=ALU.add)
    Wt = LF
    Cq = LI
    P.op('dve', 'tensor_tensor', out=Cq.a, in0=LI.a, in1=BC.a, op=ALU.subtract)
    P.op('dve', 'tensor_tensor', out=j3(Wt), in0=j3(Cq), in1=bc3(tot), op=ALU.add)
    P.op('dve', 'tensor_reduce', out=wmax.a, in_=j3(Wt), axis=AX.X, op=ALU.max)
    P.op('dve', 'tensor_reduce', out=cmax.a, in_=j3(Cq), axis=AX.X, op=ALU.max)
    zero = P.sb([64, 1], F32, "zero", es=es1)
    P.op('pool', 'memset', ap=zero.a, constant=0.0)
    P.op('pool', 'memset', ap=m_in.a, constant=0.0)
    for (rows, order) in ((slice(0, 32), order_f), (slice(32, 64), order_b)):
        prev = None
        for i, c in enumerate(order):
            pv_ = zero[rows, 0:1] if prev is None else m_new[rows, prev:prev + 1]
            if prev is not None:
                P.op('dve', 'tensor_copy', out=m_in[rows, c:c + 1], in_=pv_)
            P.op('dve', 'tensor_scalar', out=m_new[rows, c:c + 1], in0=pv_, scalar1=tot[rows, c:c + 1], scalar2=wmax[rows, c:c + 1], op0=ALU.add, op1=ALU.max)
            prev = c
    P.op('dve', 'tensor_tensor', out=Mx.a, in0=m_in.a, in1=cmax.a, op=ALU.max)
    P.op('dve', 'tensor_tensor', out=j3(Cq), in0=j3(Cq), in1=bc3(Mx), op=ALU.subtract)
    P.op('act', 'activation', out=Cq.a, in_=Cq.a, func=AF.Exp)
    P.op('dve', 'tensor_tensor', out=j3(Wt), in0=j3(Wt), in1=bc3(m_new), op=ALU.subtract)
    P.op('act', 'activation', out=Wt.a, in_=Wt.a, func=AF.Exp)
    P.op('dve', 'tensor_tensor', out=j3(BC), in0=j3(BC), in1=bc3(Mx), op=ALU.add)
    P.op('act', 'activation', out=BC.a, in_=BC.a, func=AF.Exp, scale=-1.0)
    P.op('dve', 'tensor_tensor', out=dec.a, in0=tot.a, in1=m_in.a, op=ALU.add)
    P.op('dve', 'tensor_tensor', out=dec.a, in0=dec.a, in1=m_new.a, op=ALU.subtract)
    P.op('act', 'activation', out=dec.a, in_=dec.a, func=AF.Exp)
    P.op('dve', 'tensor_tensor', out=em.a, in0=m_in.a, in1=Mx.a, op=ALU.subtract)
    P.op('act', 'activation', out=em.a, in_=em.a, func=AF.Exp)
    pt = Rot([P.ps([64, 8, 64], F32, es=es1) for _ in range(2)])
    for (src, dst) in ((Cq, Atok), (Wt, Btok), (BC, Ctok)):
        for c0 in range(0, NCH, 8):
            nb = min(8, NCH - c0)
            p_ = pt.get()
            for cc in range(nb):
                P.op('pe', 'transpose', out=p_[:, cc, :], in_=src[:, (c0 + cc) * 64:(c0 + cc + 1) * 64], identity=self.identf[0:64, 0:64])
            P.op('dve', 'tensor_copy', out=dst[:, c0:c0 + nb, 0:4], in_=p_[:, 0:nb, 0:4])
            P.op('dve', 'tensor_copy', out=dst[:, c0:c0 + nb, 4:8], in_=p_[:, 0:nb, 32:36])
    pbq = Rot([P.ps([64, 4, NCH], F32, es=es1) for _ in range(2)])
    for (src, dst) in ((dec, decb), (em, emb)):
        for half in range(2):
            p_ = pbq.get()
            for gg in range(4):
                g = half * 4 + gg
                P.mm(p_[:, gg, :], self.cst[0:64, 384 + g * 64:384 + (g + 1) * 64], src.a, start=True, stop=True)
            P.op('dve', 'tensor_copy', out=dst[:, half * 4:half * 4 + 4, :], in_=p_.a)
    es1.close()
    esA.close()
    P.barrier()
    es3 = contextlib.ExitStack()
    v1 = P.sb([64, NCH, 4, 65], BF16, "v1", es=es3)
    P.op('pool', 'memset', ap=v1.a, constant=1.0)
    ktok = P.sb([64, NCH, 256], BF16, "ktok", es=es3)
    es3a = contextlib.ExitStack()
    Wv = P.sb([128, KD, 256], BF16, "wv", es=es3a)
    for k in range(KD):
        self.load_w(Wv[:, k, :], self.w_in[l, k * 128:(k + 1) * 128, 512:768])
    hg = Rot([P.sb([128, KD, 512], BF16, "hg", es=es3a) for _ in range(2)])
    psV = Rot([P.ps([128, 512], F32, es=es3a) for _ in range(2)])
    pk = Rot([P.ps([64, 256], BF16, es=es3a) for _ in range(2)])
    for (n0, n, seg) in cfg.groups:
        h = hg.get()
        P.dma(h[:, :, 0:n], self.hT[:, :, n0:n0 + n].re("k p t -> p k t"))
        for cc in range(n // 64):
            c = n0 // 64 + cc
            pv = psV.get()
            for k in range(KD):
                P.mm(pv[0:64, 0:256], h[:, k, cc * 64:(cc + 1) * 64], Wv[:, k, :], start=(k == 0), stop=(k == KD - 1))
            P.op('act', 'activation', out=v1[:, c, :, 0:64], in_=pv[0:64, 0:256].re("p (h d) -> p h d", h=4), func=AF.Copy)
    for c in range(NCH):
        p_ = pk.get()
        for hh in range(4):
            P.op('pe', 'transpose', out=p_[:, hh * 64:(hh + 1) * 64], in_=qk8[4 + hh][:, c * 64:(c + 1) * 64], identity=self.identb[0:64, 0:64])
        P.op('act', 'activation', out=ktok[:, c, :], in_=p_.a, func=AF.Copy)
    es3a.close()
    P.barrier()
    PS1 = [P.ps([64, 4, 64], F32, es=es3) for _ in range(2)]
    PS2 = [P.ps([64, 4, 65], F32, es=es3) for _ in range(2)]
    PS3 = [P.ps([64, 4, 65], F32, es=es3) for _ in range(2)]
    Cst = [P.sb([64, 4, 65], F32, "Cst", es=es3) for _ in range(2)]
    CnS = [P.sb([64, 4, 65], BF16, "CnS", es=es3) for _ in range(2)]
    for d_ in range(2):
        P.op('pool', 'memset', ap=Cst[d_].a, constant=0.0)
        P.op('pool', 'memset', ap=CnS[d_].a, constant=0.0)
    tmp = [Rot([P.sb([64, 4, 64], F32, "stmp", es=es3) for _ in range(2)]) for _ in range(2)]
    SD = [Rot([P.sb([64, 4, 64], BF16, "SD", es=es3) for _ in range(2)]) for _ in range(2)]
    kw = [Rot([P.sb([64, 4, 64], BF16, "kw", es=es3) for _ in range(2)]) for _ in range(2)]
    dn = [Rot([P.sb([64, 8], F32, "dn", es=es3) for _ in range(2)]) for _ in range(2)]
    hb = [Rot([P.sb([64, 4, 64], F32, "hbuf", es=es3) for _ in range(3)]) for _ in range(2)]
    Hd = [self.Hf, self.Hb]
    orders = [order_f, order_b]
    prep_lst = []
    prep_es = contextlib.ExitStack()
    if self.prep_in_mlstm:
        prep_lst = self.phase_peer_prep(l, es=prep_es, defer=True) or []
    per_step = (len(prep_lst) + NCH - 1) // NCH + 1
    for i in range(NCH):
        cs_ = [orders[d_][i] for d_ in range(2)]
        tks = [slice(c * 64, (c + 1) * 64) for c in cs_]
        qh = [[qk8[hh][:, tks[d_]] for hh in range(4)] for d_ in range(2)]
        kh = [[qk8[4 + hh][:, tks[d_]] for hh in range(4)] for d_ in range(2)]
        for d_ in range(2):
            for hh in range(4):
                P.mm(PS1[d_][:, hh, :], kh[d_][hh], qh[d_][hh])
        t_ = [tmp[d_].get() for d_ in range(2)]
        for d_ in range(2):
            P.op('dve', 'tensor_tensor', out=t_[d_].a, in0=PS1[d_].a, in1=Atok[:, cs_[d_], 4 * d_:4 * d_ + 4].unsq(2).bc([64, 4, 64]), op=ALU.mult)
        sd = [SD[d_].get() for d_ in range(2)]
        for d_ in range(2):
            P.op(self.scan_eng, 'tensor_tensor', out=sd[d_].a, in0=t_[d_].a, in1=self.maskb_t[:, d_, :, :], op=ALU.mult)
        kw_ = [kw[d_].get() for d_ in range(2)]
        for d_ in range(2):
            P.op(self.scan_eng, 'tensor_tensor', out=kw_[d_].a, in0=ktok[:, cs_[d_], :].re("p (h d) -> p h d", h=4), in1=Btok[:, cs_[d_], 4 * d_:4 * d_ + 4].unsq(2).bc([64, 4, 64]), op=ALU.mult)
        for d_ in range(2):
            for hh in range(4):
                P.mm(PS2[d_][:, hh, :], qh[d_][hh], CnS[d_][:, hh, :], start=True, stop=False)
                P.mm(PS2[d_][:, hh, :], sd[d_][:, hh, :], v1[:, cs_[d_], hh, :], start=False, stop=True)
        for d_ in range(2):
            for hh in range(4):
                P.mm(PS3[d_][:, hh, :], kw_[d_][:, hh, :], v1[:, cs_[d_], hh, :])
        for d_ in range(2):
            P.op(self.scan_eng, 'tensor_tensor', out=Cst[d_].a, in0=Cst[d_].a, in1=decb[:, 4 * d_:4 * d_ + 4, cs_[d_]].unsq(2).bc([64, 4, 65]), op=ALU.mult)
        for d_ in range(2):
            P.op('dve', 'tensor_tensor', out=Cst[d_].a, in0=Cst[d_].a, in1=PS3[d_].a, op=ALU.add)
        if i + 1 < NCH:
            for d_ in range(2):
                cn = orders[d_][i + 1]
                P.op(self.scan_eng, 'tensor_tensor', out=CnS[d_].a, in0=Cst[d_].a, in1=emb[:, 4 * d_:4 * d_ + 4, cn].unsq(2).bc([64, 4, 65]), op=ALU.mult)
        dd = [dn[d_].get() for d_ in range(2)]
        for d_ in range(2):
            P.op('act', 'activation', out=dd[d_][:, 0:4], in_=PS2[d_][:, :, 64], func=AF.Abs)
        for d_ in range(2):
            P.op('dve', 'tensor_tensor', out=dd[d_][:, 0:4], in0=dd[d_][:, 0:4], in1=Ctok[:, cs_[d_], 4 * d_:4 * d_ + 4], op=ALU.max)
        for d_ in range(2):
            P.op('dve', 'reciprocal', out=dd[d_][:, 4:8], in_=dd[d_][:, 0:4])
        for d_ in range(2):
            hbuf = hb[d_].get()
            P.op('dve', 'tensor_tensor', out=hbuf.a, in0=PS2[d_][:, :, 0:64], in1=dd[d_][:, 4:8].unsq(2).bc([64, 4, 64]), op=ALU.mult)
            P.dma(Hd[d_][cs_[d_]].re("p (h d) -> p h d", h=4), hbuf.a, q='sp')
        P.run_deferred(prep_lst, per_step)
    P.run_deferred(prep_lst)
    P.barrier()
    prep_es.close()
    es3.close()
    P.barrier()
    es4 = contextlib.ExitStack()
    Wo = P.sb([128, KD, 256], BF16, "wo", es=es4)
    for k in range(KD):
        self.load_w(Wo[:, k, :], self.w_in[l, k * 128:(k + 1) * 128, 784:1040])
    ng = P.sb([64, 256], F32, "mlng", es=es4)
    P.dma(ng.a, self.ml_norm_g[l:l + 1, :].pbc(64))
    hg = Rot([P.sb([128, KD, 512], BF16, "hg", es=es4) for _ in range(2)])
    hf_ = Rot([P.sb([64, 8, 256], F32, "hf", es=es4) for _ in range(2)])
    hb_ = Rot([P.sb([64, 8, 256], F32, "hb", es=es4) for _ in range(2)])
    sq = P.sb([64, 8, 256], F32, "sq4", es=es4)
    ss = P.sb([64, 32], F32, "ss4", es=es4)
    ob = Rot([P.sb([64, 8, 256], F32, "ob", es=es4) for _ in range(2)])
    hz = Rot([P.sb([64, 8, 256], BF16, "hz", es=es4) for _ in range(2)])
    zst = Rot([P.sb([128, 2, 512], BF16, "zst", es=es4) for _ in range(2)])
    pso = Rot([P.ps([128, 512], F32, es=es4) for _ in range(2)])
    pst = Rot([P.ps([128, 8, 64], BF16, es=es4) for _ in range(2)])
    for (n0, n, seg) in cfg.groups:
        if self.skip_ctx and seg == 1:
            continue
        nc_ = n // 64
        c0 = n0 // 64
        h = hg.get()
        P.dma(h[:, :, 0:n], self.hT[:, :, n0:n0 + n].re("k p t -> p k t"))
        a = hf_.get()
        b = hb_.get()
        P.dma(a[:, 0:nc_, :], self.Hf[c0:c0 + nc_].re("c p f -> p c f"))
        P.dma(b[:, 0:nc_, :], self.Hb[c0:c0 + nc_].re("c p f -> p c f"))
        P.op('dve', 'tensor_tensor', out=a[:, 0:nc_, :], in0=a[:, 0:nc_, :], in1=b[:, 0:nc_, :], op=ALU.add)
        P.op('act', 'activation', out=sq[:, 0:nc_, :], in_=a[:, 0:nc_, :], func=AF.Square)
        P.op('dve', 'tensor_reduce', out=ss[:, 0:nc_ * 4], in_=sq[:, 0:nc_, :].re("p c (h d) -> p (c h) d", h=4), axis=AX.X, op=ALU.add)
        P.op('act', 'activation', out=ss[:, 0:nc_ * 4], in_=ss[:, 0:nc_ * 4], func=AF.Sqrt, scale=1.0 / 64, bias=self.eps[0:64, :])
        P.op('dve', 'reciprocal', out=ss[:, 0:nc_ * 4], in_=ss[:, 0:nc_ * 4])
        P.op('dve', 'tensor_tensor', out=a[:, 0:nc_, :].re("p c (h d) -> p (c h) d", h=4), in0=a[:, 0:nc_, :].re("p c (h d) -> p (c h) d", h=4),
             in1=ss[:, 0:nc_ * 4].unsq(2).bc([64, nc_ * 4, 64]), op=ALU.mult)
        P.op('dve', 'tensor_tensor', out=a[:, 0:nc_, :], in0=a[:, 0:nc_, :], in1=ng.a.unsq(1).bc([64, nc_, 256]), op=ALU.mult)
        o = ob.get()
        for cc in range(nc_):
            po = pso.get()
            for k in range(KD):
                P.mm(po[0:64, 0:256], h[:, k, cc * 64:(cc + 1) * 64], Wo[:, k, :], start=(k == 0), stop=(k == KD - 1))
            P.op('act', 'activation', out=o[:, cc, :], in_=po[0:64, 0:256], func=AF.Sigmoid)
        z = hz.get()
        P.op('dve', 'tensor_tensor', out=z[:, 0:nc_, :], in0=a[:, 0:nc_, :], in1=o[:, 0:nc_, :], op=ALU.mult)
        zs = zst.get()
        for half in range(2):
            p_ = pst.get()
            for cc in range(nc_):
                P.op('pe', 'transpose', out=p_[:, cc, :], in_=z[:, cc, half * 128:(half + 1) * 128], identity=self.identb[0:64, 0:64])
            P.op('act', 'activation', out=zs[:, half, 0:n], in_=p_[:, 0:nc_, :].re("p c t -> p (c t)"), func=AF.Copy)
        P.dma(self.Z[0, :, n0:n0 + n].re("(c p) t -> p c t", p=128), zs[:, :, 0:n], q='pool')
    es4.close()
    es.close()
    P.barrier()


MK.phase_mlstm = _ml


def _peer_prep(self, l, es=None, defer=False):
    P = self.P
    if self.prep_done.get(l):
        return None
    self.prep_done[l] = True
    own_es = es is None
    es = es or contextlib.ExitStack()
    lst = []
    if defer:
        P.deferred = lst
    ub = Rot([P.sb([128, D], BF16, "ub", es=es) for _ in range(3)])
    vb = Rot([P.sb([128, D], BF16, "vb", es=es) for _ in range(3)])
    uo = Rot([P.sb([128, D], BF16, "uo", es=es) for _ in range(3)])
    pst = Rot([P.ps([128, KD, 128], BF16, es=es) for _ in range(2 if defer else 3)])
    Uv = self.peer_u[l].re("(a b) d -> b a d", b=128)
    Vv = self.peer_v[l].re("(a b) d -> b a d", b=128)
    for e2 in range(128):
        u = ub.get()
        P.dma(u.a, Uv[e2], q='pool')
        p_ = pst.get()
        for k in range(KD):
            P.op('pe', 'transpose', out=p_[:, k, :], in_=u[:, k * 128:(k + 1) * 128], identity=self.identb)
        o = uo.get()
        if e2 % 2:
            P.op('act', 'activation', out=o.a, in_=p_.a.re("p k e -> p (k e)"), func=AF.Copy)
        else:
            P.op('dve', 'tensor_copy', out=o.a, in_=p_.a.re("p k e -> p (k e)"))
        P.dma(self.UTs[l % 2][e2], o.a, q='sp')
        v = vb.get()
        P.dma(v.a, Vv[e2], q='pool')
        P.dma(self.VBs[l % 2][e2], v.a, q='sp')
    P.deferred = None
    if own_es:
        es.close()
        P.barrier()
    return lst


MK.phase_peer_prep = _peer_prep


def _peer(self, l):
    P, cfg = self.P, self.cfg
    es = contextlib.ExitStack()
    gs, sh, gate2 = self.mod_scale_shift(l, 1, es)
    Wq = P.sb([128, KD, 2048], BF16, "wq", es=es)
    for k in range(KD):
        self.load_w(Wq[:, k, :], self.peer_w_q[l, k * 128:(k + 1) * 128, :])
    psA = Rot([P.ps([128, 512], F32, es=es) for _ in range(2)])
    Wps = Rot([P.ps([128, 512], F32, es=es) for _ in range(2)])
    acc = [P.ps([128, 2, 256], F32, es=es) for _ in range(4)]
    kl = P.sb([128, 2, 128], BF16, "kl", es=es)
    self.load_w(kl.a, self.peer_keys[l].re("p e k -> e p k"))
    keysT = P.sb([128, 2, 128], BF16, "keysT", es=es)
    for p in range(2):
        pk_ = Wps.get()
        pkb = pk_.a.re("q (a b) -> q a b", b=128)
        klf = P.sb([128, 128], F32, "klf", es=es)
        P.op('dve', 'tensor_copy', out=klf.a, in_=kl[:, p, :])
        P.op('pe', 'transpose', out=pk_[:, 0:128], in_=klf.a, identity=self.identf)
        P.op('dve', 'tensor_copy', out=keysT[:, p, :], in_=pk_[:, 0:128])
    xgs = [P.sb([128, KD, 256], F32, "xg", es=es) for _ in range(2)]
    h2s = [P.sb([128, KD, 256], BF16, "h2", es=es) for _ in range(2)]
    qT = P.sb([128, 16, 256], BF16, "qT", es=es)
    S = P.sb([128, 16, 128], F32, "S", es=es)
    S2 = P.sb([128, 16, 128], F32, "S2", es=es)
    V1 = P.sb([128, 16, 16], F32, "V1", es=es)
    I1u = P.sb([128, 16, 16], U32, "I1u", es=es)
    I1f = P.sb([128, 16, 16], F32, "I1f", es=es)
    cand = P.sb([128, 8, 256], F32, "cand", es=es)
    cand2 = S2
    SC = P.sb([128, 8, 16], F32, "SC", es=es)
    POSu = P.sb([128, 8, 16], U32, "POSu", es=es)
    PIu = P.sb([128, 8, 16], U32, "PIu", es=es)
    PJu = P.sb([128, 8, 16], U32, "PJu", es=es)
    PIf = P.sb([128, 128], F32, "PIf", es=es)
    PJf = P.sb([128, 128], F32, "PJf", es=es)
    OH = S
    E1 = P.sb([128, 128], F32, "E1", es=es)
    E2 = P.sb([128, 128], F32, "E2", es=es)
    G = P.sb([128, 128], F32, "G", es=es)
    sm = P.sb([128, 16], F32, "sm", es=es)
    E1T = P.sb([128, 256], BF16, "E1T", es=es)
    E2T = P.sb([128, 256], BF16, "E2T", es=es)
    GT = P.sb([128, 256], BF16, "GT", es=es)
    if self.wb4:
        An4 = Rot([P.sb([128, 4, 128], BF16, "An", es=es) for _ in range(2)])
        Bn4 = Rot([P.sb([128, 4, 128], BF16, "Bn", es=es) for _ in range(2)])
        iota4 = P.sb([128, 4, 128], BF16, "iota4", es=es)
        for tt in range(4):
            P.op('dve', 'tensor_copy', out=iota4[:, tt, :], in_=self.iota128b_t.a)
    else:
        An = Rot([P.sb([128, 128], BF16, "An", es=es) for _ in range(6)])
        Bn = Rot([P.sb([128, 128], BF16, "Bn", es=es) for _ in range(6)])
    Wbuf = P.sb([128, 256, 128], BF16, "Wbuf", es=es)
    ut = Rot([P.sb([128, KD, 128], BF16, "ut", es=es) for _ in range(6)])
    vt = Rot([P.sb([128, D], BF16, "vtb", es=es) for _ in range(6)])
    Ab = Rot([P.sb([128, 256], BF16, "Ab", es=es) for _ in range(3)])
    AW = Rot([P.sb([128, 256], BF16, "AW", es=es) for _ in range(3)])
    i16 = self.iota16
    V1s = [V1] + [V1.sub() for _ in range(15)]
    I1s = [I1u] + [I1u.sub() for _ in range(15)]
    S2s = [S2] + [S2.sub() for _ in range(15)]
    cands = [cand] + [cand.sub() for _ in range(7)]
    SCs = [SC] + [SC.sub() for _ in range(7)]
    POSs = [POSu] + [POSu.sub() for _ in range(7)]
    zlhs = P.sb([128, 128], BF16, "zlhs", es=es)
    zrhs = P.sb([128, 512], BF16, "zrhs", es=es)
    P.op('pool', 'memset', ap=zlhs.a, constant=0.0)
    P.op('pool', 'memset', ap=zrhs.a, constant=0.0)
    glist = [g for g in cfg.groups256 if not (self.skip_ctx and g[2] == 1)]
    sq = qT[:, 0:8, :]
    tmps = Rot([cands[i][:, i, :] for i in range(3)])
    rsb = cands[3][:, 3, :]

    def front(gi):
        (n0, n, seg) = glist[gi]
        x = xgs[gi % 2]
        h2 = h2s[gi % 2]
        P.dma(x.a, self.xT[:, :, n0:n0 + n].re("k p t -> p k t"))
        self.norm_group(x.a, n, gs, sh, seg, h2.a, Wps, tmps, sq, rsb)
        for j in range(16):
            pq = Wps.get()
            for k in range(KD):
                P.mm(pq[:, 0:n], Wq[:, k, j * 128:(j + 1) * 128], h2[:, k, :], start=(k == 0), stop=(k == KD - 1))
            P.op('act', 'activation', out=qT[:, j, :], in_=pq[:, 0:n], func=AF.Copy)
        for sub in range(2 if 'topk' in self.peer_parts else 0):
            tsl = slice(sub * 128, (sub + 1) * 128)
            for j4 in range(4):
                pscr = Wps.get()
                for jj in range(4):
                    j = j4 * 4 + jj
                    P.mm(pscr[:, jj * 128:(jj + 1) * 128], qT[:, j, tsl], keysT[:, j % 2, :])
                P.op('act', 'activation', out=S[:, j4 * 4:j4 * 4 + 4, :], in_=pscr.a.re("p (a b) -> p a b", b=128), func=AF.Copy)
            for j in range(16):
                P.op('dve', 'max', out=V1s[j][:, j, 0:8], in_=S[:, j, :])
            for j in range(16):
                P.op('dve', 'max_index', out=I1s[j][:, j, 0:8], in_max=V1s[j][:, j, 0:8], in_values=S[:, j, :])
            for j in range(16):
                P.op('dve', 'match_replace', out=S2s[j][:, j, :], in_to_replace=V1s[j][:, j, 0:8], in_values=S[:, j, :], imm_value=NEG)
            for j in range(16):
                P.op('dve', 'max', out=V1s[j][:, j, 8:16], in_=S2s[j][:, j, :])
            for j in range(16):
                P.op('dve', 'max_index', out=I1s[j][:, j, 8:16], in_max=V1s[j][:, j, 8:16], in_values=S2s[j][:, j, :])
            P.op('dve', 'tensor_copy', out=I1f.a, in_=I1s[0].a, xr=I1s[1:])
            V1v = V1s[0].a.re("q (h p) i -> q h p i", p=2)
            P.op('dve', 'tensor_tensor', out=cands[0].a.re("q h (i j) -> q h i j", j=16), in0=V1v[:, :, 0, :].unsq(3).bc([128, 8, 16, 16]),
                 in1=V1v[:, :, 1, :].unsq(2).bc([128, 8, 16, 16]), op=ALU.add, xr=V1s[1:], xw=cands[1:])
            c2v = S2.a.re("q a b -> q (a b)").re("q (h c) -> q h c", h=8)
            ohv = S.a.re("q a b -> q (a b)").re("q (j i) -> q j i", i=16)
            def c2(h):
                return V(S2s[2 * h], c2v.ap[:, h, :])
            for h in range(8):
                P.op('dve', 'max', out=SCs[h][:, h, 0:8], in_=cands[h][:, h, :])
            for h in range(8):
                P.op('dve', 'max_index', out=POSs[h][:, h, 0:8], in_max=SCs[h][:, h, 0:8], in_values=cands[h][:, h, :])
            for h in range(8):
                P.op('dve', 'match_replace', out=c2(h), in_to_replace=SCs[h][:, h, 0:8], in_values=cands[h][:, h, :], imm_value=NEG, xw=[S2s[2 * h + 1]])
            for h in range(8):
                P.op('dve', 'max', out=SCs[h][:, h, 8:16], in_=c2(h), xr=[S2s[2 * h + 1]])
            for h in range(8):
                P.op('dve', 'max_index', out=POSs[h][:, h, 8:16], in_max=SCs[h][:, h, 8:16], in_values=c2(h), xr=[S2s[2 * h + 1]])
            P.op('dve', 'tensor_single_scalar', out=PIu.a, in_=POSs[0].a, scalar=4, op=ALU.logical_shift_right, xr=POSs[1:])
            P.op('dve', 'tensor_single_scalar', out=PJu.a, in_=POSs[0].a, scalar=15, op=ALU.bitwise_and, xr=POSs[1:])
            P.op('dve', 'tensor_copy', out=PIf.a, in_=PIu.a.re("q h k -> q (h k)"))
            P.op('dve', 'tensor_copy', out=PJf.a, in_=PJu.a.re("q h k -> q (h k)"))
            I1v = I1f.a.re("q (h p) i -> q h p i", p=2)
            for (Pf, pp, Eo) in ((PIf, 0, E1), (PJf, 1, E2)):
                P.op('dve', 'tensor_tensor', out=ohv, in0=i16.unsq(1).bc([128, 128, 16]), in1=Pf.a.unsq(2).bc([128, 128, 16]), op=ALU.is_equal)
                P.op('dve', 'tensor_tensor', out=ohv.re("q (h k) i -> q h k i", h=8), in0=ohv.re("q (h k) i -> q h k i", h=8),
                     in1=I1v[:, :, pp, :].unsq(2).bc([128, 8, 16, 16]), op=ALU.mult)
                P.op('dve', 'tensor_reduce', out=Eo.a, in_=ohv, axis=AX.X, op=ALU.add)
            Gv = G.a.re("q (h k) -> q h k", h=8)
            P.op('dve', 'tensor_tensor', out=Gv, in0=SC.a, in1=SC[:, :, 0:1].bc([128, 8, 16]), op=ALU.subtract, xr=SCs[1:])
            P.op('act', 'activation', out=G.a, in_=G.a, func=AF.Exp)
            P.op('dve', 'tensor_reduce', out=sm[:, 0:8], in_=Gv, axis=AX.X, op=ALU.add)
            P.op('dve', 'reciprocal', out=sm[:, 8:16], in_=sm[:, 0:8])
            P.op('dve', 'tensor_tensor', out=Gv, in0=Gv, in1=sm[:, 8:16].unsq(2).bc([128, 8, 16]), op=ALU.mult)
            for (src, dst) in ((E1, E1T), (E2, E2T), (G, GT)):
                ptr = Wps.get()
                P.op('pe', 'transpose', out=ptr[:, 0:128], in_=src.a, identity=self.identf)
                P.op('act', 'activation', out=dst[:, tsl], in_=ptr[:, 0:128], func=AF.Copy)
    def run_front(gi):
        lst = []
        P.deferred = lst
        front(gi)
        P.deferred = None
        return lst

    P.run_deferred(run_front(0))
    for gi, (n0, n, seg) in enumerate(glist):
        x = xgs[gi % 2]
        h2 = h2s[gi % 2]
        for t4 in range(n // 4 if 'wb' in self.peer_parts else 0):
            wp = Wps.get()
            if self.wb4:
                a_ = An4.get()
                b_ = Bn4.get()
                t0_ = t4 * 4
                P.op('dve', 'tensor_tensor', out=a_.a, in0=iota4.a, in1=E1T[:, t0_:t0_ + 4].unsq(2).bc([128, 4, 128]), op=ALU.is_equal)
                P.op('dve', 'tensor_tensor', out=a_.a, in0=a_.a, in1=GT[:, t0_:t0_ + 4].unsq(2).bc([128, 4, 128]), op=ALU.mult)
                P.op('dve', 'tensor_tensor', out=b_.a, in0=iota4.a, in1=E2T[:, t0_:t0_ + 4].unsq(2).bc([128, 4, 128]), op=ALU.is_equal)
                for tt in range(4):
                    P.mm(wp[:, tt * 128:(tt + 1) * 128], a_[:, tt, :], b_[:, tt, :])
            for tt in range(0 if self.wb4 else 4):
                t = t4 * 4 + tt
                a_ = An.get()
                b_ = Bn.get()
                P.op('dve', 'tensor_scalar', out=a_.a, in0=self.iota128b_t.a, scalar1=E1T[:, t:t + 1], scalar2=GT[:, t:t + 1], op0=ALU.is_equal, op1=ALU.mult)
                P.op('dve', 'tensor_scalar', out=b_.a, in0=self.iota128b_t.a, scalar1=E2T[:, t:t + 1], scalar2=None, op0=ALU.is_equal)
                P.mm(wp[:, tt * 128:(tt + 1) * 128], a_.a, b_.a)
            P.op('act', 'activation', out=Wbuf[:, t4 * 4:t4 * 4 + 4, :], in_=wp.a.re("p (a b) -> p a b", b=128), func=AF.Copy)
        for bnk in range(4):
            P.mm(acc[bnk].a.re("p a b -> p (a b)"), zlhs.a, zrhs.a, start=True, stop=False)
        NE = 128 if 'ex' in self.peer_parts else 0

        def stage_a(e2):
            u = ut.get()
            v = vt.get()
            P.dma(u.a, self.UTs[l % 2][e2].re("p (k e) -> p k e", e=128), q='sp')
            P.dma(v.a, self.VBs[l % 2][e2], q='sp')
            pa = psA.get()
            for k in range(KD):
                P.mm(pa[:, 0:n], u[:, k, :], h2[:, k, :], start=(k == 0), stop=(k == KD - 1))
            ab = Ab.get()
            P.op('act', 'activation', out=ab.a, in_=pa[:, 0:n], func=AF.Gelu_apprx_tanh)
            aw = AW.get()
            P.op(self.aw_eng, 'tensor_tensor', out=aw.a, in0=ab.a, in1=Wbuf[:, :, e2], op=ALU.mult)
            return v, aw

        nxt = run_front(gi + 1) if gi + 1 < len(glist) else []
        if not getattr(self, 'peer_pipe', True):
            pre_nxt, nxt = nxt, []
        per_chunk = (len(nxt) + 119) // 120 if NE else len(nxt)
        pend = stage_a(0) if NE else None
        for e2 in range(NE):
            v, aw = pend
            if e2 + 1 < NE:
                pend = stage_a(e2 + 1)
            P.run_deferred(nxt, per_chunk)
            for dc in range(KD):
                P.mm(acc[dc // 2][:, dc % 2, :], v[:, dc * 128:(dc + 1) * 128], aw.a, start=False, stop=(e2 == 127 and dc % 2 == 1))
        P.run_deferred(nxt)
        if not getattr(self, 'peer_pipe', True):
            P.run_deferred(pre_nxt)
        for dc in range(KD):
            P.op('dve', 'scalar_tensor_tensor', out=x[:, dc, :], in0=acc[dc // 2][:, dc % 2, :], scalar=gate2[:, dc, seg:seg + 1], in1=x[:, dc, :], op0=ALU.mult, op1=ALU.add)
        P.dma(self.xT[:, :, n0:n0 + n].re("k p t -> p k t"), x.a, q='pool')
    es.close()
    P.barrier()


MK.phase_peer = _peer


def build_program(cfg, debug=False, phases=None, **opts):
    mk = MK(cfg, debug=debug)
    mk.skip_ctx = False
    mk.prep_done = {}
    mk.prep_in_mlstm = (phases is None) and opts.get('prep_in_mlstm', False)
    mk.peer_pipe = opts.get('peer_pipe', True)
    mk.wb4 = opts.get('wb4', False) or (phases is not None and 'wb4' in phases)
    mk.aw_eng = 'pool' if (opts.get('aw_pool', True) or (phases is not None and 'aw_pool' in phases)) else 'dve'
    mk.scan_eng = 'dve' if (opts.get('scan_dve', True) and not (phases is not None and 'scan_pool' in phases)) else 'pool'
    mk.merge_eng = 'dve' if (opts.get('merge_dve', True) and not (phases is not None and 'merge_pool' in phases)) else 'pool'
    mk.wb_pool = opts.get('wb_pool', False) or (phases is not None and 'wb_pool' in phases)
    mk.peer_parts = set(['topk', 'wb', 'ex']) if (phases is None or not any(p.startswith('pp_') for p in phases)) else set(p[3:] for p in phases if p.startswith('pp_'))
    on = lambda p: phases is None or p in phases
    mk.phase_init()
    for l in range(cfg.depth):
        mk.skip_ctx = False
        if on('mod'):
            mk.phase_mod(l)
        if on('norm1'):
            mk.phase_norm1(l)
        if on('mlstm'):
            mk.phase_mlstm(l)
        mk.skip_ctx = (l == cfg.depth - 1) and phases is None
        if on('gmlp'):
            mk.phase_gmlp(l)
        if on('conv'):
            mk.phase_conv(l)
        if on('fnet'):
            mk.phase_fnet(l)
        if on('merge'):
            mk.phase_merge(l)
        if on('prep'):
            mk.phase_peer_prep(l)
        if on('peer'):
            mk.phase_peer(l)
    mk.phase_final()
    mk.P.finish()
    return mk


_CACHE = {}


def make_in_maps(cfg, inp):
    dftc, dfts, cd, cst = host_consts(cfg)
    pos = grid_sincos(cfg.t_lat, D)
    L = cfg.depth
    shared = {"pos": pos, "dftc": dftc, "dfts": dfts, "cd": cd, "cst": cst}
    for k in ["w_ada", "b_ada", "norm1_g", "norm2_g", "w_in", "ml_conv_w", "ml_conv_b", "ml_gate_b", "gm_ln_g", "gm_ln_b",
              "gm_w_s", "cv_dw_w", "cv_dw_b", "cv_ln_g", "cv_ln_b", "w_branch", "w_out", "peer_w_q", "peer_keys",
              "peer_u", "peer_v", "final_norm_g"]:
        shared[k] = np.ascontiguousarray(np.asarray(inp[k], dtype=np.float32))
    shared["ml_norm_g"] = np.ascontiguousarray(np.asarray(inp["ml_norm_g"], np.float32).reshape(L, 256))
    shared["gm_b_s"] = np.ascontiguousarray(np.asarray(inp["gm_b_s"], np.float32).reshape(L, 512))
    x = np.asarray(inp["x"], np.float32)
    ctx = np.asarray(inp["ctx"], np.float32)
    c = np.asarray(inp["c"], np.float32)
    c_ctx = np.asarray(inp["c_ctx"], np.float32)
    maps = []
    for b in range(x.shape[0]):
        m = dict(shared)
        m["xin"] = np.ascontiguousarray(np.concatenate([ctx[b], x[b]], 0))
        m["cvec"] = np.ascontiguousarray(np.stack([c[b], c_ctx], 0))
        maps.append(m)
    return maps


def kernel(**inputs):
    x = np.asarray(inputs["x"])
    B, T, _ = x.shape
    depth = np.asarray(inputs["w_ada"]).shape[0]
    cfg = Cfg(depth=depth, t_lat=T, t_ctx=np.asarray(inputs["ctx"]).shape[1])
    key = (depth, T, cfg.t_ctx)
    if key not in _CACHE:
        _CACHE[key] = build_program(cfg)
    mk = _CACHE[key]
    maps = make_in_maps(cfg, inputs)
    res = run_bass_kernel_spmd(mk.nc, maps, core_ids=list(range(B)))
    return np.stack([np.asarray(r["out"], dtype=np.float32) for r in res.results], 0)
```
